# Optimizing a Trainium2 kernel written in Bass

```python
import jax, jax.numpy as jnp
from jax import lax
import numpy as np

D_MODEL = 1024
BATCH = 2
SEQ = 8192
DEPTH = 1

HEAD_DIM = 64
A_HEADS = 8
B_HEADS = 8
IDX_HEADS = 8
IDX_DIM = 64
DILATED_PATTERNS = ((128, 1), (512, 4), (2048, 16))
TOPK_MAX = 256
D_A = A_HEADS * HEAD_DIM
D_B = B_HEADS * HEAD_DIM
D_FF = ((8 * D_MODEL + 767) // 768) * 256
ROPE_THETA = 10000.0
RMS_EPS = 1e-6
BLOCK = 128
ATTN_SCALE = HEAD_DIM ** -0.5
IDX_SCALE = (IDX_DIM ** -0.5) * (IDX_HEADS ** -0.5)
IN_SPLITS = (D_A, D_A, D_A, D_B, HEAD_DIM, HEAD_DIM, IDX_HEADS * IDX_DIM, IDX_DIM, IDX_HEADS, D_MODEL, D_MODEL)
IN_OFFSETS = tuple(int(o) for o in np.cumsum(IN_SPLITS)[:-1])
D_IN = int(sum(IN_SPLITS))

kernel_name = 'hybrid_dilated_dsa_gated_block'


def rmsnorm(x, g):
    x32 = x.astype(jnp.float32)
    y = x32 * lax.rsqrt(jnp.mean(x32 * x32, axis=-1, keepdims=True) + RMS_EPS)
    return (y * g.astype(jnp.float32)).astype(x.dtype)


def rope(z, pos):
    half = z.shape[-1] // 2
    inv_freq = ROPE_THETA ** (-jnp.arange(half, dtype=jnp.float32) / half)
    ang = pos.astype(jnp.float32)[:, None] * inv_freq[None, :]
    cos = jnp.cos(ang)[None, :, None, :]
    sin = jnp.sin(ang)[None, :, None, :]
    z32 = z.astype(jnp.float32)
    z1, z2 = z32[..., :half], z32[..., half:]
    return jnp.concatenate([z1 * cos - z2 * sin, z2 * cos + z1 * sin], axis=-1).astype(z.dtype)


def dilated_window_attention(q, k, v, window, dilation):
    B, T, H, hd = q.shape
    steps = window // dilation
    seg = dilation * BLOCK
    t_pad = -(-T // seg) * seg
    m_len = t_pad // dilation
    nb = m_len // BLOCK

    def to_blocks(z):
        z = jnp.pad(z, ((0, 0), (0, t_pad - T), (0, 0), (0, 0)))
        z = z.reshape(B, m_len, dilation, H, hd).transpose(0, 2, 3, 1, 4)
        return z.reshape(B, dilation, H, nb, BLOCK, hd)

    qb, kb, vb = to_blocks(q), to_blocks(k), to_blocks(v)

    def with_prev(z):
        prev = jnp.pad(z[:, :, :, :-1], ((0, 0), (0, 0), (0, 0), (1, 0), (0, 0), (0, 0)))
        return jnp.concatenate([prev, z], axis=4)

    k2, v2 = with_prev(kb), with_prev(vb)
    s = jnp.einsum('brhnqd,brhnkd->brhnqk', qb, k2).astype(jnp.float32) * ATTN_SCALE
    qi = jnp.arange(BLOCK)[:, None]
    kj = jnp.arange(2 * BLOCK)[None, :]
    dist = qi + BLOCK - kj
    band = (dist >= 0) & (dist <= steps)
    has_prev = (jnp.arange(nb)[:, None, None] > 0) | (kj >= BLOCK)[None]
    mask = band[None] & has_prev
    s = jnp.where(mask, s, -jnp.inf)
    m = jnp.max(s, axis=-1, keepdims=True)
    p = jnp.exp(s - m)
    den = jnp.sum(p, axis=-1, keepdims=True)
    o = jnp.einsum('brhnqk,brhnkd->brhnqd', p.astype(v.dtype), v2).astype(jnp.float32) / den
    lse = (m + jnp.log(den))[..., 0]
    o = o.reshape(B, dilation, H, m_len, hd).transpose(0, 3, 1, 2, 4).reshape(B, t_pad, H, hd)[:, :T]
    lse = lse.reshape(B, dilation, H, m_len).transpose(0, 3, 1, 2).reshape(B, t_pad, H)[:, :T]
    return o, lse


def dilated_mixture(q, k, v):
    outs, lses = [], []
    for window, dilation in DILATED_PATTERNS:
        o, l = dilated_window_attention(q, k, v, window, dilation)
        outs.append(o)
        lses.append(l)
    w = jax.nn.softmax(jnp.stack(lses, axis=0), axis=0)
    return jnp.sum(w[..., None] * jnp.stack(outs, axis=0), axis=0)


def indexed_sparse_attention(q, k, v, qi, ki, wi):
    B, T, H, hd = q.shape
    topk = min(TOPK_MAX, T // 4)
    nb = T // BLOCK
    key_pos = jnp.arange(T)

    def blocks(z):
        return jnp.moveaxis(z.reshape((B, nb, BLOCK) + z.shape[2:]), 1, 0)

    def one_block(args):
        q_blk, qi_blk, wi_blk, start = args
        q_pos = start + jnp.arange(BLOCK)
        causal = key_pos[None, :] <= q_pos[:, None]
        dots = jnp.einsum('bqhd,bsd->bqhs', qi_blk, ki).astype(jnp.float32)
        score = jnp.einsum('bqh,bqhs->bqs', wi_blk.astype(jnp.float32), jax.nn.relu(dots)) * IDX_SCALE
        score = jnp.where(causal[None], score, -jnp.inf)
        _, sel = lax.top_k(score, topk)
        valid = sel <= q_pos[None, :, None]
        k_sel = jax.vmap(lambda kk, ii: kk[ii])(k, sel)
        v_sel = jax.vmap(lambda vv, ii: vv[ii])(v, sel)
        s = jnp.einsum('bqhd,bqkd->bhqk', q_blk, k_sel).astype(jnp.float32) * ATTN_SCALE
        s = jnp.where(valid[:, None], s, -jnp.inf)
        p = jax.nn.softmax(s, axis=-1)
        return jnp.einsum('bhqk,bqkd->bqhd', p.astype(v.dtype), v_sel)

    starts = jnp.arange(nb, dtype=jnp.int32) * BLOCK
    out = lax.map(one_block, (blocks(q), blocks(qi), blocks(wi), starts))
    return jnp.moveaxis(out, 0, 1).reshape(B, T, H, hd)


def setup_inputs(seed: int = 0) -> dict:
    key = jax.random.key(seed)
    ks = jax.random.split(key, 12)
    f32 = jnp.float32

    def dense(k, shape, fan_in):
        return jax.random.normal(k, shape, f32) * fan_in ** -0.5

    def gain(k, shape):
        return 1.0 + 0.02 * jax.random.normal(k, shape, f32)

    return {
        'x': jax.random.normal(ks[0], (BATCH, SEQ, D_MODEL), f32),
        'norm_mix': gain(ks[1], (DEPTH, D_MODEL)),
        'w_in': dense(ks[2], (DEPTH, D_MODEL, D_IN), D_MODEL),
        'w_up_a': dense(ks[3], (DEPTH, D_A, D_MODEL), D_A),
        'w_up_b': dense(ks[4], (DEPTH, D_B, D_MODEL), D_B),
        'w_out': dense(ks[5], (DEPTH, D_MODEL, D_MODEL), D_MODEL),
        'norm_ffn': gain(ks[6], (DEPTH, D_MODEL)),
        'w_gate': dense(ks[7], (DEPTH, D_MODEL, D_FF), D_MODEL),
        'w_up': dense(ks[8], (DEPTH, D_MODEL, D_FF), D_MODEL),
        'w_down': dense(ks[9], (DEPTH, D_FF, D_MODEL), D_FF),
        'norm_final': gain(ks[10], (D_MODEL,)),
    }


def reference(x, norm_mix, w_in, w_up_a, w_up_b, w_out, norm_ffn, w_gate, w_up, w_down, norm_final):
    B, T, _ = x.shape
    pos = jnp.arange(T, dtype=jnp.int32)
    for layer in range(DEPTH):
        h = rmsnorm(x, norm_mix[layer])
        proj = h @ w_in[layer]
        qa, ka, va, qb, kb, vb, qi, ki, wi, ga, gb = jnp.split(proj, IN_OFFSETS, axis=-1)
        qa = rope(qa.reshape(B, T, A_HEADS, HEAD_DIM), pos)
        ka = rope(ka.reshape(B, T, A_HEADS, HEAD_DIM), pos)
        va = va.reshape(B, T, A_HEADS, HEAD_DIM)
        y_a = dilated_mixture(qa, ka, va).astype(x.dtype).reshape(B, T, D_A)
        qb = rope(qb.reshape(B, T, B_HEADS, HEAD_DIM), pos)
        kb = rope(kb[:, :, None], pos)[:, :, 0]
        qi = rope(qi.reshape(B, T, IDX_HEADS, IDX_DIM), pos)
        ki = rope(ki[:, :, None], pos)[:, :, 0]
        y_b = indexed_sparse_attention(qb, kb, vb, qi, ki, wi).reshape(B, T, D_B)
        merged = jax.nn.sigmoid(ga) * (y_a @ w_up_a[layer]) + jax.nn.sigmoid(gb) * (y_b @ w_up_b[layer])
        x = x + merged @ w_out[layer]
        h = rmsnorm(x, norm_ffn[layer])
        x = x + (jax.nn.silu(h @ w_gate[layer]) * (h @ w_up[layer])) @ w_down[layer]
    return rmsnorm(x, norm_final)
```

```python
import os
from contextlib import ExitStack

import ml_dtypes
import numpy as np

import concourse.bass as bass
import concourse.mybir as mybir
from concourse.bass_types import AP
from concourse.bass_utils import run_bass_kernel_spmd

F32 = mybir.dt.float32
BF16 = mybir.dt.bfloat16
AF = mybir.ActivationFunctionType
ALU = mybir.AluOpType
AX = mybir.AxisListType

NB = 64
NOWN = 16
D = 1024
DFF = 2816
NFF = DFF // 128
DIN = 4808
NIT = 22
BIG = 1.0e30
NEGM = 30000.0
EPS = 1e-6
NDS = 12

_DBG = os.environ.get("KDBG", "")


class Buf:
    __slots__ = ("w", "r")

    def __init__(self):
        self.w = None
        self.r = {}


class KB:
    def __init__(self, nc):
        self.nc = nc
        self.eng = {"pe": nc.tensor, "act": nc.scalar, "dve": nc.vector, "pool": nc.gpsimd, "sp": nc.sync}
        self.sem = {e: nc.alloc_semaphore(name=f"s_{e}") for e in self.eng}
        self.cnt = {e: 0 for e in self.eng}
        self.waited = {e: {} for e in self.eng}
        self.dq = {}
        for q in ("sp", "pool"):
            self.dq[q] = {"sems": [nc.alloc_semaphore(name=f"d_{q}{i}") for i in range(NDS)], "k": 0}

    def _wait(self, e, ev):
        sem, val = ev
        if e == "pe" and sem is self.sem["pe"]:
            return
        key = sem.num
        if self.waited[e].get(key, 0) >= val:
            return
        self.eng[e].wait_ge(sem, val)
        self.waited[e][key] = val

    def _deps(self, e, reads, writes):
        for b in reads:
            if b.w is not None:
                self._wait(e, b.w)
        for b in writes:
            if b.w is not None:
                self._wait(e, b.w)
            for ev in b.r.values():
                self._wait(e, ev)

    def _mark(self, ev, reads, writes):
        key = ev[0].num
        for b in reads:
            old = b.r.get(key)
            if old is None or old[1] < ev[1]:
                b.r[key] = ev
        for b in writes:
            b.w = ev
            b.r = {}

    def op(self, e, fn, reads=(), writes=(), sig=True):
        self._deps(e, reads, writes)
        inst = fn()
        if sig:
            self.cnt[e] += 1
            inst.then_inc(self.sem[e], 1)
            ev = (self.sem[e], self.cnt[e])
        else:
            ev = (self.sem[e], self.cnt[e] + 1)
        self._mark(ev, reads, writes)
        return inst

    def dma(self, q, out, in_, reads=(), writes=()):
        dq = self.dq[q]
        k = dq["k"]
        P = len(dq["sems"])
        sem = dq["sems"][k % P]
        if k >= P:
            self._wait(q, (sem, 16 * (k // P)))
        self._deps(q, reads, writes)
        self.eng[q].dma_start(out=out, in_=in_).then_inc(sem, 16)
        dq["k"] += 1
        ev = (sem, 16 * (k // P + 1))
        self._mark(ev, reads, writes)

    def barrier(self):
        evs = [(self.sem[e], self.cnt[e]) for e in self.eng if self.cnt[e] > 0]
        for q, dq in self.dq.items():
            k = dq["k"]
            P = len(dq["sems"])
            for i in range(min(k, P)):
                kk = k - 1 - i
                evs.append((dq["sems"][kk % P], 16 * (kk // P + 1)))
        for e in self.eng:
            for ev in evs:
                if ev[0] is self.sem[e]:
                    continue
                self._wait(e, ev)

    def finish(self, bufs):
        for b in bufs:
            if b.w is not None:
                self._wait("sp", b.w)
        for q, dq in self.dq.items():
            k = dq["k"]
            P = len(dq["sems"])
            for i in range(min(k, P)):
                kk = k - 1 - i
                self._wait(q, (dq["sems"][kk % P], 16 * (kk // P + 1)))


def bc_mid(ap2d, n):
    a = [list(x) for x in ap2d.ap]
    assert len(a) == 2
    return AP(ap2d.tensor, ap2d.offset, [a[0], [0, n], a[1]])


def bc_last(ap, n):
    a = [list(x) for x in ap.ap]
    return AP(ap.tensor, ap.offset, a + [[0, n]])


def build():
    nc = bass.Bass("TRN2", target_bir_lowering=False)
    kb = KB(nc)
    op = kb.op
    dma = kb.dma
    PE, ACT, DVE, POOL = nc.tensor, nc.scalar, nc.vector, nc.gpsimd

    def din(name, shape, dt=F32):
        return nc.dram_tensor(name, list(shape), dt, kind="ExternalInput")

    def dscr(name, shape, dt):
        return nc.dram_tensor(name, list(shape), dt, kind=("ExternalOutput" if _DBG else "Internal"))

    xl = din("xl", [NB, 128, D])
    cs_t = din("cs", [NB, 128, 64])
    vmask_d = din("vmask", [128, NB])
    padbias_d = din("padbias", [128, 512])
    diagbias_d = din("diagbias", [128, 512])
    mt_d = din("mt", [128, 17, 128], BF16)
    identb_d = din("identb", [128, 128], BF16)
    pow2_d = din("pow2", [128, NIT + 1])
    gmix_d = din("gmix", [128, 8])
    gffn_d = din("gffn", [128, 8])
    gfin_d = din("gfin", [128, D])
    w_in = din("w_in", [D, DIN])
    w_up_a = din("w_up_a", [512, D])
    w_up_b = din("w_up_b", [512, D])
    w_out = din("w_out", [D, D])
    w_gate = din("w_gate", [D, DFF])
    w_up = din("w_up", [D, DFF])
    w_down = din("w_down", [DFF, D])
    out_d = nc.dram_tensor("out", [NOWN, 128, D], F32, kind="ExternalOutput")

    KAT = dscr("KAT", [128, NB, 512], BF16)
    VA = dscr("VA", [128, NB, 520], BF16)
    KBT = dscr("KBT", [64, NB * 128], BF16)
    KIT = dscr("KIT", [64, NB * 128], BF16)
    VB = dscr("VB", [128, NB, 65], BF16)
    QAT = dscr("QAT", [NOWN, 128, 512], BF16)
    QBT = dscr("QBT", [NOWN, 64, 1024], BF16)
    QIT = dscr("QIT", [NOWN, 64, 1024], BF16)
    WI = dscr("WI", [NOWN, 128, 8], F32)
    GS = dscr("GS", [NOWN, 128, 2048], BF16)
    X2 = dscr("X2", [NOWN, 128, D], F32)
    b_KAT = [Buf() for _ in range(NB)]
    b_VA = [Buf() for _ in range(NB)]
    b_KBT = [Buf() for _ in range(NB)]
    b_KIT = [Buf() for _ in range(NB)]
    b_VB = [Buf() for _ in range(NB)]
    b_QAT = [Buf() for _ in range(NOWN)]
    b_QBT = [Buf() for _ in range(NOWN)]
    b_QIT = [Buf() for _ in range(NOWN)]
    b_WI = [Buf() for _ in range(NOWN)]
    b_GS = [Buf() for _ in range(NOWN)]
    b_X2 = [Buf() for _ in range(NOWN)]
    b_out = [Buf() for _ in range(NOWN)]

    dbg_outs = {}

    def dbg_out(name, shape, dt):
        t = nc.dram_tensor("dbg_" + name, list(shape), dt, kind="ExternalOutput")
        dbg_outs[name] = t
        return t

    with ExitStack() as top:
        def sb(stack, name, shape, dt):
            return stack.enter_context(nc.sbuf_tensor("sb_" + name, list(shape), dt))

        def ps(stack, name, shape, dt):
            return stack.enter_context(nc.psum_tensor("ps_" + name, list(shape), dt))

        identb = sb(top, "identb", [128, 128], BF16)
        b_const = Buf()
        dma("sp", identb[:], identb_d.ap(), writes=[b_const])
        vmask = sb(top, "vmask", [128, NB], F32)
        dma("sp", vmask[:], vmask_d.ap(), writes=[b_const])
        YAT = sb(top, "YAT", [128, 4, NOWN * 128], BF16)
        YBT = sb(top, "YBT", [128, 4, NOWN * 128], BF16)
        b_YAT = [Buf() for _ in range(NOWN)]
        b_YBT = [Buf() for _ in range(NOWN)]

        def rope(stack_bufs, zview, H, cs_tile, b_cs, b_z, out_view, b_outv):
            tA, tB, tC, tD, bA, bB, bC, bD = stack_bufs
            cosb = bc_mid(cs_tile[:, 0:32], H)
            sinb = bc_mid(cs_tile[:, 32:64], H)
            z1 = zview[:, :, 0:32]
            z2 = zview[:, :, 32:64]
            a = tA[:, 0:H, :]
            b = tB[:, 0:H, :]
            c = tC[:, 0:H, :]
            d = tD[:, 0:H, :]
            op("dve", lambda: DVE.tensor_tensor(out=a, in0=z1, in1=cosb, op=ALU.mult), reads=[b_z, b_cs], writes=[bA])
            op("dve", lambda: DVE.tensor_tensor(out=b, in0=z2, in1=sinb, op=ALU.mult), reads=[b_z, b_cs], writes=[bB])
            op("dve", lambda: DVE.tensor_tensor(out=c, in0=z2, in1=cosb, op=ALU.mult), reads=[b_z, b_cs], writes=[bC])
            op("dve", lambda: DVE.tensor_tensor(out=d, in0=z1, in1=sinb, op=ALU.mult), reads=[b_z, b_cs], writes=[bD])
            op("dve", lambda: DVE.tensor_tensor(out=out_view[:, :, 0:32], in0=a, in1=b, op=ALU.subtract),
               reads=[bA, bB], writes=[b_outv])
            op("dve", lambda: DVE.tensor_tensor(out=out_view[:, :, 32:64], in0=c, in1=d, op=ALU.add),
               reads=[bC, bD], writes=[b_outv])

        with ExitStack() as st:
            WK = sb(st, "WK", [128, 8, 1216], BF16)
            WQ = sb(st, "WQ", [128, 8, 3592], BF16)
            wst = [sb(st, f"wst{i}", [128, 8, 512], F32) for i in range(2)]
            b_wst = [Buf(), Buf()]
            gmix = sb(st, "gmix", [128, 8], F32)
            b_g = Buf()
            dma("sp", gmix[:], gmix_d.ap(), writes=[b_g])
            b_WK = Buf()
            b_WQ = Buf()
            w_in_v = w_in.ap().rearrange("(kc p) n -> p kc n", p=128)
            kparts = [(WK, b_WK, 0, 512, 512), (WK, b_WK, 512, 1024, 512), (WK, b_WK, 1024, 2048, 128),
                      (WK, b_WK, 1152, 2688, 64)]
            qparts = [(WQ, b_WQ, 0, 0, 512), (WQ, b_WQ, 512, 1536, 512), (WQ, b_WQ, 1024, 2176, 512),
                      (WQ, b_WQ, 1536, 2752, 8), (WQ, b_WQ, 1544, 2760, 512), (WQ, b_WQ, 2056, 3272, 512),
                      (WQ, b_WQ, 2568, 3784, 512), (WQ, b_WQ, 3080, 4296, 512)]
            for i, (dst, bdst, dc, sc, n) in enumerate(kparts + qparts):
                s = wst[i % 2]
                bs = b_wst[i % 2]
                dma("sp", s[:, :, 0:n], w_in_v[:, :, sc:sc + n], writes=[bs])
                op("pool", lambda s=s, dst=dst, dc=dc, n=n: POOL.tensor_tensor(
                    out=dst[:, :, dc:dc + n], in0=s[:, :, 0:n], in1=bc_last(gmix[:, :], n), op=ALU.mult),
                   reads=[bs, b_g], writes=[bdst])

            xs = [sb(st, f"xs{i}", [128, D], F32) for i in range(2)]
            b_xs = [Buf(), Buf()]
            cst = [sb(st, f"cst{i}", [128, 64], F32) for i in range(2)]
            b_cst = [Buf(), Buf()]
            junk = sb(st, "junk", [128, D], BF16)
            b_junk = Buf()
            ss = sb(st, "ss", [128, 1], F32)
            ms = sb(st, "ms", [128, 1], F32)
            rstd = sb(st, "rstd", [128, 1], F32)
            mhalf = sb(st, "mhalf", [128, 1], F32)
            b_ss, b_ms, b_rstd, b_mh = Buf(), Buf(), Buf(), Buf()
            op("pool", lambda: POOL.memset(mhalf[:], -0.5), writes=[b_mh])
            hb = sb(st, "hb", [128, D], BF16)
            b_hb = Buf()
            hT = sb(st, "hT", [128, 8, 128], BF16)
            b_hT = Buf()
            TRH = ps(st, "TRH", [128, 1024], BF16)
            b_TRH = Buf()
            PB = [ps(st, f"PB{i}", [128, 512], F32) for i in range(5)]
            b_PB = [Buf() for _ in range(5)]
            TRO = ps(st, "TRO", [128, 1024], BF16)
            b_TRO = Buf()
            rt = [sb(st, f"rt{i}", [128, 8, 32], F32) for i in range(4)]
            rbufs = tuple(rt) + tuple(Buf() for _ in range(4))
            kab = sb(st, "kab", [128, 8, 64], BF16)
            b_kab = Buf()
            kat = sb(st, "kat", [128, 512], BF16)
            b_kat = Buf()
            kbi = sb(st, "kbi", [128, 2, 64], BF16)
            b_kbi = Buf()
            kbit = sb(st, "kbit", [64, 256], BF16)
            b_kbit = Buf()
            vaa = sb(st, "vaa", [128, 8, 65], BF16)
            b_vaa = Buf()
            vba = sb(st, "vba", [128, 65], BF16)
            b_vba = Buf()
            qab = sb(st, "qab", [128, 8, 64], BF16)
            b_qab = Buf()
            qat = sb(st, "qat", [128, 512], BF16)
            b_qat = Buf()
            qbt = sb(st, "qbt", [64, 1024], BF16)
            b_qbt = Buf()
            wis = sb(st, "wis", [128, 8], F32)
            b_wis = Buf()
            gsb = sb(st, "gsb", [128, 2048], BF16)
            b_gsb = Buf()
            pbc = [0]

            def next_pb():
                i = pbc[0] % 5
                pbc[0] += 1
                return PB[i], b_PB[i]

            def proj(W, bW, c0, n, bank, bbank, o0=0):
                for kc in range(8):
                    op("pe", lambda kc=kc: PE.matmul(bank[:, o0:o0 + n], lhsT=hT[:, kc, :], rhs=W[:, kc, c0:c0 + n],
                                                     start=(kc == 0), stop=(kc == 7)),
                       reads=[b_hT, bW], writes=[bbank], sig=(kc == 7))

            for l in range(NB):
                x_ = xs[l % 2]
                bx = b_xs[l % 2]
                c_ = cst[l % 2]
                bc = b_cst[l % 2]
                dma("sp", x_[:], xl.ap()[l], writes=[bx])
                dma("sp", c_[:], cs_t.ap()[l], writes=[bc])
                op("act", lambda: ACT.activation(out=junk[:], in_=x_[:], func=AF.Square, accum_out=ss[:]),
                   reads=[bx], writes=[b_junk, b_ss])
                op("dve", lambda: DVE.tensor_scalar(out=ms[:], in0=ss[:], scalar1=1.0 / D, scalar2=EPS,
                                                    op0=ALU.mult, op1=ALU.add), reads=[b_ss], writes=[b_ms])
                op("pool", lambda: POOL.tensor_tensor(out=rstd[:], in0=ms[:], in1=mhalf[:], op=ALU.pow),
                   reads=[b_ms, b_mh], writes=[b_rstd])
                op("pool", lambda: POOL.tensor_scalar(out=hb[:], in0=x_[:], scalar1=rstd[:, 0:1], scalar2=0.0,
                                                      op0=ALU.mult, op1=ALU.add),
                   reads=[bx, b_rstd], writes=[b_hb])
                for kc in range(8):
                    op("pe", lambda kc=kc: PE.transpose(out=TRH[:, kc * 128:(kc + 1) * 128],
                                                        in_=hb[:, kc * 128:(kc + 1) * 128], identity=identb[:]),
                       reads=[b_hb, b_const], writes=[b_TRH], sig=(kc == 7))
                op("act", lambda: ACT.copy(out=hT[:].rearrange("p a b -> p (a b)"), in_=TRH[:]),
                   reads=[b_TRH], writes=[b_hT])
                pa, bpa = next_pb()
                proj(WK, b_WK, 0, 512, pa, bpa)
                pv, bpv = next_pb()
                proj(WK, b_WK, 512, 512, pv, bpv)
                pc, bpc = next_pb()
                proj(WK, b_WK, 1024, 128, pc, bpc, 0)
                proj(WK, b_WK, 1152, 64, pc, bpc, 128)
                rope(rbufs, pa[:].rearrange("p (h d) -> p h d", h=8), 8, c_, bc, bpa, kab[:], b_kab)
                for pr in range(4):
                    op("pe", lambda pr=pr: PE.transpose(out=TRO[:, pr * 128:(pr + 1) * 128],
                                                        in_=kab[:].rearrange("p h d -> p (h d)")[:, pr * 128:(pr + 1) * 128],
                                                        identity=identb[:]),
                       reads=[b_kab, b_const], writes=[b_TRO], sig=(pr == 3))
                op("act", lambda: ACT.copy(out=kat[:], in_=TRO[:, 0:512]), reads=[b_TRO], writes=[b_kat])
                dma("pool", KAT.ap()[:, l, :], kat[:], reads=[b_kat], writes=[b_KAT[l]])
                zc = AP(pc, 0, [[512, 128], [128, 2], [1, 64]])
                rope(rbufs, zc, 2, c_, bc, bpc, kbi[:], b_kbi)
                for hh in range(2):
                    op("pe", lambda hh=hh: PE.transpose(out=TRO[0:64, 512 + hh * 128:512 + (hh + 1) * 128],
                                                        in_=kbi[:, hh, :], identity=identb[:]),
                       reads=[b_kbi, b_const], writes=[b_TRO], sig=(hh == 1))
                op("act", lambda: ACT.copy(out=kbit[:], in_=TRO[0:64, 512:768]), reads=[b_TRO], writes=[b_kbit])
                dma("pool", KBT.ap()[:, l * 128:(l + 1) * 128], kbit[:, 0:128], reads=[b_kbit], writes=[b_KBT[l]])
                dma("pool", KIT.ap()[:, l * 128:(l + 1) * 128], kbit[:, 128:256], reads=[b_kbit], writes=[b_KIT[l]])
                op("act", lambda: ACT.activation(out=vaa[:, :, 0:64], in_=pv[:].rearrange("p (h d) -> p h d", h=8),
                                                 func=AF.Copy, scale=vmask[:, l:l + 1]),
                   reads=[bpv, b_const], writes=[b_vaa])
                op("pool", lambda: POOL.tensor_copy(out=vaa[:, :, 64:65], in_=bc_mid(vmask[:, l:l + 1], 8)),
                   reads=[b_const], writes=[b_vaa])
                dma("pool", VA.ap()[:, l, :], vaa[:].rearrange("p h d -> p (h d)"), reads=[b_vaa], writes=[b_VA[l]])
                op("act", lambda: ACT.activation(out=vba[:, 0:64], in_=pc[:, 64:128], func=AF.Copy,
                                                 scale=vmask[:, l:l + 1]), reads=[bpc, b_const], writes=[b_vba])
                op("pool", lambda: POOL.tensor_copy(out=vba[:, 64:65], in_=vmask[:, l:l + 1]),
                   reads=[b_const], writes=[b_vba])
                dma("pool", VB.ap()[:, l, :], vba[:], reads=[b_vba], writes=[b_VB[l]])
                if l % 4 != 3:
                    continue
                m = l // 4
                pq, bpq = next_pb()
                proj(WQ, b_WQ, 0, 512, pq, bpq)
                rope(rbufs, pq[:].rearrange("p (h d) -> p h d", h=8), 8, c_, bc, bpq, qab[:], b_qab)
                for pr in range(4):
                    op("pe", lambda pr=pr: PE.transpose(out=TRO[:, pr * 128:(pr + 1) * 128],
                                                        in_=qab[:].rearrange("p h d -> p (h d)")[:, pr * 128:(pr + 1) * 128],
                                                        identity=identb[:]),
                       reads=[b_qab, b_const], writes=[b_TRO], sig=(pr == 3))
                op("act", lambda: ACT.copy(out=qat[:], in_=TRO[:, 0:512]), reads=[b_TRO], writes=[b_qat])
                dma("pool", QAT.ap()[m], qat[:], reads=[b_qat], writes=[b_QAT[m]])
                for which, (c0, dstT, bdst) in enumerate([(512, QBT, b_QBT), (1024, QIT, b_QIT)]):
                    pq, bpq = next_pb()
                    proj(WQ, b_WQ, c0, 512, pq, bpq)
                    rope(rbufs, pq[:].rearrange("p (h d) -> p h d", h=8), 8, c_, bc, bpq, qab[:], b_qab)
                    for hh in range(8):
                        op("pe", lambda hh=hh: PE.transpose(out=TRO[0:64, hh * 128:(hh + 1) * 128],
                                                            in_=qab[:, hh, :], identity=identb[:]),
                           reads=[b_qab, b_const], writes=[b_TRO], sig=(hh == 7))
                    op("act", lambda: ACT.copy(out=qbt[:], in_=TRO[0:64, :]), reads=[b_TRO], writes=[b_qbt])
                    dma("pool", dstT.ap()[m], qbt[:], reads=[b_qbt], writes=[bdst[m]])
                pq, bpq = next_pb()
                proj(WQ, b_WQ, 1536, 8, pq, bpq)
                op("dve", lambda: DVE.tensor_copy(out=wis[:], in_=pq[:, 0:8]), reads=[bpq], writes=[b_wis])
                dma("pool", WI.ap()[m], wis[:], reads=[b_wis], writes=[b_WI[m]])
                for gq in range(4):
                    pq, bpq = next_pb()
                    proj(WQ, b_WQ, 1544 + gq * 512, 512, pq, bpq)
                    op("act", lambda gq=gq, pq=pq: ACT.activation(out=gsb[:, gq * 512:(gq + 1) * 512], in_=pq[:],
                                                                  func=AF.Sigmoid), reads=[bpq], writes=[b_gsb])
                dma("pool", GS.ap()[m], gsb[:], reads=[b_gsb], writes=[b_GS[m]])

        if _DBG == "1":
            kb.finish(b_KAT + b_VA + b_KBT + b_KIT + b_VB + b_QAT + b_QBT + b_QIT + b_WI + b_GS)
            return nc, dbg_outs

        kb.barrier()
        with ExitStack() as st:
            MT = sb(st, "MT", [128, 17 * 128], BF16)
            b_MT = Buf()
            dma("sp", MT[:], mt_d.ap().rearrange("p a b -> p (a b)"), writes=[b_MT])
            kw = [sb(st, f"kw{i}", [128, 17, 512], BF16) for i in range(2)]
            b_kw = [Buf(), Buf()]
            vw = [sb(st, f"vw{i}", [128, 17, 520], BF16) for i in range(2)]
            b_vw = [Buf(), Buf()]
            qa_t = [sb(st, f"qa{i}", [128, 512], BF16) for i in range(2)]
            b_qa = [Buf(), Buf()]
            STb = [ps(st, f"ST{i}", [128, 512], F32) for i in range(2)]
            b_ST = [Buf(), Buf()]
            OA = [ps(st, f"OA{i}", [128, 512], F32) for i in range(2)]
            b_OA = [Buf(), Buf()]
            TR = ps(st, "TR2", [128, 1024], BF16)
            b_TR = Buf()
            E = [sb(st, f"E{i}", [128, 512], BF16) for i in range(2)]
            b_E = [Buf(), Buf()]
            Pm = [sb(st, f"P{i}", [128, 512], BF16) for i in range(2)]
            b_P = [Buf(), Buf()]
            rc = sb(st, "rc", [128, 8], F32)
            b_rc = Buf()
            ya = sb(st, "ya", [128, 8, 64], BF16)
            b_ya = Buf()
            cnt = 0
            for m in range(NOWN):
                lq = 4 * m + 3
                lo = max(0, lq - 16)
                nb = lq - lo + 1
                k_, v_, q_ = kw[m % 2], vw[m % 2], qa_t[m % 2]
                bk, bv, bq = b_kw[m % 2], b_vw[m % 2], b_qa[m % 2]
                dma("sp", k_[:, 0:nb, :], KAT.ap()[:, lo:lq + 1, :], reads=b_KAT[lo:lq + 1], writes=[bk])
                dma("sp", v_[:, 0:nb, :], VA.ap()[:, lo:lq + 1, :], reads=b_VA[lo:lq + 1], writes=[bv])
                dma("sp", q_[:], QAT.ap()[m], reads=[b_QAT[m]], writes=[bq])
                first_bank = [True, True]
                groups = [list(range(i, min(i + 4, nb))) for i in range(0, nb, 4)]
                for h in range(8):
                    base = 64 * (h % 2)
                    pair = h // 2
                    ob = OA[h // 4]
                    bob = b_OA[h // 4]
                    for gi, grp in enumerate(groups):
                        n = len(grp)
                        bank, bb = STb[cnt % 2], b_ST[cnt % 2]
                        e_, be = E[cnt % 2], b_E[cnt % 2]
                        p_, bp = Pm[cnt % 2], b_P[cnt % 2]
                        cnt += 1
                        for i, dl in enumerate(grp):
                            slot = nb - 1 - dl
                            op("pe", lambda i=i, slot=slot: PE.matmul(
                                bank[:, i * 128:(i + 1) * 128],
                                lhsT=k_[base:base + 64, slot, pair * 128:(pair + 1) * 128],
                                rhs=q_[base:base + 64, pair * 128:(pair + 1) * 128], start=True, stop=True),
                               reads=[bk, bq], writes=[bb], sig=(i == n - 1))
                        op("act", lambda: ACT.activation(out=e_[:, 0:n * 128], in_=bank[:, 0:n * 128], func=AF.Exp,
                                                         scale=0.125), reads=[bb], writes=[be])
                        d0 = grp[0]
                        op("dve", lambda: DVE.tensor_tensor(out=p_[:, 0:n * 128], in0=e_[:, 0:n * 128],
                                                            in1=MT[:, d0 * 128:(d0 + n) * 128], op=ALU.mult),
                           reads=[be, b_MT], writes=[bp])
                        for i, dl in enumerate(grp):
                            slot = nb - 1 - dl
                            is_first = first_bank[h // 4]
                            first_bank[h // 4] = False
                            is_last = (h % 4 == 3 and gi == len(groups) - 1 and i == n - 1)
                            op("pe", lambda i=i, slot=slot, is_first=is_first, is_last=is_last: PE.matmul(
                                ob[:, (h % 4) * 65:(h % 4) * 65 + 65], lhsT=p_[:, i * 128:(i + 1) * 128],
                                rhs=v_[:, slot, h * 65:(h + 1) * 65], start=is_first, stop=is_last,
                                skip_group_check=True),
                               reads=[bp, bv], writes=[bob], sig=(i == n - 1))
                for bnk in range(2):
                    ov = OA[bnk][:, 0:260].rearrange("p (h d) -> p h d", d=65)
                    op("dve", lambda: DVE.reciprocal(out=rc[:, bnk * 4:(bnk + 1) * 4], in_=ov[:, :, 64]),
                       reads=[b_OA[bnk]], writes=[b_rc])
                    op("dve", lambda: DVE.tensor_tensor(out=ya[:, bnk * 4:(bnk + 1) * 4, :], in0=ov[:, :, 0:64],
                                                        in1=bc_last(rc[:, bnk * 4:(bnk + 1) * 4], 64), op=ALU.mult),
                       reads=[b_OA[bnk], b_rc], writes=[b_ya])
                ya2 = ya[:].rearrange("p h d -> p (h d)")
                for kc in range(4):
                    op("pe", lambda kc=kc: PE.transpose(out=TR[:, kc * 128:(kc + 1) * 128],
                                                        in_=ya2[:, kc * 128:(kc + 1) * 128], identity=identb[:]),
                       reads=[b_ya, b_const], writes=[b_TR], sig=(kc == 3))
                op("act", lambda: ACT.copy(out=YAT[:, :, m * 128:(m + 1) * 128],
                                           in_=TR[:, 0:512].rearrange("p (a b) -> p a b", b=128)),
                   reads=[b_TR], writes=[b_YAT[m]])

        if _DBG == "2":
            dy = dbg_out("YAT", [128, 4, NOWN * 128], BF16)
            bo = Buf()
            dma("sp", dy.ap(), YAT[:], reads=b_YAT, writes=[bo])
            kb.finish([bo])
            return nc, dbg_outs

        kb.barrier()
        with ExitStack() as st:
            KIs = sb(st, "KIs", [64, NB * 128], BF16)
            KBs = sb(st, "KBs", [64, NB * 128], BF16)
            VBs = sb(st, "VBs", [128, NB, 65], BF16)
            b_KIs, b_KBs, b_VBs = Buf(), Buf(), Buf()
            dma("sp", KIs[:], KIT.ap(), reads=b_KIT, writes=[b_KIs])
            dma("sp", KBs[:], KBT.ap(), reads=b_KBT, writes=[b_KBs])
            dma("sp", VBs[:], VB.ap(), reads=b_VB, writes=[b_VBs])
            padb = sb(st, "padb", [128, 512], F32)
            diagb = sb(st, "diagb", [128, 512], F32)
            pow2 = sb(st, "pow2", [128, NIT + 1], F32)
            b_c3 = Buf()
            dma("sp", padb[:], padbias_d.ap(), writes=[b_c3])
            dma("sp", diagb[:], diagbias_d.ap(), writes=[b_c3])
            dma("sp", pow2[:], pow2_d.ap(), writes=[b_c3])
            qi_t = [sb(st, f"qi{i}", [64, 1024], BF16) for i in range(2)]
            qb_t = [sb(st, f"qb{i}", [64, 1024], BF16) for i in range(2)]
            wi_t = [sb(st, f"wi{i}", [128, 8], F32) for i in range(2)]
            b_qi, b_qb, b_wi = [Buf(), Buf()], [Buf(), Buf()], [Buf(), Buf()]
            scores = sb(st, "scores", [128, NB * 128], F32)
            b_sc = [Buf() for _ in range(NOWN)]
            junkc = sb(st, "junkc", [128, NB * 128], BF16)
            b_jc = Buf()
            DP = [ps(st, f"DP{i}", [128, 512], F32) for i in range(2)]
            b_DP = [Buf(), Buf()]
            SC = ps(st, "SC", [128, 512], F32)
            b_SC = Buf()
            ST3 = [ps(st, f"ST3{i}", [128, 512], F32) for i in range(2)]
            b_ST3 = [Buf(), Buf()]
            OB = [ps(st, f"OB{i}", [128, 512], F32) for i in range(2)]
            b_OB = [Buf(), Buf()]
            TR3 = ps(st, "TR3", [128, 1024], BF16)
            b_TR3 = Buf()
            R = [sb(st, f"R{i}", [128, 512], BF16) for i in range(3)]
            b_R = [Buf() for _ in range(3)]
            absw = sb(st, "absw", [128, 8], F32)
            sgh = sb(st, "sgh", [128, 8], F32)
            Dg = sb(st, "Dg", [128, 8, 128], BF16)
            b_absw, b_sgh, b_Dg = Buf(), Buf(), Buf()
            cmin = sb(st, "cmin", [128, NOWN], F32)
            b_cmin = Buf()
            rmin = sb(st, "rmin", [128, 1], F32)
            rmax = sb(st, "rmax", [128, 1], F32)
            rng = sb(st, "rng", [128, 1], F32)
            tsum = sb(st, "tsum", [128, 1], F32)
            tau = [sb(st, f"tau{i}", [128, 1], F32) for i in range(2)]
            cntt = sb(st, "cntt", [128, 1], F32)
            sg = sb(st, "sg", [128, 1], F32)
            step2 = sb(st, "step2", [128, NIT + 1], F32)
            tsel = sb(st, "tsel", [128, 1], F32)
            b_rmin, b_rmax, b_rng, b_tsum, b_cnt, b_sg, b_step2, b_tsel = (Buf() for _ in range(8))
            b_tau = [Buf(), Buf()]
            sel = [sb(st, f"sel{i}", [128, 512], BF16) for i in range(2)]
            selT = [sb(st, f"selT{i}", [128, 512], BF16) for i in range(2)]
            b_sel, b_selT = [Buf(), Buf()], [Buf(), Buf()]
            Eb = [sb(st, f"Eb{i}", [128, 512], BF16) for i in range(2)]
            b_Eb = [Buf(), Buf()]
            rcb = sb(st, "rcb", [128, 8], F32)
            b_rcb = Buf()
            yb = sb(st, "yb", [128, 8, 64], BF16)
            b_yb = Buf()
            kd = 0
            kr = 0
            ks = 0
            for m in range(NOWN):
                nch = m + 1
                S = 512 * nch
                qi_, qb_, wi_ = qi_t[m % 2], qb_t[m % 2], wi_t[m % 2]
                bqi, bqb, bwi = b_qi[m % 2], b_qb[m % 2], b_wi[m % 2]
                dma("sp", qi_[:], QIT.ap()[m], reads=[b_QIT[m]], writes=[bqi])
                dma("sp", qb_[:], QBT.ap()[m], reads=[b_QBT[m]], writes=[bqb])
                dma("sp", wi_[:], WI.ap()[m], reads=[b_WI[m]], writes=[bwi])
                op("dve", lambda: DVE.tensor_scalar(out=sgh[:], in0=wi_[:], scalar1=0.0, scalar2=0.5,
                                                    op0=ALU.is_ge, op1=ALU.subtract), reads=[bwi], writes=[b_sgh])
                op("dve", lambda: DVE.scalar_tensor_tensor(out=absw[:], in0=sgh[:], scalar=2.0, in1=wi_[:],
                                                           op0=ALU.mult, op1=ALU.mult),
                   reads=[bwi, b_sgh], writes=[b_absw])
                for h in range(8):
                    op("dve", lambda h=h: DVE.tensor_scalar(out=Dg[:, h, :], in0=identb[:], scalar1=sgh[:, h:h + 1],
                                                            scalar2=2.0, op0=ALU.mult, op1=ALU.mult),
                       reads=[b_sgh, b_const], writes=[b_Dg])
                for c in range(nch):
                    for h in range(8):
                        dp, bdp = DP[kd % 2], b_DP[kd % 2]
                        kd += 1
                        r_, br = R[kr % 3], b_R[kr % 3]
                        kr += 1
                        op("pe", lambda h=h, dp=dp: PE.matmul(dp[:], lhsT=qi_[:, h * 128:(h + 1) * 128],
                                                              rhs=KIs[:, c * 512:(c + 1) * 512], start=True, stop=True),
                           reads=[bqi, b_KIs], writes=[bdp])
                        op("act", lambda h=h, dp=dp, r_=r_: ACT.activation(out=r_[:], in_=dp[:], func=AF.Relu,
                                                                           scale=absw[:, h:h + 1]),
                           reads=[bdp, b_absw], writes=[br])
                        op("pe", lambda h=h, r_=r_: PE.matmul(SC[:], lhsT=Dg[:, h, :], rhs=r_[:], start=(h == 0),
                                                              stop=(h == 7)),
                           reads=[br, b_Dg], writes=[b_SC])
                    sch = scores[:, c * 512:(c + 1) * 512]
                    op("dve", lambda: DVE.tensor_scalar(out=sch, in0=SC[:], scalar1=1.0, scalar2=None, op0=ALU.mult,
                                                        op1=ALU.min, accum_out=cmin[:, c:c + 1]),
                       reads=[b_SC], writes=[b_sc[c], b_cmin])
                    if c == 0:
                        op("dve", lambda: DVE.tensor_tensor(out=sch, in0=sch, in1=padb[:], op=ALU.add),
                           reads=[b_c3, b_sc[c]], writes=[b_sc[c]])
                    if c == nch - 1:
                        op("dve", lambda: DVE.tensor_tensor(out=sch, in0=sch, in1=diagb[:], op=ALU.add),
                           reads=[b_c3, b_sc[c]], writes=[b_sc[c]])
                bsc = b_sc[0:nch]
                op("dve", lambda: DVE.tensor_reduce(out=rmax[:], in_=scores[:, 0:S], axis=AX.X, op=ALU.max),
                   reads=bsc, writes=[b_rmax])
                op("dve", lambda: DVE.tensor_reduce(out=rmin[:], in_=cmin[:, 0:nch], axis=AX.X, op=ALU.min),
                   reads=[b_cmin], writes=[b_rmin])
                op("dve", lambda: DVE.scalar_tensor_tensor(out=rng[:], in0=rmax[:], scalar=2.0, in1=rmin[:],
                                                           op0=ALU.add, op1=ALU.subtract),
                   reads=[b_rmax, b_rmin], writes=[b_rng])
                op("dve", lambda: DVE.tensor_tensor(out=tsum[:], in0=rmax[:], in1=rmin[:], op=ALU.add),
                   reads=[b_rmax, b_rmin], writes=[b_tsum])
                op("dve", lambda: DVE.tensor_scalar(out=tau[0][:], in0=tsum[:], scalar1=0.5, scalar2=None, op0=ALU.mult),
                   reads=[b_tsum], writes=[b_tau[0]])
                op("dve", lambda: DVE.tensor_scalar(out=step2[:], in0=pow2[:], scalar1=rng[:, 0:1], scalar2=None,
                                                    op0=ALU.mult), reads=[b_rng, b_c3], writes=[b_step2])
                for it in range(NIT):
                    tc_, tn_ = tau[it % 2], tau[(it + 1) % 2]
                    btc, btn = b_tau[it % 2], b_tau[(it + 1) % 2]
                    op("dve", lambda: DVE.tensor_scalar(out=junkc[:, 0:S], in0=scores[:, 0:S], scalar1=tc_[:, 0:1],
                                                        scalar2=None, op0=ALU.is_ge, op1=ALU.add, accum_out=cntt[:]),
                       reads=bsc + [btc], writes=[b_jc, b_cnt])
                    op("dve", lambda: DVE.tensor_scalar(out=sg[:], in0=cntt[:], scalar1=255.5, scalar2=0.5,
                                                        op0=ALU.is_ge, op1=ALU.subtract), reads=[b_cnt], writes=[b_sg])
                    op("dve", lambda: DVE.scalar_tensor_tensor(out=tn_[:], in0=sg[:], scalar=step2[:, it:it + 1],
                                                               in1=tc_[:], op0=ALU.mult, op1=ALU.add),
                       reads=[b_sg, b_step2, btc], writes=[btn])
                tf_, btf = tau[NIT % 2], b_tau[NIT % 2]
                op("dve", lambda: DVE.tensor_tensor(out=tsel[:], in0=tf_[:], in1=step2[:, NIT:NIT + 1], op=ALU.subtract),
                   reads=[btf, b_step2], writes=[b_tsel])
                first_ob = [True, True]
                for c in range(nch):
                    s_, bs_ = sel[c % 2], b_sel[c % 2]
                    sT_, bsT_ = selT[c % 2], b_selT[c % 2]
                    op("dve", lambda: DVE.tensor_scalar(out=s_[:], in0=scores[:, c * 512:(c + 1) * 512],
                                                        scalar1=tsel[:, 0:1], scalar2=None, op0=ALU.is_ge),
                       reads=[b_sc[c], b_tsel], writes=[bs_])
                    for kq in range(4):
                        op("pe", lambda kq=kq: PE.transpose(out=TR3[:, kq * 128:(kq + 1) * 128],
                                                            in_=s_[:, kq * 128:(kq + 1) * 128], identity=identb[:]),
                           reads=[bs_, b_const], writes=[b_TR3], sig=(kq == 3))
                    op("act", lambda: ACT.activation(out=sT_[:], in_=TR3[:, 0:512], func=AF.Identity, scale=NEGM,
                                                     bias=-NEGM), reads=[b_TR3], writes=[bsT_])
                    for kq in range(4):
                        ls = 4 * c + kq
                        for i in range(2):
                            bank, bb = ST3[ks % 2], b_ST3[ks % 2]
                            e_, be = Eb[ks % 2], b_Eb[ks % 2]
                            ks += 1
                            bank3 = bank[:].rearrange("p (a b) -> p a b", b=128)
                            op("pe", lambda i=i, ls=ls, bank3=bank3: PE.matmul(
                                bank3, lhsT=KBs[:, ls * 128:(ls + 1) * 128],
                                rhs=qb_[:, i * 512:(i + 1) * 512].rearrange("p (a b) -> p a b", b=128),
                                start=True, stop=False),
                               reads=[b_KBs, bqb], writes=[bb], sig=False)
                            op("pe", lambda kq=kq, bank3=bank3: PE.matmul(
                                bank3, lhsT=identb[:], rhs=bc_mid(sT_[:, kq * 128:(kq + 1) * 128], 4),
                                start=False, stop=True),
                               reads=[bsT_, b_const], writes=[bb])
                            op("act", lambda bank=bank, e_=e_: ACT.activation(out=e_[:], in_=bank[:], func=AF.Exp,
                                                                              scale=0.125), reads=[bb], writes=[be])
                            for hh in range(4):
                                is_first = first_ob[i]
                                first_ob[i] = False
                                is_last = (c == nch - 1 and kq == 3 and hh == 3)
                                op("pe", lambda hh=hh, i=i, ls=ls, e_=e_, is_first=is_first, is_last=is_last: PE.matmul(
                                    OB[i][:, hh * 65:hh * 65 + 65], lhsT=e_[:, hh * 128:(hh + 1) * 128],
                                    rhs=VBs[:, ls, :], start=is_first, stop=is_last, skip_group_check=True),
                                   reads=[be, b_VBs], writes=[b_OB[i]], sig=(hh == 3))
                for bnk in range(2):
                    ov = OB[bnk][:, 0:260].rearrange("p (h d) -> p h d", d=65)
                    op("dve", lambda: DVE.reciprocal(out=rcb[:, bnk * 4:(bnk + 1) * 4], in_=ov[:, :, 64]),
                       reads=[b_OB[bnk]], writes=[b_rcb])
                    op("dve", lambda: DVE.tensor_tensor(out=yb[:, bnk * 4:(bnk + 1) * 4, :], in0=ov[:, :, 0:64],
                                                        in1=bc_last(rcb[:, bnk * 4:(bnk + 1) * 4], 64), op=ALU.mult),
                       reads=[b_OB[bnk], b_rcb], writes=[b_yb])
                yb2 = yb[:].rearrange("p h d -> p (h d)")
                for kc in range(4):
                    op("pe", lambda kc=kc: PE.transpose(out=TR3[:, kc * 128:(kc + 1) * 128],
                                                        in_=yb2[:, kc * 128:(kc + 1) * 128], identity=identb[:]),
                       reads=[b_yb, b_const], writes=[b_TR3], sig=(kc == 3))
                op("act", lambda: ACT.copy(out=YBT[:, :, m * 128:(m + 1) * 128],
                                           in_=TR3[:, 0:512].rearrange("p (a b) -> p a b", b=128)),
                   reads=[b_TR3], writes=[b_YBT[m]])

        if _DBG == "3":
            dy = dbg_out("YBT", [128, 4, NOWN * 128], BF16)
            bo = Buf()
            dma("sp", dy.ap(), YBT[:], reads=b_YBT, writes=[bo])
            kb.finish([bo])
            return nc, dbg_outs

        kb.barrier()
        with ExitStack() as st45:
            H2T = sb(st45, "H2T", [128, 8, NOWN * 128], BF16)
            b_H2T = [Buf() for _ in range(NOWN)]
            ss = sb(st45, "ss2", [128, 1], F32)
            ms = sb(st45, "ms2", [128, 1], F32)
            rstd = sb(st45, "rstd2", [128, 1], F32)
            mhalf = sb(st45, "mhalf2", [128, 1], F32)
            b_ss, b_ms, b_rstd, b_mh = Buf(), Buf(), Buf(), Buf()
            op("pool", lambda: POOL.memset(mhalf[:], -0.5), writes=[b_mh])
            junk = sb(st45, "junk2", [128, D], BF16)
            b_junk = Buf()
            with ExitStack() as st:
                WUA = sb(st, "WUA", [128, 4, D], BF16)
                WUB = sb(st, "WUB", [128, 4, D], BF16)
                WO = sb(st, "WO", [128, 8, D], BF16)
                wst2 = sb(st, "wst2", [128, 8, D], F32)
                b_w2, b_WUA, b_WUB, b_WO = Buf(), Buf(), Buf(), Buf()
                dma("sp", wst2[:, 0:4, :], w_up_a.ap().rearrange("(kc p) n -> p kc n", p=128), writes=[b_w2])
                op("pool", lambda: POOL.tensor_copy(out=WUA[:], in_=wst2[:, 0:4, :]), reads=[b_w2], writes=[b_WUA])
                dma("sp", wst2[:, 0:4, :], w_up_b.ap().rearrange("(kc p) n -> p kc n", p=128), writes=[b_w2])
                op("pool", lambda: POOL.tensor_copy(out=WUB[:], in_=wst2[:, 0:4, :]), reads=[b_w2], writes=[b_WUB])
                dma("sp", wst2[:], w_out.ap().rearrange("(kc p) n -> p kc n", p=128), writes=[b_w2])
                op("pool", lambda: POOL.tensor_copy(out=WO[:], in_=wst2[:]), reads=[b_w2], writes=[b_WO])
                gs_t = [sb(st, f"gs{i}", [128, 2048], BF16) for i in range(2)]
                x_t = [sb(st, f"x4{i}", [128, D], F32) for i in range(2)]
                b_gs, b_x4 = [Buf(), Buf()], [Buf(), Buf()]
                UA = [ps(st, f"UA{i}", [128, 512], F32) for i in range(2)]
                UB = [ps(st, f"UB{i}", [128, 512], F32) for i in range(2)]
                WOp = [ps(st, f"WOp{i}", [128, 512], F32) for i in range(2)]
                TR4 = ps(st, "TR4", [128, 1024], BF16)
                b_UA, b_UB, b_WOp = [Buf(), Buf()], [Buf(), Buf()], [Buf(), Buf()]
                b_TR4 = Buf()
                t1 = [sb(st, f"t1{i}", [128, 512], F32) for i in range(2)]
                t2 = [sb(st, f"t2{i}", [128, 512], F32) for i in range(2)]
                b_t1, b_t2 = [Buf(), Buf()], [Buf(), Buf()]
                mg = sb(st, "mg", [128, D], BF16)
                mgT = sb(st, "mgT", [128, 8, 128], BF16)
                b_mg, b_mgT = Buf(), Buf()
                x2 = [sb(st, f"x2{i}", [128, D], F32) for i in range(2)]
                b_x2 = [Buf(), Buf()]
                h2b = sb(st, "h2b", [128, D], BF16)
                b_h2b = Buf()
                for m in range(NOWN):
                    g_, bg_ = gs_t[m % 2], b_gs[m % 2]
                    x_, bx_ = x_t[m % 2], b_x4[m % 2]
                    x2_, bx2_ = x2[m % 2], b_x2[m % 2]
                    dma("sp", g_[:], GS.ap()[m], reads=[b_GS[m]], writes=[bg_])
                    dma("sp", x_[:], xl.ap()[4 * m + 3], writes=[bx_])
                    tk = slice(m * 128, (m + 1) * 128)
                    for half in range(2):
                        cs_ = slice(half * 512, (half + 1) * 512)
                        for kc in range(4):
                            op("pe", lambda kc=kc: PE.matmul(UA[half][:], lhsT=YAT[:, kc, tk], rhs=WUA[:, kc, cs_],
                                                             start=(kc == 0), stop=(kc == 3)),
                               reads=[b_YAT[m], b_WUA], writes=[b_UA[half]], sig=(kc == 3))
                        for kc in range(4):
                            op("pe", lambda kc=kc: PE.matmul(UB[half][:], lhsT=YBT[:, kc, tk], rhs=WUB[:, kc, cs_],
                                                             start=(kc == 0), stop=(kc == 3)),
                               reads=[b_YBT[m], b_WUB], writes=[b_UB[half]], sig=(kc == 3))
                        op("dve", lambda: DVE.tensor_tensor(out=t1[half][:], in0=UA[half][:], in1=g_[:, cs_], op=ALU.mult),
                           reads=[b_UA[half], bg_], writes=[b_t1[half]])
                        op("dve", lambda: DVE.tensor_tensor(out=t2[half][:], in0=UB[half][:],
                                                            in1=g_[:, 1024 + half * 512:1024 + (half + 1) * 512], op=ALU.mult),
                           reads=[b_UB[half], bg_], writes=[b_t2[half]])
                        op("pool", lambda: POOL.tensor_tensor(out=mg[:, cs_], in0=t1[half][:], in1=t2[half][:], op=ALU.add),
                           reads=[b_t1[half], b_t2[half]], writes=[b_mg])
                    for kc in range(8):
                        op("pe", lambda kc=kc: PE.transpose(out=TR4[:, kc * 128:(kc + 1) * 128],
                                                            in_=mg[:, kc * 128:(kc + 1) * 128], identity=identb[:]),
                           reads=[b_mg, b_const], writes=[b_TR4], sig=(kc == 7))
                    op("act", lambda: ACT.copy(out=mgT[:].rearrange("p a b -> p (a b)"), in_=TR4[:]),
                       reads=[b_TR4], writes=[b_mgT])
                    for half in range(2):
                        cs_ = slice(half * 512, (half + 1) * 512)
                        for kc in range(8):
                            op("pe", lambda kc=kc: PE.matmul(WOp[half][:], lhsT=mgT[:, kc, :], rhs=WO[:, kc, cs_],
                                                             start=(kc == 0), stop=(kc == 7)),
                               reads=[b_mgT, b_WO], writes=[b_WOp[half]], sig=(kc == 7))
                        op("dve", lambda: DVE.tensor_tensor(out=x2_[:, cs_], in0=WOp[half][:], in1=x_[:, cs_], op=ALU.add),
                           reads=[b_WOp[half], bx_], writes=[bx2_])
                    dma("pool", X2.ap()[m], x2_[:], reads=[bx2_], writes=[b_X2[m]])
                    op("act", lambda: ACT.activation(out=junk[:], in_=x2_[:], func=AF.Square, accum_out=ss[:]),
                       reads=[bx2_], writes=[b_junk, b_ss])
                    op("dve", lambda: DVE.tensor_scalar(out=ms[:], in0=ss[:], scalar1=1.0 / D, scalar2=EPS,
                                                        op0=ALU.mult, op1=ALU.add), reads=[b_ss], writes=[b_ms])
                    op("pool", lambda: POOL.tensor_tensor(out=rstd[:], in0=ms[:], in1=mhalf[:], op=ALU.pow),
                       reads=[b_ms, b_mh], writes=[b_rstd])
                    op("pool", lambda: POOL.tensor_scalar(out=h2b[:], in0=x2_[:], scalar1=rstd[:, 0:1], scalar2=0.0,
                                                          op0=ALU.mult, op1=ALU.add),
                       reads=[bx2_, b_rstd], writes=[b_h2b])
                    for kc in range(8):
                        op("pe", lambda kc=kc: PE.transpose(out=TR4[:, kc * 128:(kc + 1) * 128],
                                                            in_=h2b[:, kc * 128:(kc + 1) * 128], identity=identb[:]),
                           reads=[b_h2b, b_const], writes=[b_TR4], sig=(kc == 7))
                    op("act", lambda: ACT.copy(out=H2T[:, :, tk], in_=TR4[:].rearrange("p (a b) -> p a b", b=128)),
                       reads=[b_TR4], writes=[b_H2T[m]])

            if _DBG == "4":
                dx = dbg_out("X2o", [NOWN, 128, D], F32)
                bo = Buf()
                with ExitStack() as st:
                    tt = sb(st, "dbgt", [128, D], F32)
                    bt = Buf()
                    for m in range(NOWN):
                        dma("sp", tt[:], X2.ap()[m], reads=[b_X2[m]], writes=[bt])
                        dma("sp", dx.ap()[m], tt[:], reads=[bt], writes=[bo])
                    kb.finish([bo])
                return nc, dbg_outs

            kb.barrier()
            with ExitStack() as st:
                gffn = sb(st, "gffn", [128, 8], F32)
                gfin = sb(st, "gfin", [128, D], F32)
                b_g5 = Buf()
                dma("sp", gffn[:], gffn_d.ap(), writes=[b_g5])
                dma("sp", gfin[:], gfin_d.ap(), writes=[b_g5])
                ACTT = sb(st, "ACTT", [128, NFF, NOWN * 128], BF16)
                b_ACTT = [Buf() for _ in range(4)]
                with ExitStack() as sg_:
                    wgst = [sb(sg_, f"wgst{i}", [128, 8, 128], F32) for i in range(2)]
                    wust = [sb(sg_, f"wust{i}", [128, 8, 128], F32) for i in range(2)]
                    wgb = [sb(sg_, f"wgb{i}", [128, 8, 128], BF16) for i in range(2)]
                    wub = [sb(sg_, f"wub{i}", [128, 8, 128], BF16) for i in range(2)]
                    b_wgst, b_wust, b_wgb, b_wub = ([Buf(), Buf()] for _ in range(4))
                    G = [ps(sg_, f"G{i}", [128, 512], F32) for i in range(2)]
                    U = [ps(sg_, f"U{i}", [128, 512], F32) for i in range(2)]
                    b_G, b_U = [Buf(), Buf()], [Buf(), Buf()]
                    sgt = [sb(sg_, f"sgt{i}", [128, 512], F32) for i in range(2)]
                    b_sgt = [Buf(), Buf()]
                    wgv = w_gate.ap().rearrange("(kc p) n -> p kc n", p=128)
                    wuv = w_up.ap().rearrange("(kc p) n -> p kc n", p=128)
                    kk = 0
                    for ffc in range(NFF):
                        i2 = ffc % 2
                        dma("sp", wgst[i2][:], wgv[:, :, ffc * 128:(ffc + 1) * 128], writes=[b_wgst[i2]])
                        dma("sp", wust[i2][:], wuv[:, :, ffc * 128:(ffc + 1) * 128], writes=[b_wust[i2]])
                        op("pool", lambda: POOL.tensor_tensor(out=wgb[i2][:], in0=wgst[i2][:], in1=bc_last(gffn[:, :], 128),
                                                              op=ALU.mult), reads=[b_wgst[i2], b_g5], writes=[b_wgb[i2]])
                        op("pool", lambda: POOL.tensor_tensor(out=wub[i2][:], in0=wust[i2][:], in1=bc_last(gffn[:, :], 128),
                                                              op=ALU.mult), reads=[b_wust[i2], b_g5], writes=[b_wub[i2]])
                        for tg in range(4):
                            ts_ = slice(tg * 512, (tg + 1) * 512)
                            g_, bg_ = G[kk % 2], b_G[kk % 2]
                            u_, bu_ = U[kk % 2], b_U[kk % 2]
                            s_, bs_ = sgt[kk % 2], b_sgt[kk % 2]
                            kk += 1
                            for kc in range(8):
                                op("pe", lambda kc=kc, g_=g_: PE.matmul(g_[:], lhsT=wgb[i2][:, kc, :], rhs=H2T[:, kc, ts_],
                                                                        start=(kc == 0), stop=(kc == 7)),
                                   reads=[b_wgb[i2]] + b_H2T[tg * 4:(tg + 1) * 4], writes=[bg_], sig=(kc == 7))
                            for kc in range(8):
                                op("pe", lambda kc=kc, u_=u_: PE.matmul(u_[:], lhsT=wub[i2][:, kc, :], rhs=H2T[:, kc, ts_],
                                                                        start=(kc == 0), stop=(kc == 7)),
                                   reads=[b_wub[i2]] + b_H2T[tg * 4:(tg + 1) * 4], writes=[bu_], sig=(kc == 7))
                            op("act", lambda g_=g_, s_=s_: ACT.activation(out=s_[:], in_=g_[:], func=AF.Silu),
                               reads=[bg_], writes=[bs_])
                            op("dve", lambda u_=u_, s_=s_: DVE.tensor_tensor(out=ACTT[:, ffc, ts_], in0=u_[:], in1=s_[:],
                                                                             op=ALU.mult),
                               reads=[bu_, bs_], writes=[b_ACTT[tg]])
                kb.barrier()
                with ExitStack() as sd_:
                    wdst = [sb(sd_, f"wdst{i}", [128, D], F32) for i in range(2)]
                    wdb = [sb(sd_, f"wdb{i}", [128, D], BF16) for i in range(2)]
                    b_wdst, b_wdb = [Buf(), Buf()], [Buf(), Buf()]
                    DN = [ps(sd_, f"DN{i}", [128, 512], F32) for i in range(8)]
                    b_DN = [Buf() for _ in range(8)]
                    x2t = [sb(sd_, f"x2t{i}", [128, D], F32) for i in range(2)]
                    x3 = [sb(sd_, f"x3{i}", [128, D], F32) for i in range(2)]
                    ot = [sb(sd_, f"ot{i}", [128, D], F32) for i in range(2)]
                    b_x2t, b_x3, b_ot = [Buf(), Buf()], [Buf(), Buf()], [Buf(), Buf()]
                    kw_ = 0
                    for tg in range(4):
                        for ffc in range(NFF):
                            i2 = kw_ % 2
                            kw_ += 1
                            dma("sp", wdst[i2][:], w_down.ap()[ffc * 128:(ffc + 1) * 128, :], writes=[b_wdst[i2]])
                            op("pool", lambda: POOL.tensor_copy(out=wdb[i2][:], in_=wdst[i2][:]),
                               reads=[b_wdst[i2]], writes=[b_wdb[i2]])
                            for tb in range(4):
                                mm = tg * 4 + tb
                                for half in range(2):
                                    op("pe", lambda tb=tb, half=half, mm=mm: PE.matmul(
                                        DN[tb * 2 + half][:], lhsT=ACTT[:, ffc, mm * 128:(mm + 1) * 128],
                                        rhs=wdb[i2][:, half * 512:(half + 1) * 512], start=(ffc == 0), stop=(ffc == NFF - 1)),
                                       reads=[b_wdb[i2], b_ACTT[tg]], writes=[b_DN[tb * 2 + half]],
                                       sig=(tb == 3 and half == 1))
                        for tb in range(4):
                            mm = tg * 4 + tb
                            xx, bxx = x2t[mm % 2], b_x2t[mm % 2]
                            x3_, bx3 = x3[mm % 2], b_x3[mm % 2]
                            o_, bo_ = ot[mm % 2], b_ot[mm % 2]
                            dma("sp", xx[:], X2.ap()[mm], reads=[b_X2[mm]], writes=[bxx])
                            for half in range(2):
                                cs_ = slice(half * 512, (half + 1) * 512)
                                op("dve", lambda: DVE.tensor_tensor(out=x3_[:, cs_], in0=DN[tb * 2 + half][:], in1=xx[:, cs_],
                                                                    op=ALU.add),
                                   reads=[b_DN[tb * 2 + half], bxx], writes=[bx3])
                            op("act", lambda: ACT.activation(out=junk[:], in_=x3_[:], func=AF.Square, accum_out=ss[:]),
                               reads=[bx3], writes=[b_junk, b_ss])
                            op("dve", lambda: DVE.tensor_scalar(out=ms[:], in0=ss[:], scalar1=1.0 / D, scalar2=EPS,
                                                                op0=ALU.mult, op1=ALU.add), reads=[b_ss], writes=[b_ms])
                            op("pool", lambda: POOL.tensor_tensor(out=rstd[:], in0=ms[:], in1=mhalf[:], op=ALU.pow),
                               reads=[b_ms, b_mh], writes=[b_rstd])
                            op("dve", lambda: DVE.scalar_tensor_tensor(out=o_[:], in0=x3_[:], scalar=rstd[:, 0:1], in1=gfin[:],
                                                                       op0=ALU.mult, op1=ALU.mult),
                               reads=[bx3, b_rstd, b_g5], writes=[bo_])
                            dma("sp", out_d.ap()[mm], o_[:], reads=[bo_], writes=[b_out[mm]])

        kb.finish(b_out)
    return nc, dbg_outs


def host_prep(x, norm_mix, w_in, w_up_a, w_up_b, w_out, norm_ffn, w_gate, w_up, w_down, norm_final):
    B, T, _ = x.shape
    x = np.asarray(x, np.float32)
    half = 32
    inv_freq = (10000.0 ** (-np.arange(half, dtype=np.float32) / half)).astype(np.float32)
    identb = np.eye(128, dtype=np.float32).astype(ml_dtypes.bfloat16)
    s_i = np.arange(128)[:, None]
    t_i = np.arange(128)[None, :]
    mt = np.zeros((128, 17, 128), np.float32)
    for dl in range(17):
        diff = 128 * dl + t_i - s_i
        tot = np.zeros((128, 128), np.float32)
        for (wdw, dil) in ((128, 1), (512, 4), (2048, 16)):
            ok = (diff >= 0) & (diff <= wdw) & (diff % dil == 0)
            tot += ok.astype(np.float32)
        mt[:, dl, :] = tot
    mt = mt.astype(ml_dtypes.bfloat16)
    diagbias = np.zeros((128, 512), np.float32)
    diagbias[:, 384:512] = np.where(np.arange(128)[None, :] > np.arange(128)[:, None], -BIG, 0.0)
    pow2 = np.broadcast_to((2.0 ** (-(np.arange(NIT + 1) + 1.0))).astype(np.float32)[None, :], (128, NIT + 1)).copy()
    gmix = np.ascontiguousarray(np.asarray(norm_mix, np.float32).reshape(8, 128).T)
    gffn = np.ascontiguousarray(np.asarray(norm_ffn, np.float32).reshape(8, 128).T)
    gfin = np.ascontiguousarray(np.broadcast_to(np.asarray(norm_final, np.float32)[None, :], (128, D)))
    common = {
        "identb": identb, "mt": mt, "diagbias": diagbias, "pow2": pow2, "gmix": gmix, "gffn": gffn, "gfin": gfin,
        "w_in": np.ascontiguousarray(np.asarray(w_in, np.float32)[0]),
        "w_up_a": np.ascontiguousarray(np.asarray(w_up_a, np.float32)[0]),
        "w_up_b": np.ascontiguousarray(np.asarray(w_up_b, np.float32)[0]),
        "w_out": np.ascontiguousarray(np.asarray(w_out, np.float32)[0]),
        "w_gate": np.ascontiguousarray(np.asarray(w_gate, np.float32)[0]),
        "w_up": np.ascontiguousarray(np.asarray(w_up, np.float32)[0]),
        "w_down": np.ascontiguousarray(np.asarray(w_down, np.float32)[0]),
    }
    in_maps = []
    for core in range(8):
        b, j = core // 4, core % 4
        xl = np.zeros((NB, 128, D), np.float32)
        pos = np.zeros((NB, 128), np.float32)
        valid = np.zeros((NB,), np.float32)
        for l in range(NB):
            g = l + j - 3
            if g >= 0:
                xl[l] = x[b, g * 128:(g + 1) * 128]
                pos[l] = np.arange(g * 128, (g + 1) * 128, dtype=np.float32)
                valid[l] = 1.0
        ang = pos[:, :, None] * inv_freq[None, None, :]
        cs = np.concatenate([np.cos(ang), np.sin(ang)], axis=-1).astype(np.float32)
        vmask = np.ascontiguousarray(np.broadcast_to(valid[None, :], (128, NB))).astype(np.float32)
        padbias = np.zeros((128, 512), np.float32)
        for l in range(4):
            if valid[l] == 0.0:
                padbias[:, l * 128:(l + 1) * 128] = -BIG
        m = dict(common)
        m.update({"xl": xl, "cs": cs, "vmask": vmask, "padbias": padbias})
        in_maps.append(m)
    return in_maps


def kernel(x, norm_mix, w_in, w_up_a, w_up_b, w_out, norm_ffn, w_gate, w_up, w_down, norm_final):
    in_maps = host_prep(x, norm_mix, w_in, w_up_a, w_up_b, w_out, norm_ffn, w_gate, w_up, w_down, norm_final)
    nc, dbg = build()
    res = run_bass_kernel_spmd(nc, in_maps, core_ids=list(range(8)))
    if _DBG:
        return res
    B, T, _ = x.shape
    out = np.zeros((B, T, D), np.float32)
    for core in range(8):
        b, j = core // 4, core % 4
        o = res.results[core]["out"]
        for m in range(NOWN):
            g = 4 * m + j
            out[b, g * 128:(g + 1) * 128] = o[m]
    return out
```

```python
import os
from contextlib import ExitStack

import ml_dtypes
import numpy as np

import concourse.bass as bass
import concourse.mybir as mybir
from concourse.bass_types import AP
from concourse.bass_utils import run_bass_kernel_spmd

F32 = mybir.dt.float32
BF16 = mybir.dt.bfloat16
AF = mybir.ActivationFunctionType
ALU = mybir.AluOpType
AX = mybir.AxisListType

NB = 64
NOWN = 16
D = 1024
DFF = 2816
NFF = DFF // 128
DIN = 4808
NIT = 22
BIG = 1.0e30
NEGM = 30000.0
EPS = 1e-6
NDS = 12

_DBG = os.environ.get("KDBG", "")


class Buf:
    __slots__ = ("w", "r")

    def __init__(self):
        self.w = None
        self.r = {}


class KB:
    def __init__(self, nc):
        self.nc = nc
        self.eng = {"pe": nc.tensor, "act": nc.scalar, "dve": nc.vector, "pool": nc.gpsimd, "sp": nc.sync}
        self.sem = {e: nc.alloc_semaphore(name=f"s_{e}") for e in self.eng}
        self.cnt = {e: 0 for e in self.eng}
        self.waited = {e: {} for e in self.eng}
        self.dq = {}
        for q in ("sp", "pool"):
            self.dq[q] = {"sems": [nc.alloc_semaphore(name=f"d_{q}{i}") for i in range(NDS)], "k": 0}

    def _wait(self, e, ev):
        sem, val = ev
        if e == "pe" and sem is self.sem["pe"]:
            return
        key = sem.num
        if self.waited[e].get(key, 0) >= val:
            return
        self.eng[e].wait_ge(sem, val)
        self.waited[e][key] = val

    def _deps(self, e, reads, writes):
        for b in reads:
            if b.w is not None:
                self._wait(e, b.w)
        for b in writes:
            if b.w is not None:
                self._wait(e, b.w)
            for ev in b.r.values():
                self._wait(e, ev)

    def _mark(self, ev, reads, writes):
        key = ev[0].num
        for b in reads:
            old = b.r.get(key)
            if old is None or old[1] < ev[1]:
                b.r[key] = ev
        for b in writes:
            b.w = ev
            b.r = {}

    def op(self, e, fn, reads=(), writes=(), sig=True):
        self._deps(e, reads, writes)
        inst = fn()
        if sig:
            self.cnt[e] += 1
            inst.then_inc(self.sem[e], 1)
            ev = (self.sem[e], self.cnt[e])
        else:
            ev = (self.sem[e], self.cnt[e] + 1)
        self._mark(ev, reads, writes)
        return inst

    def dma(self, q, out, in_, reads=(), writes=()):
        dq = self.dq[q]
        k = dq["k"]
        P = len(dq["sems"])
        sem = dq["sems"][k % P]
        if k >= P:
            self._wait(q, (sem, 16 * (k // P)))
        self._deps(q, reads, writes)
        self.eng[q].dma_start(out=out, in_=in_).then_inc(sem, 16)
        dq["k"] += 1
        ev = (sem, 16 * (k // P + 1))
        self._mark(ev, reads, writes)

    def barrier(self):
        evs = [(self.sem[e], self.cnt[e]) for e in self.eng if self.cnt[e] > 0]
        for q, dq in self.dq.items():
            k = dq["k"]
            P = len(dq["sems"])
            for i in range(min(k, P)):
                kk = k - 1 - i
                evs.append((dq["sems"][kk % P], 16 * (kk // P + 1)))
        for e in self.eng:
            for ev in evs:
                if ev[0] is self.sem[e]:
                    continue
                self._wait(e, ev)

    def finish(self, bufs):
        for b in bufs:
            if b.w is not None:
                self._wait("sp", b.w)
        for q, dq in self.dq.items():
            k = dq["k"]
            P = len(dq["sems"])
            for i in range(min(k, P)):
                kk = k - 1 - i
                self._wait(q, (dq["sems"][kk % P], 16 * (kk // P + 1)))


def bc_mid(ap2d, n):
    a = [list(x) for x in ap2d.ap]
    assert len(a) == 2
    return AP(ap2d.tensor, ap2d.offset, [a[0], [0, n], a[1]])


def bc_last(ap, n):
    a = [list(x) for x in ap.ap]
    return AP(ap.tensor, ap.offset, a + [[0, n]])


def build():
    nc = bass.Bass("TRN2", target_bir_lowering=False)
    kb = KB(nc)
    op = kb.op
    dma = kb.dma
    PE, ACT, DVE, POOL = nc.tensor, nc.scalar, nc.vector, nc.gpsimd

    def din(name, shape, dt=F32):
        return nc.dram_tensor(name, list(shape), dt, kind="ExternalInput")

    def dscr(name, shape, dt):
        return nc.dram_tensor(name, list(shape), dt, kind=("ExternalOutput" if _DBG else "Internal"))

    xl = din("xl", [NB, 128, D])
    cs_t = din("cs", [NB, 128, 64])
    vmask_d = din("vmask", [128, NB])
    padbias_d = din("padbias", [128, 512])
    diagbias_d = din("diagbias", [128, 512])
    mt_d = din("mt", [128, 17, 128], BF16)
    identb_d = din("identb", [128, 128], BF16)
    pow2_d = din("pow2", [128, NIT + 1])
    gmix_d = din("gmix", [128, 8])
    gffn_d = din("gffn", [128, 8])
    gfin_d = din("gfin", [128, D])
    w_in = din("w_in", [D, DIN])
    w_up_a = din("w_up_a", [512, D])
    w_up_b = din("w_up_b", [512, D])
    w_out = din("w_out", [D, D])
    w_gate = din("w_gate", [D, DFF])
    w_up = din("w_up", [D, DFF])
    w_down = din("w_down", [DFF, D])
    out_d = nc.dram_tensor("out", [NOWN, 128, D], F32, kind="ExternalOutput")

    KAT = dscr("KAT", [128, NB, 512], BF16)
    VA = dscr("VA", [128, NB, 520], BF16)
    KBT = dscr("KBT", [64, NB * 128], BF16)
    KIT = dscr("KIT", [64, NB * 128], BF16)
    VB = dscr("VB", [128, NB, 65], BF16)
    QAT = dscr("QAT", [NOWN, 128, 512], BF16)
    QBT = dscr("QBT", [NOWN, 64, 1024], BF16)
    QIT = dscr("QIT", [NOWN, 64, 1024], BF16)
    WI = dscr("WI", [NOWN, 128, 8], F32)
    GS = dscr("GS", [NOWN, 128, 2048], BF16)
    X2 = dscr("X2", [NOWN, 128, D], F32)
    b_KAT = [Buf() for _ in range(NB)]
    b_VA = [Buf() for _ in range(NB)]
    b_KBT = [Buf() for _ in range(NB)]
    b_KIT = [Buf() for _ in range(NB)]
    b_VB = [Buf() for _ in range(NB)]
    b_QAT = [Buf() for _ in range(NOWN)]
    b_QBT = [Buf() for _ in range(NOWN)]
    b_QIT = [Buf() for _ in range(NOWN)]
    b_WI = [Buf() for _ in range(NOWN)]
    b_GS = [Buf() for _ in range(NOWN)]
    b_X2 = [Buf() for _ in range(NOWN)]
    b_out = [Buf() for _ in range(NOWN)]

    dbg_outs = {}

    def dbg_out(name, shape, dt):
        t = nc.dram_tensor("dbg_" + name, list(shape), dt, kind="ExternalOutput")
        dbg_outs[name] = t
        return t

    with ExitStack() as top:
        def sb(stack, name, shape, dt):
            return stack.enter_context(nc.sbuf_tensor("sb_" + name, list(shape), dt))

        def ps(stack, name, shape, dt):
            return stack.enter_context(nc.psum_tensor("ps_" + name, list(shape), dt))

        identb = sb(top, "identb", [128, 128], BF16)
        b_const = Buf()
        dma("sp", identb[:], identb_d.ap(), writes=[b_const])
        vmask = sb(top, "vmask", [128, NB], F32)
        dma("sp", vmask[:], vmask_d.ap(), writes=[b_const])
        YAT = sb(top, "YAT", [128, 4, NOWN * 128], BF16)
        YBT = sb(top, "YBT", [128, 4, NOWN * 128], BF16)
        b_YAT = [Buf() for _ in range(NOWN)]
        b_YBT = [Buf() for _ in range(NOWN)]

        def rope(stack_bufs, zview, H, cs_tile, b_cs, b_z, out_view, b_outv):
            tA, tB, tC, tD, bA, bB, bC, bD = stack_bufs
            cosb = bc_mid(cs_tile[:, 0:32], H)
            sinb = bc_mid(cs_tile[:, 32:64], H)
            z1 = zview[:, :, 0:32]
            z2 = zview[:, :, 32:64]
            a = tA[:, 0:H, :]
            b = tB[:, 0:H, :]
            c = tC[:, 0:H, :]
            d = tD[:, 0:H, :]
            op("dve", lambda: DVE.tensor_tensor(out=a, in0=z1, in1=cosb, op=ALU.mult), reads=[b_z, b_cs], writes=[bA])
            op("dve", lambda: DVE.tensor_tensor(out=b, in0=z2, in1=sinb, op=ALU.mult), reads=[b_z, b_cs], writes=[bB])
            op("dve", lambda: DVE.tensor_tensor(out=c, in0=z2, in1=cosb, op=ALU.mult), reads=[b_z, b_cs], writes=[bC])
            op("dve", lambda: DVE.tensor_tensor(out=d, in0=z1, in1=sinb, op=ALU.mult), reads=[b_z, b_cs], writes=[bD])
            op("dve", lambda: DVE.tensor_tensor(out=out_view[:, :, 0:32], in0=a, in1=b, op=ALU.subtract),
               reads=[bA, bB], writes=[b_outv])
            op("dve", lambda: DVE.tensor_tensor(out=out_view[:, :, 32:64], in0=c, in1=d, op=ALU.add),
               reads=[bC, bD], writes=[b_outv])

        with ExitStack() as st:
            WK = sb(st, "WK", [128, 8, 1216], BF16)
            WQ = sb(st, "WQ", [128, 8, 3592], BF16)
            wst = [sb(st, f"wst{i}", [128, 8, 512], F32) for i in range(2)]
            b_wst = [Buf(), Buf()]
            gmix = sb(st, "gmix", [128, 8], F32)
            b_g = Buf()
            dma("sp", gmix[:], gmix_d.ap(), writes=[b_g])
            b_WK = Buf()
            b_WQ = Buf()
            w_in_v = w_in.ap().rearrange("(kc p) n -> p kc n", p=128)
            kparts = [(WK, b_WK, 0, 512, 512), (WK, b_WK, 512, 1024, 512), (WK, b_WK, 1024, 2048, 128),
                      (WK, b_WK, 1152, 2688, 64)]
            qparts = [(WQ, b_WQ, 0, 0, 512), (WQ, b_WQ, 512, 1536, 512), (WQ, b_WQ, 1024, 2176, 512),
                      (WQ, b_WQ, 1536, 2752, 8), (WQ, b_WQ, 1544, 2760, 512), (WQ, b_WQ, 2056, 3272, 512),
                      (WQ, b_WQ, 2568, 3784, 512), (WQ, b_WQ, 3080, 4296, 512)]
            for i, (dst, bdst, dc, sc, n) in enumerate(kparts + qparts):
                s = wst[i % 2]
                bs = b_wst[i % 2]
                dma("sp", s[:, :, 0:n], w_in_v[:, :, sc:sc + n], writes=[bs])
                op("pool", lambda s=s, dst=dst, dc=dc, n=n: POOL.tensor_tensor(
                    out=dst[:, :, dc:dc + n], in0=s[:, :, 0:n], in1=bc_last(gmix[:, :], n), op=ALU.mult),
                   reads=[bs, b_g], writes=[bdst])

            xs = [sb(st, f"xs{i}", [128, D], F32) for i in range(2)]
            b_xs = [Buf(), Buf()]
            cst = [sb(st, f"cst{i}", [128, 64], F32) for i in range(2)]
            b_cst = [Buf(), Buf()]
            junk = sb(st, "junk", [128, D], BF16)
            b_junk = Buf()
            ss = sb(st, "ss", [128, 1], F32)
            ms = sb(st, "ms", [128, 1], F32)
            rstd = sb(st, "rstd", [128, 1], F32)
            mhalf = sb(st, "mhalf", [128, 1], F32)
            b_ss, b_ms, b_rstd, b_mh = Buf(), Buf(), Buf(), Buf()
            op("pool", lambda: POOL.memset(mhalf[:], -0.5), writes=[b_mh])
            hb = sb(st, "hb", [128, D], BF16)
            b_hb = Buf()
            hT = sb(st, "hT", [128, 8, 128], BF16)
            b_hT = Buf()
            TRH = ps(st, "TRH", [128, 1024], BF16)
            b_TRH = Buf()
            PB = [ps(st, f"PB{i}", [128, 512], F32) for i in range(5)]
            b_PB = [Buf() for _ in range(5)]
            TRO = ps(st, "TRO", [128, 1024], BF16)
            b_TRO = Buf()
            rt = [sb(st, f"rt{i}", [128, 8, 32], F32) for i in range(4)]
            rbufs = tuple(rt) + tuple(Buf() for _ in range(4))
            kab = sb(st, "kab", [128, 8, 64], BF16)
            b_kab = Buf()
            kat = sb(st, "kat", [128, 512], BF16)
            b_kat = Buf()
            kbi = sb(st, "kbi", [128, 2, 64], BF16)
            b_kbi = Buf()
            kbit = sb(st, "kbit", [64, 256], BF16)
            b_kbit = Buf()
            vaa = sb(st, "vaa", [128, 8, 65], BF16)
            b_vaa = Buf()
            vba = sb(st, "vba", [128, 65], BF16)
            b_vba = Buf()
            qab = sb(st, "qab", [128, 8, 64], BF16)
            b_qab = Buf()
            qat = sb(st, "qat", [128, 512], BF16)
            b_qat = Buf()
            qbt = sb(st, "qbt", [64, 1024], BF16)
            b_qbt = Buf()
            wis = sb(st, "wis", [128, 8], F32)
            b_wis = Buf()
            gsb = sb(st, "gsb", [128, 2048], BF16)
            b_gsb = Buf()
            pbc = [0]

            def next_pb():
                i = pbc[0] % 5
                pbc[0] += 1
                return PB[i], b_PB[i]

            def proj(W, bW, c0, n, bank, bbank, o0=0):
                for kc in range(8):
                    op("pe", lambda kc=kc: PE.matmul(bank[:, o0:o0 + n], lhsT=hT[:, kc, :], rhs=W[:, kc, c0:c0 + n],
                                                     start=(kc == 0), stop=(kc == 7)),
                       reads=[b_hT, bW], writes=[bbank], sig=(kc == 7))

            for l in range(NB):
                x_ = xs[l % 2]
                bx = b_xs[l % 2]
                c_ = cst[l % 2]
                bc = b_cst[l % 2]
                dma("sp", x_[:], xl.ap()[l], writes=[bx])
                dma("sp", c_[:], cs_t.ap()[l], writes=[bc])
                op("act", lambda: ACT.activation(out=junk[:], in_=x_[:], func=AF.Square, accum_out=ss[:]),
                   reads=[bx], writes=[b_junk, b_ss])
                op("dve", lambda: DVE.tensor_scalar(out=ms[:], in0=ss[:], scalar1=1.0 / D, scalar2=EPS,
                                                    op0=ALU.mult, op1=ALU.add), reads=[b_ss], writes=[b_ms])
                op("pool", lambda: POOL.tensor_tensor(out=rstd[:], in0=ms[:], in1=mhalf[:], op=ALU.pow),
                   reads=[b_ms, b_mh], writes=[b_rstd])
                op("pool", lambda: POOL.tensor_scalar(out=hb[:], in0=x_[:], scalar1=rstd[:, 0:1], scalar2=0.0,
                                                      op0=ALU.mult, op1=ALU.add),
                   reads=[bx, b_rstd], writes=[b_hb])
                for kc in range(8):
                    op("pe", lambda kc=kc: PE.transpose(out=TRH[:, kc * 128:(kc + 1) * 128],
                                                        in_=hb[:, kc * 128:(kc + 1) * 128], identity=identb[:]),
                       reads=[b_hb, b_const], writes=[b_TRH], sig=(kc == 7))
                op("act", lambda: ACT.copy(out=hT[:].rearrange("p a b -> p (a b)"), in_=TRH[:]),
                   reads=[b_TRH], writes=[b_hT])
                pa, bpa = next_pb()
                proj(WK, b_WK, 0, 512, pa, bpa)
                pv, bpv = next_pb()
                proj(WK, b_WK, 512, 512, pv, bpv)
                pc, bpc = next_pb()
                proj(WK, b_WK, 1024, 128, pc, bpc, 0)
                proj(WK, b_WK, 1152, 64, pc, bpc, 128)
                rope(rbufs, pa[:].rearrange("p (h d) -> p h d", h=8), 8, c_, bc, bpa, kab[:], b_kab)
                for pr in range(4):
                    op("pe", lambda pr=pr: PE.transpose(out=TRO[:, pr * 128:(pr + 1) * 128],
                                                        in_=kab[:].rearrange("p h d -> p (h d)")[:, pr * 128:(pr + 1) * 128],
                                                        identity=identb[:]),
                       reads=[b_kab, b_const], writes=[b_TRO], sig=(pr == 3))
                op("act", lambda: ACT.copy(out=kat[:], in_=TRO[:, 0:512]), reads=[b_TRO], writes=[b_kat])
                dma("pool", KAT.ap()[:, l, :], kat[:], reads=[b_kat], writes=[b_KAT[l]])
                zc = AP(pc, 0, [[512, 128], [128, 2], [1, 64]])
                rope(rbufs, zc, 2, c_, bc, bpc, kbi[:], b_kbi)
                for hh in range(2):
                    op("pe", lambda hh=hh: PE.transpose(out=TRO[0:64, 512 + hh * 128:512 + (hh + 1) * 128],
                                                        in_=kbi[:, hh, :], identity=identb[:]),
                       reads=[b_kbi, b_const], writes=[b_TRO], sig=(hh == 1))
                op("act", lambda: ACT.copy(out=kbit[:], in_=TRO[0:64, 512:768]), reads=[b_TRO], writes=[b_kbit])
                dma("pool", KBT.ap()[:, l * 128:(l + 1) * 128], kbit[:, 0:128], reads=[b_kbit], writes=[b_KBT[l]])
                dma("pool", KIT.ap()[:, l * 128:(l + 1) * 128], kbit[:, 128:256], reads=[b_kbit], writes=[b_KIT[l]])
                op("act", lambda: ACT.activation(out=vaa[:, :, 0:64], in_=pv[:].rearrange("p (h d) -> p h d", h=8),
                                                 func=AF.Copy, scale=vmask[:, l:l + 1]),
                   reads=[bpv, b_const], writes=[b_vaa])
                op("pool", lambda: POOL.tensor_copy(out=vaa[:, :, 64:65], in_=bc_mid(vmask[:, l:l + 1], 8)),
                   reads=[b_const], writes=[b_vaa])
                dma("pool", VA.ap()[:, l, :], vaa[:].rearrange("p h d -> p (h d)"), reads=[b_vaa], writes=[b_VA[l]])
                op("act", lambda: ACT.activation(out=vba[:, 0:64], in_=pc[:, 64:128], func=AF.Copy,
                                                 scale=vmask[:, l:l + 1]), reads=[bpc, b_const], writes=[b_vba])
                op("pool", lambda: POOL.tensor_copy(out=vba[:, 64:65], in_=vmask[:, l:l + 1]),
                   reads=[b_const], writes=[b_vba])
                dma("pool", VB.ap()[:, l, :], vba[:], reads=[b_vba], writes=[b_VB[l]])
                if l % 4 != 3:
                    continue
                m = l // 4
                pq, bpq = next_pb()
                proj(WQ, b_WQ, 0, 512, pq, bpq)
                rope(rbufs, pq[:].rearrange("p (h d) -> p h d", h=8), 8, c_, bc, bpq, qab[:], b_qab)
                for pr in range(4):
                    op("pe", lambda pr=pr: PE.transpose(out=TRO[:, pr * 128:(pr + 1) * 128],
                                                        in_=qab[:].rearrange("p h d -> p (h d)")[:, pr * 128:(pr + 1) * 128],
                                                        identity=identb[:]),
                       reads=[b_qab, b_const], writes=[b_TRO], sig=(pr == 3))
                op("act", lambda: ACT.copy(out=qat[:], in_=TRO[:, 0:512]), reads=[b_TRO], writes=[b_qat])
                dma("pool", QAT.ap()[m], qat[:], reads=[b_qat], writes=[b_QAT[m]])
                for which, (c0, dstT, bdst) in enumerate([(512, QBT, b_QBT), (1024, QIT, b_QIT)]):
                    pq, bpq = next_pb()
                    proj(WQ, b_WQ, c0, 512, pq, bpq)
                    rope(rbufs, pq[:].rearrange("p (h d) -> p h d", h=8), 8, c_, bc, bpq, qab[:], b_qab)
                    for hh in range(8):
                        op("pe", lambda hh=hh: PE.transpose(out=TRO[0:64, hh * 128:(hh + 1) * 128],
                                                            in_=qab[:, hh, :], identity=identb[:]),
                           reads=[b_qab, b_const], writes=[b_TRO], sig=(hh == 7))
                    op("act", lambda: ACT.copy(out=qbt[:], in_=TRO[0:64, :]), reads=[b_TRO], writes=[b_qbt])
                    dma("pool", dstT.ap()[m], qbt[:], reads=[b_qbt], writes=[bdst[m]])
                pq, bpq = next_pb()
                proj(WQ, b_WQ, 1536, 8, pq, bpq)
                op("dve", lambda: DVE.tensor_copy(out=wis[:], in_=pq[:, 0:8]), reads=[bpq], writes=[b_wis])
                dma("pool", WI.ap()[m], wis[:], reads=[b_wis], writes=[b_WI[m]])
                for gq in range(4):
                    pq, bpq = next_pb()
                    proj(WQ, b_WQ, 1544 + gq * 512, 512, pq, bpq)
                    op("act", lambda gq=gq, pq=pq: ACT.activation(out=gsb[:, gq * 512:(gq + 1) * 512], in_=pq[:],
                                                                  func=AF.Sigmoid), reads=[bpq], writes=[b_gsb])
                dma("pool", GS.ap()[m], gsb[:], reads=[b_gsb], writes=[b_GS[m]])

        if _DBG == "1":
            kb.finish(b_KAT + b_VA + b_KBT + b_KIT + b_VB + b_QAT + b_QBT + b_QIT + b_WI + b_GS)
            return nc, dbg_outs

        kb.barrier()
        with ExitStack() as st:
            MT = sb(st, "MT", [128, 17 * 128], BF16)
            b_MT = Buf()
            dma("sp", MT[:], mt_d.ap().rearrange("p a b -> p (a b)"), writes=[b_MT])
            kw = [sb(st, f"kw{i}", [128, 17, 512], BF16) for i in range(2)]
            b_kw = [Buf(), Buf()]
            vw = [sb(st, f"vw{i}", [128, 17, 520], BF16) for i in range(2)]
            b_vw = [Buf(), Buf()]
            qa_t = [sb(st, f"qa{i}", [128, 512], BF16) for i in range(2)]
            b_qa = [Buf(), Buf()]
            STb = [ps(st, f"ST{i}", [128, 512], F32) for i in range(2)]
            b_ST = [Buf(), Buf()]
            OA = [ps(st, f"OA{i}", [128, 512], F32) for i in range(2)]
            b_OA = [Buf(), Buf()]
            TR = ps(st, "TR2", [128, 1024], BF16)
            b_TR = Buf()
            E = [sb(st, f"E{i}", [128, 512], BF16) for i in range(2)]
            b_E = [Buf(), Buf()]
            Pm = [sb(st, f"P{i}", [128, 512], BF16) for i in range(2)]
            b_P = [Buf(), Buf()]
            rc = sb(st, "rc", [128, 8], F32)
            b_rc = Buf()
            ya = sb(st, "ya", [128, 8, 64], BF16)
            b_ya = Buf()
            cnt = 0
            for m in range(NOWN):
                lq = 4 * m + 3
                lo = max(0, lq - 16)
                nb = lq - lo + 1
                k_, v_, q_ = kw[m % 2], vw[m % 2], qa_t[m % 2]
                bk, bv, bq = b_kw[m % 2], b_vw[m % 2], b_qa[m % 2]
                dma("sp", k_[:, 0:nb, :], KAT.ap()[:, lo:lq + 1, :], reads=b_KAT[lo:lq + 1], writes=[bk])
                dma("sp", v_[:, 0:nb, :], VA.ap()[:, lo:lq + 1, :], reads=b_VA[lo:lq + 1], writes=[bv])
                dma("sp", q_[:], QAT.ap()[m], reads=[b_QAT[m]], writes=[bq])
                first_bank = [True, True]
                groups = [list(range(i, min(i + 4, nb))) for i in range(0, nb, 4)]
                for h in range(8):
                    base = 64 * (h % 2)
                    pair = h // 2
                    ob = OA[h // 4]
                    bob = b_OA[h // 4]
                    for gi, grp in enumerate(groups):
                        n = len(grp)
                        bank, bb = STb[cnt % 2], b_ST[cnt % 2]
                        e_, be = E[cnt % 2], b_E[cnt % 2]
                        p_, bp = Pm[cnt % 2], b_P[cnt % 2]
                        cnt += 1
                        for i, dl in enumerate(grp):
                            slot = nb - 1 - dl
                            op("pe", lambda i=i, slot=slot: PE.matmul(
                                bank[:, i * 128:(i + 1) * 128],
                                lhsT=k_[base:base + 64, slot, pair * 128:(pair + 1) * 128],
                                rhs=q_[base:base + 64, pair * 128:(pair + 1) * 128], start=True, stop=True),
                               reads=[bk, bq], writes=[bb], sig=(i == n - 1))
                        op("act", lambda: ACT.activation(out=e_[:, 0:n * 128], in_=bank[:, 0:n * 128], func=AF.Exp,
                                                         scale=0.125), reads=[bb], writes=[be])
                        d0 = grp[0]
                        op("dve", lambda: DVE.tensor_tensor(out=p_[:, 0:n * 128], in0=e_[:, 0:n * 128],
                                                            in1=MT[:, d0 * 128:(d0 + n) * 128], op=ALU.mult),
                           reads=[be, b_MT], writes=[bp])
                        for i, dl in enumerate(grp):
                            slot = nb - 1 - dl
                            is_first = first_bank[h // 4]
                            first_bank[h // 4] = False
                            is_last = (h % 4 == 3 and gi == len(groups) - 1 and i == n - 1)
                            op("pe", lambda i=i, slot=slot, is_first=is_first, is_last=is_last: PE.matmul(
                                ob[:, (h % 4) * 65:(h % 4) * 65 + 65], lhsT=p_[:, i * 128:(i + 1) * 128],
                                rhs=v_[:, slot, h * 65:(h + 1) * 65], start=is_first, stop=is_last,
                                skip_group_check=True),
                               reads=[bp, bv], writes=[bob], sig=(i == n - 1))
                for bnk in range(2):
                    ov = OA[bnk][:, 0:260].rearrange("p (h d) -> p h d", d=65)
                    op("dve", lambda: DVE.reciprocal(out=rc[:, bnk * 4:(bnk + 1) * 4], in_=ov[:, :, 64]),
                       reads=[b_OA[bnk]], writes=[b_rc])
                    op("dve", lambda: DVE.tensor_tensor(out=ya[:, bnk * 4:(bnk + 1) * 4, :], in0=ov[:, :, 0:64],
                                                        in1=bc_last(rc[:, bnk * 4:(bnk + 1) * 4], 64), op=ALU.mult),
                       reads=[b_OA[bnk], b_rc], writes=[b_ya])
                ya2 = ya[:].rearrange("p h d -> p (h d)")
                for kc in range(4):
                    op("pe", lambda kc=kc: PE.transpose(out=TR[:, kc * 128:(kc + 1) * 128],
                                                        in_=ya2[:, kc * 128:(kc + 1) * 128], identity=identb[:]),
                       reads=[b_ya, b_const], writes=[b_TR], sig=(kc == 3))
                op("act", lambda: ACT.copy(out=YAT[:, :, m * 128:(m + 1) * 128],
                                           in_=TR[:, 0:512].rearrange("p (a b) -> p a b", b=128)),
                   reads=[b_TR], writes=[b_YAT[m]])

        if _DBG == "2":
            dy = dbg_out("YAT", [128, 4, NOWN * 128], BF16)
            bo = Buf()
            dma("sp", dy.ap(), YAT[:], reads=b_YAT, writes=[bo])
            kb.finish([bo])
            return nc, dbg_outs

        kb.barrier()
        with ExitStack() as st:
            KIs = sb(st, "KIs", [64, NB * 128], BF16)
            KBs = sb(st, "KBs", [64, NB * 128], BF16)
            VBs = sb(st, "VBs", [128, NB, 65], BF16)
            b_KIs, b_KBs, b_VBs = Buf(), Buf(), Buf()
            dma("sp", KIs[:], KIT.ap(), reads=b_KIT, writes=[b_KIs])
            dma("sp", KBs[:], KBT.ap(), reads=b_KBT, writes=[b_KBs])
            dma("sp", VBs[:], VB.ap(), reads=b_VB, writes=[b_VBs])
            padb = sb(st, "padb", [128, 512], F32)
            diagb = sb(st, "diagb", [128, 512], F32)
            pow2 = sb(st, "pow2", [128, NIT + 1], F32)
            b_c3 = Buf()
            dma("sp", padb[:], padbias_d.ap(), writes=[b_c3])
            dma("sp", diagb[:], diagbias_d.ap(), writes=[b_c3])
            dma("sp", pow2[:], pow2_d.ap(), writes=[b_c3])
            qi_t = [sb(st, f"qi{i}", [64, 1024], BF16) for i in range(2)]
            qb_t = [sb(st, f"qb{i}", [64, 1024], BF16) for i in range(2)]
            wi_t = [sb(st, f"wi{i}", [128, 8], F32) for i in range(2)]
            b_qi, b_qb, b_wi = [Buf(), Buf()], [Buf(), Buf()], [Buf(), Buf()]
            scores = sb(st, "scores", [128, NB * 128], F32)
            b_sc = [Buf() for _ in range(NOWN)]
            junkc = sb(st, "junkc", [128, NB * 128], BF16)
            b_jc = Buf()
            PX = [ps(st, f"PX{i}", [128, 512], F32) for i in range(3)]
            b_PX = [Buf() for _ in range(3)]
            SCb = [ps(st, f"SC{i}", [128, 512], F32) for i in range(2)]
            b_SCb = [Buf(), Buf()]
            OB = [ps(st, f"OB{i}", [128, 512], F32) for i in range(2)]
            b_OB = [Buf(), Buf()]
            TR3 = ps(st, "TR3", [128, 1024], BF16)
            b_TR3 = Buf()
            R = [sb(st, f"R{i}", [128, 512], BF16) for i in range(4)]
            b_R = [Buf() for _ in range(4)]
            selfull = sb(st, "selfull", [128, NB * 128], BF16)
            b_selfull = Buf()
            absw = sb(st, "absw", [128, 8], F32)
            sgh = sb(st, "sgh", [128, 8], F32)
            Dg = sb(st, "Dg", [128, 8, 128], BF16)
            b_absw, b_sgh, b_Dg = Buf(), Buf(), Buf()
            cmin = sb(st, "cmin", [128, NOWN], F32)
            b_cmin = Buf()
            rmin = sb(st, "rmin", [128, 1], F32)
            rmax = sb(st, "rmax", [128, 1], F32)
            rng = sb(st, "rng", [128, 1], F32)
            tsum = sb(st, "tsum", [128, 1], F32)
            tau = [sb(st, f"tau{i}", [128, 1], F32) for i in range(2)]
            cntt = sb(st, "cntt", [128, 1], F32)
            sg = sb(st, "sg", [128, 1], F32)
            step2 = sb(st, "step2", [128, NIT + 1], F32)
            tsel = sb(st, "tsel", [128, 1], F32)
            b_rmin, b_rmax, b_rng, b_tsum, b_cnt, b_sg, b_step2, b_tsel = (Buf() for _ in range(8))
            b_tau = [Buf(), Buf()]
            sel = [sb(st, f"sel{i}", [128, 512], BF16) for i in range(2)]
            selT = [sb(st, f"selT{i}", [128, 512], BF16) for i in range(2)]
            b_sel, b_selT = [Buf(), Buf()], [Buf(), Buf()]
            Eb = [sb(st, f"Eb{i}", [128, 512], BF16) for i in range(3)]
            b_Eb = [Buf() for _ in range(3)]
            rcb = sb(st, "rcb", [128, 8], F32)
            b_rcb = Buf()
            yb = sb(st, "yb", [128, 8, 64], BF16)
            b_yb = Buf()
            scb = [scores, sb(st, "scores1", [128, NB * 128], F32)]
            b_scb = [b_sc, [Buf() for _ in range(NOWN)]]
            tselb = [tsel, sb(st, "tsel1", [128, 1], F32)]
            b_tselb = [b_tsel, Buf()]
            ctr = {"px": 0, "kr": 0, "ke": 0}

            def stage_S(m):
                nch = m + 1
                qi_, wi_ = qi_t[m % 2], wi_t[m % 2]
                bqi, bwi = b_qi[m % 2], b_wi[m % 2]
                sc_, bsc_ = scb[m % 2], b_scb[m % 2]
                dma("sp", qi_[:], QIT.ap()[m], reads=[b_QIT[m]], writes=[bqi])
                dma("sp", wi_[:], WI.ap()[m], reads=[b_WI[m]], writes=[bwi])
                op("dve", lambda: DVE.tensor_scalar(out=sgh[:], in0=wi_[:], scalar1=0.0, scalar2=0.5,
                                                    op0=ALU.is_ge, op1=ALU.subtract), reads=[bwi], writes=[b_sgh])
                op("dve", lambda: DVE.scalar_tensor_tensor(out=absw[:], in0=sgh[:], scalar=2.0, in1=wi_[:],
                                                           op0=ALU.mult, op1=ALU.mult),
                   reads=[bwi, b_sgh], writes=[b_absw])
                for h in range(8):
                    op("dve", lambda h=h: DVE.tensor_scalar(out=Dg[:, h, :], in0=identb[:], scalar1=sgh[:, h:h + 1],
                                                            scalar2=2.0, op0=ALU.mult, op1=ALU.mult),
                       reads=[b_sgh, b_const], writes=[b_Dg])
                n = 8 * nch
                slots = {}

                def emit_d(i):
                    c, h = divmod(i, 8)
                    k = ctr["px"] % 3
                    ctr["px"] += 1
                    slots[i] = k
                    op("pe", lambda: PE.matmul(PX[k][:], lhsT=qi_[:, h * 128:(h + 1) * 128],
                                               rhs=KIs[:, c * 512:(c + 1) * 512], start=True, stop=True),
                       reads=[bqi, b_KIs], writes=[b_PX[k]])

                def emit_evac(c):
                    op("act", lambda: ACT.copy(out=sc_[:, c * 512:(c + 1) * 512], in_=SCb[c % 2][:]),
                       reads=[b_SCb[c % 2]], writes=[bsc_[c]])

                for i in range(min(3, n)):
                    emit_d(i)
                pend = []
                for i in range(n):
                    c, h = divmod(i, 8)
                    k = slots[i]
                    r_, br = R[ctr["kr"] % 4], b_R[ctr["kr"] % 4]
                    ctr["kr"] += 1
                    op("act", lambda: ACT.activation(out=r_[:], in_=PX[k][:], func=AF.Relu, scale=absw[:, h:h + 1]),
                       reads=[b_PX[k], b_absw], writes=[br])
                    op("pe", lambda: PE.matmul(SCb[c % 2][:], lhsT=Dg[:, h, :], rhs=r_[:], start=(h == 0), stop=(h == 7)),
                       reads=[br, b_Dg], writes=[b_SCb[c % 2]])
                    if i + 3 < n:
                        emit_d(i + 3)
                    if pend and pend[0][1] <= i:
                        emit_evac(pend.pop(0)[0])
                    if h == 7:
                        pend.append((c, i + 2))
                for c_, _ in pend:
                    emit_evac(c_)

            def stage_B(m):
                nch = m + 1
                S = 512 * nch
                sc_, bsc_ = scb[m % 2], b_scb[m % 2]
                bsc = bsc_[0:nch]
                op("dve", lambda: DVE.tensor_reduce(out=rmin[:], in_=sc_[:, 0:S], axis=AX.X, op=ALU.min),
                   reads=bsc, writes=[b_rmin])
                op("dve", lambda: DVE.tensor_tensor(out=sc_[:, 0:512], in0=sc_[:, 0:512], in1=padb[:], op=ALU.add),
                   reads=[b_c3, bsc_[0]], writes=[bsc_[0]])
                op("dve", lambda: DVE.tensor_tensor(out=sc_[:, S - 512:S], in0=sc_[:, S - 512:S], in1=diagb[:], op=ALU.add),
                   reads=[b_c3, bsc_[nch - 1]], writes=[bsc_[nch - 1]])
                op("dve", lambda: DVE.tensor_reduce(out=rmax[:], in_=sc_[:, 0:S], axis=AX.X, op=ALU.max),
                   reads=bsc, writes=[b_rmax])
                op("dve", lambda: DVE.scalar_tensor_tensor(out=rng[:], in0=rmax[:], scalar=2.0, in1=rmin[:],
                                                           op0=ALU.add, op1=ALU.subtract),
                   reads=[b_rmax, b_rmin], writes=[b_rng])
                op("dve", lambda: DVE.tensor_tensor(out=tsum[:], in0=rmax[:], in1=rmin[:], op=ALU.add),
                   reads=[b_rmax, b_rmin], writes=[b_tsum])
                op("dve", lambda: DVE.tensor_scalar(out=tau[0][:], in0=tsum[:], scalar1=0.5, scalar2=None, op0=ALU.mult),
                   reads=[b_tsum], writes=[b_tau[0]])
                op("dve", lambda: DVE.tensor_scalar(out=step2[:], in0=pow2[:], scalar1=rng[:, 0:1], scalar2=None,
                                                    op0=ALU.mult), reads=[b_rng, b_c3], writes=[b_step2])
                for it in range(NIT):
                    tc_, tn_ = tau[it % 2], tau[(it + 1) % 2]
                    btc, btn = b_tau[it % 2], b_tau[(it + 1) % 2]
                    op("dve", lambda: DVE.tensor_scalar(out=junkc[:, 0:S], in0=sc_[:, 0:S], scalar1=tc_[:, 0:1],
                                                        scalar2=None, op0=ALU.is_ge, op1=ALU.add, accum_out=cntt[:]),
                       reads=bsc + [btc], writes=[b_jc, b_cnt])
                    op("dve", lambda: DVE.tensor_scalar(out=sg[:], in0=cntt[:], scalar1=255.5, scalar2=0.5,
                                                        op0=ALU.is_ge, op1=ALU.subtract), reads=[b_cnt], writes=[b_sg])
                    op("dve", lambda: DVE.scalar_tensor_tensor(out=tn_[:], in0=sg[:], scalar=step2[:, it:it + 1],
                                                               in1=tc_[:], op0=ALU.mult, op1=ALU.add),
                       reads=[b_sg, b_step2, btc], writes=[btn])
                tf_, btf = tau[NIT % 2], b_tau[NIT % 2]
                op("dve", lambda: DVE.tensor_tensor(out=tsel[:], in0=tf_[:], in1=step2[:, NIT:NIT + 1], op=ALU.subtract),
                   reads=[btf, b_step2], writes=[b_tsel])
                op("dve", lambda: DVE.tensor_scalar(out=selfull[:, 0:S], in0=sc_[:, 0:S], scalar1=tsel[:, 0:1],
                                                    scalar2=None, op0=ALU.is_ge),
                   reads=bsc + [b_tsel], writes=[b_selfull])

            def stage_A(m):
                nch = m + 1
                qb_, bqb = qb_t[m % 2], b_qb[m % 2]
                dma("sp", qb_[:], QBT.ap()[m], reads=[b_QBT[m]], writes=[bqb])
                first_ob = [True, True]
                n = 8 * nch
                slots = {}

                def prep(c):
                    sT_, bsT_ = selT[c % 2], b_selT[c % 2]
                    for kq in range(4):
                        op("pe", lambda kq=kq: PE.transpose(out=TR3[:, kq * 128:(kq + 1) * 128],
                                                            in_=selfull[:, c * 512 + kq * 128:c * 512 + (kq + 1) * 128],
                                                            identity=identb[:]),
                           reads=[b_selfull, b_const], writes=[b_TR3], sig=(kq == 3))
                    op("act", lambda: ACT.activation(out=sT_[:], in_=TR3[:, 0:512], func=AF.Identity, scale=NEGM,
                                                     bias=-NEGM), reads=[b_TR3], writes=[bsT_])

                def emit_st(u):
                    c, rem = divmod(u, 8)
                    kq, i = divmod(rem, 2)
                    if rem == 0 and c + 1 < nch:
                        prep(c + 1)
                    ls = 4 * c + kq
                    k = ctr["px"] % 3
                    ctr["px"] += 1
                    slots[u] = k
                    sT_, bsT_ = selT[c % 2], b_selT[c % 2]
                    bank3 = PX[k][:].rearrange("p (a b) -> p a b", b=128)
                    op("pe", lambda: PE.matmul(bank3, lhsT=KBs[:, ls * 128:(ls + 1) * 128],
                                               rhs=qb_[:, i * 512:(i + 1) * 512].rearrange("p (a b) -> p a b", b=128),
                                               start=True, stop=False),
                       reads=[b_KBs, bqb], writes=[b_PX[k]], sig=False)
                    op("pe", lambda: PE.matmul(bank3, lhsT=identb[:], rhs=bc_mid(sT_[:, kq * 128:(kq + 1) * 128], 4),
                                               start=False, stop=True),
                       reads=[bsT_, b_const], writes=[b_PX[k]])

                prep(0)
                for u in range(min(3, n)):
                    emit_st(u)
                for u in range(n):
                    c, rem = divmod(u, 8)
                    kq, i = divmod(rem, 2)
                    ls = 4 * c + kq
                    k = slots[u]
                    e_, be = Eb[ctr["ke"] % 3], b_Eb[ctr["ke"] % 3]
                    ctr["ke"] += 1
                    op("act", lambda: ACT.activation(out=e_[:], in_=PX[k][:], func=AF.Exp, scale=0.125),
                       reads=[b_PX[k]], writes=[be])
                    for hh in range(4):
                        is_first = first_ob[i]
                        first_ob[i] = False
                        is_last = (u >= n - 2 and hh == 3)
                        op("pe", lambda hh=hh, is_first=is_first, is_last=is_last: PE.matmul(
                            OB[i][:, hh * 65:hh * 65 + 65], lhsT=e_[:, hh * 128:(hh + 1) * 128],
                            rhs=VBs[:, ls, :], start=is_first, stop=is_last, skip_group_check=True),
                           reads=[be, b_VBs], writes=[b_OB[i]], sig=(hh == 3))
                    if u + 3 < n:
                        emit_st(u + 3)

            def stage_norm(m):
                for bnk in range(2):
                    ov = OB[bnk][:, 0:260].rearrange("p (h d) -> p h d", d=65)
                    op("dve", lambda: DVE.reciprocal(out=rcb[:, bnk * 4:(bnk + 1) * 4], in_=ov[:, :, 64]),
                       reads=[b_OB[bnk]], writes=[b_rcb])
                    op("dve", lambda: DVE.tensor_tensor(out=yb[:, bnk * 4:(bnk + 1) * 4, :], in0=ov[:, :, 0:64],
                                                        in1=bc_last(rcb[:, bnk * 4:(bnk + 1) * 4], 64), op=ALU.mult),
                       reads=[b_OB[bnk], b_rcb], writes=[b_yb])

            def stage_fin(m):
                yb2 = yb[:].rearrange("p h d -> p (h d)")
                for kc in range(4):
                    op("pe", lambda kc=kc: PE.transpose(out=TR3[:, kc * 128:(kc + 1) * 128],
                                                        in_=yb2[:, kc * 128:(kc + 1) * 128], identity=identb[:]),
                       reads=[b_yb, b_const], writes=[b_TR3], sig=(kc == 3))
                op("act", lambda: ACT.copy(out=YBT[:, :, m * 128:(m + 1) * 128],
                                           in_=TR3[:, 0:512].rearrange("p (a b) -> p a b", b=128)),
                   reads=[b_TR3], writes=[b_YBT[m]])

            stage_S(0)
            for m in range(NOWN):
                if m + 1 < NOWN:
                    stage_S(m + 1)
                stage_B(m)
                if m >= 1:
                    stage_norm(m - 1)
                    stage_fin(m - 1)
                stage_A(m)
            stage_norm(NOWN - 1)
            stage_fin(NOWN - 1)

        if _DBG == "3":
            dy = dbg_out("YBT", [128, 4, NOWN * 128], BF16)
            bo = Buf()
            dma("sp", dy.ap(), YBT[:], reads=b_YBT, writes=[bo])
            kb.finish([bo])
            return nc, dbg_outs

        kb.barrier()
        with ExitStack() as st45:
            H2T = sb(st45, "H2T", [128, 8, NOWN * 128], BF16)
            b_H2T = [Buf() for _ in range(NOWN)]
            ss = sb(st45, "ss2", [128, 1], F32)
            ms = sb(st45, "ms2", [128, 1], F32)
            rstd = sb(st45, "rstd2", [128, 1], F32)
            mhalf = sb(st45, "mhalf2", [128, 1], F32)
            b_ss, b_ms, b_rstd, b_mh = Buf(), Buf(), Buf(), Buf()
            op("pool", lambda: POOL.memset(mhalf[:], -0.5), writes=[b_mh])
            junk = sb(st45, "junk2", [128, D], BF16)
            b_junk = Buf()
            with ExitStack() as st:
                WUA = sb(st, "WUA", [128, 4, D], BF16)
                WUB = sb(st, "WUB", [128, 4, D], BF16)
                WO = sb(st, "WO", [128, 8, D], BF16)
                wst2 = sb(st, "wst2", [128, 8, D], F32)
                b_w2, b_WUA, b_WUB, b_WO = Buf(), Buf(), Buf(), Buf()
                dma("sp", wst2[:, 0:4, :], w_up_a.ap().rearrange("(kc p) n -> p kc n", p=128), writes=[b_w2])
                op("pool", lambda: POOL.tensor_copy(out=WUA[:], in_=wst2[:, 0:4, :]), reads=[b_w2], writes=[b_WUA])
                dma("sp", wst2[:, 0:4, :], w_up_b.ap().rearrange("(kc p) n -> p kc n", p=128), writes=[b_w2])
                op("pool", lambda: POOL.tensor_copy(out=WUB[:], in_=wst2[:, 0:4, :]), reads=[b_w2], writes=[b_WUB])
                dma("sp", wst2[:], w_out.ap().rearrange("(kc p) n -> p kc n", p=128), writes=[b_w2])
                op("pool", lambda: POOL.tensor_copy(out=WO[:], in_=wst2[:]), reads=[b_w2], writes=[b_WO])
                gs_t = [sb(st, f"gs{i}", [128, 2048], BF16) for i in range(2)]
                x_t = [sb(st, f"x4{i}", [128, D], F32) for i in range(2)]
                b_gs, b_x4 = [Buf(), Buf()], [Buf(), Buf()]
                UA = [ps(st, f"UA{i}", [128, 512], F32) for i in range(2)]
                UB = [ps(st, f"UB{i}", [128, 512], F32) for i in range(2)]
                WOp = [ps(st, f"WOp{i}", [128, 512], F32) for i in range(2)]
                TR4 = ps(st, "TR4", [128, 1024], BF16)
                b_UA, b_UB, b_WOp = [Buf(), Buf()], [Buf(), Buf()], [Buf(), Buf()]
                b_TR4 = Buf()
                t1 = [sb(st, f"t1{i}", [128, 512], F32) for i in range(2)]
                t2 = [sb(st, f"t2{i}", [128, 512], F32) for i in range(2)]
                b_t1, b_t2 = [Buf(), Buf()], [Buf(), Buf()]
                mg = sb(st, "mg", [128, D], BF16)
                mgT = sb(st, "mgT", [128, 8, 128], BF16)
                b_mg, b_mgT = Buf(), Buf()
                x2 = [sb(st, f"x2{i}", [128, D], F32) for i in range(2)]
                b_x2 = [Buf(), Buf()]
                h2b = sb(st, "h2b", [128, D], BF16)
                b_h2b = Buf()
                for m in range(NOWN):
                    g_, bg_ = gs_t[m % 2], b_gs[m % 2]
                    x_, bx_ = x_t[m % 2], b_x4[m % 2]
                    x2_, bx2_ = x2[m % 2], b_x2[m % 2]
                    dma("sp", g_[:], GS.ap()[m], reads=[b_GS[m]], writes=[bg_])
                    dma("sp", x_[:], xl.ap()[4 * m + 3], writes=[bx_])
                    tk = slice(m * 128, (m + 1) * 128)
                    for half in range(2):
                        cs_ = slice(half * 512, (half + 1) * 512)
                        for kc in range(4):
                            op("pe", lambda kc=kc: PE.matmul(UA[half][:], lhsT=YAT[:, kc, tk], rhs=WUA[:, kc, cs_],
                                                             start=(kc == 0), stop=(kc == 3)),
                               reads=[b_YAT[m], b_WUA], writes=[b_UA[half]], sig=(kc == 3))
                        for kc in range(4):
                            op("pe", lambda kc=kc: PE.matmul(UB[half][:], lhsT=YBT[:, kc, tk], rhs=WUB[:, kc, cs_],
                                                             start=(kc == 0), stop=(kc == 3)),
                               reads=[b_YBT[m], b_WUB], writes=[b_UB[half]], sig=(kc == 3))
                        op("dve", lambda: DVE.tensor_tensor(out=t1[half][:], in0=UA[half][:], in1=g_[:, cs_], op=ALU.mult),
                           reads=[b_UA[half], bg_], writes=[b_t1[half]])
                        op("dve", lambda: DVE.tensor_tensor(out=t2[half][:], in0=UB[half][:],
                                                            in1=g_[:, 1024 + half * 512:1024 + (half + 1) * 512], op=ALU.mult),
                           reads=[b_UB[half], bg_], writes=[b_t2[half]])
                        op("pool", lambda: POOL.tensor_tensor(out=mg[:, cs_], in0=t1[half][:], in1=t2[half][:], op=ALU.add),
                           reads=[b_t1[half], b_t2[half]], writes=[b_mg])
                    for kc in range(8):
                        op("pe", lambda kc=kc: PE.transpose(out=TR4[:, kc * 128:(kc + 1) * 128],
                                                            in_=mg[:, kc * 128:(kc + 1) * 128], identity=identb[:]),
                           reads=[b_mg, b_const], writes=[b_TR4], sig=(kc == 7))
                    op("act", lambda: ACT.copy(out=mgT[:].rearrange("p a b -> p (a b)"), in_=TR4[:]),
                       reads=[b_TR4], writes=[b_mgT])
                    for half in range(2):
                        cs_ = slice(half * 512, (half + 1) * 512)
                        for kc in range(8):
                            op("pe", lambda kc=kc: PE.matmul(WOp[half][:], lhsT=mgT[:, kc, :], rhs=WO[:, kc, cs_],
                                                             start=(kc == 0), stop=(kc == 7)),
                               reads=[b_mgT, b_WO], writes=[b_WOp[half]], sig=(kc == 7))
                        op("dve", lambda: DVE.tensor_tensor(out=x2_[:, cs_], in0=WOp[half][:], in1=x_[:, cs_], op=ALU.add),
                           reads=[b_WOp[half], bx_], writes=[bx2_])
                    dma("pool", X2.ap()[m], x2_[:], reads=[bx2_], writes=[b_X2[m]])
                    op("act", lambda: ACT.activation(out=junk[:], in_=x2_[:], func=AF.Square, accum_out=ss[:]),
                       reads=[bx2_], writes=[b_junk, b_ss])
                    op("dve", lambda: DVE.tensor_scalar(out=ms[:], in0=ss[:], scalar1=1.0 / D, scalar2=EPS,
                                                        op0=ALU.mult, op1=ALU.add), reads=[b_ss], writes=[b_ms])
                    op("pool", lambda: POOL.tensor_tensor(out=rstd[:], in0=ms[:], in1=mhalf[:], op=ALU.pow),
                       reads=[b_ms, b_mh], writes=[b_rstd])
                    op("pool", lambda: POOL.tensor_scalar(out=h2b[:], in0=x2_[:], scalar1=rstd[:, 0:1], scalar2=0.0,
                                                          op0=ALU.mult, op1=ALU.add),
                       reads=[bx2_, b_rstd], writes=[b_h2b])
                    for kc in range(8):
                        op("pe", lambda kc=kc: PE.transpose(out=TR4[:, kc * 128:(kc + 1) * 128],
                                                            in_=h2b[:, kc * 128:(kc + 1) * 128], identity=identb[:]),
                           reads=[b_h2b, b_const], writes=[b_TR4], sig=(kc == 7))
                    op("act", lambda: ACT.copy(out=H2T[:, :, tk], in_=TR4[:].rearrange("p (a b) -> p a b", b=128)),
                       reads=[b_TR4], writes=[b_H2T[m]])

            if _DBG == "4":
                dx = dbg_out("X2o", [NOWN, 128, D], F32)
                bo = Buf()
                with ExitStack() as st:
                    tt = sb(st, "dbgt", [128, D], F32)
                    bt = Buf()
                    for m in range(NOWN):
                        dma("sp", tt[:], X2.ap()[m], reads=[b_X2[m]], writes=[bt])
                        dma("sp", dx.ap()[m], tt[:], reads=[bt], writes=[bo])
                    kb.finish([bo])
                return nc, dbg_outs

            kb.barrier()
            with ExitStack() as st:
                gffn = sb(st, "gffn", [128, 8], F32)
                gfin = sb(st, "gfin", [128, D], F32)
                b_g5 = Buf()
                dma("sp", gffn[:], gffn_d.ap(), writes=[b_g5])
                dma("sp", gfin[:], gfin_d.ap(), writes=[b_g5])
                ACTT = sb(st, "ACTT", [128, NFF, NOWN * 128], BF16)
                b_ACTT = [Buf() for _ in range(4)]
                with ExitStack() as sg_:
                    wgst = [sb(sg_, f"wgst{i}", [128, 8, 128], F32) for i in range(2)]
                    wust = [sb(sg_, f"wust{i}", [128, 8, 128], F32) for i in range(2)]
                    wgb = [sb(sg_, f"wgb{i}", [128, 8, 128], BF16) for i in range(2)]
                    wub = [sb(sg_, f"wub{i}", [128, 8, 128], BF16) for i in range(2)]
                    b_wgst, b_wust, b_wgb, b_wub = ([Buf(), Buf()] for _ in range(4))
                    G = [ps(sg_, f"G{i}", [128, 512], F32) for i in range(2)]
                    U = [ps(sg_, f"U{i}", [128, 512], F32) for i in range(2)]
                    b_G, b_U = [Buf(), Buf()], [Buf(), Buf()]
                    sgt = [sb(sg_, f"sgt{i}", [128, 512], F32) for i in range(2)]
                    b_sgt = [Buf(), Buf()]
                    wgv = w_gate.ap().rearrange("(kc p) n -> p kc n", p=128)
                    wuv = w_up.ap().rearrange("(kc p) n -> p kc n", p=128)
                    kk = 0
                    for ffc in range(NFF):
                        i2 = ffc % 2
                        dma("sp", wgst[i2][:], wgv[:, :, ffc * 128:(ffc + 1) * 128], writes=[b_wgst[i2]])
                        dma("sp", wust[i2][:], wuv[:, :, ffc * 128:(ffc + 1) * 128], writes=[b_wust[i2]])
                        op("pool", lambda: POOL.tensor_tensor(out=wgb[i2][:], in0=wgst[i2][:], in1=bc_last(gffn[:, :], 128),
                                                              op=ALU.mult), reads=[b_wgst[i2], b_g5], writes=[b_wgb[i2]])
                        op("pool", lambda: POOL.tensor_tensor(out=wub[i2][:], in0=wust[i2][:], in1=bc_last(gffn[:, :], 128),
                                                              op=ALU.mult), reads=[b_wust[i2], b_g5], writes=[b_wub[i2]])
                        for tg in range(4):
                            ts_ = slice(tg * 512, (tg + 1) * 512)
                            g_, bg_ = G[kk % 2], b_G[kk % 2]
                            u_, bu_ = U[kk % 2], b_U[kk % 2]
                            s_, bs_ = sgt[kk % 2], b_sgt[kk % 2]
                            kk += 1
                            for kc in range(8):
                                op("pe", lambda kc=kc, g_=g_: PE.matmul(g_[:], lhsT=wgb[i2][:, kc, :], rhs=H2T[:, kc, ts_],
                                                                        start=(kc == 0), stop=(kc == 7)),
                                   reads=[b_wgb[i2]] + b_H2T[tg * 4:(tg + 1) * 4], writes=[bg_], sig=(kc == 7))
                            for kc in range(8):
                                op("pe", lambda kc=kc, u_=u_: PE.matmul(u_[:], lhsT=wub[i2][:, kc, :], rhs=H2T[:, kc, ts_],
                                                                        start=(kc == 0), stop=(kc == 7)),
                                   reads=[b_wub[i2]] + b_H2T[tg * 4:(tg + 1) * 4], writes=[bu_], sig=(kc == 7))
                            op("act", lambda g_=g_, s_=s_: ACT.activation(out=s_[:], in_=g_[:], func=AF.Silu),
                               reads=[bg_], writes=[bs_])
                            op("dve", lambda u_=u_, s_=s_: DVE.tensor_tensor(out=ACTT[:, ffc, ts_], in0=u_[:], in1=s_[:],
                                                                             op=ALU.mult),
                               reads=[bu_, bs_], writes=[b_ACTT[tg]])
                kb.barrier()
                with ExitStack() as sd_:
                    wdst = [sb(sd_, f"wdst{i}", [128, D], F32) for i in range(2)]
                    wdb = [sb(sd_, f"wdb{i}", [128, D], BF16) for i in range(2)]
                    b_wdst, b_wdb = [Buf(), Buf()], [Buf(), Buf()]
                    DN = [ps(sd_, f"DN{i}", [128, 512], F32) for i in range(8)]
                    b_DN = [Buf() for _ in range(8)]
                    x2t = [sb(sd_, f"x2t{i}", [128, D], F32) for i in range(2)]
                    x3 = [sb(sd_, f"x3{i}", [128, D], F32) for i in range(2)]
                    ot = [sb(sd_, f"ot{i}", [128, D], F32) for i in range(2)]
                    b_x2t, b_x3, b_ot = [Buf(), Buf()], [Buf(), Buf()], [Buf(), Buf()]
                    kw_ = 0
                    for tg in range(4):
                        for ffc in range(NFF):
                            i2 = kw_ % 2
                            kw_ += 1
                            dma("sp", wdst[i2][:], w_down.ap()[ffc * 128:(ffc + 1) * 128, :], writes=[b_wdst[i2]])
                            op("pool", lambda: POOL.tensor_copy(out=wdb[i2][:], in_=wdst[i2][:]),
                               reads=[b_wdst[i2]], writes=[b_wdb[i2]])
                            for tb in range(4):
                                mm = tg * 4 + tb
                                for half in range(2):
                                    op("pe", lambda tb=tb, half=half, mm=mm: PE.matmul(
                                        DN[tb * 2 + half][:], lhsT=ACTT[:, ffc, mm * 128:(mm + 1) * 128],
                                        rhs=wdb[i2][:, half * 512:(half + 1) * 512], start=(ffc == 0), stop=(ffc == NFF - 1)),
                                       reads=[b_wdb[i2], b_ACTT[tg]], writes=[b_DN[tb * 2 + half]],
                                       sig=(tb == 3 and half == 1))
                        for tb in range(4):
                            mm = tg * 4 + tb
                            xx, bxx = x2t[mm % 2], b_x2t[mm % 2]
                            x3_, bx3 = x3[mm % 2], b_x3[mm % 2]
                            o_, bo_ = ot[mm % 2], b_ot[mm % 2]
                            dma("sp", xx[:], X2.ap()[mm], reads=[b_X2[mm]], writes=[bxx])
                            for half in range(2):
                                cs_ = slice(half * 512, (half + 1) * 512)
                                op("dve", lambda: DVE.tensor_tensor(out=x3_[:, cs_], in0=DN[tb * 2 + half][:], in1=xx[:, cs_],
                                                                    op=ALU.add),
                                   reads=[b_DN[tb * 2 + half], bxx], writes=[bx3])
                            op("act", lambda: ACT.activation(out=junk[:], in_=x3_[:], func=AF.Square, accum_out=ss[:]),
                               reads=[bx3], writes=[b_junk, b_ss])
                            op("dve", lambda: DVE.tensor_scalar(out=ms[:], in0=ss[:], scalar1=1.0 / D, scalar2=EPS,
                                                                op0=ALU.mult, op1=ALU.add), reads=[b_ss], writes=[b_ms])
                            op("pool", lambda: POOL.tensor_tensor(out=rstd[:], in0=ms[:], in1=mhalf[:], op=ALU.pow),
                               reads=[b_ms, b_mh], writes=[b_rstd])
                            op("dve", lambda: DVE.scalar_tensor_tensor(out=o_[:], in0=x3_[:], scalar=rstd[:, 0:1], in1=gfin[:],
                                                                       op0=ALU.mult, op1=ALU.mult),
                               reads=[bx3, b_rstd, b_g5], writes=[bo_])
                            dma("sp", out_d.ap()[mm], o_[:], reads=[bo_], writes=[b_out[mm]])

        kb.finish(b_out)
    return nc, dbg_outs


def host_prep(x, norm_mix, w_in, w_up_a, w_up_b, w_out, norm_ffn, w_gate, w_up, w_down, norm_final):
    B, T, _ = x.shape
    x = np.asarray(x, np.float32)
    half = 32
    inv_freq = (10000.0 ** (-np.arange(half, dtype=np.float32) / half)).astype(np.float32)
    identb = np.eye(128, dtype=np.float32).astype(ml_dtypes.bfloat16)
    s_i = np.arange(128)[:, None]
    t_i = np.arange(128)[None, :]
    mt = np.zeros((128, 17, 128), np.float32)
    for dl in range(17):
        diff = 128 * dl + t_i - s_i
        tot = np.zeros((128, 128), np.float32)
        for (wdw, dil) in ((128, 1), (512, 4), (2048, 16)):
            ok = (diff >= 0) & (diff <= wdw) & (diff % dil == 0)
            tot += ok.astype(np.float32)
        mt[:, dl, :] = tot
    mt = mt.astype(ml_dtypes.bfloat16)
    diagbias = np.zeros((128, 512), np.float32)
    diagbias[:, 384:512] = np.where(np.arange(128)[None, :] > np.arange(128)[:, None], -BIG, 0.0)
    pow2 = np.broadcast_to((2.0 ** (-(np.arange(NIT + 1) + 1.0))).astype(np.float32)[None, :], (128, NIT + 1)).copy()
    gmix = np.ascontiguousarray(np.asarray(norm_mix, np.float32).reshape(8, 128).T)
    gffn = np.ascontiguousarray(np.asarray(norm_ffn, np.float32).reshape(8, 128).T)
    gfin = np.ascontiguousarray(np.broadcast_to(np.asarray(norm_final, np.float32)[None, :], (128, D)))
    common = {
        "identb": identb, "mt": mt, "diagbias": diagbias, "pow2": pow2, "gmix": gmix, "gffn": gffn, "gfin": gfin,
        "w_in": np.ascontiguousarray(np.asarray(w_in, np.float32)[0]),
        "w_up_a": np.ascontiguousarray(np.asarray(w_up_a, np.float32)[0]),
        "w_up_b": np.ascontiguousarray(np.asarray(w_up_b, np.float32)[0]),
        "w_out": np.ascontiguousarray(np.asarray(w_out, np.float32)[0]),
        "w_gate": np.ascontiguousarray(np.asarray(w_gate, np.float32)[0]),
        "w_up": np.ascontiguousarray(np.asarray(w_up, np.float32)[0]),
        "w_down": np.ascontiguousarray(np.asarray(w_down, np.float32)[0]),
    }
    in_maps = []
    for core in range(8):
        b, j = core // 4, core % 4
        xl = np.zeros((NB, 128, D), np.float32)
        pos = np.zeros((NB, 128), np.float32)
        valid = np.zeros((NB,), np.float32)
        for l in range(NB):
            g = l + j - 3
            if g >= 0:
                xl[l] = x[b, g * 128:(g + 1) * 128]
                pos[l] = np.arange(g * 128, (g + 1) * 128, dtype=np.float32)
                valid[l] = 1.0
        ang = pos[:, :, None] * inv_freq[None, None, :]
        cs = np.concatenate([np.cos(ang), np.sin(ang)], axis=-1).astype(np.float32)
        vmask = np.ascontiguousarray(np.broadcast_to(valid[None, :], (128, NB))).astype(np.float32)
        padbias = np.zeros((128, 512), np.float32)
        for l in range(4):
            if valid[l] == 0.0:
                padbias[:, l * 128:(l + 1) * 128] = -BIG
        m = dict(common)
        m.update({"xl": xl, "cs": cs, "vmask": vmask, "padbias": padbias})
        in_maps.append(m)
    return in_maps


def kernel(x, norm_mix, w_in, w_up_a, w_up_b, w_out, norm_ffn, w_gate, w_up, w_down, norm_final):
    in_maps = host_prep(x, norm_mix, w_in, w_up_a, w_up_b, w_out, norm_ffn, w_gate, w_up, w_down, norm_final)
    nc, dbg = build()
    if _DBG:
        res = run_bass_kernel_spmd(nc, in_maps, core_ids=list(range(8)), trace=bool(os.environ.get("KTRACE")))
        print("DBG exec_time_ns", res.exec_time_ns)
        return res
    res = run_bass_kernel_spmd(nc, in_maps, core_ids=list(range(8)))
    B, T, _ = x.shape
    out = np.zeros((B, T, D), np.float32)
    for core in range(8):
        b, j = core // 4, core % 4
        o = res.results[core]["out"]
        for m in range(NOWN):
            g = 4 * m + j
            out[b, g * 128:(g + 1) * 128] = o[m]
    return out
```

```python
import os
from contextlib import ExitStack

import ml_dtypes
import numpy as np

import concourse.bass as bass
import concourse.mybir as mybir
from concourse.bass_types import AP
from concourse.bass_utils import run_bass_kernel_spmd

F32 = mybir.dt.float32
BF16 = mybir.dt.bfloat16
AF = mybir.ActivationFunctionType
ALU = mybir.AluOpType
AX = mybir.AxisListType

NB = 64
NOWN = 16
D = 1024
DFF = 2816
NFF = DFF // 128
DIN = 4808
NIT = 22
BIG = 1.0e30
NEGM = 30000.0
EPS = 1e-6
NDS = 12

_DBG = os.environ.get("KDBG", "")


class Buf:
    __slots__ = ("w", "r")

    def __init__(self):
        self.w = None
        self.r = {}


class KB:
    def __init__(self, nc):
        self.nc = nc
        self.eng = {"pe": nc.tensor, "act": nc.scalar, "dve": nc.vector, "pool": nc.gpsimd, "sp": nc.sync}
        self.sem = {e: nc.alloc_semaphore(name=f"s_{e}") for e in self.eng}
        self.cnt = {e: 0 for e in self.eng}
        self.waited = {e: {} for e in self.eng}
        self.fence_fn = {}
        self.last_fence = {}
        self.dq = {}
        for q in ("sp", "pool"):
            self.dq[q] = {"sems": [nc.alloc_semaphore(name=f"d_{q}{i}") for i in range(NDS)], "k": 0}

    def _wait(self, e, ev):
        sem, val = ev
        if e == "pe" and sem is self.sem["pe"]:
            return
        key = sem.num
        if self.waited[e].get(key, 0) >= val:
            return
        self.eng[e].wait_ge(sem, val)
        self.waited[e][key] = val

    def _deps(self, e, reads, writes):
        for b in reads:
            if b.w is not None:
                self._wait(e, b.w)
        for b in writes:
            if b.w is not None:
                self._wait(e, b.w)
            for ev in b.r.values():
                self._wait(e, ev)

    def _mark(self, ev, reads, writes):
        key = ev[0].num
        for b in reads:
            old = b.r.get(key)
            if old is None or old[1] < ev[1]:
                b.r[key] = ev
        for b in writes:
            b.w = ev
            b.r = {}

    def op(self, e, fn, reads=(), writes=(), sig=True):
        self._deps(e, reads, writes)
        inst = fn()
        if sig:
            self.cnt[e] += 1
            inst.then_inc(self.sem[e], 1)
            ev = (self.sem[e], self.cnt[e])
        else:
            ev = (self.sem[e], self.cnt[e] + 1)
        self._mark(ev, reads, writes)
        return inst

    def dma(self, q, out, in_, reads=(), writes=(), fence=()):
        dq = self.dq[q]
        k = dq["k"]
        P = len(dq["sems"])
        sem = dq["sems"][k % P]
        if k >= P:
            self._wait(q, (sem, 16 * (k // P)))
        self._deps(q, reads, writes)
        for e in fence:
            last = self.last_fence.get(e)
            if last is not None:
                self._wait(e, last)
            self.fence_fn[e]().then_inc(self.sem[e], 1)
            self.cnt[e] += 1
            self.last_fence[e] = (self.sem[e], self.cnt[e])
            self._wait(q, (self.sem[e], self.cnt[e]))
        self.eng[q].dma_start(out=out, in_=in_).then_inc(sem, 16)
        dq["k"] += 1
        ev = (sem, 16 * (k // P + 1))
        self._mark(ev, reads, writes)

    def barrier(self):
        evs = [(self.sem[e], self.cnt[e]) for e in self.eng if self.cnt[e] > 0]
        for q, dq in self.dq.items():
            k = dq["k"]
            P = len(dq["sems"])
            for i in range(min(k, P)):
                kk = k - 1 - i
                evs.append((dq["sems"][kk % P], 16 * (kk // P + 1)))
        for e in self.eng:
            for ev in evs:
                if ev[0] is self.sem[e]:
                    continue
                self._wait(e, ev)

    def finish(self, bufs):
        for b in bufs:
            if b.w is not None:
                self._wait("sp", b.w)
        for q, dq in self.dq.items():
            k = dq["k"]
            P = len(dq["sems"])
            for i in range(min(k, P)):
                kk = k - 1 - i
                self._wait(q, (dq["sems"][kk % P], 16 * (kk // P + 1)))


def bc_mid(ap2d, n):
    a = [list(x) for x in ap2d.ap]
    assert len(a) == 2
    return AP(ap2d.tensor, ap2d.offset, [a[0], [0, n], a[1]])


def bc_last(ap, n):
    a = [list(x) for x in ap.ap]
    return AP(ap.tensor, ap.offset, a + [[0, n]])


def build():
    nc = bass.Bass("TRN2", target_bir_lowering=False)
    kb = KB(nc)
    op = kb.op
    dma = kb.dma
    PE, ACT, DVE, POOL = nc.tensor, nc.scalar, nc.vector, nc.gpsimd

    def din(name, shape, dt=F32):
        return nc.dram_tensor(name, list(shape), dt, kind="ExternalInput")

    def dscr(name, shape, dt):
        return nc.dram_tensor(name, list(shape), dt, kind=("ExternalOutput" if _DBG else "Internal"))

    xl = din("xl", [NB, 128, D])
    cs_t = din("cs", [NB, 128, 64])
    vmask_d = din("vmask", [128, NB])
    padbias_d = din("padbias", [128, 512])
    diagbias_d = din("diagbias", [128, 512])
    mt_d = din("mt", [128, 17, 128], BF16)
    identb_d = din("identb", [128, 128], BF16)
    pow2_d = din("pow2", [128, NIT + 1])
    gmix_d = din("gmix", [128, 8])
    gffn_d = din("gffn", [128, 8])
    gfin_d = din("gfin", [128, D])
    w_in = din("w_in", [D, DIN])
    w_up_a = din("w_up_a", [512, D])
    w_up_b = din("w_up_b", [512, D])
    w_out = din("w_out", [D, D])
    w_gate = din("w_gate", [D, DFF])
    w_up = din("w_up", [D, DFF])
    w_down = din("w_down", [DFF, D])
    out_d = nc.dram_tensor("out", [NOWN, 128, D], F32, kind="ExternalOutput")

    KAT = dscr("KAT", [128, NB, 512], BF16)
    VA = dscr("VA", [128, NB, 520], BF16)
    KBT = dscr("KBT", [64, NB * 128], BF16)
    KIT = dscr("KIT", [64, NB * 128], BF16)
    VB = dscr("VB", [128, NB, 65], BF16)
    QAT = dscr("QAT", [NOWN, 128, 512], BF16)
    QBT = dscr("QBT", [NOWN, 64, 1024], BF16)
    QIT = dscr("QIT", [NOWN, 64, 1024], BF16)
    WI = dscr("WI", [NOWN, 128, 8], F32)
    GS = dscr("GS", [NOWN, 128, 2048], BF16)
    X2 = dscr("X2", [NOWN, 128, D], F32)
    b_KAT = [Buf() for _ in range(NB)]
    b_VA = [Buf() for _ in range(NB)]
    b_KBT = [Buf() for _ in range(NB)]
    b_KIT = [Buf() for _ in range(NB)]
    b_VB = [Buf() for _ in range(NB)]
    b_QAT = [Buf() for _ in range(NOWN)]
    b_QBT = [Buf() for _ in range(NOWN)]
    b_QIT = [Buf() for _ in range(NOWN)]
    b_WI = [Buf() for _ in range(NOWN)]
    b_GS = [Buf() for _ in range(NOWN)]
    b_X2 = [Buf() for _ in range(NOWN)]
    b_out = [Buf() for _ in range(NOWN)]

    dbg_outs = {}

    def dbg_out(name, shape, dt):
        t = nc.dram_tensor("dbg_" + name, list(shape), dt, kind="ExternalOutput")
        dbg_outs[name] = t
        return t

    with ExitStack() as top:
        def sb(stack, name, shape, dt):
            return stack.enter_context(nc.sbuf_tensor("sb_" + name, list(shape), dt))

        def ps(stack, name, shape, dt):
            return stack.enter_context(nc.psum_tensor("ps_" + name, list(shape), dt))

        identb = sb(top, "identb", [128, 128], BF16)
        b_const = Buf()
        dma("sp", identb[:], identb_d.ap(), writes=[b_const])
        vmask = sb(top, "vmask", [128, NB], F32)
        dma("sp", vmask[:], vmask_d.ap(), writes=[b_const])
        fsc = sb(top, "fsc", [128, 8], F32)
        op("pool", lambda: POOL.memset(fsc[:], 0.0), writes=[Buf()])
        kb.barrier()
        kb.fence_fn["act"] = lambda: ACT.copy(out=fsc[:, 0:1], in_=fsc[:, 1:2])
        kb.fence_fn["dve"] = lambda: DVE.tensor_copy(out=fsc[:, 2:3], in_=fsc[:, 3:4])
        kb.fence_fn["pool"] = lambda: POOL.tensor_copy(out=fsc[:, 4:5], in_=fsc[:, 5:6])
        YAT = sb(top, "YAT", [128, 4, NOWN * 128], BF16)
        YBT = sb(top, "YBT", [128, 4, NOWN * 128], BF16)
        b_YAT = [Buf() for _ in range(NOWN)]
        b_YBT = [Buf() for _ in range(NOWN)]

        def rope(stack_bufs, zview, H, cs_tile, b_cs, b_z, out_view, b_outv):
            tA, tB, tC, tD, bA, bB, bC, bD = stack_bufs
            cosb = bc_mid(cs_tile[:, 0:32], H)
            sinb = bc_mid(cs_tile[:, 32:64], H)
            z1 = zview[:, :, 0:32]
            z2 = zview[:, :, 32:64]
            a = tA[:, 0:H, :]
            b = tB[:, 0:H, :]
            c = tC[:, 0:H, :]
            d = tD[:, 0:H, :]
            op("dve", lambda: DVE.tensor_tensor(out=a, in0=z1, in1=cosb, op=ALU.mult), reads=[b_z, b_cs], writes=[bA])
            op("dve", lambda: DVE.tensor_tensor(out=b, in0=z2, in1=sinb, op=ALU.mult), reads=[b_z, b_cs], writes=[bB])
            op("dve", lambda: DVE.tensor_tensor(out=c, in0=z2, in1=cosb, op=ALU.mult), reads=[b_z, b_cs], writes=[bC])
            op("dve", lambda: DVE.tensor_tensor(out=d, in0=z1, in1=sinb, op=ALU.mult), reads=[b_z, b_cs], writes=[bD])
            op("dve", lambda: DVE.tensor_tensor(out=out_view[:, :, 0:32], in0=a, in1=b, op=ALU.subtract),
               reads=[bA, bB], writes=[b_outv])
            op("dve", lambda: DVE.tensor_tensor(out=out_view[:, :, 32:64], in0=c, in1=d, op=ALU.add),
               reads=[bC, bD], writes=[b_outv])

        with ExitStack() as st:
            WK = sb(st, "WK", [128, 8, 1216], BF16)
            WQ = sb(st, "WQ", [128, 8, 3592], BF16)
            wst = [sb(st, f"wst{i}", [128, 8, 512], F32) for i in range(2)]
            b_wst = [Buf(), Buf()]
            gmix = sb(st, "gmix", [128, 8], F32)
            b_g = Buf()
            dma("sp", gmix[:], gmix_d.ap(), writes=[b_g])
            b_WK = Buf()
            b_WQ = Buf()
            w_in_v = w_in.ap().rearrange("(kc p) n -> p kc n", p=128)
            kparts = [(WK, b_WK, 0, 512, 512), (WK, b_WK, 512, 1024, 512), (WK, b_WK, 1024, 2048, 128),
                      (WK, b_WK, 1152, 2688, 64)]
            qparts = [(WQ, b_WQ, 0, 0, 512), (WQ, b_WQ, 512, 1536, 512), (WQ, b_WQ, 1024, 2176, 512),
                      (WQ, b_WQ, 1536, 2752, 8), (WQ, b_WQ, 1544, 2760, 512), (WQ, b_WQ, 2056, 3272, 512),
                      (WQ, b_WQ, 2568, 3784, 512), (WQ, b_WQ, 3080, 4296, 512)]
            for i, (dst, bdst, dc, sc, n) in enumerate(kparts + qparts):
                s = wst[i % 2]
                bs = b_wst[i % 2]
                dma("sp", s[:, :, 0:n], w_in_v[:, :, sc:sc + n], writes=[bs])
                op("pool", lambda s=s, dst=dst, dc=dc, n=n: POOL.tensor_tensor(
                    out=dst[:, :, dc:dc + n], in0=s[:, :, 0:n], in1=bc_last(gmix[:, :], n), op=ALU.mult),
                   reads=[bs, b_g], writes=[bdst])

            xs = [sb(st, f"xs{i}", [128, D], F32) for i in range(3)]
            b_xs = [Buf() for _ in range(3)]
            cst = [sb(st, f"cst{i}", [128, 64], F32) for i in range(4)]
            b_cst = [Buf() for _ in range(4)]
            junk = sb(st, "junk", [128, D], BF16)
            b_junk = Buf()
            ss = [sb(st, f"ss{i}", [128, 1], F32) for i in range(2)]
            ms = [sb(st, f"ms{i}", [128, 1], F32) for i in range(2)]
            rstd = [sb(st, f"rstd{i}", [128, 1], F32) for i in range(2)]
            mhalf = sb(st, "mhalf", [128, 1], F32)
            b_ss, b_ms, b_rstd = [Buf(), Buf()], [Buf(), Buf()], [Buf(), Buf()]
            b_mh = Buf()
            op("pool", lambda: POOL.memset(mhalf[:], -0.5), writes=[b_mh])
            hb = [sb(st, f"hb{i}", [128, D], BF16) for i in range(2)]
            b_hb = [Buf(), Buf()]
            hT = [sb(st, f"hT{i}", [128, 8, 128], BF16) for i in range(3)]
            b_hT = [Buf() for _ in range(3)]
            TRH = ps(st, "TRH", [128, 1024], BF16)
            b_TRH = Buf()
            NPB = 6
            PB = [ps(st, f"PB{i}", [128, 512], F32) for i in range(NPB)]
            b_PB = [Buf() for _ in range(NPB)]
            TRO = ps(st, "TRO", [128, 1024], BF16)
            b_TRO = Buf()
            rt = [sb(st, f"rt{i}", [128, 8, 32], F32) for i in range(4)]
            rbufs = tuple(rt) + tuple(Buf() for _ in range(4))
            kab = sb(st, "kab", [128, 8, 64], BF16)
            b_kab = Buf()
            kat = [sb(st, f"kat{i}", [128, 512], BF16) for i in range(2)]
            b_kat = [Buf(), Buf()]
            kbi = sb(st, "kbi", [128, 2, 64], BF16)
            b_kbi = Buf()
            kbit = [sb(st, f"kbit{i}", [64, 256], BF16) for i in range(2)]
            b_kbit = [Buf(), Buf()]
            vaa = [sb(st, f"vaa{i}", [128, 8, 65], BF16) for i in range(2)]
            b_vaa = [Buf(), Buf()]
            vba = [sb(st, f"vba{i}", [128, 65], BF16) for i in range(2)]
            b_vba = [Buf(), Buf()]
            qab = sb(st, "qab", [128, 8, 64], BF16)
            b_qab = Buf()
            qat = sb(st, "qat", [128, 512], BF16)
            b_qat = Buf()
            qbt = [sb(st, f"qbt{i}", [64, 1024], BF16) for i in range(2)]
            b_qbt = [Buf(), Buf()]
            wis = sb(st, "wis", [128, 8], F32)
            b_wis = Buf()
            gsb = sb(st, "gsb", [128, 2048], BF16)
            b_gsb = Buf()
            pbc = [0]
            kbanks = {}

            def next_pb():
                i = pbc[0] % NPB
                pbc[0] += 1
                return PB[i], b_PB[i]

            def proj(hT_, bhT_, W, bW, c0, n, bank, bbank, o0=0):
                for kc in range(8):
                    op("pe", lambda kc=kc: PE.matmul(bank[:, o0:o0 + n], lhsT=hT_[:, kc, :], rhs=W[:, kc, c0:c0 + n],
                                                     start=(kc == 0), stop=(kc == 7)),
                       reads=[bhT_, bW], writes=[bbank], sig=(kc == 7))

            def stage_F(l):
                x_, bx = xs[l % 3], b_xs[l % 3]
                c_, bc = cst[l % 4], b_cst[l % 4]
                ss_, ms_, rs_ = ss[l % 2], ms[l % 2], rstd[l % 2]
                hb_, bhb = hb[l % 2], b_hb[l % 2]
                hT_, bhT_ = hT[l % 3], b_hT[l % 3]
                dma("sp", x_[:], xl.ap()[l], writes=[bx])
                dma("sp", c_[:], cs_t.ap()[l], writes=[bc])
                op("act", lambda: ACT.activation(out=junk[:], in_=x_[:], func=AF.Square, accum_out=ss_[:]),
                   reads=[bx], writes=[b_junk, b_ss[l % 2]])
                op("dve", lambda: DVE.tensor_scalar(out=ms_[:], in0=ss_[:], scalar1=1.0 / D, scalar2=EPS,
                                                    op0=ALU.mult, op1=ALU.add), reads=[b_ss[l % 2]], writes=[b_ms[l % 2]])
                op("pool", lambda: POOL.tensor_tensor(out=rs_[:], in0=ms_[:], in1=mhalf[:], op=ALU.pow),
                   reads=[b_ms[l % 2], b_mh], writes=[b_rstd[l % 2]])
                op("pool", lambda: POOL.tensor_scalar(out=hb_[:], in0=x_[:], scalar1=rs_[:, 0:1], scalar2=0.0,
                                                      op0=ALU.mult, op1=ALU.add),
                   reads=[bx, b_rstd[l % 2]], writes=[bhb])
                for kc in range(8):
                    op("pe", lambda kc=kc: PE.transpose(out=TRH[:, kc * 128:(kc + 1) * 128],
                                                        in_=hb_[:, kc * 128:(kc + 1) * 128], identity=identb[:]),
                       reads=[bhb, b_const], writes=[b_TRH], sig=(kc == 7))
                op("act", lambda: ACT.copy(out=hT_[:].rearrange("p a b -> p (a b)"), in_=TRH[:]),
                   reads=[b_TRH], writes=[bhT_])

            def stage_P(l):
                hT_, bhT_ = hT[l % 3], b_hT[l % 3]
                pa, bpa = next_pb()
                proj(hT_, bhT_, WK, b_WK, 0, 512, pa, bpa)
                pv, bpv = next_pb()
                proj(hT_, bhT_, WK, b_WK, 512, 512, pv, bpv)
                pc, bpc = next_pb()
                proj(hT_, bhT_, WK, b_WK, 1024, 128, pc, bpc, 0)
                proj(hT_, bhT_, WK, b_WK, 1152, 64, pc, bpc, 128)
                kbanks[l] = (pa, bpa, pv, bpv, pc, bpc)

            def stage_R(l):
                pa, bpa, pv, bpv, pc, bpc = kbanks.pop(l)
                c_, bc = cst[l % 4], b_cst[l % 4]
                kat_, bkat_ = kat[l % 2], b_kat[l % 2]
                kbit_, bkbit_ = kbit[l % 2], b_kbit[l % 2]
                vaa_, bvaa_ = vaa[l % 2], b_vaa[l % 2]
                vba_, bvba_ = vba[l % 2], b_vba[l % 2]
                op("act", lambda: ACT.activation(out=vaa_[:, :, 0:64], in_=pv[:].rearrange("p (h d) -> p h d", h=8),
                                                 func=AF.Copy, scale=vmask[:, l:l + 1]),
                   reads=[bpv, b_const], writes=[bvaa_])
                op("pool", lambda: POOL.tensor_copy(out=vaa_[:, :, 64:65], in_=bc_mid(vmask[:, l:l + 1], 8)),
                   reads=[b_const], writes=[bvaa_])
                dma("pool", VA.ap()[:, l, :], vaa_[:].rearrange("p h d -> p (h d)"), reads=[bvaa_], writes=[b_VA[l]], fence=("act", "pool"))
                op("act", lambda: ACT.activation(out=vba_[:, 0:64], in_=pc[:, 64:128], func=AF.Copy,
                                                 scale=vmask[:, l:l + 1]), reads=[bpc, b_const], writes=[bvba_])
                op("pool", lambda: POOL.tensor_copy(out=vba_[:, 64:65], in_=vmask[:, l:l + 1]),
                   reads=[b_const], writes=[bvba_])
                dma("pool", VB.ap()[:, l, :], vba_[:], reads=[bvba_], writes=[b_VB[l]], fence=("act", "pool"))
                rope(rbufs, pa[:].rearrange("p (h d) -> p h d", h=8), 8, c_, bc, bpa, kab[:], b_kab)
                zc = AP(pc, 0, [[512, 128], [128, 2], [1, 64]])
                rope(rbufs, zc, 2, c_, bc, bpc, kbi[:], b_kbi)
                for pr in range(4):
                    op("pe", lambda pr=pr: PE.transpose(out=TRO[:, pr * 128:(pr + 1) * 128],
                                                        in_=kab[:].rearrange("p h d -> p (h d)")[:, pr * 128:(pr + 1) * 128],
                                                        identity=identb[:]),
                       reads=[b_kab, b_const], writes=[b_TRO], sig=False)
                for hh in range(2):
                    op("pe", lambda hh=hh: PE.transpose(out=TRO[0:64, 512 + hh * 128:512 + (hh + 1) * 128],
                                                        in_=kbi[:, hh, :], identity=identb[:]),
                       reads=[b_kbi, b_const], writes=[b_TRO], sig=(hh == 1))
                op("act", lambda: ACT.copy(out=kat_[:], in_=TRO[:, 0:512]), reads=[b_TRO], writes=[bkat_])
                op("act", lambda: ACT.copy(out=kbit_[:], in_=TRO[0:64, 512:768]), reads=[b_TRO], writes=[bkbit_])
                dma("pool", KAT.ap()[:, l, :], kat_[:], reads=[bkat_], writes=[b_KAT[l]], fence=("act",))
                dma("pool", KBT.ap()[:, l * 128:(l + 1) * 128], kbit_[:, 0:128], reads=[bkbit_], writes=[b_KBT[l]])
                dma("pool", KIT.ap()[:, l * 128:(l + 1) * 128], kbit_[:, 128:256], reads=[bkbit_], writes=[b_KIT[l]])

            def stage_Q(l):
                m = l // 4
                hT_, bhT_ = hT[l % 3], b_hT[l % 3]
                c_, bc = cst[l % 4], b_cst[l % 4]
                pq, bpq = next_pb()
                proj(hT_, bhT_, WQ, b_WQ, 0, 512, pq, bpq)
                pq2, bpq2 = next_pb()
                proj(hT_, bhT_, WQ, b_WQ, 512, 512, pq2, bpq2)
                pq3, bpq3 = next_pb()
                proj(hT_, bhT_, WQ, b_WQ, 1024, 512, pq3, bpq3)
                pq4, bpq4 = next_pb()
                proj(hT_, bhT_, WQ, b_WQ, 1536, 8, pq4, bpq4)
                op("dve", lambda: DVE.tensor_copy(out=wis[:], in_=pq4[:, 0:8]), reads=[bpq4], writes=[b_wis])
                dma("pool", WI.ap()[m], wis[:], reads=[b_wis], writes=[b_WI[m]], fence=("dve",))
                rope(rbufs, pq[:].rearrange("p (h d) -> p h d", h=8), 8, c_, bc, bpq, qab[:], b_qab)
                for pr in range(4):
                    op("pe", lambda pr=pr: PE.transpose(out=TRO[:, pr * 128:(pr + 1) * 128],
                                                        in_=qab[:].rearrange("p h d -> p (h d)")[:, pr * 128:(pr + 1) * 128],
                                                        identity=identb[:]),
                       reads=[b_qab, b_const], writes=[b_TRO], sig=(pr == 3))
                op("act", lambda: ACT.copy(out=qat[:], in_=TRO[:, 0:512]), reads=[b_TRO], writes=[b_qat])
                dma("pool", QAT.ap()[m], qat[:], reads=[b_qat], writes=[b_QAT[m]], fence=("act",))
                for gq in range(4):
                    pg, bpg = next_pb()
                    proj(hT_, bhT_, WQ, b_WQ, 1544 + gq * 512, 512, pg, bpg)
                    op("act", lambda gq=gq, pg=pg: ACT.activation(out=gsb[:, gq * 512:(gq + 1) * 512], in_=pg[:],
                                                                  func=AF.Sigmoid), reads=[bpg], writes=[b_gsb])
                    if gq < 2:
                        pz, bpz, dstT, bdst = ((pq2, bpq2, QBT, b_QBT), (pq3, bpq3, QIT, b_QIT))[gq]
                        qbt_, bqbt_ = qbt[gq], b_qbt[gq]
                        rope(rbufs, pz[:].rearrange("p (h d) -> p h d", h=8), 8, c_, bc, bpz, qab[:], b_qab)
                        for hh in range(8):
                            op("pe", lambda hh=hh: PE.transpose(out=TRO[0:64, hh * 128:(hh + 1) * 128],
                                                                in_=qab[:, hh, :], identity=identb[:]),
                               reads=[b_qab, b_const], writes=[b_TRO], sig=(hh == 7))
                        op("act", lambda: ACT.copy(out=qbt_[:], in_=TRO[0:64, :]), reads=[b_TRO], writes=[bqbt_])
                        dma("pool", dstT.ap()[m], qbt_[:], reads=[bqbt_], writes=[bdst[m]], fence=("act",))
                dma("pool", GS.ap()[m], gsb[:], reads=[b_gsb], writes=[b_GS[m]], fence=("act",))

            stage_F(0)
            stage_F(1)
            stage_P(0)
            for l in range(NB):
                if l + 2 < NB:
                    stage_F(l + 2)
                if l % 4 == 3:
                    stage_R(l)
                    stage_Q(l)
                    if l + 1 < NB:
                        stage_P(l + 1)
                else:
                    if l + 1 < NB:
                        stage_P(l + 1)
                    stage_R(l)

        if _DBG == "1":
            kb.finish(b_KAT + b_VA + b_KBT + b_KIT + b_VB + b_QAT + b_QBT + b_QIT + b_WI + b_GS)
            return nc, dbg_outs

        kb.barrier()
        with ExitStack() as st:
            MT = sb(st, "MT", [128, 17 * 128], BF16)
            b_MT = Buf()
            dma("sp", MT[:], mt_d.ap().rearrange("p a b -> p (a b)"), writes=[b_MT])
            kw = [sb(st, f"kw{i}", [128, 17, 512], BF16) for i in range(2)]
            b_kw = [Buf(), Buf()]
            vw = [sb(st, f"vw{i}", [128, 17, 520], BF16) for i in range(2)]
            b_vw = [Buf(), Buf()]
            qa_t = [sb(st, f"qa{i}", [128, 512], BF16) for i in range(2)]
            b_qa = [Buf(), Buf()]
            STb = [ps(st, f"ST{i}", [128, 512], F32) for i in range(3)]
            b_ST = [Buf() for _ in range(3)]
            OA = [ps(st, f"OA{i}", [128, 512], F32) for i in range(4)]
            b_OA = [Buf() for _ in range(4)]
            TR = ps(st, "TR2", [128, 1024], BF16)
            b_TR = Buf()
            E = [sb(st, f"E{i}", [128, 512], BF16) for i in range(3)]
            b_E = [Buf() for _ in range(3)]
            Pm = [sb(st, f"P{i}", [128, 512], BF16) for i in range(3)]
            b_P = [Buf() for _ in range(3)]
            rc = sb(st, "rc", [128, 8], F32)
            b_rc = Buf()
            ya = sb(st, "ya", [128, 8, 64], BF16)
            b_ya = Buf()
            c2 = {"st": 0, "e": 0}
            pending_fin = []

            def fin2(m):
                oa = OA[2 * (m % 2):2 * (m % 2) + 2]
                boa = b_OA[2 * (m % 2):2 * (m % 2) + 2]
                for bnk in range(2):
                    ov = oa[bnk][:, 0:260].rearrange("p (h d) -> p h d", d=65)
                    op("dve", lambda: DVE.reciprocal(out=rc[:, bnk * 4:(bnk + 1) * 4], in_=ov[:, :, 64]),
                       reads=[boa[bnk]], writes=[b_rc])
                    op("dve", lambda: DVE.tensor_tensor(out=ya[:, bnk * 4:(bnk + 1) * 4, :], in0=ov[:, :, 0:64],
                                                        in1=bc_last(rc[:, bnk * 4:(bnk + 1) * 4], 64), op=ALU.mult),
                       reads=[boa[bnk], b_rc], writes=[b_ya])
                ya2 = ya[:].rearrange("p h d -> p (h d)")
                for kc in range(4):
                    op("pe", lambda kc=kc: PE.transpose(out=TR[:, kc * 128:(kc + 1) * 128],
                                                        in_=ya2[:, kc * 128:(kc + 1) * 128], identity=identb[:]),
                       reads=[b_ya, b_const], writes=[b_TR], sig=(kc == 3))
                op("act", lambda: ACT.copy(out=YAT[:, :, m * 128:(m + 1) * 128],
                                           in_=TR[:, 0:512].rearrange("p (a b) -> p a b", b=128)),
                   reads=[b_TR], writes=[b_YAT[m]])

            for m in range(NOWN):
                lq = 4 * m + 3
                lo = max(0, lq - 16)
                nb = lq - lo + 1
                k_, v_, q_ = kw[m % 2], vw[m % 2], qa_t[m % 2]
                bk, bv, bq = b_kw[m % 2], b_vw[m % 2], b_qa[m % 2]
                oa = OA[2 * (m % 2):2 * (m % 2) + 2]
                boa = b_OA[2 * (m % 2):2 * (m % 2) + 2]
                dma("sp", k_[:, 0:nb, :], KAT.ap()[:, lo:lq + 1, :], reads=b_KAT[lo:lq + 1], writes=[bk])
                dma("sp", v_[:, 0:nb, :], VA.ap()[:, lo:lq + 1, :], reads=b_VA[lo:lq + 1], writes=[bv])
                dma("sp", q_[:], QAT.ap()[m], reads=[b_QAT[m]], writes=[bq])
                first_bank = [True, True]
                groups = [list(range(i, min(i + 4, nb))) for i in range(0, nb, 4)]
                units = [(h, gi) for h in range(8) for gi in range(len(groups))]
                nu = len(units)
                slots = {}

                def emit_qk(u):
                    h, gi = units[u]
                    grp = groups[gi]
                    n = len(grp)
                    base = 64 * (h % 2)
                    pair = h // 2
                    k = c2["st"] % 3
                    c2["st"] += 1
                    slots[u] = k
                    for i, dl in enumerate(grp):
                        slot = nb - 1 - dl
                        op("pe", lambda i=i, slot=slot: PE.matmul(
                            STb[k][:, i * 128:(i + 1) * 128],
                            lhsT=k_[base:base + 64, slot, pair * 128:(pair + 1) * 128],
                            rhs=q_[base:base + 64, pair * 128:(pair + 1) * 128], start=True, stop=True),
                           reads=[bk, bq], writes=[b_ST[k]], sig=(i == n - 1))

                for u in range(min(3, nu)):
                    emit_qk(u)
                for u in range(nu):
                    h, gi = units[u]
                    grp = groups[gi]
                    n = len(grp)
                    k = slots[u]
                    ke = c2["e"] % 3
                    c2["e"] += 1
                    e_, be, p_, bp = E[ke], b_E[ke], Pm[ke], b_P[ke]
                    op("act", lambda: ACT.activation(out=e_[:, 0:n * 128], in_=STb[k][:, 0:n * 128], func=AF.Exp,
                                                     scale=0.125), reads=[b_ST[k]], writes=[be])
                    d0 = grp[0]
                    op("dve", lambda: DVE.tensor_tensor(out=p_[:, 0:n * 128], in0=e_[:, 0:n * 128],
                                                        in1=MT[:, d0 * 128:(d0 + n) * 128], op=ALU.mult),
                       reads=[be, b_MT], writes=[bp])
                    for i, dl in enumerate(grp):
                        slot = nb - 1 - dl
                        is_first = first_bank[h // 4]
                        first_bank[h // 4] = False
                        is_last = (h % 4 == 3 and gi == len(groups) - 1 and i == n - 1)
                        op("pe", lambda i=i, slot=slot, is_first=is_first, is_last=is_last: PE.matmul(
                            oa[h // 4][:, (h % 4) * 65:(h % 4) * 65 + 65], lhsT=p_[:, i * 128:(i + 1) * 128],
                            rhs=v_[:, slot, h * 65:(h + 1) * 65], start=is_first, stop=is_last,
                            skip_group_check=True),
                           reads=[bp, bv], writes=[boa[h // 4]], sig=(i == n - 1))
                    if u + 3 < nu:
                        emit_qk(u + 3)
                    if u == min(3, nu - 1) and pending_fin:
                        fin2(pending_fin.pop(0))
                pending_fin.append(m)
            while pending_fin:
                fin2(pending_fin.pop(0))

        if _DBG == "2":
            dy = dbg_out("YAT", [128, 4, NOWN * 128], BF16)
            bo = Buf()
            dma("sp", dy.ap(), YAT[:], reads=b_YAT, writes=[bo])
            kb.finish([bo])
            return nc, dbg_outs

        kb.barrier()
        with ExitStack() as st:
            KIs = sb(st, "KIs", [64, NB * 128], BF16)
            KBs = sb(st, "KBs", [64, NB * 128], BF16)
            VBs = sb(st, "VBs", [128, NB, 65], BF16)
            b_KIs, b_KBs, b_VBs = Buf(), Buf(), Buf()
            dma("sp", KIs[:], KIT.ap(), reads=b_KIT, writes=[b_KIs])
            dma("sp", KBs[:], KBT.ap(), reads=b_KBT, writes=[b_KBs])
            dma("sp", VBs[:], VB.ap(), reads=b_VB, writes=[b_VBs])
            padb = sb(st, "padb", [128, 512], F32)
            diagb = sb(st, "diagb", [128, 512], F32)
            pow2 = sb(st, "pow2", [128, NIT + 1], F32)
            b_c3 = Buf()
            dma("sp", padb[:], padbias_d.ap(), writes=[b_c3])
            dma("sp", diagb[:], diagbias_d.ap(), writes=[b_c3])
            dma("sp", pow2[:], pow2_d.ap(), writes=[b_c3])
            qi_t = [sb(st, f"qi{i}", [64, 1024], BF16) for i in range(2)]
            qb_t = [sb(st, f"qb{i}", [64, 1024], BF16) for i in range(2)]
            wi_t = [sb(st, f"wi{i}", [128, 8], F32) for i in range(2)]
            b_qi, b_qb, b_wi = [Buf(), Buf()], [Buf(), Buf()], [Buf(), Buf()]
            scores = sb(st, "scores", [128, NB * 128], F32)
            b_sc = [Buf() for _ in range(NOWN)]
            junkc = sb(st, "junkc", [128, NB * 128], BF16)
            b_jc = Buf()
            PX = [ps(st, f"PX{i}", [128, 512], F32) for i in range(3)]
            b_PX = [Buf() for _ in range(3)]
            SCb = [ps(st, f"SC{i}", [128, 512], F32) for i in range(2)]
            b_SCb = [Buf(), Buf()]
            OB = [ps(st, f"OB{i}", [128, 512], F32) for i in range(2)]
            b_OB = [Buf(), Buf()]
            TR3 = ps(st, "TR3", [128, 1024], BF16)
            b_TR3 = Buf()
            R = [sb(st, f"R{i}", [128, 512], BF16) for i in range(4)]
            b_R = [Buf() for _ in range(4)]
            selfull = sb(st, "selfull", [128, NB * 128], BF16)
            b_selfull = Buf()
            absw = sb(st, "absw", [128, 8], F32)
            sgh = sb(st, "sgh", [128, 8], F32)
            Dg = sb(st, "Dg", [128, 8, 128], BF16)
            b_absw, b_sgh, b_Dg = Buf(), Buf(), Buf()
            cmin = sb(st, "cmin", [128, NOWN], F32)
            b_cmin = Buf()
            rmin = sb(st, "rmin", [128, 1], F32)
            rmax = sb(st, "rmax", [128, 1], F32)
            rng = sb(st, "rng", [128, 1], F32)
            tsum = sb(st, "tsum", [128, 1], F32)
            tau = [sb(st, f"tau{i}", [128, 1], F32) for i in range(2)]
            cntt = sb(st, "cntt", [128, 1], F32)
            sg = sb(st, "sg", [128, 1], F32)
            step2 = sb(st, "step2", [128, NIT + 1], F32)
            tsel = sb(st, "tsel", [128, 1], F32)
            b_rmin, b_rmax, b_rng, b_tsum, b_cnt, b_sg, b_step2, b_tsel = (Buf() for _ in range(8))
            b_tau = [Buf(), Buf()]
            sel = [sb(st, f"sel{i}", [128, 512], BF16) for i in range(2)]
            selT = [sb(st, f"selT{i}", [128, 512], BF16) for i in range(2)]
            b_sel, b_selT = [Buf(), Buf()], [Buf(), Buf()]
            Eb = [sb(st, f"Eb{i}", [128, 512], BF16) for i in range(3)]
            b_Eb = [Buf() for _ in range(3)]
            rcb = sb(st, "rcb", [128, 8], F32)
            b_rcb = Buf()
            yb = sb(st, "yb", [128, 8, 64], BF16)
            b_yb = Buf()
            scb = [scores, sb(st, "scores1", [128, NB * 128], F32)]
            b_scb = [b_sc, [Buf() for _ in range(NOWN)]]
            tselb = [tsel, sb(st, "tsel1", [128, 1], F32)]
            b_tselb = [b_tsel, Buf()]
            ctr = {"px": 0, "kr": 0, "ke": 0}

            def stage_S(m):
                nch = m + 1
                qi_, wi_ = qi_t[m % 2], wi_t[m % 2]
                bqi, bwi = b_qi[m % 2], b_wi[m % 2]
                sc_, bsc_ = scb[m % 2], b_scb[m % 2]
                dma("sp", qi_[:], QIT.ap()[m], reads=[b_QIT[m]], writes=[bqi])
                dma("sp", wi_[:], WI.ap()[m], reads=[b_WI[m]], writes=[bwi])
                op("dve", lambda: DVE.tensor_scalar(out=sgh[:], in0=wi_[:], scalar1=0.0, scalar2=0.5,
                                                    op0=ALU.is_ge, op1=ALU.subtract), reads=[bwi], writes=[b_sgh])
                op("dve", lambda: DVE.scalar_tensor_tensor(out=absw[:], in0=sgh[:], scalar=2.0, in1=wi_[:],
                                                           op0=ALU.mult, op1=ALU.mult),
                   reads=[bwi, b_sgh], writes=[b_absw])
                for h in range(8):
                    op("dve", lambda h=h: DVE.tensor_scalar(out=Dg[:, h, :], in0=identb[:], scalar1=sgh[:, h:h + 1],
                                                            scalar2=2.0, op0=ALU.mult, op1=ALU.mult),
                       reads=[b_sgh, b_const], writes=[b_Dg])
                n = 8 * nch
                slots = {}

                def emit_d(i):
                    c, h = divmod(i, 8)
                    k = ctr["px"] % 3
                    ctr["px"] += 1
                    slots[i] = k
                    op("pe", lambda: PE.matmul(PX[k][:], lhsT=qi_[:, h * 128:(h + 1) * 128],
                                               rhs=KIs[:, c * 512:(c + 1) * 512], start=True, stop=True),
                       reads=[bqi, b_KIs], writes=[b_PX[k]])

                def emit_evac(c):
                    op("act", lambda: ACT.copy(out=sc_[:, c * 512:(c + 1) * 512], in_=SCb[c % 2][:]),
                       reads=[b_SCb[c % 2]], writes=[bsc_[c]])

                for i in range(min(3, n)):
                    emit_d(i)
                pend = []
                for i in range(n):
                    c, h = divmod(i, 8)
                    k = slots[i]
                    r_, br = R[ctr["kr"] % 4], b_R[ctr["kr"] % 4]
                    ctr["kr"] += 1
                    op("act", lambda: ACT.activation(out=r_[:], in_=PX[k][:], func=AF.Relu, scale=absw[:, h:h + 1]),
                       reads=[b_PX[k], b_absw], writes=[br])
                    op("pe", lambda: PE.matmul(SCb[c % 2][:], lhsT=Dg[:, h, :], rhs=r_[:], start=(h == 0), stop=(h == 7)),
                       reads=[br, b_Dg], writes=[b_SCb[c % 2]])
                    if i + 3 < n:
                        emit_d(i + 3)
                    if pend and pend[0][1] <= i:
                        emit_evac(pend.pop(0)[0])
                    if h == 7:
                        pend.append((c, i + 2))
                for c_, _ in pend:
                    emit_evac(c_)

            def stage_B(m):
                nch = m + 1
                S = 512 * nch
                sc_, bsc_ = scb[m % 2], b_scb[m % 2]
                bsc = bsc_[0:nch]
                op("dve", lambda: DVE.tensor_reduce(out=rmin[:], in_=sc_[:, 0:S], axis=AX.X, op=ALU.min),
                   reads=bsc, writes=[b_rmin])
                op("dve", lambda: DVE.tensor_tensor(out=sc_[:, 0:512], in0=sc_[:, 0:512], in1=padb[:], op=ALU.add),
                   reads=[b_c3, bsc_[0]], writes=[bsc_[0]])
                op("dve", lambda: DVE.tensor_tensor(out=sc_[:, S - 512:S], in0=sc_[:, S - 512:S], in1=diagb[:], op=ALU.add),
                   reads=[b_c3, bsc_[nch - 1]], writes=[bsc_[nch - 1]])
                op("dve", lambda: DVE.tensor_reduce(out=rmax[:], in_=sc_[:, 0:S], axis=AX.X, op=ALU.max),
                   reads=bsc, writes=[b_rmax])
                op("dve", lambda: DVE.scalar_tensor_tensor(out=rng[:], in0=rmax[:], scalar=2.0, in1=rmin[:],
                                                           op0=ALU.add, op1=ALU.subtract),
                   reads=[b_rmax, b_rmin], writes=[b_rng])
                op("dve", lambda: DVE.tensor_tensor(out=tsum[:], in0=rmax[:], in1=rmin[:], op=ALU.add),
                   reads=[b_rmax, b_rmin], writes=[b_tsum])
                op("dve", lambda: DVE.tensor_scalar(out=tau[0][:], in0=tsum[:], scalar1=0.5, scalar2=None, op0=ALU.mult),
                   reads=[b_tsum], writes=[b_tau[0]])
                op("dve", lambda: DVE.tensor_scalar(out=step2[:], in0=pow2[:], scalar1=rng[:, 0:1], scalar2=None,
                                                    op0=ALU.mult), reads=[b_rng, b_c3], writes=[b_step2])
                for it in range(NIT):
                    tc_, tn_ = tau[it % 2], tau[(it + 1) % 2]
                    btc, btn = b_tau[it % 2], b_tau[(it + 1) % 2]
                    op("dve", lambda: DVE.tensor_scalar(out=junkc[:, 0:S], in0=sc_[:, 0:S], scalar1=tc_[:, 0:1],
                                                        scalar2=None, op0=ALU.is_ge, op1=ALU.add, accum_out=cntt[:]),
                       reads=bsc + [btc], writes=[b_jc, b_cnt])
                    op("dve", lambda: DVE.tensor_scalar(out=sg[:], in0=cntt[:], scalar1=255.5, scalar2=0.5,
                                                        op0=ALU.is_ge, op1=ALU.subtract), reads=[b_cnt], writes=[b_sg])
                    op("dve", lambda: DVE.scalar_tensor_tensor(out=tn_[:], in0=sg[:], scalar=step2[:, it:it + 1],
                                                               in1=tc_[:], op0=ALU.mult, op1=ALU.add),
                       reads=[b_sg, b_step2, btc], writes=[btn])
                tf_, btf = tau[NIT % 2], b_tau[NIT % 2]
                op("dve", lambda: DVE.tensor_tensor(out=tsel[:], in0=tf_[:], in1=step2[:, NIT:NIT + 1], op=ALU.subtract),
                   reads=[btf, b_step2], writes=[b_tsel])
                op("dve", lambda: DVE.tensor_scalar(out=selfull[:, 0:S], in0=sc_[:, 0:S], scalar1=tsel[:, 0:1],
                                                    scalar2=None, op0=ALU.is_ge),
                   reads=bsc + [b_tsel], writes=[b_selfull])

            def stage_A(m):
                nch = m + 1
                qb_, bqb = qb_t[m % 2], b_qb[m % 2]
                dma("sp", qb_[:], QBT.ap()[m], reads=[b_QBT[m]], writes=[bqb])
                first_ob = [True, True]
                n = 8 * nch
                slots = {}

                def prep(c):
                    sT_, bsT_ = selT[c % 2], b_selT[c % 2]
                    for kq in range(4):
                        op("pe", lambda kq=kq: PE.transpose(out=TR3[:, kq * 128:(kq + 1) * 128],
                                                            in_=selfull[:, c * 512 + kq * 128:c * 512 + (kq + 1) * 128],
                                                            identity=identb[:]),
                           reads=[b_selfull, b_const], writes=[b_TR3], sig=(kq == 3))
                    op("act", lambda: ACT.activation(out=sT_[:], in_=TR3[:, 0:512], func=AF.Identity, scale=NEGM,
                                                     bias=-NEGM), reads=[b_TR3], writes=[bsT_])

                def emit_st(u):
                    c, rem = divmod(u, 8)
                    kq, i = divmod(rem, 2)
                    if rem == 0 and c + 1 < nch:
                        prep(c + 1)
                    ls = 4 * c + kq
                    k = ctr["px"] % 3
                    ctr["px"] += 1
                    slots[u] = k
                    sT_, bsT_ = selT[c % 2], b_selT[c % 2]
                    bank3 = PX[k][:].rearrange("p (a b) -> p a b", b=128)
                    op("pe", lambda: PE.matmul(bank3, lhsT=KBs[:, ls * 128:(ls + 1) * 128],
                                               rhs=qb_[:, i * 512:(i + 1) * 512].rearrange("p (a b) -> p a b", b=128),
                                               start=True, stop=False),
                       reads=[b_KBs, bqb], writes=[b_PX[k]], sig=False)
                    op("pe", lambda: PE.matmul(bank3, lhsT=identb[:], rhs=bc_mid(sT_[:, kq * 128:(kq + 1) * 128], 4),
                                               start=False, stop=True),
                       reads=[bsT_, b_const], writes=[b_PX[k]])

                prep(0)
                for u in range(min(3, n)):
                    emit_st(u)
                for u in range(n):
                    c, rem = divmod(u, 8)
                    kq, i = divmod(rem, 2)
                    ls = 4 * c + kq
                    k = slots[u]
                    e_, be = Eb[ctr["ke"] % 3], b_Eb[ctr["ke"] % 3]
                    ctr["ke"] += 1
                    op("act", lambda: ACT.activation(out=e_[:], in_=PX[k][:], func=AF.Exp, scale=0.125),
                       reads=[b_PX[k]], writes=[be])
                    for hh in range(4):
                        is_first = first_ob[i]
                        first_ob[i] = False
                        is_last = (u >= n - 2 and hh == 3)
                        op("pe", lambda hh=hh, is_first=is_first, is_last=is_last: PE.matmul(
                            OB[i][:, hh * 65:hh * 65 + 65], lhsT=e_[:, hh * 128:(hh + 1) * 128],
                            rhs=VBs[:, ls, :], start=is_first, stop=is_last, skip_group_check=True),
                           reads=[be, b_VBs], writes=[b_OB[i]], sig=(hh == 3))
                    if u + 3 < n:
                        emit_st(u + 3)

            def stage_norm(m):
                for bnk in range(2):
                    ov = OB[bnk][:, 0:260].rearrange("p (h d) -> p h d", d=65)
                    op("dve", lambda: DVE.reciprocal(out=rcb[:, bnk * 4:(bnk + 1) * 4], in_=ov[:, :, 64]),
                       reads=[b_OB[bnk]], writes=[b_rcb])
                    op("dve", lambda: DVE.tensor_tensor(out=yb[:, bnk * 4:(bnk + 1) * 4, :], in0=ov[:, :, 0:64],
                                                        in1=bc_last(rcb[:, bnk * 4:(bnk + 1) * 4], 64), op=ALU.mult),
                       reads=[b_OB[bnk], b_rcb], writes=[b_yb])

            def stage_fin(m):
                yb2 = yb[:].rearrange("p h d -> p (h d)")
                for kc in range(4):
                    op("pe", lambda kc=kc: PE.transpose(out=TR3[:, kc * 128:(kc + 1) * 128],
                                                        in_=yb2[:, kc * 128:(kc + 1) * 128], identity=identb[:]),
                       reads=[b_yb, b_const], writes=[b_TR3], sig=(kc == 3))
                op("act", lambda: ACT.copy(out=YBT[:, :, m * 128:(m + 1) * 128],
                                           in_=TR3[:, 0:512].rearrange("p (a b) -> p a b", b=128)),
                   reads=[b_TR3], writes=[b_YBT[m]])

            stage_S(0)
            for m in range(NOWN):
                if m + 1 < NOWN:
                    stage_S(m + 1)
                stage_B(m)
                if m >= 1:
                    stage_norm(m - 1)
                    stage_fin(m - 1)
                stage_A(m)
            stage_norm(NOWN - 1)
            stage_fin(NOWN - 1)

        if _DBG == "3":
            dy = dbg_out("YBT", [128, 4, NOWN * 128], BF16)
            bo = Buf()
            dma("sp", dy.ap(), YBT[:], reads=b_YBT, writes=[bo])
            kb.finish([bo])
            return nc, dbg_outs

        kb.barrier()
        with ExitStack() as st45:
            H2T = sb(st45, "H2T", [128, 8, NOWN * 128], BF16)
            b_H2T = [Buf() for _ in range(NOWN)]
            ss = sb(st45, "ss2", [128, 1], F32)
            ms = sb(st45, "ms2", [128, 1], F32)
            rstd = sb(st45, "rstd2", [128, 1], F32)
            mhalf = sb(st45, "mhalf2", [128, 1], F32)
            b_ss, b_ms, b_rstd, b_mh = Buf(), Buf(), Buf(), Buf()
            op("pool", lambda: POOL.memset(mhalf[:], -0.5), writes=[b_mh])
            junk = sb(st45, "junk2", [128, D], BF16)
            b_junk = Buf()
            with ExitStack() as st:
                WUA = sb(st, "WUA", [128, 4, D], BF16)
                WUB = sb(st, "WUB", [128, 4, D], BF16)
                WO = sb(st, "WO", [128, 8, D], BF16)
                wst2 = sb(st, "wst2", [128, 8, D], F32)
                b_w2, b_WUA, b_WUB, b_WO = Buf(), Buf(), Buf(), Buf()
                dma("sp", wst2[:, 0:4, :], w_up_a.ap().rearrange("(kc p) n -> p kc n", p=128), writes=[b_w2])
                op("pool", lambda: POOL.tensor_copy(out=WUA[:], in_=wst2[:, 0:4, :]), reads=[b_w2], writes=[b_WUA])
                dma("sp", wst2[:, 0:4, :], w_up_b.ap().rearrange("(kc p) n -> p kc n", p=128), writes=[b_w2])
                op("pool", lambda: POOL.tensor_copy(out=WUB[:], in_=wst2[:, 0:4, :]), reads=[b_w2], writes=[b_WUB])
                dma("sp", wst2[:], w_out.ap().rearrange("(kc p) n -> p kc n", p=128), writes=[b_w2])
                op("pool", lambda: POOL.tensor_copy(out=WO[:], in_=wst2[:]), reads=[b_w2], writes=[b_WO])
                gs_t = [sb(st, f"gs{i}", [128, 2048], BF16) for i in range(2)]
                x_t = [sb(st, f"x4{i}", [128, D], F32) for i in range(2)]
                b_gs, b_x4 = [Buf(), Buf()], [Buf(), Buf()]
                UA = [ps(st, f"UA{i}", [128, 512], F32) for i in range(2)]
                UB = [ps(st, f"UB{i}", [128, 512], F32) for i in range(2)]
                WOp = [ps(st, f"WOp{i}", [128, 512], F32) for i in range(2)]
                TR4 = ps(st, "TR4", [128, 1024], BF16)
                b_UA, b_UB, b_WOp = [Buf(), Buf()], [Buf(), Buf()], [Buf(), Buf()]
                b_TR4 = Buf()
                t1 = [sb(st, f"t1{i}", [128, 512], F32) for i in range(2)]
                t2 = [sb(st, f"t2{i}", [128, 512], F32) for i in range(2)]
                b_t1, b_t2 = [Buf(), Buf()], [Buf(), Buf()]
                mg = sb(st, "mg", [128, D], BF16)
                mgT = sb(st, "mgT", [128, 8, 128], BF16)
                b_mg, b_mgT = Buf(), Buf()
                x2 = [sb(st, f"x2{i}", [128, D], F32) for i in range(2)]
                b_x2 = [Buf(), Buf()]
                h2b = sb(st, "h2b", [128, D], BF16)
                b_h2b = Buf()
                for m in range(NOWN):
                    g_, bg_ = gs_t[m % 2], b_gs[m % 2]
                    x_, bx_ = x_t[m % 2], b_x4[m % 2]
                    x2_, bx2_ = x2[m % 2], b_x2[m % 2]
                    dma("sp", g_[:], GS.ap()[m], reads=[b_GS[m]], writes=[bg_])
                    dma("sp", x_[:], xl.ap()[4 * m + 3], writes=[bx_])
                    tk = slice(m * 128, (m + 1) * 128)
                    for half in range(2):
                        cs_ = slice(half * 512, (half + 1) * 512)
                        for kc in range(4):
                            op("pe", lambda kc=kc: PE.matmul(UA[half][:], lhsT=YAT[:, kc, tk], rhs=WUA[:, kc, cs_],
                                                             start=(kc == 0), stop=(kc == 3)),
                               reads=[b_YAT[m], b_WUA], writes=[b_UA[half]], sig=(kc == 3))
                        for kc in range(4):
                            op("pe", lambda kc=kc: PE.matmul(UB[half][:], lhsT=YBT[:, kc, tk], rhs=WUB[:, kc, cs_],
                                                             start=(kc == 0), stop=(kc == 3)),
                               reads=[b_YBT[m], b_WUB], writes=[b_UB[half]], sig=(kc == 3))
                        op("dve", lambda: DVE.tensor_tensor(out=t1[half][:], in0=UA[half][:], in1=g_[:, cs_], op=ALU.mult),
                           reads=[b_UA[half], bg_], writes=[b_t1[half]])
                        op("dve", lambda: DVE.tensor_tensor(out=t2[half][:], in0=UB[half][:],
                                                            in1=g_[:, 1024 + half * 512:1024 + (half + 1) * 512], op=ALU.mult),
                           reads=[b_UB[half], bg_], writes=[b_t2[half]])
                        op("pool", lambda: POOL.tensor_tensor(out=mg[:, cs_], in0=t1[half][:], in1=t2[half][:], op=ALU.add),
                           reads=[b_t1[half], b_t2[half]], writes=[b_mg])
                    for kc in range(8):
                        op("pe", lambda kc=kc: PE.transpose(out=TR4[:, kc * 128:(kc + 1) * 128],
                                                            in_=mg[:, kc * 128:(kc + 1) * 128], identity=identb[:]),
                           reads=[b_mg, b_const], writes=[b_TR4], sig=(kc == 7))
                    op("act", lambda: ACT.copy(out=mgT[:].rearrange("p a b -> p (a b)"), in_=TR4[:]),
                       reads=[b_TR4], writes=[b_mgT])
                    for half in range(2):
                        cs_ = slice(half * 512, (half + 1) * 512)
                        for kc in range(8):
                            op("pe", lambda kc=kc: PE.matmul(WOp[half][:], lhsT=mgT[:, kc, :], rhs=WO[:, kc, cs_],
                                                             start=(kc == 0), stop=(kc == 7)),
                               reads=[b_mgT, b_WO], writes=[b_WOp[half]], sig=(kc == 7))
                        op("dve", lambda: DVE.tensor_tensor(out=x2_[:, cs_], in0=WOp[half][:], in1=x_[:, cs_], op=ALU.add),
                           reads=[b_WOp[half], bx_], writes=[bx2_])
                    dma("pool", X2.ap()[m], x2_[:], reads=[bx2_], writes=[b_X2[m]], fence=("dve",))
                    op("act", lambda: ACT.activation(out=junk[:], in_=x2_[:], func=AF.Square, accum_out=ss[:]),
                       reads=[bx2_], writes=[b_junk, b_ss])
                    op("dve", lambda: DVE.tensor_scalar(out=ms[:], in0=ss[:], scalar1=1.0 / D, scalar2=EPS,
                                                        op0=ALU.mult, op1=ALU.add), reads=[b_ss], writes=[b_ms])
                    op("pool", lambda: POOL.tensor_tensor(out=rstd[:], in0=ms[:], in1=mhalf[:], op=ALU.pow),
                       reads=[b_ms, b_mh], writes=[b_rstd])
                    op("pool", lambda: POOL.tensor_scalar(out=h2b[:], in0=x2_[:], scalar1=rstd[:, 0:1], scalar2=0.0,
                                                          op0=ALU.mult, op1=ALU.add),
                       reads=[bx2_, b_rstd], writes=[b_h2b])
                    for kc in range(8):
                        op("pe", lambda kc=kc: PE.transpose(out=TR4[:, kc * 128:(kc + 1) * 128],
                                                            in_=h2b[:, kc * 128:(kc + 1) * 128], identity=identb[:]),
                           reads=[b_h2b, b_const], writes=[b_TR4], sig=(kc == 7))
                    op("act", lambda: ACT.copy(out=H2T[:, :, tk], in_=TR4[:].rearrange("p (a b) -> p a b", b=128)),
                       reads=[b_TR4], writes=[b_H2T[m]])

            if _DBG == "4":
                dx = dbg_out("X2o", [NOWN, 128, D], F32)
                bo = Buf()
                with ExitStack() as st:
                    tt = sb(st, "dbgt", [128, D], F32)
                    bt = Buf()
                    for m in range(NOWN):
                        dma("sp", tt[:], X2.ap()[m], reads=[b_X2[m]], writes=[bt])
                        dma("sp", dx.ap()[m], tt[:], reads=[bt], writes=[bo])
                    kb.finish([bo])
                return nc, dbg_outs

            kb.barrier()
            with ExitStack() as st:
                gffn = sb(st, "gffn", [128, 8], F32)
                gfin = sb(st, "gfin", [128, D], F32)
                b_g5 = Buf()
                dma("sp", gffn[:], gffn_d.ap(), writes=[b_g5])
                dma("sp", gfin[:], gfin_d.ap(), writes=[b_g5])
                ACTT = sb(st, "ACTT", [128, NFF, NOWN * 128], BF16)
                b_ACTT = [Buf() for _ in range(4)]
                with ExitStack() as sg_:
                    wgst = [sb(sg_, f"wgst{i}", [128, 8, 128], F32) for i in range(2)]
                    wust = [sb(sg_, f"wust{i}", [128, 8, 128], F32) for i in range(2)]
                    wgb = [sb(sg_, f"wgb{i}", [128, 8, 128], BF16) for i in range(2)]
                    wub = [sb(sg_, f"wub{i}", [128, 8, 128], BF16) for i in range(2)]
                    b_wgst, b_wust, b_wgb, b_wub = ([Buf(), Buf()] for _ in range(4))
                    G = [ps(sg_, f"G{i}", [128, 512], F32) for i in range(2)]
                    U = [ps(sg_, f"U{i}", [128, 512], F32) for i in range(2)]
                    b_G, b_U = [Buf(), Buf()], [Buf(), Buf()]
                    sgt = [sb(sg_, f"sgt{i}", [128, 512], F32) for i in range(2)]
                    b_sgt = [Buf(), Buf()]
                    wgv = w_gate.ap().rearrange("(kc p) n -> p kc n", p=128)
                    wuv = w_up.ap().rearrange("(kc p) n -> p kc n", p=128)
                    kk = 0
                    for ffc in range(NFF):
                        i2 = ffc % 2
                        dma("sp", wgst[i2][:], wgv[:, :, ffc * 128:(ffc + 1) * 128], writes=[b_wgst[i2]])
                        dma("sp", wust[i2][:], wuv[:, :, ffc * 128:(ffc + 1) * 128], writes=[b_wust[i2]])
                        op("pool", lambda: POOL.tensor_tensor(out=wgb[i2][:], in0=wgst[i2][:], in1=bc_last(gffn[:, :], 128),
                                                              op=ALU.mult), reads=[b_wgst[i2], b_g5], writes=[b_wgb[i2]])
                        op("pool", lambda: POOL.tensor_tensor(out=wub[i2][:], in0=wust[i2][:], in1=bc_last(gffn[:, :], 128),
                                                              op=ALU.mult), reads=[b_wust[i2], b_g5], writes=[b_wub[i2]])
                        for tg in range(4):
                            ts_ = slice(tg * 512, (tg + 1) * 512)
                            g_, bg_ = G[kk % 2], b_G[kk % 2]
                            u_, bu_ = U[kk % 2], b_U[kk % 2]
                            s_, bs_ = sgt[kk % 2], b_sgt[kk % 2]
                            kk += 1
                            for kc in range(8):
                                op("pe", lambda kc=kc, g_=g_: PE.matmul(g_[:], lhsT=wgb[i2][:, kc, :], rhs=H2T[:, kc, ts_],
                                                                        start=(kc == 0), stop=(kc == 7)),
                                   reads=[b_wgb[i2]] + b_H2T[tg * 4:(tg + 1) * 4], writes=[bg_], sig=(kc == 7))
                            for kc in range(8):
                                op("pe", lambda kc=kc, u_=u_: PE.matmul(u_[:], lhsT=wub[i2][:, kc, :], rhs=H2T[:, kc, ts_],
                                                                        start=(kc == 0), stop=(kc == 7)),
                                   reads=[b_wub[i2]] + b_H2T[tg * 4:(tg + 1) * 4], writes=[bu_], sig=(kc == 7))
                            op("act", lambda g_=g_, s_=s_: ACT.activation(out=s_[:], in_=g_[:], func=AF.Silu),
                               reads=[bg_], writes=[bs_])
                            op("dve", lambda u_=u_, s_=s_: DVE.tensor_tensor(out=ACTT[:, ffc, ts_], in0=u_[:], in1=s_[:],
                                                                             op=ALU.mult),
                               reads=[bu_, bs_], writes=[b_ACTT[tg]])
                kb.barrier()
                with ExitStack() as sd_:
                    wdst = [sb(sd_, f"wdst{i}", [128, D], F32) for i in range(2)]
                    wdb = [sb(sd_, f"wdb{i}", [128, D], BF16) for i in range(2)]
                    b_wdst, b_wdb = [Buf(), Buf()], [Buf(), Buf()]
                    DN = [ps(sd_, f"DN{i}", [128, 512], F32) for i in range(8)]
                    b_DN = [Buf() for _ in range(8)]
                    x2t = [sb(sd_, f"x2t{i}", [128, D], F32) for i in range(2)]
                    x3 = [sb(sd_, f"x3{i}", [128, D], F32) for i in range(2)]
                    ot = [sb(sd_, f"ot{i}", [128, D], F32) for i in range(2)]
                    b_x2t, b_x3, b_ot = [Buf(), Buf()], [Buf(), Buf()], [Buf(), Buf()]
                    kw_ = 0
                    for tg in range(4):
                        for ffc in range(NFF):
                            i2 = kw_ % 2
                            kw_ += 1
                            dma("sp", wdst[i2][:], w_down.ap()[ffc * 128:(ffc + 1) * 128, :], writes=[b_wdst[i2]])
                            op("pool", lambda: POOL.tensor_copy(out=wdb[i2][:], in_=wdst[i2][:]),
                               reads=[b_wdst[i2]], writes=[b_wdb[i2]])
                            for tb in range(4):
                                mm = tg * 4 + tb
                                for half in range(2):
                                    op("pe", lambda tb=tb, half=half, mm=mm: PE.matmul(
                                        DN[tb * 2 + half][:], lhsT=ACTT[:, ffc, mm * 128:(mm + 1) * 128],
                                        rhs=wdb[i2][:, half * 512:(half + 1) * 512], start=(ffc == 0), stop=(ffc == NFF - 1)),
                                       reads=[b_wdb[i2], b_ACTT[tg]], writes=[b_DN[tb * 2 + half]],
                                       sig=(tb == 3 and half == 1))
                        for tb in range(4):
                            mm = tg * 4 + tb
                            xx, bxx = x2t[mm % 2], b_x2t[mm % 2]
                            x3_, bx3 = x3[mm % 2], b_x3[mm % 2]
                            o_, bo_ = ot[mm % 2], b_ot[mm % 2]
                            dma("sp", xx[:], X2.ap()[mm], reads=[b_X2[mm]], writes=[bxx])
                            for half in range(2):
                                cs_ = slice(half * 512, (half + 1) * 512)
                                op("dve", lambda: DVE.tensor_tensor(out=x3_[:, cs_], in0=DN[tb * 2 + half][:], in1=xx[:, cs_],
                                                                    op=ALU.add),
                                   reads=[b_DN[tb * 2 + half], bxx], writes=[bx3])
                            op("act", lambda: ACT.activation(out=junk[:], in_=x3_[:], func=AF.Square, accum_out=ss[:]),
                               reads=[bx3], writes=[b_junk, b_ss])
                            op("dve", lambda: DVE.tensor_scalar(out=ms[:], in0=ss[:], scalar1=1.0 / D, scalar2=EPS,
                                                                op0=ALU.mult, op1=ALU.add), reads=[b_ss], writes=[b_ms])
                            op("pool", lambda: POOL.tensor_tensor(out=rstd[:], in0=ms[:], in1=mhalf[:], op=ALU.pow),
                               reads=[b_ms, b_mh], writes=[b_rstd])
                            op("dve", lambda: DVE.scalar_tensor_tensor(out=o_[:], in0=x3_[:], scalar=rstd[:, 0:1], in1=gfin[:],
                                                                       op0=ALU.mult, op1=ALU.mult),
                               reads=[bx3, b_rstd, b_g5], writes=[bo_])
                            dma("sp", out_d.ap()[mm], o_[:], reads=[bo_], writes=[b_out[mm]], fence=("dve",))

        kb.finish(b_out)
    return nc, dbg_outs


def host_prep(x, norm_mix, w_in, w_up_a, w_up_b, w_out, norm_ffn, w_gate, w_up, w_down, norm_final):
    B, T, _ = x.shape
    x = np.asarray(x, np.float32)
    half = 32
    inv_freq = (10000.0 ** (-np.arange(half, dtype=np.float32) / half)).astype(np.float32)
    identb = np.eye(128, dtype=np.float32).astype(ml_dtypes.bfloat16)
    s_i = np.arange(128)[:, None]
    t_i = np.arange(128)[None, :]
    mt = np.zeros((128, 17, 128), np.float32)
    for dl in range(17):
        diff = 128 * dl + t_i - s_i
        tot = np.zeros((128, 128), np.float32)
        for (wdw, dil) in ((128, 1), (512, 4), (2048, 16)):
            ok = (diff >= 0) & (diff <= wdw) & (diff % dil == 0)
            tot += ok.astype(np.float32)
        mt[:, dl, :] = tot
    mt = mt.astype(ml_dtypes.bfloat16)
    diagbias = np.zeros((128, 512), np.float32)
    diagbias[:, 384:512] = np.where(np.arange(128)[None, :] > np.arange(128)[:, None], -BIG, 0.0)
    pow2 = np.broadcast_to((2.0 ** (-(np.arange(NIT + 1) + 1.0))).astype(np.float32)[None, :], (128, NIT + 1)).copy()
    gmix = np.ascontiguousarray(np.asarray(norm_mix, np.float32).reshape(8, 128).T)
    gffn = np.ascontiguousarray(np.asarray(norm_ffn, np.float32).reshape(8, 128).T)
    gfin = np.ascontiguousarray(np.broadcast_to(np.asarray(norm_final, np.float32)[None, :], (128, D)))
    common = {
        "identb": identb, "mt": mt, "diagbias": diagbias, "pow2": pow2, "gmix": gmix, "gffn": gffn, "gfin": gfin,
        "w_in": np.ascontiguousarray(np.asarray(w_in, np.float32)[0]),
        "w_up_a": np.ascontiguousarray(np.asarray(w_up_a, np.float32)[0]),
        "w_up_b": np.ascontiguousarray(np.asarray(w_up_b, np.float32)[0]),
        "w_out": np.ascontiguousarray(np.asarray(w_out, np.float32)[0]),
        "w_gate": np.ascontiguousarray(np.asarray(w_gate, np.float32)[0]),
        "w_up": np.ascontiguousarray(np.asarray(w_up, np.float32)[0]),
        "w_down": np.ascontiguousarray(np.asarray(w_down, np.float32)[0]),
    }
    in_maps = []
    for core in range(8):
        b, j = core // 4, core % 4
        xl = np.zeros((NB, 128, D), np.float32)
        pos = np.zeros((NB, 128), np.float32)
        valid = np.zeros((NB,), np.float32)
        for l in range(NB):
            g = l + j - 3
            if g >= 0:
                xl[l] = x[b, g * 128:(g + 1) * 128]
                pos[l] = np.arange(g * 128, (g + 1) * 128, dtype=np.float32)
                valid[l] = 1.0
        ang = pos[:, :, None] * inv_freq[None, None, :]
        cs = np.concatenate([np.cos(ang), np.sin(ang)], axis=-1).astype(np.float32)
        vmask = np.ascontiguousarray(np.broadcast_to(valid[None, :], (128, NB))).astype(np.float32)
        padbias = np.zeros((128, 512), np.float32)
        for l in range(4):
            if valid[l] == 0.0:
                padbias[:, l * 128:(l + 1) * 128] = -BIG
        m = dict(common)
        m.update({"xl": xl, "cs": cs, "vmask": vmask, "padbias": padbias})
        in_maps.append(m)
    return in_maps


def kernel(x, norm_mix, w_in, w_up_a, w_up_b, w_out, norm_ffn, w_gate, w_up, w_down, norm_final):
    in_maps = host_prep(x, norm_mix, w_in, w_up_a, w_up_b, w_out, norm_ffn, w_gate, w_up, w_down, norm_final)
    nc, dbg = build()
    if _DBG:
        res = run_bass_kernel_spmd(nc, in_maps, core_ids=list(range(8)), trace=bool(os.environ.get("KTRACE")))
        print("DBG exec_time_ns", res.exec_time_ns)
        return res
    res = run_bass_kernel_spmd(nc, in_maps, core_ids=list(range(8)))
    B, T, _ = x.shape
    out = np.zeros((B, T, D), np.float32)
    for core in range(8):
        b, j = core // 4, core % 4
        o = res.results[core]["out"]
        for m in range(NOWN):
            g = 4 * m + j
            out[b, g * 128:(g + 1) * 128] = o[m]
    return out
```

```python
import os
from contextlib import ExitStack

import ml_dtypes
import numpy as np

import concourse.bass as bass
import concourse.mybir as mybir
from concourse.bass_types import AP
from concourse.bass_utils import run_bass_kernel_spmd

F32 = mybir.dt.float32
BF16 = mybir.dt.bfloat16
AF = mybir.ActivationFunctionType
ALU = mybir.AluOpType
AX = mybir.AxisListType

NB = 64
NOWN = 16
D = 1024
DFF = 2816
NFF = DFF // 128
DIN = 4808
NIT = 16
BIG = 1.0e30
NEGM = 30000.0
EPS = 1e-6
NDS = 12

_DBG = os.environ.get("KDBG", "")


class Buf:
    __slots__ = ("w", "r")

    def __init__(self):
        self.w = None
        self.r = {}


class KB:
    def __init__(self, nc):
        self.nc = nc
        self.eng = {"pe": nc.tensor, "act": nc.scalar, "dve": nc.vector, "pool": nc.gpsimd, "sp": nc.sync}
        self.sem = {e: nc.alloc_semaphore(name=f"s_{e}") for e in self.eng}
        self.cnt = {e: 0 for e in self.eng}
        self.waited = {e: {} for e in self.eng}
        self.fence_fn = {}
        self.last_fence = {}
        self.dq = {}
        for q in ("sp", "pool"):
            self.dq[q] = {"sems": [nc.alloc_semaphore(name=f"d_{q}{i}") for i in range(NDS)], "k": 0}

    def _wait(self, e, ev):
        sem, val = ev
        if e == "pe" and sem is self.sem["pe"]:
            return
        key = sem.num
        if self.waited[e].get(key, 0) >= val:
            return
        self.eng[e].wait_ge(sem, val)
        self.waited[e][key] = val

    def _deps(self, e, reads, writes):
        for b in reads:
            if b.w is not None:
                self._wait(e, b.w)
        for b in writes:
            if b.w is not None:
                self._wait(e, b.w)
            for ev in b.r.values():
                self._wait(e, ev)

    def _mark(self, ev, reads, writes):
        key = ev[0].num
        for b in reads:
            old = b.r.get(key)
            if old is None or old[1] < ev[1]:
                b.r[key] = ev
        for b in writes:
            b.w = ev
            b.r = {}

    def op(self, e, fn, reads=(), writes=(), sig=True):
        self._deps(e, reads, writes)
        inst = fn()
        if sig:
            self.cnt[e] += 1
            inst.then_inc(self.sem[e], 1)
            ev = (self.sem[e], self.cnt[e])
        else:
            ev = (self.sem[e], self.cnt[e] + 1)
        self._mark(ev, reads, writes)
        return inst

    def dma(self, q, out, in_, reads=(), writes=(), fence=()):
        dq = self.dq[q]
        k = dq["k"]
        P = len(dq["sems"])
        sem = dq["sems"][k % P]
        if k >= P:
            self._wait(q, (sem, 16 * (k // P)))
        self._deps(q, reads, writes)
        for e in fence:
            last = self.last_fence.get(e)
            if last is not None:
                self._wait(e, last)
            self.fence_fn[e]().then_inc(self.sem[e], 1)
            self.cnt[e] += 1
            self.last_fence[e] = (self.sem[e], self.cnt[e])
            self._wait(q, (self.sem[e], self.cnt[e]))
        self.eng[q].dma_start(out=out, in_=in_).then_inc(sem, 16)
        dq["k"] += 1
        ev = (sem, 16 * (k // P + 1))
        self._mark(ev, reads, writes)

    def barrier(self):
        evs = [(self.sem[e], self.cnt[e]) for e in self.eng if self.cnt[e] > 0]
        for q, dq in self.dq.items():
            k = dq["k"]
            P = len(dq["sems"])
            for i in range(min(k, P)):
                kk = k - 1 - i
                evs.append((dq["sems"][kk % P], 16 * (kk // P + 1)))
        for e in self.eng:
            for ev in evs:
                if ev[0] is self.sem[e]:
                    continue
                self._wait(e, ev)

    def finish(self, bufs):
        for b in bufs:
            if b.w is not None:
                self._wait("sp", b.w)
        for q, dq in self.dq.items():
            k = dq["k"]
            P = len(dq["sems"])
            for i in range(min(k, P)):
                kk = k - 1 - i
                self._wait(q, (dq["sems"][kk % P], 16 * (kk // P + 1)))


def bc_mid(ap2d, n):
    a = [list(x) for x in ap2d.ap]
    assert len(a) == 2
    return AP(ap2d.tensor, ap2d.offset, [a[0], [0, n], a[1]])


def bc_last(ap, n):
    a = [list(x) for x in ap.ap]
    return AP(ap.tensor, ap.offset, a + [[0, n]])


def build():
    nc = bass.Bass("TRN2", target_bir_lowering=False)
    kb = KB(nc)
    op = kb.op
    dma = kb.dma
    PE, ACT, DVE, POOL = nc.tensor, nc.scalar, nc.vector, nc.gpsimd

    def din(name, shape, dt=F32):
        return nc.dram_tensor(name, list(shape), dt, kind="ExternalInput")

    def dscr(name, shape, dt):
        return nc.dram_tensor(name, list(shape), dt, kind=("ExternalOutput" if _DBG else "Internal"))

    xl = din("xl", [NB, 128, D])
    cs_t = din("cs", [NB, 128, 64])
    vmask_d = din("vmask", [128, NB])
    padbias_d = din("padbias", [128, 512])
    diagbias_d = din("diagbias", [128, 512])
    mt_d = din("mt", [128, 17, 128], BF16)
    identb_d = din("identb", [128, 128], BF16)
    pow2_d = din("pow2", [128, NIT + 1])
    gmix_d = din("gmix", [128, 8])
    gffn_d = din("gffn", [128, 8])
    gfin_d = din("gfin", [128, D])
    w_in = din("w_in", [D, DIN])
    w_up_a = din("w_up_a", [512, D])
    w_up_b = din("w_up_b", [512, D])
    w_out = din("w_out", [D, D])
    w_gate = din("w_gate", [D, DFF])
    w_up = din("w_up", [D, DFF])
    w_down = din("w_down", [DFF, D])
    out_d = nc.dram_tensor("out", [NOWN, 128, D], F32, kind="ExternalOutput")

    KAT = dscr("KAT", [128, NB, 512], BF16)
    VA = dscr("VA", [128, NB, 520], BF16)
    KBT = dscr("KBT", [64, NB * 128], BF16)
    KIT = dscr("KIT", [64, NB * 128], BF16)
    VB = dscr("VB", [128, NB, 65], BF16)
    QAT = dscr("QAT", [NOWN, 128, 512], BF16)
    QBT = dscr("QBT", [NOWN, 128, 512], BF16)
    QIT = dscr("QIT", [NOWN, 128, 512], BF16)
    WI = dscr("WI", [NOWN, 128, 8], F32)
    GS = dscr("GS", [NOWN, 128, 2048], BF16)
    X2 = dscr("X2", [NOWN, 128, D], F32)
    b_KAT = [Buf() for _ in range(NB)]
    b_VA = [Buf() for _ in range(NB)]
    b_KBT = [Buf() for _ in range(NB)]
    b_KIT = [Buf() for _ in range(NB)]
    b_VB = [Buf() for _ in range(NB)]
    b_QAT = [Buf() for _ in range(NOWN)]
    b_QBT = [Buf() for _ in range(NOWN)]
    b_QIT = [Buf() for _ in range(NOWN)]
    b_WI = [Buf() for _ in range(NOWN)]
    b_GS = [Buf() for _ in range(NOWN)]
    b_X2 = [Buf() for _ in range(NOWN)]
    b_out = [Buf() for _ in range(NOWN)]

    dbg_outs = {}

    def dbg_out(name, shape, dt):
        t = nc.dram_tensor("dbg_" + name, list(shape), dt, kind="ExternalOutput")
        dbg_outs[name] = t
        return t

    with ExitStack() as top:
        def sb(stack, name, shape, dt):
            return stack.enter_context(nc.sbuf_tensor("sb_" + name, list(shape), dt))

        def ps(stack, name, shape, dt):
            return stack.enter_context(nc.psum_tensor("ps_" + name, list(shape), dt))

        identb = sb(top, "identb", [128, 128], BF16)
        b_const = Buf()
        dma("sp", identb[:], identb_d.ap(), writes=[b_const])
        vmask = sb(top, "vmask", [128, NB], F32)
        dma("sp", vmask[:], vmask_d.ap(), writes=[b_const])
        fsc = sb(top, "fsc", [128, 8], F32)
        op("pool", lambda: POOL.memset(fsc[:], 0.0), writes=[Buf()])
        kb.barrier()
        kb.fence_fn["act"] = lambda: ACT.copy(out=fsc[:, 0:1], in_=fsc[:, 1:2])
        kb.fence_fn["dve"] = lambda: DVE.tensor_copy(out=fsc[:, 2:3], in_=fsc[:, 3:4])
        kb.fence_fn["pool"] = lambda: POOL.tensor_copy(out=fsc[:, 4:5], in_=fsc[:, 5:6])
        YAT = sb(top, "YAT", [128, 4, NOWN * 128], BF16)
        YBT = sb(top, "YBT", [128, 4, NOWN * 128], BF16)
        b_YAT = [Buf() for _ in range(NOWN)]
        b_YBT = [Buf() for _ in range(NOWN)]

        def rope(stack_bufs, zview, H, cs_tile, b_cs, b_z, out_view, b_outv):
            tA, tB, tC, tD, bA, bB, bC, bD = stack_bufs
            cosb = bc_mid(cs_tile[:, 0:32], H)
            sinb = bc_mid(cs_tile[:, 32:64], H)
            z1 = zview[:, :, 0:32]
            z2 = zview[:, :, 32:64]
            a = tA[:, 0:H, :]
            b = tB[:, 0:H, :]
            c = tC[:, 0:H, :]
            d = tD[:, 0:H, :]
            op("dve", lambda: DVE.tensor_tensor(out=a, in0=z1, in1=cosb, op=ALU.mult), reads=[b_z, b_cs], writes=[bA])
            op("dve", lambda: DVE.tensor_tensor(out=b, in0=z2, in1=sinb, op=ALU.mult), reads=[b_z, b_cs], writes=[bB])
            op("dve", lambda: DVE.tensor_tensor(out=c, in0=z2, in1=cosb, op=ALU.mult), reads=[b_z, b_cs], writes=[bC])
            op("dve", lambda: DVE.tensor_tensor(out=d, in0=z1, in1=sinb, op=ALU.mult), reads=[b_z, b_cs], writes=[bD])
            op("dve", lambda: DVE.tensor_tensor(out=out_view[:, :, 0:32], in0=a, in1=b, op=ALU.subtract),
               reads=[bA, bB], writes=[b_outv])
            op("dve", lambda: DVE.tensor_tensor(out=out_view[:, :, 32:64], in0=c, in1=d, op=ALU.add),
               reads=[bC, bD], writes=[b_outv])

        with ExitStack() as st:
            WK = sb(st, "WK", [128, 8, 1216], BF16)
            WQ = sb(st, "WQ", [128, 8, 3592], BF16)
            wst = [sb(st, f"wst{i}", [128, 8, 512], F32) for i in range(2)]
            b_wst = [Buf(), Buf()]
            gmix = sb(st, "gmix", [128, 8], F32)
            b_g = Buf()
            dma("sp", gmix[:], gmix_d.ap(), writes=[b_g])
            b_WK = Buf()
            b_WQ = Buf()
            w_in_v = w_in.ap().rearrange("(kc p) n -> p kc n", p=128)
            kparts = [(WK, b_WK, 0, 512, 512), (WK, b_WK, 512, 1024, 512), (WK, b_WK, 1024, 2048, 128),
                      (WK, b_WK, 1152, 2688, 64)]
            qparts = [(WQ, b_WQ, 0, 0, 512), (WQ, b_WQ, 512, 1536, 512), (WQ, b_WQ, 1024, 2176, 512),
                      (WQ, b_WQ, 1536, 2752, 8), (WQ, b_WQ, 1544, 2760, 512), (WQ, b_WQ, 2056, 3272, 512),
                      (WQ, b_WQ, 2568, 3784, 512), (WQ, b_WQ, 3080, 4296, 512)]
            for i, (dst, bdst, dc, sc, n) in enumerate(kparts + qparts):
                s = wst[i % 2]
                bs = b_wst[i % 2]
                dma("sp", s[:, :, 0:n], w_in_v[:, :, sc:sc + n], writes=[bs])
                op("pool", lambda s=s, dst=dst, dc=dc, n=n: POOL.tensor_tensor(
                    out=dst[:, :, dc:dc + n], in0=s[:, :, 0:n], in1=bc_last(gmix[:, :], n), op=ALU.mult),
                   reads=[bs, b_g], writes=[bdst])

            xs = [sb(st, f"xs{i}", [128, D], F32) for i in range(3)]
            b_xs = [Buf() for _ in range(3)]
            cst = [sb(st, f"cst{i}", [128, 64], F32) for i in range(4)]
            b_cst = [Buf() for _ in range(4)]
            junk = sb(st, "junk", [128, D], BF16)
            b_junk = Buf()
            ss = [sb(st, f"ss{i}", [128, 1], F32) for i in range(2)]
            ms = [sb(st, f"ms{i}", [128, 1], F32) for i in range(2)]
            rstd = [sb(st, f"rstd{i}", [128, 1], F32) for i in range(2)]
            mhalf = sb(st, "mhalf", [128, 1], F32)
            b_ss, b_ms, b_rstd = [Buf(), Buf()], [Buf(), Buf()], [Buf(), Buf()]
            b_mh = Buf()
            op("pool", lambda: POOL.memset(mhalf[:], -0.5), writes=[b_mh])
            hb = [sb(st, f"hb{i}", [128, D], BF16) for i in range(2)]
            b_hb = [Buf(), Buf()]
            hT = [sb(st, f"hT{i}", [128, 8, 128], BF16) for i in range(3)]
            b_hT = [Buf() for _ in range(3)]
            TRH = ps(st, "TRH", [128, 1024], BF16)
            b_TRH = Buf()
            NPB = 6
            PB = [ps(st, f"PB{i}", [128, 512], F32) for i in range(NPB)]
            b_PB = [Buf() for _ in range(NPB)]
            TRO = ps(st, "TRO", [128, 1024], BF16)
            b_TRO = Buf()
            rt = [sb(st, f"rt{i}", [128, 8, 32], F32) for i in range(4)]
            rbufs = tuple(rt) + tuple(Buf() for _ in range(4))
            kab = sb(st, "kab", [128, 8, 64], BF16)
            b_kab = Buf()
            kat = [sb(st, f"kat{i}", [128, 512], BF16) for i in range(2)]
            b_kat = [Buf(), Buf()]
            kbi = sb(st, "kbi", [128, 2, 64], BF16)
            b_kbi = Buf()
            kbit = [sb(st, f"kbit{i}", [64, 256], BF16) for i in range(2)]
            b_kbit = [Buf(), Buf()]
            vaa = [sb(st, f"vaa{i}", [128, 8, 65], BF16) for i in range(2)]
            b_vaa = [Buf(), Buf()]
            vba = [sb(st, f"vba{i}", [128, 65], BF16) for i in range(2)]
            b_vba = [Buf(), Buf()]
            qab = sb(st, "qab", [128, 8, 64], BF16)
            b_qab = Buf()
            qat = sb(st, "qat", [128, 512], BF16)
            b_qat = Buf()
            qbt = [sb(st, f"qbt{i}", [128, 512], BF16) for i in range(2)]
            b_qbt = [Buf(), Buf()]
            sgq = sb(st, "sgq", [128, 8], F32)
            absq = sb(st, "absq", [128, 8], F32)
            qis = sb(st, "qis", [128, 8, 64], F32)
            b_sgq, b_absq, b_qis = Buf(), Buf(), Buf()
            wis = sb(st, "wis", [128, 8], F32)
            b_wis = Buf()
            gsb = sb(st, "gsb", [128, 2048], BF16)
            b_gsb = Buf()
            pbc = [0]
            kbanks = {}

            def next_pb():
                i = pbc[0] % NPB
                pbc[0] += 1
                return PB[i], b_PB[i]

            def proj(hT_, bhT_, W, bW, c0, n, bank, bbank, o0=0):
                for kc in range(8):
                    op("pe", lambda kc=kc: PE.matmul(bank[:, o0:o0 + n], lhsT=hT_[:, kc, :], rhs=W[:, kc, c0:c0 + n],
                                                     start=(kc == 0), stop=(kc == 7)),
                       reads=[bhT_, bW], writes=[bbank], sig=(kc == 7))

            def stage_F(l):
                x_, bx = xs[l % 3], b_xs[l % 3]
                c_, bc = cst[l % 4], b_cst[l % 4]
                ss_, ms_, rs_ = ss[l % 2], ms[l % 2], rstd[l % 2]
                hb_, bhb = hb[l % 2], b_hb[l % 2]
                hT_, bhT_ = hT[l % 3], b_hT[l % 3]
                dma("sp", x_[:], xl.ap()[l], writes=[bx])
                dma("sp", c_[:], cs_t.ap()[l], writes=[bc])
                op("act", lambda: ACT.activation(out=junk[:], in_=x_[:], func=AF.Square, accum_out=ss_[:]),
                   reads=[bx], writes=[b_junk, b_ss[l % 2]])
                op("dve", lambda: DVE.tensor_scalar(out=ms_[:], in0=ss_[:], scalar1=1.0 / D, scalar2=EPS,
                                                    op0=ALU.mult, op1=ALU.add), reads=[b_ss[l % 2]], writes=[b_ms[l % 2]])
                op("pool", lambda: POOL.tensor_tensor(out=rs_[:], in0=ms_[:], in1=mhalf[:], op=ALU.pow),
                   reads=[b_ms[l % 2], b_mh], writes=[b_rstd[l % 2]])
                op("pool", lambda: POOL.tensor_scalar(out=hb_[:], in0=x_[:], scalar1=rs_[:, 0:1], scalar2=0.0,
                                                      op0=ALU.mult, op1=ALU.add),
                   reads=[bx, b_rstd[l % 2]], writes=[bhb])
                for kc in range(8):
                    op("pe", lambda kc=kc: PE.transpose(out=TRH[:, kc * 128:(kc + 1) * 128],
                                                        in_=hb_[:, kc * 128:(kc + 1) * 128], identity=identb[:]),
                       reads=[bhb, b_const], writes=[b_TRH], sig=(kc == 7))
                op("act", lambda: ACT.copy(out=hT_[:].rearrange("p a b -> p (a b)"), in_=TRH[:]),
                   reads=[b_TRH], writes=[bhT_])

            def stage_P(l):
                hT_, bhT_ = hT[l % 3], b_hT[l % 3]
                pa, bpa = next_pb()
                proj(hT_, bhT_, WK, b_WK, 0, 512, pa, bpa)
                pv, bpv = next_pb()
                proj(hT_, bhT_, WK, b_WK, 512, 512, pv, bpv)
                pc, bpc = next_pb()
                proj(hT_, bhT_, WK, b_WK, 1024, 128, pc, bpc, 0)
                proj(hT_, bhT_, WK, b_WK, 1152, 64, pc, bpc, 128)
                kbanks[l] = (pa, bpa, pv, bpv, pc, bpc)

            def stage_R(l):
                pa, bpa, pv, bpv, pc, bpc = kbanks.pop(l)
                c_, bc = cst[l % 4], b_cst[l % 4]
                kat_, bkat_ = kat[l % 2], b_kat[l % 2]
                kbit_, bkbit_ = kbit[l % 2], b_kbit[l % 2]
                vaa_, bvaa_ = vaa[l % 2], b_vaa[l % 2]
                vba_, bvba_ = vba[l % 2], b_vba[l % 2]
                op("act", lambda: ACT.activation(out=vaa_[:, :, 0:64], in_=pv[:].rearrange("p (h d) -> p h d", h=8),
                                                 func=AF.Copy, scale=vmask[:, l:l + 1]),
                   reads=[bpv, b_const], writes=[bvaa_])
                op("pool", lambda: POOL.tensor_copy(out=vaa_[:, :, 64:65], in_=bc_mid(vmask[:, l:l + 1], 8)),
                   reads=[b_const], writes=[bvaa_])
                dma("pool", VA.ap()[:, l, :], vaa_[:].rearrange("p h d -> p (h d)"), reads=[bvaa_], writes=[b_VA[l]], fence=("act", "pool"))
                op("act", lambda: ACT.activation(out=vba_[:, 0:64], in_=pc[:, 64:128], func=AF.Copy,
                                                 scale=vmask[:, l:l + 1]), reads=[bpc, b_const], writes=[bvba_])
                op("pool", lambda: POOL.tensor_copy(out=vba_[:, 64:65], in_=vmask[:, l:l + 1]),
                   reads=[b_const], writes=[bvba_])
                dma("pool", VB.ap()[:, l, :], vba_[:], reads=[bvba_], writes=[b_VB[l]], fence=("act", "pool"))
                rope(rbufs, pa[:].rearrange("p (h d) -> p h d", h=8), 8, c_, bc, bpa, kab[:], b_kab)
                zc = AP(pc, 0, [[512, 128], [128, 2], [1, 64]])
                rope(rbufs, zc, 2, c_, bc, bpc, kbi[:], b_kbi)
                for pr in range(4):
                    op("pe", lambda pr=pr: PE.transpose(out=TRO[:, pr * 128:(pr + 1) * 128],
                                                        in_=kab[:].rearrange("p h d -> p (h d)")[:, pr * 128:(pr + 1) * 128],
                                                        identity=identb[:]),
                       reads=[b_kab, b_const], writes=[b_TRO], sig=False)
                for hh in range(2):
                    op("pe", lambda hh=hh: PE.transpose(out=TRO[0:64, 512 + hh * 128:512 + (hh + 1) * 128],
                                                        in_=kbi[:, hh, :], identity=identb[:]),
                       reads=[b_kbi, b_const], writes=[b_TRO], sig=(hh == 1))
                op("act", lambda: ACT.copy(out=kat_[:], in_=TRO[:, 0:512]), reads=[b_TRO], writes=[bkat_])
                op("act", lambda: ACT.copy(out=kbit_[:], in_=TRO[0:64, 512:768]), reads=[b_TRO], writes=[bkbit_])
                dma("pool", KAT.ap()[:, l, :], kat_[:], reads=[bkat_], writes=[b_KAT[l]], fence=("act",))
                dma("pool", KBT.ap()[:, l * 128:(l + 1) * 128], kbit_[:, 0:128], reads=[bkbit_], writes=[b_KBT[l]])
                dma("pool", KIT.ap()[:, l * 128:(l + 1) * 128], kbit_[:, 128:256], reads=[bkbit_], writes=[b_KIT[l]])

            def stage_Q(l):
                m = l // 4
                hT_, bhT_ = hT[l % 3], b_hT[l % 3]
                c_, bc = cst[l % 4], b_cst[l % 4]
                pq, bpq = next_pb()
                proj(hT_, bhT_, WQ, b_WQ, 0, 512, pq, bpq)
                pq2, bpq2 = next_pb()
                proj(hT_, bhT_, WQ, b_WQ, 512, 512, pq2, bpq2)
                pq3, bpq3 = next_pb()
                proj(hT_, bhT_, WQ, b_WQ, 1024, 512, pq3, bpq3)
                pq4, bpq4 = next_pb()
                proj(hT_, bhT_, WQ, b_WQ, 1536, 8, pq4, bpq4)
                op("dve", lambda: DVE.tensor_copy(out=wis[:], in_=pq4[:, 0:8]), reads=[bpq4], writes=[b_wis])
                dma("pool", WI.ap()[m], wis[:], reads=[b_wis], writes=[b_WI[m]], fence=("dve",))
                rope(rbufs, pq[:].rearrange("p (h d) -> p h d", h=8), 8, c_, bc, bpq, qab[:], b_qab)
                for pr in range(4):
                    op("pe", lambda pr=pr: PE.transpose(out=TRO[:, pr * 128:(pr + 1) * 128],
                                                        in_=qab[:].rearrange("p h d -> p (h d)")[:, pr * 128:(pr + 1) * 128],
                                                        identity=identb[:]),
                       reads=[b_qab, b_const], writes=[b_TRO], sig=(pr == 3))
                op("act", lambda: ACT.copy(out=qat[:], in_=TRO[:, 0:512]), reads=[b_TRO], writes=[b_qat])
                dma("pool", QAT.ap()[m], qat[:], reads=[b_qat], writes=[b_QAT[m]], fence=("act",))
                for gq in range(4):
                    pg, bpg = next_pb()
                    proj(hT_, bhT_, WQ, b_WQ, 1544 + gq * 512, 512, pg, bpg)
                    op("act", lambda gq=gq, pg=pg: ACT.activation(out=gsb[:, gq * 512:(gq + 1) * 512], in_=pg[:],
                                                                  func=AF.Sigmoid), reads=[bpg], writes=[b_gsb])
                    if gq == 0:
                        qbt_, bqbt_ = qbt[0], b_qbt[0]
                        rope(rbufs, pq2[:].rearrange("p (h d) -> p h d", h=8), 8, c_, bc, bpq2, qab[:], b_qab)
                        for h8 in range(8):
                            ro = 64 * (h8 // 4)
                            op("pe", lambda h8=h8, ro=ro: PE.transpose(
                                out=TRO[ro:ro + 64, (h8 % 4) * 128:(h8 % 4 + 1) * 128], in_=qab[:, h8, :],
                                identity=identb[:]),
                               reads=[b_qab, b_const], writes=[b_TRO], sig=(h8 == 7))
                        op("act", lambda: ACT.copy(out=qbt_[:], in_=TRO[:, 0:512]), reads=[b_TRO], writes=[bqbt_])
                        dma("pool", QBT.ap()[m], qbt_[:], reads=[bqbt_], writes=[b_QBT[m]], fence=("act",))
                    if gq == 1:
                        qbt_, bqbt_ = qbt[1], b_qbt[1]
                        op("dve", lambda: DVE.tensor_scalar(out=sgq[:], in0=wis[:], scalar1=0.0, scalar2=0.5,
                                                            op0=ALU.is_ge, op1=ALU.subtract), reads=[b_wis], writes=[b_sgq])
                        op("dve", lambda: DVE.scalar_tensor_tensor(out=absq[:], in0=sgq[:], scalar=2.0, in1=wis[:],
                                                                   op0=ALU.mult, op1=ALU.mult),
                           reads=[b_wis, b_sgq], writes=[b_absq])
                        op("dve", lambda: DVE.tensor_tensor(out=qis[:], in0=pq3[:].rearrange("p (h d) -> p h d", h=8),
                                                            in1=bc_last(absq[:, :], 64), op=ALU.mult),
                           reads=[bpq3, b_absq], writes=[b_qis])
                        rope(rbufs, qis[:], 8, c_, bc, b_qis, qab[:], b_qab)
                        for pr in range(4):
                            op("pe", lambda pr=pr: PE.transpose(
                                out=TRO[:, pr * 128:(pr + 1) * 128],
                                in_=qab[:].rearrange("p h d -> p (h d)")[:, pr * 128:(pr + 1) * 128], identity=identb[:]),
                               reads=[b_qab, b_const], writes=[b_TRO], sig=(pr == 3))
                        op("act", lambda: ACT.copy(out=qbt_[:], in_=TRO[:, 0:512]), reads=[b_TRO], writes=[bqbt_])
                        dma("pool", QIT.ap()[m], qbt_[:], reads=[bqbt_], writes=[b_QIT[m]], fence=("act",))
                dma("pool", GS.ap()[m], gsb[:], reads=[b_gsb], writes=[b_GS[m]], fence=("act",))

            stage_F(0)
            stage_F(1)
            stage_P(0)
            for l in range(NB):
                if l + 2 < NB:
                    stage_F(l + 2)
                if l % 4 == 3:
                    stage_R(l)
                    stage_Q(l)
                    if l + 1 < NB:
                        stage_P(l + 1)
                else:
                    if l + 1 < NB:
                        stage_P(l + 1)
                    stage_R(l)

        if _DBG == "1":
            kb.finish(b_KAT + b_VA + b_KBT + b_KIT + b_VB + b_QAT + b_QBT + b_QIT + b_WI + b_GS)
            return nc, dbg_outs

        kb.barrier()
        with ExitStack() as st:
            MT = sb(st, "MT", [128, 17 * 128], BF16)
            b_MT = Buf()
            dma("sp", MT[:], mt_d.ap().rearrange("p a b -> p (a b)"), writes=[b_MT])
            kw = [sb(st, f"kw{i}", [128, 17, 512], BF16) for i in range(2)]
            b_kw = [Buf(), Buf()]
            vw = [sb(st, f"vw{i}", [128, 17, 520], BF16) for i in range(2)]
            b_vw = [Buf(), Buf()]
            qa_t = [sb(st, f"qa{i}", [128, 512], BF16) for i in range(2)]
            b_qa = [Buf(), Buf()]
            STb = [ps(st, f"ST{i}", [128, 512], F32) for i in range(3)]
            b_ST = [Buf() for _ in range(3)]
            OA = [ps(st, f"OA{i}", [128, 512], F32) for i in range(4)]
            b_OA = [Buf() for _ in range(4)]
            TR = ps(st, "TR2", [128, 1024], BF16)
            b_TR = Buf()
            E = [sb(st, f"E{i}", [128, 512], BF16) for i in range(3)]
            b_E = [Buf() for _ in range(3)]
            Pm = [sb(st, f"P{i}", [128, 512], BF16) for i in range(3)]
            b_P = [Buf() for _ in range(3)]
            rc = sb(st, "rc", [128, 8], F32)
            b_rc = Buf()
            ya = sb(st, "ya", [128, 8, 64], BF16)
            b_ya = Buf()
            c2 = {"st": 0, "e": 0}
            pending_fin = []

            def fin2(m):
                oa = OA[2 * (m % 2):2 * (m % 2) + 2]
                boa = b_OA[2 * (m % 2):2 * (m % 2) + 2]
                for bnk in range(2):
                    ov = oa[bnk][:, 0:260].rearrange("p (h d) -> p h d", d=65)
                    op("dve", lambda: DVE.reciprocal(out=rc[:, bnk * 4:(bnk + 1) * 4], in_=ov[:, :, 64]),
                       reads=[boa[bnk]], writes=[b_rc])
                    op("dve", lambda: DVE.tensor_tensor(out=ya[:, bnk * 4:(bnk + 1) * 4, :], in0=ov[:, :, 0:64],
                                                        in1=bc_last(rc[:, bnk * 4:(bnk + 1) * 4], 64), op=ALU.mult),
                       reads=[boa[bnk], b_rc], writes=[b_ya])
                ya2 = ya[:].rearrange("p h d -> p (h d)")
                for kc in range(4):
                    op("pe", lambda kc=kc: PE.transpose(out=TR[:, kc * 128:(kc + 1) * 128],
                                                        in_=ya2[:, kc * 128:(kc + 1) * 128], identity=identb[:]),
                       reads=[b_ya, b_const], writes=[b_TR], sig=(kc == 3))
                op("act", lambda: ACT.copy(out=YAT[:, :, m * 128:(m + 1) * 128],
                                           in_=TR[:, 0:512].rearrange("p (a b) -> p a b", b=128)),
                   reads=[b_TR], writes=[b_YAT[m]])

            for m in range(NOWN):
                lq = 4 * m + 3
                lo = max(0, lq - 16)
                nb = lq - lo + 1
                k_, v_, q_ = kw[m % 2], vw[m % 2], qa_t[m % 2]
                bk, bv, bq = b_kw[m % 2], b_vw[m % 2], b_qa[m % 2]
                oa = OA[2 * (m % 2):2 * (m % 2) + 2]
                boa = b_OA[2 * (m % 2):2 * (m % 2) + 2]
                dma("sp", k_[:, 0:nb, :], KAT.ap()[:, lo:lq + 1, :], reads=b_KAT[lo:lq + 1], writes=[bk])
                dma("sp", v_[:, 0:nb, :], VA.ap()[:, lo:lq + 1, :], reads=b_VA[lo:lq + 1], writes=[bv])
                dma("sp", q_[:], QAT.ap()[m], reads=[b_QAT[m]], writes=[bq])
                first_bank = [True, True]
                groups = [list(range(i, min(i + 4, nb))) for i in range(0, nb, 4)]
                units = [(h, gi) for h in range(8) for gi in range(len(groups))]
                nu = len(units)
                slots = {}

                def emit_qk(u):
                    h, gi = units[u]
                    grp = groups[gi]
                    n = len(grp)
                    base = 64 * (h % 2)
                    pair = h // 2
                    k = c2["st"] % 3
                    c2["st"] += 1
                    slots[u] = k
                    for i, dl in enumerate(grp):
                        slot = nb - 1 - dl
                        op("pe", lambda i=i, slot=slot: PE.matmul(
                            STb[k][:, i * 128:(i + 1) * 128],
                            lhsT=k_[base:base + 64, slot, pair * 128:(pair + 1) * 128],
                            rhs=q_[base:base + 64, pair * 128:(pair + 1) * 128], start=True, stop=True),
                           reads=[bk, bq], writes=[b_ST[k]], sig=(i == n - 1))

                for u in range(min(3, nu)):
                    emit_qk(u)
                for u in range(nu):
                    h, gi = units[u]
                    grp = groups[gi]
                    n = len(grp)
                    k = slots[u]
                    ke = c2["e"] % 3
                    c2["e"] += 1
                    e_, be, p_, bp = E[ke], b_E[ke], Pm[ke], b_P[ke]
                    op("act", lambda: ACT.activation(out=e_[:, 0:n * 128], in_=STb[k][:, 0:n * 128], func=AF.Exp,
                                                     scale=0.125), reads=[b_ST[k]], writes=[be])
                    d0 = grp[0]
                    op("dve", lambda: DVE.tensor_tensor(out=p_[:, 0:n * 128], in0=e_[:, 0:n * 128],
                                                        in1=MT[:, d0 * 128:(d0 + n) * 128], op=ALU.mult),
                       reads=[be, b_MT], writes=[bp])
                    for i, dl in enumerate(grp):
                        slot = nb - 1 - dl
                        is_first = first_bank[h // 4]
                        first_bank[h // 4] = False
                        is_last = (h % 4 == 3 and gi == len(groups) - 1 and i == n - 1)
                        op("pe", lambda i=i, slot=slot, is_first=is_first, is_last=is_last: PE.matmul(
                            oa[h // 4][:, (h % 4) * 65:(h % 4) * 65 + 65], lhsT=p_[:, i * 128:(i + 1) * 128],
                            rhs=v_[:, slot, h * 65:(h + 1) * 65], start=is_first, stop=is_last,
                            skip_group_check=True),
                           reads=[bp, bv], writes=[boa[h // 4]], sig=(i == n - 1))
                    if u + 3 < nu:
                        emit_qk(u + 3)
                    if u == min(3, nu - 1) and pending_fin:
                        fin2(pending_fin.pop(0))
                pending_fin.append(m)
            while pending_fin:
                fin2(pending_fin.pop(0))

        if _DBG == "2":
            dy = dbg_out("YAT", [128, 4, NOWN * 128], BF16)
            bo = Buf()
            dma("sp", dy.ap(), YAT[:], reads=b_YAT, writes=[bo])
            kb.finish([bo])
            return nc, dbg_outs

        kb.barrier()
        with ExitStack() as st:
            KIs = sb(st, "KIs", [128, NB * 128], BF16)
            KBs = sb(st, "KBs", [128, NB * 128], BF16)
            VBs = sb(st, "VBs", [128, NB, 65], BF16)
            b_KIs, b_KBs, b_VBs = Buf(), Buf(), Buf()
            dma("sp", KIs[0:64, :], KIT.ap(), reads=b_KIT, writes=[b_KIs])
            dma("sp", KIs[64:128, :], KIT.ap(), reads=b_KIT, writes=[b_KIs])
            dma("sp", KBs[0:64, :], KBT.ap(), reads=b_KBT, writes=[b_KBs])
            dma("sp", KBs[64:128, :], KBT.ap(), reads=b_KBT, writes=[b_KBs])
            dma("sp", VBs[:], VB.ap(), reads=b_VB, writes=[b_VBs])
            padb = sb(st, "padb", [128, 512], F32)
            diagb = sb(st, "diagb", [128, 512], F32)
            pow2 = sb(st, "pow2", [128, NIT + 1], F32)
            b_c3 = Buf()
            dma("sp", padb[:], padbias_d.ap(), writes=[b_c3])
            dma("sp", diagb[:], diagbias_d.ap(), writes=[b_c3])
            dma("sp", pow2[:], pow2_d.ap(), writes=[b_c3])
            qi_t = [sb(st, f"qi{i}", [128, 512], BF16) for i in range(2)]
            qb_t = [sb(st, f"qb{i}", [128, 512], BF16) for i in range(2)]
            wi_t = [sb(st, f"wi{i}", [128, 8], F32) for i in range(2)]
            b_qi, b_qb, b_wi = [Buf(), Buf()], [Buf(), Buf()], [Buf(), Buf()]
            scores = sb(st, "scores", [128, NB * 128], F32)
            b_sc = [Buf() for _ in range(NOWN)]
            junkc = sb(st, "junkc", [128, NB * 128], BF16)
            b_jc = Buf()
            PXP = [ps(st, f"PXP{i}", [128, 1024], F32) for i in range(2)]
            b_PXP = [Buf(), Buf()]
            SC = ps(st, "SC", [128, 512], F32)
            b_SC = Buf()
            OB = [ps(st, f"OB{i}", [128, 512], F32) for i in range(2)]
            b_OB = [Buf(), Buf()]
            TR3 = ps(st, "TR3", [128, 1024], BF16)
            b_TR3 = Buf()
            R2 = [sb(st, f"R2{i}", [128, 1024], BF16) for i in range(3)]
            b_R2 = [Buf() for _ in range(3)]
            selfull = sb(st, "selfull", [128, NB * 128], BF16)
            b_selfull = Buf()
            absw = sb(st, "absw", [128, 8], F32)
            sgh = sb(st, "sgh", [128, 8], F32)
            Dg = sb(st, "Dg", [128, 8, 128], BF16)
            b_absw, b_sgh, b_Dg = Buf(), Buf(), Buf()
            cmin = sb(st, "cmin", [128, NOWN], F32)
            b_cmin = Buf()
            rmin = sb(st, "rmin", [128, 1], F32)
            rmax = sb(st, "rmax", [128, 1], F32)
            rng = sb(st, "rng", [128, 1], F32)
            tsum = sb(st, "tsum", [128, 1], F32)
            tau = [sb(st, f"tau{i}", [128, 1], F32) for i in range(2)]
            cntt = sb(st, "cntt", [128, 1], F32)
            sg = sb(st, "sg", [128, 1], F32)
            step2 = sb(st, "step2", [128, NIT + 1], F32)
            tsel = sb(st, "tsel", [128, 1], F32)
            b_rmin, b_rmax, b_rng, b_tsum, b_cnt, b_sg, b_step2, b_tsel = (Buf() for _ in range(8))
            b_tau = [Buf(), Buf()]
            sel = [sb(st, f"sel{i}", [128, 512], BF16) for i in range(2)]
            selT = [sb(st, f"selT{i}", [128, 512], BF16) for i in range(3)]
            b_sel, b_selT = [Buf(), Buf()], [Buf(), Buf(), Buf()]
            E2 = [sb(st, f"E2{i}", [128, 1024], BF16) for i in range(2)]
            b_E2 = [Buf(), Buf()]
            P2 = [sb(st, f"P2{i}", [128, 1024], BF16) for i in range(2)]
            b_P2 = [Buf(), Buf()]
            rcb = sb(st, "rcb", [128, 8], F32)
            b_rcb = Buf()
            yb = sb(st, "yb", [128, 8, 64], BF16)
            b_yb = Buf()
            scb = [scores, sb(st, "scores1", [128, NB * 128], F32)]
            b_scb = [b_sc, [Buf() for _ in range(NOWN)]]
            tselb = [tsel, sb(st, "tsel1", [128, 1], F32)]
            b_tselb = [b_tsel, Buf()]
            ctr = {"px": 0, "kr": 0, "ke": 0}

            def stage_S(m):
                nch = m + 1
                qi_, wi_ = qi_t[m % 2], wi_t[m % 2]
                bqi, bwi = b_qi[m % 2], b_wi[m % 2]
                sc_, bsc_ = scb[m % 2], b_scb[m % 2]
                dma("sp", qi_[:], QIT.ap()[m], reads=[b_QIT[m]], writes=[bqi])
                dma("sp", wi_[:], WI.ap()[m], reads=[b_WI[m]], writes=[bwi])
                op("dve", lambda: DVE.tensor_scalar(out=sgh[:], in0=wi_[:], scalar1=0.0, scalar2=0.5,
                                                    op0=ALU.is_ge, op1=ALU.subtract), reads=[bwi], writes=[b_sgh])
                for h in range(8):
                    op("dve", lambda h=h: DVE.tensor_scalar(out=Dg[:, h, :], in0=identb[:], scalar1=sgh[:, h:h + 1],
                                                            scalar2=2.0, op0=ALU.mult, op1=ALU.mult),
                       reads=[b_sgh, b_const], writes=[b_Dg])
                n = 4 * nch
                slots = {}

                def emit_d(j):
                    c, p = divmod(j, 4)
                    k = ctr["px"] % 2
                    ctr["px"] += 1
                    slots[j] = k
                    for half in range(2):
                        rows = slice(64 * half, 64 * half + 64)
                        op("pe", lambda: PE.matmul(PXP[k][:, half * 512:(half + 1) * 512],
                                                   lhsT=qi_[rows, p * 128:(p + 1) * 128],
                                                   rhs=KIs[rows, c * 512:(c + 1) * 512], start=True, stop=True),
                           reads=[bqi, b_KIs], writes=[b_PXP[k]], sig=(half == 1))

                for j in range(min(2, n)):
                    emit_d(j)
                pend_evac = None
                for j in range(n):
                    c, p = divmod(j, 4)
                    k = slots[j]
                    r_, br = R2[ctr["kr"] % 3], b_R2[ctr["kr"] % 3]
                    ctr["kr"] += 1
                    op("act", lambda: ACT.activation(out=r_[:], in_=PXP[k][:], func=AF.Relu),
                       reads=[b_PXP[k]], writes=[br])
                    if pend_evac is not None:
                        cc = pend_evac
                        pend_evac = None
                        op("act", lambda: ACT.copy(out=sc_[:, cc * 512:(cc + 1) * 512], in_=SC[:]),
                           reads=[b_SC], writes=[bsc_[cc]])
                    for half in range(2):
                        h = 2 * p + half
                        op("pe", lambda: PE.matmul(SC[:], lhsT=Dg[:, h, :], rhs=r_[:, half * 512:(half + 1) * 512],
                                                   start=(h == 0), stop=(h == 7)),
                           reads=[br, b_Dg], writes=[b_SC], sig=(half == 1))
                    if j + 2 < n:
                        emit_d(j + 2)
                    if p == 3:
                        pend_evac = c
                if pend_evac is not None:
                    cc = pend_evac
                    op("act", lambda: ACT.copy(out=sc_[:, cc * 512:(cc + 1) * 512], in_=SC[:]),
                       reads=[b_SC], writes=[bsc_[cc]])

            def stage_B(m):
                nch = m + 1
                S = 512 * nch
                sc_, bsc_ = scb[m % 2], b_scb[m % 2]
                bsc = bsc_[0:nch]
                op("dve", lambda: DVE.tensor_reduce(out=rmin[:], in_=sc_[:, 0:S], axis=AX.X, op=ALU.min),
                   reads=bsc, writes=[b_rmin])
                op("dve", lambda: DVE.tensor_tensor(out=sc_[:, 0:512], in0=sc_[:, 0:512], in1=padb[:], op=ALU.add),
                   reads=[b_c3, bsc_[0]], writes=[bsc_[0]])
                op("dve", lambda: DVE.tensor_tensor(out=sc_[:, S - 512:S], in0=sc_[:, S - 512:S], in1=diagb[:], op=ALU.add),
                   reads=[b_c3, bsc_[nch - 1]], writes=[bsc_[nch - 1]])
                op("dve", lambda: DVE.tensor_reduce(out=rmax[:], in_=sc_[:, 0:S], axis=AX.X, op=ALU.max),
                   reads=bsc, writes=[b_rmax])
                op("dve", lambda: DVE.scalar_tensor_tensor(out=rng[:], in0=rmax[:], scalar=2.0, in1=rmin[:],
                                                           op0=ALU.add, op1=ALU.subtract),
                   reads=[b_rmax, b_rmin], writes=[b_rng])
                op("dve", lambda: DVE.tensor_tensor(out=tsum[:], in0=rmax[:], in1=rmin[:], op=ALU.add),
                   reads=[b_rmax, b_rmin], writes=[b_tsum])
                op("dve", lambda: DVE.tensor_scalar(out=tau[0][:], in0=tsum[:], scalar1=0.5, scalar2=None, op0=ALU.mult),
                   reads=[b_tsum], writes=[b_tau[0]])
                op("dve", lambda: DVE.tensor_scalar(out=step2[:], in0=pow2[:], scalar1=rng[:, 0:1], scalar2=None,
                                                    op0=ALU.mult), reads=[b_rng, b_c3], writes=[b_step2])
                for it in range(NIT):
                    tc_, tn_ = tau[it % 2], tau[(it + 1) % 2]
                    btc, btn = b_tau[it % 2], b_tau[(it + 1) % 2]
                    op("dve", lambda: DVE.tensor_scalar(out=junkc[:, 0:S], in0=sc_[:, 0:S], scalar1=tc_[:, 0:1],
                                                        scalar2=None, op0=ALU.is_ge, op1=ALU.add, accum_out=cntt[:]),
                       reads=bsc + [btc], writes=[b_jc, b_cnt])
                    op("dve", lambda: DVE.tensor_scalar(out=sg[:], in0=cntt[:], scalar1=255.5, scalar2=0.5,
                                                        op0=ALU.is_ge, op1=ALU.subtract), reads=[b_cnt], writes=[b_sg])
                    op("dve", lambda: DVE.scalar_tensor_tensor(out=tn_[:], in0=sg[:], scalar=step2[:, it:it + 1],
                                                               in1=tc_[:], op0=ALU.mult, op1=ALU.add),
                       reads=[b_sg, b_step2, btc], writes=[btn])
                tf_, btf = tau[NIT % 2], b_tau[NIT % 2]
                op("dve", lambda: DVE.tensor_tensor(out=tsel[:], in0=tf_[:], in1=step2[:, NIT:NIT + 1], op=ALU.subtract),
                   reads=[btf, b_step2], writes=[b_tsel])
                op("dve", lambda: DVE.tensor_scalar(out=selfull[:, 0:S], in0=sc_[:, 0:S], scalar1=tsel[:, 0:1],
                                                    scalar2=None, op0=ALU.is_ge),
                   reads=bsc + [b_tsel], writes=[b_selfull])

            def stage_A(m):
                nch = m + 1
                qb_, bqb = qb_t[m % 2], b_qb[m % 2]
                dma("sp", qb_[:], QBT.ap()[m], reads=[b_QBT[m]], writes=[bqb])
                first_ob = [True, True]
                n = 4 * nch
                slots = {}

                def prep(c):
                    sT_, bsT_ = selT[c % 3], b_selT[c % 3]
                    for kq in range(4):
                        op("pe", lambda kq=kq: PE.transpose(out=TR3[:, kq * 128:(kq + 1) * 128],
                                                            in_=selfull[:, c * 512 + kq * 128:c * 512 + (kq + 1) * 128],
                                                            identity=identb[:]),
                           reads=[b_selfull, b_const], writes=[b_TR3], sig=(kq == 3))
                    op("act", lambda: ACT.copy(out=sT_[:], in_=TR3[:, 0:512]), reads=[b_TR3], writes=[bsT_])

                def emit_st(u):
                    c, kq = divmod(u, 4)
                    if kq == 0 and c + 1 < nch:
                        prep(c + 1)
                    ls = 4 * c + kq
                    k = ctr["px"] % 2
                    ctr["px"] += 1
                    slots[u] = k
                    for half in range(2):
                        rows = slice(64 * half, 64 * half + 64)
                        op("pe", lambda: PE.matmul(
                            PXP[k][:, half * 512:(half + 1) * 512].rearrange("p (a b) -> p a b", b=128),
                            lhsT=KBs[rows, ls * 128:(ls + 1) * 128],
                            rhs=qb_[rows, :].rearrange("p (a b) -> p a b", b=128), start=True, stop=True),
                           reads=[b_KBs, bqb], writes=[b_PXP[k]], sig=(half == 1))

                prep(0)
                for u in range(min(2, n)):
                    emit_st(u)
                for u in range(n):
                    c, kq = divmod(u, 4)
                    ls = 4 * c + kq
                    k = slots[u]
                    ke = ctr["ke"] % 2
                    ctr["ke"] += 1
                    e_, be, p_, bp = E2[ke], b_E2[ke], P2[ke], b_P2[ke]
                    sT_, bsT_ = selT[c % 3], b_selT[c % 3]
                    op("act", lambda: ACT.activation(out=e_[:], in_=PXP[k][:], func=AF.Exp, scale=0.125),
                       reads=[b_PXP[k]], writes=[be])
                    op("pool", lambda: POOL.tensor_tensor(out=p_[:].rearrange("p (a b) -> p a b", b=128),
                                                          in0=e_[:].rearrange("p (a b) -> p a b", b=128),
                                                          in1=bc_mid(sT_[:, kq * 128:(kq + 1) * 128], 8), op=ALU.mult),
                       reads=[be, bsT_], writes=[bp])
                    for hh in range(8):
                        i = hh // 4
                        is_first = first_ob[i]
                        first_ob[i] = False
                        is_last = (u == n - 1 and hh % 4 == 3)
                        op("pe", lambda hh=hh, i=i, is_first=is_first, is_last=is_last: PE.matmul(
                            OB[i][:, (hh % 4) * 65:(hh % 4) * 65 + 65], lhsT=p_[:, hh * 128:(hh + 1) * 128],
                            rhs=VBs[:, ls, :], start=is_first, stop=is_last, skip_group_check=True),
                           reads=[bp, b_VBs], writes=[b_OB[i]], sig=(hh % 4 == 3))
                    if u + 2 < n:
                        emit_st(u + 2)

            def stage_norm(m):
                for bnk in range(2):
                    ov = OB[bnk][:, 0:260].rearrange("p (h d) -> p h d", d=65)
                    op("dve", lambda: DVE.reciprocal(out=rcb[:, bnk * 4:(bnk + 1) * 4], in_=ov[:, :, 64]),
                       reads=[b_OB[bnk]], writes=[b_rcb])
                    op("dve", lambda: DVE.tensor_tensor(out=yb[:, bnk * 4:(bnk + 1) * 4, :], in0=ov[:, :, 0:64],
                                                        in1=bc_last(rcb[:, bnk * 4:(bnk + 1) * 4], 64), op=ALU.mult),
                       reads=[b_OB[bnk], b_rcb], writes=[b_yb])

            def stage_fin(m):
                yb2 = yb[:].rearrange("p h d -> p (h d)")
                for kc in range(4):
                    op("pe", lambda kc=kc: PE.transpose(out=TR3[:, kc * 128:(kc + 1) * 128],
                                                        in_=yb2[:, kc * 128:(kc + 1) * 128], identity=identb[:]),
                       reads=[b_yb, b_const], writes=[b_TR3], sig=(kc == 3))
                op("act", lambda: ACT.copy(out=YBT[:, :, m * 128:(m + 1) * 128],
                                           in_=TR3[:, 0:512].rearrange("p (a b) -> p a b", b=128)),
                   reads=[b_TR3], writes=[b_YBT[m]])

            stage_S(0)
            for m in range(NOWN):
                if m + 1 < NOWN:
                    stage_S(m + 1)
                stage_B(m)
                if m >= 1:
                    stage_norm(m - 1)
                    stage_fin(m - 1)
                stage_A(m)
            stage_norm(NOWN - 1)
            stage_fin(NOWN - 1)

        if _DBG == "3":
            dy = dbg_out("YBT", [128, 4, NOWN * 128], BF16)
            bo = Buf()
            dma("sp", dy.ap(), YBT[:], reads=b_YBT, writes=[bo])
            kb.finish([bo])
            return nc, dbg_outs

        kb.barrier()
        with ExitStack() as st45:
            H2T = sb(st45, "H2T", [128, 8, NOWN * 128], BF16)
            b_H2T = [Buf() for _ in range(NOWN)]
            ss = sb(st45, "ss2", [128, 1], F32)
            ms = sb(st45, "ms2", [128, 1], F32)
            rstd = sb(st45, "rstd2", [128, 1], F32)
            mhalf = sb(st45, "mhalf2", [128, 1], F32)
            b_ss, b_ms, b_rstd, b_mh = Buf(), Buf(), Buf(), Buf()
            op("pool", lambda: POOL.memset(mhalf[:], -0.5), writes=[b_mh])
            junk = sb(st45, "junk2", [128, D], BF16)
            b_junk = Buf()
            with ExitStack() as st:
                WUA = sb(st, "WUA", [128, 4, D], BF16)
                WUB = sb(st, "WUB", [128, 4, D], BF16)
                WO = sb(st, "WO", [128, 8, D], BF16)
                wst2 = sb(st, "wst2", [128, 8, D], F32)
                b_w2, b_WUA, b_WUB, b_WO = Buf(), Buf(), Buf(), Buf()
                dma("sp", wst2[:, 0:4, :], w_up_a.ap().rearrange("(kc p) n -> p kc n", p=128), writes=[b_w2])
                op("pool", lambda: POOL.tensor_copy(out=WUA[:], in_=wst2[:, 0:4, :]), reads=[b_w2], writes=[b_WUA])
                dma("sp", wst2[:, 0:4, :], w_up_b.ap().rearrange("(kc p) n -> p kc n", p=128), writes=[b_w2])
                op("pool", lambda: POOL.tensor_copy(out=WUB[:], in_=wst2[:, 0:4, :]), reads=[b_w2], writes=[b_WUB])
                dma("sp", wst2[:], w_out.ap().rearrange("(kc p) n -> p kc n", p=128), writes=[b_w2])
                op("pool", lambda: POOL.tensor_copy(out=WO[:], in_=wst2[:]), reads=[b_w2], writes=[b_WO])
                gs_t = [sb(st, f"gs{i}", [128, 2048], BF16) for i in range(2)]
                x_t = [sb(st, f"x4{i}", [128, D], F32) for i in range(2)]
                b_gs, b_x4 = [Buf(), Buf()], [Buf(), Buf()]
                UA = [ps(st, f"UA{i}", [128, 512], F32) for i in range(2)]
                UB = [ps(st, f"UB{i}", [128, 512], F32) for i in range(2)]
                WOp = [ps(st, f"WOp{i}", [128, 512], F32) for i in range(2)]
                TR4 = ps(st, "TR4", [128, 1024], BF16)
                b_UA, b_UB, b_WOp = [Buf(), Buf()], [Buf(), Buf()], [Buf(), Buf()]
                b_TR4 = Buf()
                t1 = [sb(st, f"t1{i}", [128, 512], F32) for i in range(2)]
                t2 = [sb(st, f"t2{i}", [128, 512], F32) for i in range(2)]
                b_t1, b_t2 = [Buf(), Buf()], [Buf(), Buf()]
                mg = sb(st, "mg", [128, D], BF16)
                mgT = sb(st, "mgT", [128, 8, 128], BF16)
                b_mg, b_mgT = Buf(), Buf()
                x2 = [sb(st, f"x2{i}", [128, D], F32) for i in range(2)]
                b_x2 = [Buf(), Buf()]
                h2b = sb(st, "h2b", [128, D], BF16)
                b_h2b = Buf()
                for m in range(NOWN):
                    g_, bg_ = gs_t[m % 2], b_gs[m % 2]
                    x_, bx_ = x_t[m % 2], b_x4[m % 2]
                    x2_, bx2_ = x2[m % 2], b_x2[m % 2]
                    dma("sp", g_[:], GS.ap()[m], reads=[b_GS[m]], writes=[bg_])
                    dma("sp", x_[:], xl.ap()[4 * m + 3], writes=[bx_])
                    tk = slice(m * 128, (m + 1) * 128)
                    for half in range(2):
                        cs_ = slice(half * 512, (half + 1) * 512)
                        for kc in range(4):
                            op("pe", lambda kc=kc: PE.matmul(UA[half][:], lhsT=YAT[:, kc, tk], rhs=WUA[:, kc, cs_],
                                                             start=(kc == 0), stop=(kc == 3)),
                               reads=[b_YAT[m], b_WUA], writes=[b_UA[half]], sig=(kc == 3))
                        for kc in range(4):
                            op("pe", lambda kc=kc: PE.matmul(UB[half][:], lhsT=YBT[:, kc, tk], rhs=WUB[:, kc, cs_],
                                                             start=(kc == 0), stop=(kc == 3)),
                               reads=[b_YBT[m], b_WUB], writes=[b_UB[half]], sig=(kc == 3))
                        op("dve", lambda: DVE.tensor_tensor(out=t1[half][:], in0=UA[half][:], in1=g_[:, cs_], op=ALU.mult),
                           reads=[b_UA[half], bg_], writes=[b_t1[half]])
                        op("dve", lambda: DVE.tensor_tensor(out=t2[half][:], in0=UB[half][:],
                                                            in1=g_[:, 1024 + half * 512:1024 + (half + 1) * 512], op=ALU.mult),
                           reads=[b_UB[half], bg_], writes=[b_t2[half]])
                        op("pool", lambda: POOL.tensor_tensor(out=mg[:, cs_], in0=t1[half][:], in1=t2[half][:], op=ALU.add),
                           reads=[b_t1[half], b_t2[half]], writes=[b_mg])
                    for kc in range(8):
                        op("pe", lambda kc=kc: PE.transpose(out=TR4[:, kc * 128:(kc + 1) * 128],
                                                            in_=mg[:, kc * 128:(kc + 1) * 128], identity=identb[:]),
                           reads=[b_mg, b_const], writes=[b_TR4], sig=(kc == 7))
                    op("act", lambda: ACT.copy(out=mgT[:].rearrange("p a b -> p (a b)"), in_=TR4[:]),
                       reads=[b_TR4], writes=[b_mgT])
                    for half in range(2):
                        cs_ = slice(half * 512, (half + 1) * 512)
                        for kc in range(8):
                            op("pe", lambda kc=kc: PE.matmul(WOp[half][:], lhsT=mgT[:, kc, :], rhs=WO[:, kc, cs_],
                                                             start=(kc == 0), stop=(kc == 7)),
                               reads=[b_mgT, b_WO], writes=[b_WOp[half]], sig=(kc == 7))
                        op("dve", lambda: DVE.tensor_tensor(out=x2_[:, cs_], in0=WOp[half][:], in1=x_[:, cs_], op=ALU.add),
                           reads=[b_WOp[half], bx_], writes=[bx2_])
                    dma("pool", X2.ap()[m], x2_[:], reads=[bx2_], writes=[b_X2[m]], fence=("dve",))
                    op("act", lambda: ACT.activation(out=junk[:], in_=x2_[:], func=AF.Square, accum_out=ss[:]),
                       reads=[bx2_], writes=[b_junk, b_ss])
                    op("dve", lambda: DVE.tensor_scalar(out=ms[:], in0=ss[:], scalar1=1.0 / D, scalar2=EPS,
                                                        op0=ALU.mult, op1=ALU.add), reads=[b_ss], writes=[b_ms])
                    op("pool", lambda: POOL.tensor_tensor(out=rstd[:], in0=ms[:], in1=mhalf[:], op=ALU.pow),
                       reads=[b_ms, b_mh], writes=[b_rstd])
                    op("pool", lambda: POOL.tensor_scalar(out=h2b[:], in0=x2_[:], scalar1=rstd[:, 0:1], scalar2=0.0,
                                                          op0=ALU.mult, op1=ALU.add),
                       reads=[bx2_, b_rstd], writes=[b_h2b])
                    for kc in range(8):
                        op("pe", lambda kc=kc: PE.transpose(out=TR4[:, kc * 128:(kc + 1) * 128],
                                                            in_=h2b[:, kc * 128:(kc + 1) * 128], identity=identb[:]),
                           reads=[b_h2b, b_const], writes=[b_TR4], sig=(kc == 7))
                    op("act", lambda: ACT.copy(out=H2T[:, :, tk], in_=TR4[:].rearrange("p (a b) -> p a b", b=128)),
                       reads=[b_TR4], writes=[b_H2T[m]])

            if _DBG == "4":
                dx = dbg_out("X2o", [NOWN, 128, D], F32)
                bo = Buf()
                with ExitStack() as st:
                    tt = sb(st, "dbgt", [128, D], F32)
                    bt = Buf()
                    for m in range(NOWN):
                        dma("sp", tt[:], X2.ap()[m], reads=[b_X2[m]], writes=[bt])
                        dma("sp", dx.ap()[m], tt[:], reads=[bt], writes=[bo])
                    kb.finish([bo])
                return nc, dbg_outs

            kb.barrier()
            with ExitStack() as st:
                gffn = sb(st, "gffn", [128, 8], F32)
                gfin = sb(st, "gfin", [128, D], F32)
                b_g5 = Buf()
                dma("sp", gffn[:], gffn_d.ap(), writes=[b_g5])
                dma("sp", gfin[:], gfin_d.ap(), writes=[b_g5])
                ACTT = sb(st, "ACTT", [128, NFF, NOWN * 128], BF16)
                b_ACTT = [Buf() for _ in range(4)]
                with ExitStack() as sg_:
                    wgst = [sb(sg_, f"wgst{i}", [128, 8, 128], F32) for i in range(2)]
                    wust = [sb(sg_, f"wust{i}", [128, 8, 128], F32) for i in range(2)]
                    wgb = [sb(sg_, f"wgb{i}", [128, 8, 128], BF16) for i in range(2)]
                    wub = [sb(sg_, f"wub{i}", [128, 8, 128], BF16) for i in range(2)]
                    b_wgst, b_wust, b_wgb, b_wub = ([Buf(), Buf()] for _ in range(4))
                    G = [ps(sg_, f"G{i}", [128, 512], F32) for i in range(2)]
                    U = [ps(sg_, f"U{i}", [128, 512], F32) for i in range(2)]
                    b_G, b_U = [Buf(), Buf()], [Buf(), Buf()]
                    sgt = [sb(sg_, f"sgt{i}", [128, 512], F32) for i in range(2)]
                    b_sgt = [Buf(), Buf()]
                    wgv = w_gate.ap().rearrange("(kc p) n -> p kc n", p=128)
                    wuv = w_up.ap().rearrange("(kc p) n -> p kc n", p=128)
                    kk = 0
                    for ffc in range(NFF):
                        i2 = ffc % 2
                        dma("sp", wgst[i2][:], wgv[:, :, ffc * 128:(ffc + 1) * 128], writes=[b_wgst[i2]])
                        dma("sp", wust[i2][:], wuv[:, :, ffc * 128:(ffc + 1) * 128], writes=[b_wust[i2]])
                        op("pool", lambda: POOL.tensor_tensor(out=wgb[i2][:], in0=wgst[i2][:], in1=bc_last(gffn[:, :], 128),
                                                              op=ALU.mult), reads=[b_wgst[i2], b_g5], writes=[b_wgb[i2]])
                        op("pool", lambda: POOL.tensor_tensor(out=wub[i2][:], in0=wust[i2][:], in1=bc_last(gffn[:, :], 128),
                                                              op=ALU.mult), reads=[b_wust[i2], b_g5], writes=[b_wub[i2]])
                        for tg in range(4):
                            ts_ = slice(tg * 512, (tg + 1) * 512)
                            g_, bg_ = G[kk % 2], b_G[kk % 2]
                            u_, bu_ = U[kk % 2], b_U[kk % 2]
                            s_, bs_ = sgt[kk % 2], b_sgt[kk % 2]
                            kk += 1
                            for kc in range(8):
                                op("pe", lambda kc=kc, g_=g_: PE.matmul(g_[:], lhsT=wgb[i2][:, kc, :], rhs=H2T[:, kc, ts_],
                                                                        start=(kc == 0), stop=(kc == 7)),
                                   reads=[b_wgb[i2]] + b_H2T[tg * 4:(tg + 1) * 4], writes=[bg_], sig=(kc == 7))
                            for kc in range(8):
                                op("pe", lambda kc=kc, u_=u_: PE.matmul(u_[:], lhsT=wub[i2][:, kc, :], rhs=H2T[:, kc, ts_],
                                                                        start=(kc == 0), stop=(kc == 7)),
                                   reads=[b_wub[i2]] + b_H2T[tg * 4:(tg + 1) * 4], writes=[bu_], sig=(kc == 7))
                            op("act", lambda g_=g_, s_=s_: ACT.activation(out=s_[:], in_=g_[:], func=AF.Silu),
                               reads=[bg_], writes=[bs_])
                            op("dve", lambda u_=u_, s_=s_: DVE.tensor_tensor(out=ACTT[:, ffc, ts_], in0=u_[:], in1=s_[:],
                                                                             op=ALU.mult),
                               reads=[bu_, bs_], writes=[b_ACTT[tg]])
                kb.barrier()
                with ExitStack() as sd_:
                    wdst = [sb(sd_, f"wdst{i}", [128, D], F32) for i in range(2)]
                    wdb = [sb(sd_, f"wdb{i}", [128, D], BF16) for i in range(2)]
                    b_wdst, b_wdb = [Buf(), Buf()], [Buf(), Buf()]
                    DN = [ps(sd_, f"DN{i}", [128, 512], F32) for i in range(8)]
                    b_DN = [Buf() for _ in range(8)]
                    x2t = [sb(sd_, f"x2t{i}", [128, D], F32) for i in range(2)]
                    x3 = [sb(sd_, f"x3{i}", [128, D], F32) for i in range(2)]
                    ot = [sb(sd_, f"ot{i}", [128, D], F32) for i in range(2)]
                    b_x2t, b_x3, b_ot = [Buf(), Buf()], [Buf(), Buf()], [Buf(), Buf()]
                    kw_ = 0
                    for tg in range(4):
                        for ffc in range(NFF):
                            i2 = kw_ % 2
                            kw_ += 1
                            dma("sp", wdst[i2][:], w_down.ap()[ffc * 128:(ffc + 1) * 128, :], writes=[b_wdst[i2]])
                            op("pool", lambda: POOL.tensor_copy(out=wdb[i2][:], in_=wdst[i2][:]),
                               reads=[b_wdst[i2]], writes=[b_wdb[i2]])
                            for tb in range(4):
                                mm = tg * 4 + tb
                                for half in range(2):
                                    op("pe", lambda tb=tb, half=half, mm=mm: PE.matmul(
                                        DN[tb * 2 + half][:], lhsT=ACTT[:, ffc, mm * 128:(mm + 1) * 128],
                                        rhs=wdb[i2][:, half * 512:(half + 1) * 512], start=(ffc == 0), stop=(ffc == NFF - 1)),
                                       reads=[b_wdb[i2], b_ACTT[tg]], writes=[b_DN[tb * 2 + half]],
                                       sig=(tb == 3 and half == 1))
                        for tb in range(4):
                            mm = tg * 4 + tb
                            xx, bxx = x2t[mm % 2], b_x2t[mm % 2]
                            x3_, bx3 = x3[mm % 2], b_x3[mm % 2]
                            o_, bo_ = ot[mm % 2], b_ot[mm % 2]
                            dma("sp", xx[:], X2.ap()[mm], reads=[b_X2[mm]], writes=[bxx])
                            for half in range(2):
                                cs_ = slice(half * 512, (half + 1) * 512)
                                op("dve", lambda: DVE.tensor_tensor(out=x3_[:, cs_], in0=DN[tb * 2 + half][:], in1=xx[:, cs_],
                                                                    op=ALU.add),
                                   reads=[b_DN[tb * 2 + half], bxx], writes=[bx3])
                            op("act", lambda: ACT.activation(out=junk[:], in_=x3_[:], func=AF.Square, accum_out=ss[:]),
                               reads=[bx3], writes=[b_junk, b_ss])
                            op("dve", lambda: DVE.tensor_scalar(out=ms[:], in0=ss[:], scalar1=1.0 / D, scalar2=EPS,
                                                                op0=ALU.mult, op1=ALU.add), reads=[b_ss], writes=[b_ms])
                            op("pool", lambda: POOL.tensor_tensor(out=rstd[:], in0=ms[:], in1=mhalf[:], op=ALU.pow),
                               reads=[b_ms, b_mh], writes=[b_rstd])
                            op("dve", lambda: DVE.scalar_tensor_tensor(out=o_[:], in0=x3_[:], scalar=rstd[:, 0:1], in1=gfin[:],
                                                                       op0=ALU.mult, op1=ALU.mult),
                               reads=[bx3, b_rstd, b_g5], writes=[bo_])
                            dma("sp", out_d.ap()[mm], o_[:], reads=[bo_], writes=[b_out[mm]], fence=("dve",))

        kb.finish(b_out)
    return nc, dbg_outs


def host_prep(x, norm_mix, w_in, w_up_a, w_up_b, w_out, norm_ffn, w_gate, w_up, w_down, norm_final):
    B, T, _ = x.shape
    x = np.asarray(x, np.float32)
    half = 32
    inv_freq = (10000.0 ** (-np.arange(half, dtype=np.float32) / half)).astype(np.float32)
    identb = np.eye(128, dtype=np.float32).astype(ml_dtypes.bfloat16)
    s_i = np.arange(128)[:, None]
    t_i = np.arange(128)[None, :]
    mt = np.zeros((128, 17, 128), np.float32)
    for dl in range(17):
        diff = 128 * dl + t_i - s_i
        tot = np.zeros((128, 128), np.float32)
        for (wdw, dil) in ((128, 1), (512, 4), (2048, 16)):
            ok = (diff >= 0) & (diff <= wdw) & (diff % dil == 0)
            tot += ok.astype(np.float32)
        mt[:, dl, :] = tot
    mt = mt.astype(ml_dtypes.bfloat16)
    diagbias = np.zeros((128, 512), np.float32)
    diagbias[:, 384:512] = np.where(np.arange(128)[None, :] > np.arange(128)[:, None], -BIG, 0.0)
    pow2 = np.broadcast_to((2.0 ** (-(np.arange(NIT + 1) + 1.0))).astype(np.float32)[None, :], (128, NIT + 1)).copy()
    gmix = np.ascontiguousarray(np.asarray(norm_mix, np.float32).reshape(8, 128).T)
    gffn = np.ascontiguousarray(np.asarray(norm_ffn, np.float32).reshape(8, 128).T)
    gfin = np.ascontiguousarray(np.broadcast_to(np.asarray(norm_final, np.float32)[None, :], (128, D)))
    common = {
        "identb": identb, "mt": mt, "diagbias": diagbias, "pow2": pow2, "gmix": gmix, "gffn": gffn, "gfin": gfin,
        "w_in": np.ascontiguousarray(np.asarray(w_in, np.float32)[0]),
        "w_up_a": np.ascontiguousarray(np.asarray(w_up_a, np.float32)[0]),
        "w_up_b": np.ascontiguousarray(np.asarray(w_up_b, np.float32)[0]),
        "w_out": np.ascontiguousarray(np.asarray(w_out, np.float32)[0]),
        "w_gate": np.ascontiguousarray(np.asarray(w_gate, np.float32)[0]),
        "w_up": np.ascontiguousarray(np.asarray(w_up, np.float32)[0]),
        "w_down": np.ascontiguousarray(np.asarray(w_down, np.float32)[0]),
    }
    in_maps = []
    for core in range(8):
        b, j = core // 4, core % 4
        xl = np.zeros((NB, 128, D), np.float32)
        pos = np.zeros((NB, 128), np.float32)
        valid = np.zeros((NB,), np.float32)
        for l in range(NB):
            g = l + j - 3
            if g >= 0:
                xl[l] = x[b, g * 128:(g + 1) * 128]
                pos[l] = np.arange(g * 128, (g + 1) * 128, dtype=np.float32)
                valid[l] = 1.0
        ang = pos[:, :, None] * inv_freq[None, None, :]
        cs = np.concatenate([np.cos(ang), np.sin(ang)], axis=-1).astype(np.float32)
        vmask = np.ascontiguousarray(np.broadcast_to(valid[None, :], (128, NB))).astype(np.float32)
        padbias = np.zeros((128, 512), np.float32)
        for l in range(4):
            if valid[l] == 0.0:
                padbias[:, l * 128:(l + 1) * 128] = -BIG
        m = dict(common)
        m.update({"xl": xl, "cs": cs, "vmask": vmask, "padbias": padbias})
        in_maps.append(m)
    return in_maps


def kernel(x, norm_mix, w_in, w_up_a, w_up_b, w_out, norm_ffn, w_gate, w_up, w_down, norm_final):
    in_maps = host_prep(x, norm_mix, w_in, w_up_a, w_up_b, w_out, norm_ffn, w_gate, w_up, w_down, norm_final)
    nc, dbg = build()
    if _DBG:
        res = run_bass_kernel_spmd(nc, in_maps, core_ids=list(range(8)), trace=bool(os.environ.get("KTRACE")))
        print("DBG exec_time_ns", res.exec_time_ns)
        return res
    res = run_bass_kernel_spmd(nc, in_maps, core_ids=list(range(8)))
    B, T, _ = x.shape
    out = np.zeros((B, T, D), np.float32)
    for core in range(8):
        b, j = core // 4, core % 4
        o = res.results[core]["out"]
        for m in range(NOWN):
            g = 4 * m + j
            out[b, g * 128:(g + 1) * 128] = o[m]
    return out
```

```python
import os
from contextlib import ExitStack

import ml_dtypes
import numpy as np

import concourse.bass as bass
import concourse.mybir as mybir
from concourse.bass_types import AP
from concourse.bass_utils import run_bass_kernel_spmd

F32 = mybir.dt.float32
BF16 = mybir.dt.bfloat16
AF = mybir.ActivationFunctionType
ALU = mybir.AluOpType
AX = mybir.AxisListType

NB = 64
NOWN = 16
D = 1024
DFF = 2816
NFF = DFF // 128
DIN = 4808
NIT = 16
BIG = 1.0e30
NEGM = 30000.0
EPS = 1e-6
NDS = 12

_DBG = os.environ.get("KDBG", "")


class Buf:
    __slots__ = ("w", "r")

    def __init__(self):
        self.w = None
        self.r = {}


class KB:
    def __init__(self, nc):
        self.nc = nc
        self.eng = {"pe": nc.tensor, "act": nc.scalar, "dve": nc.vector, "pool": nc.gpsimd, "sp": nc.sync}
        self.sem = {e: nc.alloc_semaphore(name=f"s_{e}") for e in self.eng}
        self.cnt = {e: 0 for e in self.eng}
        self.waited = {e: {} for e in self.eng}
        self.fence_fn = {}
        self.last_fence = {}
        self.dq = {}
        for q in ("sp", "pool", "act"):
            self.dq[q] = {"sems": [nc.alloc_semaphore(name=f"d_{q}{i}") for i in range(NDS)], "k": 0}

    def _wait(self, e, ev):
        sem, val = ev
        if e == "pe" and sem is self.sem["pe"]:
            return
        key = sem.num
        if self.waited[e].get(key, 0) >= val:
            return
        self.eng[e].wait_ge(sem, val)
        self.waited[e][key] = val

    def _deps(self, e, reads, writes):
        for b in reads:
            if b.w is not None:
                self._wait(e, b.w)
        for b in writes:
            if b.w is not None:
                self._wait(e, b.w)
            for ev in b.r.values():
                self._wait(e, ev)

    def _mark(self, ev, reads, writes):
        key = ev[0].num
        for b in reads:
            old = b.r.get(key)
            if old is None or old[1] < ev[1]:
                b.r[key] = ev
        for b in writes:
            b.w = ev
            b.r = {}

    def op(self, e, fn, reads=(), writes=(), sig=True):
        self._deps(e, reads, writes)
        inst = fn()
        if sig:
            self.cnt[e] += 1
            inst.then_inc(self.sem[e], 1)
            ev = (self.sem[e], self.cnt[e])
        else:
            ev = (self.sem[e], self.cnt[e] + 1)
        self._mark(ev, reads, writes)
        return inst

    def dma(self, q, out, in_, reads=(), writes=(), fence=()):
        dq = self.dq[q]
        k = dq["k"]
        P = len(dq["sems"])
        sem = dq["sems"][k % P]
        if k >= P:
            self._wait(q, (sem, 16 * (k // P)))
        self._deps(q, reads, writes)
        for e in fence:
            last = self.last_fence.get(e)
            if last is not None:
                self._wait(e, last)
            self.fence_fn[e]().then_inc(self.sem[e], 1)
            self.cnt[e] += 1
            self.last_fence[e] = (self.sem[e], self.cnt[e])
            self._wait(q, (self.sem[e], self.cnt[e]))
        self.eng[q].dma_start(out=out, in_=in_).then_inc(sem, 16)
        dq["k"] += 1
        ev = (sem, 16 * (k // P + 1))
        self._mark(ev, reads, writes)

    def barrier(self):
        evs = [(self.sem[e], self.cnt[e]) for e in self.eng if self.cnt[e] > 0]
        for q, dq in self.dq.items():
            k = dq["k"]
            P = len(dq["sems"])
            for i in range(min(k, P)):
                kk = k - 1 - i
                evs.append((dq["sems"][kk % P], 16 * (kk // P + 1)))
        for e in self.eng:
            for ev in evs:
                if ev[0] is self.sem[e]:
                    continue
                self._wait(e, ev)

    def finish(self, bufs):
        for b in bufs:
            if b.w is not None:
                self._wait("sp", b.w)
        for q, dq in self.dq.items():
            k = dq["k"]
            P = len(dq["sems"])
            for i in range(min(k, P)):
                kk = k - 1 - i
                self._wait(q, (dq["sems"][kk % P], 16 * (kk // P + 1)))


def bc_mid(ap2d, n):
    a = [list(x) for x in ap2d.ap]
    assert len(a) == 2
    return AP(ap2d.tensor, ap2d.offset, [a[0], [0, n], a[1]])


def bc_last(ap, n):
    a = [list(x) for x in ap.ap]
    return AP(ap.tensor, ap.offset, a + [[0, n]])


def build():
    nc = bass.Bass("TRN2", target_bir_lowering=False)
    kb = KB(nc)
    op = kb.op
    dma = kb.dma
    PE, ACT, DVE, POOL = nc.tensor, nc.scalar, nc.vector, nc.gpsimd

    def din(name, shape, dt=F32):
        return nc.dram_tensor(name, list(shape), dt, kind="ExternalInput")

    def dscr(name, shape, dt):
        return nc.dram_tensor(name, list(shape), dt, kind=("ExternalOutput" if _DBG else "Internal"))

    xl = din("xl", [NB, 128, D])
    cs_t = din("cs", [NB, 128, 64])
    vmask_d = din("vmask", [128, NB])
    padbias_d = din("padbias", [128, 512])
    diagbias_d = din("diagbias", [128, 512])
    mt_d = din("mt", [128, 17, 128], BF16)
    identb_d = din("identb", [128, 128], BF16)
    pow2_d = din("pow2", [128, NIT + 1])
    gmix_d = din("gmix", [128, 8])
    gffn_d = din("gffn", [128, 8])
    gfin_d = din("gfin", [128, D])
    w_in = din("w_in", [D, DIN])
    w_up_a = din("w_up_a", [512, D])
    w_up_b = din("w_up_b", [512, D])
    w_out = din("w_out", [D, D])
    w_gate = din("w_gate", [D, DFF])
    w_up = din("w_up", [D, DFF])
    w_down = din("w_down", [DFF, D])
    out_d = nc.dram_tensor("out", [NOWN, 128, D], F32, kind="ExternalOutput")

    KAT = dscr("KAT", [128, NB, 512], BF16)
    VA = dscr("VA", [128, NB, 520], BF16)
    KBT = dscr("KBT", [64, NB * 128], BF16)
    KIT = dscr("KIT", [64, NB * 128], BF16)
    VB = dscr("VB", [128, NB, 65], BF16)
    QAT = dscr("QAT", [NOWN, 128, 512], BF16)
    QBT = dscr("QBT", [NOWN, 128, 512], BF16)
    QIT = dscr("QIT", [NOWN, 128, 512], BF16)
    WI = dscr("WI", [NOWN, 128, 8], F32)
    GS = dscr("GS", [NOWN, 128, 2048], BF16)
    X2 = dscr("X2", [NOWN, 128, D], F32)
    b_KAT = [Buf() for _ in range(NB)]
    b_VA = [Buf() for _ in range(NB)]
    b_KBT = [Buf() for _ in range(NB)]
    b_KIT = [Buf() for _ in range(NB)]
    b_VB = [Buf() for _ in range(NB)]
    b_QAT = [Buf() for _ in range(NOWN)]
    b_QBT = [Buf() for _ in range(NOWN)]
    b_QIT = [Buf() for _ in range(NOWN)]
    b_WI = [Buf() for _ in range(NOWN)]
    b_GS = [Buf() for _ in range(NOWN)]
    b_X2 = [Buf() for _ in range(NOWN)]
    b_out = [Buf() for _ in range(NOWN)]

    dbg_outs = {}

    def dbg_out(name, shape, dt):
        t = nc.dram_tensor("dbg_" + name, list(shape), dt, kind="ExternalOutput")
        dbg_outs[name] = t
        return t

    with ExitStack() as top:
        def sb(stack, name, shape, dt):
            return stack.enter_context(nc.sbuf_tensor("sb_" + name, list(shape), dt))

        def ps(stack, name, shape, dt):
            return stack.enter_context(nc.psum_tensor("ps_" + name, list(shape), dt))

        identb = sb(top, "identb", [128, 128], BF16)
        b_const = Buf()
        dma("sp", identb[:], identb_d.ap(), writes=[b_const])
        vmask = sb(top, "vmask", [128, NB], F32)
        dma("sp", vmask[:], vmask_d.ap(), writes=[b_const])
        fsc = sb(top, "fsc", [128, 8], F32)
        op("pool", lambda: POOL.memset(fsc[:], 0.0), writes=[Buf()])
        kb.barrier()
        kb.fence_fn["act"] = lambda: ACT.copy(out=fsc[:, 0:1], in_=fsc[:, 1:2])
        kb.fence_fn["dve"] = lambda: DVE.tensor_copy(out=fsc[:, 2:3], in_=fsc[:, 3:4])
        kb.fence_fn["pool"] = lambda: POOL.tensor_copy(out=fsc[:, 4:5], in_=fsc[:, 5:6])
        YAT = sb(top, "YAT", [128, 4, NOWN * 128], BF16)
        YBT = sb(top, "YBT", [128, 4, NOWN * 128], BF16)
        b_YAT = [Buf() for _ in range(NOWN)]
        b_YBT = [Buf() for _ in range(NOWN)]

        def rope(stack_bufs, zview, H, cs_tile, b_cs, b_z, out_view, b_outv):
            tA, tB, tC, tD, bA, bB, bC, bD = stack_bufs
            cosb = bc_mid(cs_tile[:, 0:32], H)
            sinb = bc_mid(cs_tile[:, 32:64], H)
            z1 = zview[:, :, 0:32]
            z2 = zview[:, :, 32:64]
            a = tA[:, 0:H, :]
            b = tB[:, 0:H, :]
            c = tC[:, 0:H, :]
            d = tD[:, 0:H, :]
            op("dve", lambda: DVE.tensor_tensor(out=a, in0=z1, in1=cosb, op=ALU.mult), reads=[b_z, b_cs], writes=[bA])
            op("dve", lambda: DVE.tensor_tensor(out=b, in0=z2, in1=sinb, op=ALU.mult), reads=[b_z, b_cs], writes=[bB])
            op("dve", lambda: DVE.tensor_tensor(out=c, in0=z2, in1=cosb, op=ALU.mult), reads=[b_z, b_cs], writes=[bC])
            op("dve", lambda: DVE.tensor_tensor(out=d, in0=z1, in1=sinb, op=ALU.mult), reads=[b_z, b_cs], writes=[bD])
            op("dve", lambda: DVE.tensor_tensor(out=out_view[:, :, 0:32], in0=a, in1=b, op=ALU.subtract),
               reads=[bA, bB], writes=[b_outv])
            op("dve", lambda: DVE.tensor_tensor(out=out_view[:, :, 32:64], in0=c, in1=d, op=ALU.add),
               reads=[bC, bD], writes=[b_outv])

        with ExitStack() as st:
            WK = sb(st, "WK", [128, 8, 1216], BF16)
            WQ = sb(st, "WQ", [128, 8, 3592], BF16)
            wst = [sb(st, f"wst{i}", [128, 8, 512], F32) for i in range(2)]
            b_wst = [Buf(), Buf()]
            gmix = sb(st, "gmix", [128, 8], F32)
            b_g = Buf()
            dma("sp", gmix[:], gmix_d.ap(), writes=[b_g])
            b_WK = Buf()
            b_WQ = Buf()
            w_in_v = w_in.ap().rearrange("(kc p) n -> p kc n", p=128)
            kparts = [(WK, b_WK, 0, 512, 512), (WK, b_WK, 512, 1024, 512), (WK, b_WK, 1024, 2048, 128),
                      (WK, b_WK, 1152, 2688, 64)]
            qparts = [(WQ, b_WQ, 0, 0, 512), (WQ, b_WQ, 512, 1536, 512), (WQ, b_WQ, 1024, 2176, 512),
                      (WQ, b_WQ, 1536, 2752, 8), (WQ, b_WQ, 1544, 2760, 512), (WQ, b_WQ, 2056, 3272, 512),
                      (WQ, b_WQ, 2568, 3784, 512), (WQ, b_WQ, 3080, 4296, 512)]
            for i, (dst, bdst, dc, sc, n) in enumerate(kparts + qparts):
                s = wst[i % 2]
                bs = b_wst[i % 2]
                dma("sp", s[:, :, 0:n], w_in_v[:, :, sc:sc + n], writes=[bs])
                op("pool", lambda s=s, dst=dst, dc=dc, n=n: POOL.tensor_tensor(
                    out=dst[:, :, dc:dc + n], in0=s[:, :, 0:n], in1=bc_last(gmix[:, :], n), op=ALU.mult),
                   reads=[bs, b_g], writes=[bdst])

            xs = [sb(st, f"xs{i}", [128, D], F32) for i in range(3)]
            b_xs = [Buf() for _ in range(3)]
            cst = [sb(st, f"cst{i}", [128, 64], F32) for i in range(4)]
            b_cst = [Buf() for _ in range(4)]
            junk = sb(st, "junk", [128, D], BF16)
            b_junk = Buf()
            ss = [sb(st, f"ss{i}", [128, 1], F32) for i in range(2)]
            ms = [sb(st, f"ms{i}", [128, 1], F32) for i in range(2)]
            rstd = [sb(st, f"rstd{i}", [128, 1], F32) for i in range(2)]
            mhalf = sb(st, "mhalf", [128, 1], F32)
            b_ss, b_ms, b_rstd = [Buf(), Buf()], [Buf(), Buf()], [Buf(), Buf()]
            b_mh = Buf()
            op("pool", lambda: POOL.memset(mhalf[:], -0.5), writes=[b_mh])
            hb = [sb(st, f"hb{i}", [128, D], BF16) for i in range(2)]
            b_hb = [Buf(), Buf()]
            hT = [sb(st, f"hT{i}", [128, 8, 128], BF16) for i in range(3)]
            b_hT = [Buf() for _ in range(3)]
            TRH = ps(st, "TRH", [128, 1024], BF16)
            b_TRH = Buf()
            NPB = 6
            PB = [ps(st, f"PB{i}", [128, 512], F32) for i in range(NPB)]
            b_PB = [Buf() for _ in range(NPB)]
            TRO = ps(st, "TRO", [128, 1024], BF16)
            b_TRO = Buf()
            rt = [sb(st, f"rt{i}", [128, 8, 32], F32) for i in range(4)]
            rbufs = tuple(rt) + tuple(Buf() for _ in range(4))
            kab = sb(st, "kab", [128, 8, 64], BF16)
            b_kab = Buf()
            kat = [sb(st, f"kat{i}", [128, 512], BF16) for i in range(2)]
            b_kat = [Buf(), Buf()]
            kbi = sb(st, "kbi", [128, 2, 64], BF16)
            b_kbi = Buf()
            kbit = [sb(st, f"kbit{i}", [64, 256], BF16) for i in range(2)]
            b_kbit = [Buf(), Buf()]
            vaa = [sb(st, f"vaa{i}", [128, 8, 65], BF16) for i in range(2)]
            b_vaa = [Buf(), Buf()]
            vba = [sb(st, f"vba{i}", [128, 65], BF16) for i in range(2)]
            b_vba = [Buf(), Buf()]
            qab = sb(st, "qab", [128, 8, 64], BF16)
            b_qab = Buf()
            qat = sb(st, "qat", [128, 512], BF16)
            b_qat = Buf()
            qbt = [sb(st, f"qbt{i}", [128, 512], BF16) for i in range(2)]
            b_qbt = [Buf(), Buf()]
            sgq = sb(st, "sgq", [128, 8], F32)
            absq = sb(st, "absq", [128, 8], F32)
            qis = sb(st, "qis", [128, 8, 64], F32)
            b_sgq, b_absq, b_qis = Buf(), Buf(), Buf()
            wis = sb(st, "wis", [128, 8], F32)
            b_wis = Buf()
            gsb = sb(st, "gsb", [128, 2048], BF16)
            b_gsb = Buf()
            pbc = [0]
            kbanks = {}

            def next_pb():
                i = pbc[0] % NPB
                pbc[0] += 1
                return PB[i], b_PB[i]

            def proj(hT_, bhT_, W, bW, c0, n, bank, bbank, o0=0):
                for kc in range(8):
                    op("pe", lambda kc=kc: PE.matmul(bank[:, o0:o0 + n], lhsT=hT_[:, kc, :], rhs=W[:, kc, c0:c0 + n],
                                                     start=(kc == 0), stop=(kc == 7)),
                       reads=[bhT_, bW], writes=[bbank], sig=(kc == 7))

            def stage_F(l):
                x_, bx = xs[l % 3], b_xs[l % 3]
                c_, bc = cst[l % 4], b_cst[l % 4]
                ss_, ms_, rs_ = ss[l % 2], ms[l % 2], rstd[l % 2]
                hb_, bhb = hb[l % 2], b_hb[l % 2]
                hT_, bhT_ = hT[l % 3], b_hT[l % 3]
                dma("sp", x_[:], xl.ap()[l], writes=[bx])
                dma("sp", c_[:], cs_t.ap()[l], writes=[bc])
                op("act", lambda: ACT.activation(out=junk[:], in_=x_[:], func=AF.Square, accum_out=ss_[:]),
                   reads=[bx], writes=[b_junk, b_ss[l % 2]])
                op("dve", lambda: DVE.tensor_scalar(out=ms_[:], in0=ss_[:], scalar1=1.0 / D, scalar2=EPS,
                                                    op0=ALU.mult, op1=ALU.add), reads=[b_ss[l % 2]], writes=[b_ms[l % 2]])
                op("pool", lambda: POOL.tensor_tensor(out=rs_[:], in0=ms_[:], in1=mhalf[:], op=ALU.pow),
                   reads=[b_ms[l % 2], b_mh], writes=[b_rstd[l % 2]])
                op("pool", lambda: POOL.tensor_scalar(out=hb_[:], in0=x_[:], scalar1=rs_[:, 0:1], scalar2=0.0,
                                                      op0=ALU.mult, op1=ALU.add),
                   reads=[bx, b_rstd[l % 2]], writes=[bhb])
                for kc in range(8):
                    op("pe", lambda kc=kc: PE.transpose(out=TRH[:, kc * 128:(kc + 1) * 128],
                                                        in_=hb_[:, kc * 128:(kc + 1) * 128], identity=identb[:]),
                       reads=[bhb, b_const], writes=[b_TRH], sig=(kc == 7))
                op("act", lambda: ACT.copy(out=hT_[:].rearrange("p a b -> p (a b)"), in_=TRH[:]),
                   reads=[b_TRH], writes=[bhT_])

            def stage_P(l):
                hT_, bhT_ = hT[l % 3], b_hT[l % 3]
                pa, bpa = next_pb()
                proj(hT_, bhT_, WK, b_WK, 0, 512, pa, bpa)
                pv, bpv = next_pb()
                proj(hT_, bhT_, WK, b_WK, 512, 512, pv, bpv)
                pc, bpc = next_pb()
                proj(hT_, bhT_, WK, b_WK, 1024, 128, pc, bpc, 0)
                proj(hT_, bhT_, WK, b_WK, 1152, 64, pc, bpc, 128)
                kbanks[l] = (pa, bpa, pv, bpv, pc, bpc)

            def stage_R(l):
                pa, bpa, pv, bpv, pc, bpc = kbanks.pop(l)
                c_, bc = cst[l % 4], b_cst[l % 4]
                kat_, bkat_ = kat[l % 2], b_kat[l % 2]
                kbit_, bkbit_ = kbit[l % 2], b_kbit[l % 2]
                vaa_, bvaa_ = vaa[l % 2], b_vaa[l % 2]
                vba_, bvba_ = vba[l % 2], b_vba[l % 2]
                op("act", lambda: ACT.activation(out=vaa_[:, :, 0:64], in_=pv[:].rearrange("p (h d) -> p h d", h=8),
                                                 func=AF.Copy, scale=vmask[:, l:l + 1]),
                   reads=[bpv, b_const], writes=[bvaa_])
                op("pool", lambda: POOL.tensor_copy(out=vaa_[:, :, 64:65], in_=bc_mid(vmask[:, l:l + 1], 8)),
                   reads=[b_const], writes=[bvaa_])
                dma("act", VA.ap()[:, l, :], vaa_[:].rearrange("p h d -> p (h d)"), reads=[bvaa_], writes=[b_VA[l]], fence=("act", "pool"))
                op("act", lambda: ACT.activation(out=vba_[:, 0:64], in_=pc[:, 64:128], func=AF.Copy,
                                                 scale=vmask[:, l:l + 1]), reads=[bpc, b_const], writes=[bvba_])
                op("pool", lambda: POOL.tensor_copy(out=vba_[:, 64:65], in_=vmask[:, l:l + 1]),
                   reads=[b_const], writes=[bvba_])
                dma("act", VB.ap()[:, l, :], vba_[:], reads=[bvba_], writes=[b_VB[l]], fence=("act", "pool"))
                rope(rbufs, pa[:].rearrange("p (h d) -> p h d", h=8), 8, c_, bc, bpa, kab[:], b_kab)
                zc = AP(pc, 0, [[512, 128], [128, 2], [1, 64]])
                rope(rbufs, zc, 2, c_, bc, bpc, kbi[:], b_kbi)
                for pr in range(4):
                    op("pe", lambda pr=pr: PE.transpose(out=TRO[:, pr * 128:(pr + 1) * 128],
                                                        in_=kab[:].rearrange("p h d -> p (h d)")[:, pr * 128:(pr + 1) * 128],
                                                        identity=identb[:]),
                       reads=[b_kab, b_const], writes=[b_TRO], sig=False)
                for hh in range(2):
                    op("pe", lambda hh=hh: PE.transpose(out=TRO[0:64, 512 + hh * 128:512 + (hh + 1) * 128],
                                                        in_=kbi[:, hh, :], identity=identb[:]),
                       reads=[b_kbi, b_const], writes=[b_TRO], sig=(hh == 1))
                op("act", lambda: ACT.copy(out=kat_[:], in_=TRO[:, 0:512]), reads=[b_TRO], writes=[bkat_])
                op("act", lambda: ACT.copy(out=kbit_[:], in_=TRO[0:64, 512:768]), reads=[b_TRO], writes=[bkbit_])
                dma("act", KAT.ap()[:, l, :], kat_[:], reads=[bkat_], writes=[b_KAT[l]], fence=("act",))
                dma("act", KBT.ap()[:, l * 128:(l + 1) * 128], kbit_[:, 0:128], reads=[bkbit_], writes=[b_KBT[l]])
                dma("act", KIT.ap()[:, l * 128:(l + 1) * 128], kbit_[:, 128:256], reads=[bkbit_], writes=[b_KIT[l]])

            def stage_Q(l):
                m = l // 4
                hT_, bhT_ = hT[l % 3], b_hT[l % 3]
                c_, bc = cst[l % 4], b_cst[l % 4]
                pq, bpq = next_pb()
                proj(hT_, bhT_, WQ, b_WQ, 0, 512, pq, bpq)
                pq2, bpq2 = next_pb()
                proj(hT_, bhT_, WQ, b_WQ, 512, 512, pq2, bpq2)
                pq3, bpq3 = next_pb()
                proj(hT_, bhT_, WQ, b_WQ, 1024, 512, pq3, bpq3)
                pq4, bpq4 = next_pb()
                proj(hT_, bhT_, WQ, b_WQ, 1536, 8, pq4, bpq4)
                op("dve", lambda: DVE.tensor_copy(out=wis[:], in_=pq4[:, 0:8]), reads=[bpq4], writes=[b_wis])
                dma("pool", WI.ap()[m], wis[:], reads=[b_wis], writes=[b_WI[m]], fence=("dve",))
                rope(rbufs, pq[:].rearrange("p (h d) -> p h d", h=8), 8, c_, bc, bpq, qab[:], b_qab)
                for pr in range(4):
                    op("pe", lambda pr=pr: PE.transpose(out=TRO[:, pr * 128:(pr + 1) * 128],
                                                        in_=qab[:].rearrange("p h d -> p (h d)")[:, pr * 128:(pr + 1) * 128],
                                                        identity=identb[:]),
                       reads=[b_qab, b_const], writes=[b_TRO], sig=(pr == 3))
                op("act", lambda: ACT.copy(out=qat[:], in_=TRO[:, 0:512]), reads=[b_TRO], writes=[b_qat])
                dma("act", QAT.ap()[m], qat[:], reads=[b_qat], writes=[b_QAT[m]], fence=("act",))
                for gq in range(4):
                    pg, bpg = next_pb()
                    proj(hT_, bhT_, WQ, b_WQ, 1544 + gq * 512, 512, pg, bpg)
                    op("act", lambda gq=gq, pg=pg: ACT.activation(out=gsb[:, gq * 512:(gq + 1) * 512], in_=pg[:],
                                                                  func=AF.Sigmoid), reads=[bpg], writes=[b_gsb])
                    if gq == 0:
                        qbt_, bqbt_ = qbt[0], b_qbt[0]
                        rope(rbufs, pq2[:].rearrange("p (h d) -> p h d", h=8), 8, c_, bc, bpq2, qab[:], b_qab)
                        for h8 in range(8):
                            ro = 64 * (h8 // 4)
                            op("pe", lambda h8=h8, ro=ro: PE.transpose(
                                out=TRO[ro:ro + 64, (h8 % 4) * 128:(h8 % 4 + 1) * 128], in_=qab[:, h8, :],
                                identity=identb[:]),
                               reads=[b_qab, b_const], writes=[b_TRO], sig=(h8 == 7))
                        op("act", lambda: ACT.copy(out=qbt_[:], in_=TRO[:, 0:512]), reads=[b_TRO], writes=[bqbt_])
                        dma("act", QBT.ap()[m], qbt_[:], reads=[bqbt_], writes=[b_QBT[m]], fence=("act",))
                    if gq == 1:
                        qbt_, bqbt_ = qbt[1], b_qbt[1]
                        op("dve", lambda: DVE.tensor_scalar(out=sgq[:], in0=wis[:], scalar1=0.0, scalar2=0.5,
                                                            op0=ALU.is_ge, op1=ALU.subtract), reads=[b_wis], writes=[b_sgq])
                        op("dve", lambda: DVE.scalar_tensor_tensor(out=absq[:], in0=sgq[:], scalar=2.0, in1=wis[:],
                                                                   op0=ALU.mult, op1=ALU.mult),
                           reads=[b_wis, b_sgq], writes=[b_absq])
                        op("dve", lambda: DVE.tensor_tensor(out=qis[:], in0=pq3[:].rearrange("p (h d) -> p h d", h=8),
                                                            in1=bc_last(absq[:, :], 64), op=ALU.mult),
                           reads=[bpq3, b_absq], writes=[b_qis])
                        rope(rbufs, qis[:], 8, c_, bc, b_qis, qab[:], b_qab)
                        for pr in range(4):
                            op("pe", lambda pr=pr: PE.transpose(
                                out=TRO[:, pr * 128:(pr + 1) * 128],
                                in_=qab[:].rearrange("p h d -> p (h d)")[:, pr * 128:(pr + 1) * 128], identity=identb[:]),
                               reads=[b_qab, b_const], writes=[b_TRO], sig=(pr == 3))
                        op("act", lambda: ACT.copy(out=qbt_[:], in_=TRO[:, 0:512]), reads=[b_TRO], writes=[bqbt_])
                        dma("act", QIT.ap()[m], qbt_[:], reads=[bqbt_], writes=[b_QIT[m]], fence=("act",))
                dma("act", GS.ap()[m], gsb[:], reads=[b_gsb], writes=[b_GS[m]], fence=("act",))

            stage_F(0)
            stage_F(1)
            stage_P(0)
            for l in range(NB):
                if l + 2 < NB:
                    stage_F(l + 2)
                if l % 4 == 3:
                    stage_R(l)
                    stage_Q(l)
                    if l + 1 < NB:
                        stage_P(l + 1)
                else:
                    if l + 1 < NB:
                        stage_P(l + 1)
                    stage_R(l)

        if _DBG == "1":
            kb.finish(b_KAT + b_VA + b_KBT + b_KIT + b_VB + b_QAT + b_QBT + b_QIT + b_WI + b_GS)
            return nc, dbg_outs

        kb.barrier()
        with ExitStack() as st:
            MT = sb(st, "MT", [128, 17 * 128], BF16)
            b_MT = Buf()
            dma("sp", MT[:], mt_d.ap().rearrange("p a b -> p (a b)"), writes=[b_MT])
            kw = [sb(st, f"kw{i}", [128, 17, 512], BF16) for i in range(2)]
            b_kw = [Buf(), Buf()]
            vw = [sb(st, f"vw{i}", [128, 17, 520], BF16) for i in range(2)]
            b_vw = [Buf(), Buf()]
            qa_t = [sb(st, f"qa{i}", [128, 512], BF16) for i in range(2)]
            b_qa = [Buf(), Buf()]
            STb = [ps(st, f"ST{i}", [128, 512], F32) for i in range(3)]
            b_ST = [Buf() for _ in range(3)]
            OA = [ps(st, f"OA{i}", [128, 512], F32) for i in range(4)]
            b_OA = [Buf() for _ in range(4)]
            TR = ps(st, "TR2", [128, 1024], BF16)
            b_TR = Buf()
            E = [sb(st, f"E{i}", [128, 512], BF16) for i in range(3)]
            b_E = [Buf() for _ in range(3)]
            Pm = [sb(st, f"P{i}", [128, 512], BF16) for i in range(3)]
            b_P = [Buf() for _ in range(3)]
            rc = sb(st, "rc", [128, 8], F32)
            b_rc = Buf()
            ya = sb(st, "ya", [128, 8, 64], BF16)
            b_ya = Buf()
            c2 = {"st": 0, "e": 0}
            pending_fin = []

            def fin2(m):
                oa = OA[2 * (m % 2):2 * (m % 2) + 2]
                boa = b_OA[2 * (m % 2):2 * (m % 2) + 2]
                for bnk in range(2):
                    ov = oa[bnk][:, 0:260].rearrange("p (h d) -> p h d", d=65)
                    op("dve", lambda: DVE.reciprocal(out=rc[:, bnk * 4:(bnk + 1) * 4], in_=ov[:, :, 64]),
                       reads=[boa[bnk]], writes=[b_rc])
                    op("dve", lambda: DVE.tensor_tensor(out=ya[:, bnk * 4:(bnk + 1) * 4, :], in0=ov[:, :, 0:64],
                                                        in1=bc_last(rc[:, bnk * 4:(bnk + 1) * 4], 64), op=ALU.mult),
                       reads=[boa[bnk], b_rc], writes=[b_ya])
                ya2 = ya[:].rearrange("p h d -> p (h d)")
                for kc in range(4):
                    op("pe", lambda kc=kc: PE.transpose(out=TR[:, kc * 128:(kc + 1) * 128],
                                                        in_=ya2[:, kc * 128:(kc + 1) * 128], identity=identb[:]),
                       reads=[b_ya, b_const], writes=[b_TR], sig=(kc == 3))
                op("act", lambda: ACT.copy(out=YAT[:, :, m * 128:(m + 1) * 128],
                                           in_=TR[:, 0:512].rearrange("p (a b) -> p a b", b=128)),
                   reads=[b_TR], writes=[b_YAT[m]])

            for m in range(NOWN):
                lq = 4 * m + 3
                lo = max(0, lq - 16)
                nb = lq - lo + 1
                k_, v_, q_ = kw[m % 2], vw[m % 2], qa_t[m % 2]
                bk, bv, bq = b_kw[m % 2], b_vw[m % 2], b_qa[m % 2]
                oa = OA[2 * (m % 2):2 * (m % 2) + 2]
                boa = b_OA[2 * (m % 2):2 * (m % 2) + 2]
                dma("sp", k_[:, 0:nb, :], KAT.ap()[:, lo:lq + 1, :], reads=b_KAT[lo:lq + 1], writes=[bk])
                dma("sp", v_[:, 0:nb, :], VA.ap()[:, lo:lq + 1, :], reads=b_VA[lo:lq + 1], writes=[bv])
                dma("sp", q_[:], QAT.ap()[m], reads=[b_QAT[m]], writes=[bq])
                first_bank = [True, True]
                groups = [list(range(i, min(i + 4, nb))) for i in range(0, nb, 4)]
                units = [(h, gi) for h in range(8) for gi in range(len(groups))]
                nu = len(units)
                slots = {}

                def emit_qk(u):
                    h, gi = units[u]
                    grp = groups[gi]
                    n = len(grp)
                    base = 64 * (h % 2)
                    pair = h // 2
                    k = c2["st"] % 3
                    c2["st"] += 1
                    slots[u] = k
                    for i, dl in enumerate(grp):
                        slot = nb - 1 - dl
                        op("pe", lambda i=i, slot=slot: PE.matmul(
                            STb[k][:, i * 128:(i + 1) * 128],
                            lhsT=k_[base:base + 64, slot, pair * 128:(pair + 1) * 128],
                            rhs=q_[base:base + 64, pair * 128:(pair + 1) * 128], start=True, stop=True),
                           reads=[bk, bq], writes=[b_ST[k]], sig=(i == n - 1))

                for u in range(min(3, nu)):
                    emit_qk(u)
                for u in range(nu):
                    h, gi = units[u]
                    grp = groups[gi]
                    n = len(grp)
                    k = slots[u]
                    ke = c2["e"] % 3
                    c2["e"] += 1
                    e_, be, p_, bp = E[ke], b_E[ke], Pm[ke], b_P[ke]
                    op("act", lambda: ACT.activation(out=e_[:, 0:n * 128], in_=STb[k][:, 0:n * 128], func=AF.Exp,
                                                     scale=0.125), reads=[b_ST[k]], writes=[be])
                    d0 = grp[0]
                    op("dve", lambda: DVE.tensor_tensor(out=p_[:, 0:n * 128], in0=e_[:, 0:n * 128],
                                                        in1=MT[:, d0 * 128:(d0 + n) * 128], op=ALU.mult),
                       reads=[be, b_MT], writes=[bp])
                    for i, dl in enumerate(grp):
                        slot = nb - 1 - dl
                        is_first = first_bank[h // 4]
                        first_bank[h // 4] = False
                        is_last = (h % 4 == 3 and gi == len(groups) - 1 and i == n - 1)
                        op("pe", lambda i=i, slot=slot, is_first=is_first, is_last=is_last: PE.matmul(
                            oa[h // 4][:, (h % 4) * 65:(h % 4) * 65 + 65], lhsT=p_[:, i * 128:(i + 1) * 128],
                            rhs=v_[:, slot, h * 65:(h + 1) * 65], start=is_first, stop=is_last,
                            skip_group_check=True),
                           reads=[bp, bv], writes=[boa[h // 4]], sig=(i == n - 1))
                    if u + 3 < nu:
                        emit_qk(u + 3)
                    if u == min(3, nu - 1) and pending_fin:
                        fin2(pending_fin.pop(0))
                pending_fin.append(m)
            while pending_fin:
                fin2(pending_fin.pop(0))

        if _DBG == "2":
            dy = dbg_out("YAT", [128, 4, NOWN * 128], BF16)
            bo = Buf()
            dma("sp", dy.ap(), YAT[:], reads=b_YAT, writes=[bo])
            kb.finish([bo])
            return nc, dbg_outs

        kb.barrier()
        with ExitStack() as st:
            KIs = sb(st, "KIs", [128, NB * 128], BF16)
            KBs = sb(st, "KBs", [128, NB * 128], BF16)
            VBs = sb(st, "VBs", [128, NB, 65], BF16)
            b_KIs, b_KBs, b_VBs = Buf(), Buf(), Buf()
            dma("sp", KIs[0:64, :], KIT.ap(), reads=b_KIT, writes=[b_KIs])
            dma("sp", KIs[64:128, :], KIT.ap(), reads=b_KIT, writes=[b_KIs])
            dma("sp", KBs[0:64, :], KBT.ap(), reads=b_KBT, writes=[b_KBs])
            dma("sp", KBs[64:128, :], KBT.ap(), reads=b_KBT, writes=[b_KBs])
            dma("sp", VBs[:], VB.ap(), reads=b_VB, writes=[b_VBs])
            padb = sb(st, "padb", [128, 512], F32)
            diagb = sb(st, "diagb", [128, 512], F32)
            pow2 = sb(st, "pow2", [128, NIT + 1], F32)
            b_c3 = Buf()
            dma("sp", padb[:], padbias_d.ap(), writes=[b_c3])
            dma("sp", diagb[:], diagbias_d.ap(), writes=[b_c3])
            dma("sp", pow2[:], pow2_d.ap(), writes=[b_c3])
            qi_t = [sb(st, f"qi{i}", [128, 512], BF16) for i in range(2)]
            qb_t = [sb(st, f"qb{i}", [128, 512], BF16) for i in range(2)]
            wi_t = [sb(st, f"wi{i}", [128, 8], F32) for i in range(2)]
            b_qi, b_qb, b_wi = [Buf(), Buf()], [Buf(), Buf()], [Buf(), Buf()]
            scores = sb(st, "scores", [128, NB * 128], F32)
            b_sc = [Buf() for _ in range(NOWN)]
            junkc = sb(st, "junkc", [128, NB * 128], BF16)
            b_jc = Buf()
            PXP = [ps(st, f"PXP{i}", [128, 1024], F32) for i in range(2)]
            b_PXP = [Buf(), Buf()]
            SC = ps(st, "SC", [128, 512], F32)
            b_SC = Buf()
            OB = [ps(st, f"OB{i}", [128, 512], F32) for i in range(2)]
            b_OB = [Buf(), Buf()]
            TR3 = ps(st, "TR3", [128, 1024], BF16)
            b_TR3 = Buf()
            R2 = [sb(st, f"R2{i}", [128, 1024], BF16) for i in range(3)]
            b_R2 = [Buf() for _ in range(3)]
            selfull = sb(st, "selfull", [128, NB * 128], BF16)
            b_selfull = Buf()
            absw = sb(st, "absw", [128, 8], F32)
            sgh = sb(st, "sgh", [128, 8], F32)
            Dg = sb(st, "Dg", [128, 8, 128], BF16)
            b_absw, b_sgh, b_Dg = Buf(), Buf(), Buf()
            cmin = sb(st, "cmin", [128, NOWN], F32)
            b_cmin = Buf()
            rmin = sb(st, "rmin", [128, 1], F32)
            rmax = sb(st, "rmax", [128, 1], F32)
            rng = sb(st, "rng", [128, 1], F32)
            tsum = sb(st, "tsum", [128, 1], F32)
            tau = [sb(st, f"tau{i}", [128, 1], F32) for i in range(2)]
            cntt = sb(st, "cntt", [128, 1], F32)
            sg = sb(st, "sg", [128, 1], F32)
            step2 = sb(st, "step2", [128, NIT + 1], F32)
            tsel = sb(st, "tsel", [128, 1], F32)
            b_rmin, b_rmax, b_rng, b_tsum, b_cnt, b_sg, b_step2, b_tsel = (Buf() for _ in range(8))
            b_tau = [Buf(), Buf()]
            sel = [sb(st, f"sel{i}", [128, 512], BF16) for i in range(2)]
            selT = [sb(st, f"selT{i}", [128, 512], BF16) for i in range(3)]
            b_sel, b_selT = [Buf(), Buf()], [Buf(), Buf(), Buf()]
            E2 = [sb(st, f"E2{i}", [128, 1024], BF16) for i in range(2)]
            b_E2 = [Buf(), Buf()]
            P2 = [sb(st, f"P2{i}", [128, 1024], BF16) for i in range(2)]
            b_P2 = [Buf(), Buf()]
            rcb = sb(st, "rcb", [128, 8], F32)
            b_rcb = Buf()
            yb = sb(st, "yb", [128, 8, 64], BF16)
            b_yb = Buf()
            scb = [scores, sb(st, "scores1", [128, NB * 128], F32)]
            b_scb = [b_sc, [Buf() for _ in range(NOWN)]]
            tselb = [tsel, sb(st, "tsel1", [128, 1], F32)]
            b_tselb = [b_tsel, Buf()]
            ctr = {"px": 0, "kr": 0, "ke": 0}

            def stage_S(m):
                nch = m + 1
                qi_, wi_ = qi_t[m % 2], wi_t[m % 2]
                bqi, bwi = b_qi[m % 2], b_wi[m % 2]
                sc_, bsc_ = scb[m % 2], b_scb[m % 2]
                dma("sp", qi_[:], QIT.ap()[m], reads=[b_QIT[m]], writes=[bqi])
                dma("sp", wi_[:], WI.ap()[m], reads=[b_WI[m]], writes=[bwi])
                op("dve", lambda: DVE.tensor_scalar(out=sgh[:], in0=wi_[:], scalar1=0.0, scalar2=0.5,
                                                    op0=ALU.is_ge, op1=ALU.subtract), reads=[bwi], writes=[b_sgh])
                for h in range(8):
                    op("dve", lambda h=h: DVE.tensor_scalar(out=Dg[:, h, :], in0=identb[:], scalar1=sgh[:, h:h + 1],
                                                            scalar2=2.0, op0=ALU.mult, op1=ALU.mult),
                       reads=[b_sgh, b_const], writes=[b_Dg])
                n = 4 * nch
                slots = {}

                def emit_d(j):
                    c, p = divmod(j, 4)
                    k = ctr["px"] % 2
                    ctr["px"] += 1
                    slots[j] = k
                    for half in range(2):
                        rows = slice(64 * half, 64 * half + 64)
                        op("pe", lambda: PE.matmul(PXP[k][:, half * 512:(half + 1) * 512],
                                                   lhsT=qi_[rows, p * 128:(p + 1) * 128],
                                                   rhs=KIs[rows, c * 512:(c + 1) * 512], start=True, stop=True),
                           reads=[bqi, b_KIs], writes=[b_PXP[k]], sig=(half == 1))

                for j in range(min(2, n)):
                    emit_d(j)
                pend_evac = None
                for j in range(n):
                    c, p = divmod(j, 4)
                    k = slots[j]
                    r_, br = R2[ctr["kr"] % 3], b_R2[ctr["kr"] % 3]
                    ctr["kr"] += 1
                    op("act", lambda: ACT.activation(out=r_[:], in_=PXP[k][:], func=AF.Relu),
                       reads=[b_PXP[k]], writes=[br])
                    if pend_evac is not None:
                        cc = pend_evac
                        pend_evac = None
                        op("act", lambda: ACT.copy(out=sc_[:, cc * 512:(cc + 1) * 512], in_=SC[:]),
                           reads=[b_SC], writes=[bsc_[cc]])
                    for half in range(2):
                        h = 2 * p + half
                        op("pe", lambda: PE.matmul(SC[:], lhsT=Dg[:, h, :], rhs=r_[:, half * 512:(half + 1) * 512],
                                                   start=(h == 0), stop=(h == 7)),
                           reads=[br, b_Dg], writes=[b_SC], sig=(half == 1))
                    if j + 2 < n:
                        emit_d(j + 2)
                    if p == 3:
                        pend_evac = c
                if pend_evac is not None:
                    cc = pend_evac
                    op("act", lambda: ACT.copy(out=sc_[:, cc * 512:(cc + 1) * 512], in_=SC[:]),
                       reads=[b_SC], writes=[bsc_[cc]])

            def stage_B(m):
                nch = m + 1
                S = 512 * nch
                sc_, bsc_ = scb[m % 2], b_scb[m % 2]
                bsc = bsc_[0:nch]
                op("dve", lambda: DVE.tensor_reduce(out=rmin[:], in_=sc_[:, 0:S], axis=AX.X, op=ALU.min),
                   reads=bsc, writes=[b_rmin])
                op("dve", lambda: DVE.tensor_tensor(out=sc_[:, 0:512], in0=sc_[:, 0:512], in1=padb[:], op=ALU.add),
                   reads=[b_c3, bsc_[0]], writes=[bsc_[0]])
                op("dve", lambda: DVE.tensor_tensor(out=sc_[:, S - 512:S], in0=sc_[:, S - 512:S], in1=diagb[:], op=ALU.add),
                   reads=[b_c3, bsc_[nch - 1]], writes=[bsc_[nch - 1]])
                op("dve", lambda: DVE.tensor_reduce(out=rmax[:], in_=sc_[:, 0:S], axis=AX.X, op=ALU.max),
                   reads=bsc, writes=[b_rmax])
                op("dve", lambda: DVE.scalar_tensor_tensor(out=rng[:], in0=rmax[:], scalar=2.0, in1=rmin[:],
                                                           op0=ALU.add, op1=ALU.subtract),
                   reads=[b_rmax, b_rmin], writes=[b_rng])
                op("dve", lambda: DVE.tensor_tensor(out=tsum[:], in0=rmax[:], in1=rmin[:], op=ALU.add),
                   reads=[b_rmax, b_rmin], writes=[b_tsum])
                op("dve", lambda: DVE.tensor_scalar(out=tau[0][:], in0=tsum[:], scalar1=0.5, scalar2=None, op0=ALU.mult),
                   reads=[b_tsum], writes=[b_tau[0]])
                op("dve", lambda: DVE.tensor_scalar(out=step2[:], in0=pow2[:], scalar1=rng[:, 0:1], scalar2=None,
                                                    op0=ALU.mult), reads=[b_rng, b_c3], writes=[b_step2])
                for it in range(NIT):
                    tc_, tn_ = tau[it % 2], tau[(it + 1) % 2]
                    btc, btn = b_tau[it % 2], b_tau[(it + 1) % 2]
                    op("dve", lambda: DVE.tensor_scalar(out=junkc[:, 0:S], in0=sc_[:, 0:S], scalar1=tc_[:, 0:1],
                                                        scalar2=None, op0=ALU.is_ge, op1=ALU.add, accum_out=cntt[:]),
                       reads=bsc + [btc], writes=[b_jc, b_cnt])
                    op("dve", lambda: DVE.tensor_scalar(out=sg[:], in0=cntt[:], scalar1=255.5, scalar2=0.5,
                                                        op0=ALU.is_ge, op1=ALU.subtract), reads=[b_cnt], writes=[b_sg])
                    op("dve", lambda: DVE.scalar_tensor_tensor(out=tn_[:], in0=sg[:], scalar=step2[:, it:it + 1],
                                                               in1=tc_[:], op0=ALU.mult, op1=ALU.add),
                       reads=[b_sg, b_step2, btc], writes=[btn])
                tf_, btf = tau[NIT % 2], b_tau[NIT % 2]
                op("dve", lambda: DVE.tensor_tensor(out=tsel[:], in0=tf_[:], in1=step2[:, NIT:NIT + 1], op=ALU.subtract),
                   reads=[btf, b_step2], writes=[b_tsel])
                op("dve", lambda: DVE.tensor_scalar(out=selfull[:, 0:S], in0=sc_[:, 0:S], scalar1=tsel[:, 0:1],
                                                    scalar2=None, op0=ALU.is_ge),
                   reads=bsc + [b_tsel], writes=[b_selfull])

            def stage_A(m):
                nch = m + 1
                qb_, bqb = qb_t[m % 2], b_qb[m % 2]
                dma("sp", qb_[:], QBT.ap()[m], reads=[b_QBT[m]], writes=[bqb])
                first_ob = [True, True]
                n = 4 * nch
                slots = {}

                def prep(c):
                    sT_, bsT_ = selT[c % 3], b_selT[c % 3]
                    for kq in range(4):
                        op("pe", lambda kq=kq: PE.transpose(out=TR3[:, kq * 128:(kq + 1) * 128],
                                                            in_=selfull[:, c * 512 + kq * 128:c * 512 + (kq + 1) * 128],
                                                            identity=identb[:]),
                           reads=[b_selfull, b_const], writes=[b_TR3], sig=(kq == 3))
                    op("act", lambda: ACT.copy(out=sT_[:], in_=TR3[:, 0:512]), reads=[b_TR3], writes=[bsT_])

                def emit_st(u):
                    c, kq = divmod(u, 4)
                    if kq == 0 and c + 1 < nch:
                        prep(c + 1)
                    ls = 4 * c + kq
                    k = ctr["px"] % 2
                    ctr["px"] += 1
                    slots[u] = k
                    for half in range(2):
                        rows = slice(64 * half, 64 * half + 64)
                        op("pe", lambda: PE.matmul(
                            PXP[k][:, half * 512:(half + 1) * 512].rearrange("p (a b) -> p a b", b=128),
                            lhsT=KBs[rows, ls * 128:(ls + 1) * 128],
                            rhs=qb_[rows, :].rearrange("p (a b) -> p a b", b=128), start=True, stop=True),
                           reads=[b_KBs, bqb], writes=[b_PXP[k]], sig=(half == 1))

                prep(0)
                for u in range(min(2, n)):
                    emit_st(u)
                for u in range(n):
                    c, kq = divmod(u, 4)
                    ls = 4 * c + kq
                    k = slots[u]
                    ke = ctr["ke"] % 2
                    ctr["ke"] += 1
                    e_, be, p_, bp = E2[ke], b_E2[ke], P2[ke], b_P2[ke]
                    sT_, bsT_ = selT[c % 3], b_selT[c % 3]
                    op("act", lambda: ACT.activation(out=e_[:], in_=PXP[k][:], func=AF.Exp, scale=0.125),
                       reads=[b_PXP[k]], writes=[be])
                    op("pool", lambda: POOL.tensor_tensor(out=p_[:].rearrange("p (a b) -> p a b", b=128),
                                                          in0=e_[:].rearrange("p (a b) -> p a b", b=128),
                                                          in1=bc_mid(sT_[:, kq * 128:(kq + 1) * 128], 8), op=ALU.mult),
                       reads=[be, bsT_], writes=[bp])
                    for hh in range(8):
                        i = hh // 4
                        is_first = first_ob[i]
                        first_ob[i] = False
                        is_last = (u == n - 1 and hh % 4 == 3)
                        op("pe", lambda hh=hh, i=i, is_first=is_first, is_last=is_last: PE.matmul(
                            OB[i][:, (hh % 4) * 65:(hh % 4) * 65 + 65], lhsT=p_[:, hh * 128:(hh + 1) * 128],
                            rhs=VBs[:, ls, :], start=is_first, stop=is_last, skip_group_check=True),
                           reads=[bp, b_VBs], writes=[b_OB[i]], sig=(hh % 4 == 3))
                    if u + 2 < n:
                        emit_st(u + 2)

            def stage_norm(m):
                for bnk in range(2):
                    ov = OB[bnk][:, 0:260].rearrange("p (h d) -> p h d", d=65)
                    op("dve", lambda: DVE.reciprocal(out=rcb[:, bnk * 4:(bnk + 1) * 4], in_=ov[:, :, 64]),
                       reads=[b_OB[bnk]], writes=[b_rcb])
                    op("dve", lambda: DVE.tensor_tensor(out=yb[:, bnk * 4:(bnk + 1) * 4, :], in0=ov[:, :, 0:64],
                                                        in1=bc_last(rcb[:, bnk * 4:(bnk + 1) * 4], 64), op=ALU.mult),
                       reads=[b_OB[bnk], b_rcb], writes=[b_yb])

            def stage_fin(m):
                yb2 = yb[:].rearrange("p h d -> p (h d)")
                for kc in range(4):
                    op("pe", lambda kc=kc: PE.transpose(out=TR3[:, kc * 128:(kc + 1) * 128],
                                                        in_=yb2[:, kc * 128:(kc + 1) * 128], identity=identb[:]),
                       reads=[b_yb, b_const], writes=[b_TR3], sig=(kc == 3))
                op("act", lambda: ACT.copy(out=YBT[:, :, m * 128:(m + 1) * 128],
                                           in_=TR3[:, 0:512].rearrange("p (a b) -> p a b", b=128)),
                   reads=[b_TR3], writes=[b_YBT[m]])

            stage_S(0)
            for m in range(NOWN):
                if m + 1 < NOWN:
                    stage_S(m + 1)
                stage_B(m)
                if m >= 1:
                    stage_norm(m - 1)
                    stage_fin(m - 1)
                stage_A(m)
            stage_norm(NOWN - 1)
            stage_fin(NOWN - 1)

        if _DBG == "3":
            dy = dbg_out("YBT", [128, 4, NOWN * 128], BF16)
            bo = Buf()
            dma("sp", dy.ap(), YBT[:], reads=b_YBT, writes=[bo])
            kb.finish([bo])
            return nc, dbg_outs

        kb.barrier()
        with ExitStack() as st45:
            H2T = sb(st45, "H2T", [128, 8, NOWN * 128], BF16)
            b_H2T = [Buf() for _ in range(NOWN)]
            ss = sb(st45, "ss2", [128, 1], F32)
            ms = sb(st45, "ms2", [128, 1], F32)
            rstd = sb(st45, "rstd2", [128, 1], F32)
            mhalf = sb(st45, "mhalf2", [128, 1], F32)
            b_ss, b_ms, b_rstd, b_mh = Buf(), Buf(), Buf(), Buf()
            op("pool", lambda: POOL.memset(mhalf[:], -0.5), writes=[b_mh])
            junk = sb(st45, "junk2", [128, D], BF16)
            b_junk = Buf()
            with ExitStack() as st:
                WUA = sb(st, "WUA", [128, 4, D], BF16)
                WUB = sb(st, "WUB", [128, 4, D], BF16)
                WO = sb(st, "WO", [128, 8, D], BF16)
                wst2 = sb(st, "wst2", [128, 8, D], F32)
                b_w2, b_WUA, b_WUB, b_WO = Buf(), Buf(), Buf(), Buf()
                dma("sp", wst2[:, 0:4, :], w_up_a.ap().rearrange("(kc p) n -> p kc n", p=128), writes=[b_w2])
                op("pool", lambda: POOL.tensor_copy(out=WUA[:], in_=wst2[:, 0:4, :]), reads=[b_w2], writes=[b_WUA])
                dma("sp", wst2[:, 0:4, :], w_up_b.ap().rearrange("(kc p) n -> p kc n", p=128), writes=[b_w2])
                op("pool", lambda: POOL.tensor_copy(out=WUB[:], in_=wst2[:, 0:4, :]), reads=[b_w2], writes=[b_WUB])
                dma("sp", wst2[:], w_out.ap().rearrange("(kc p) n -> p kc n", p=128), writes=[b_w2])
                op("pool", lambda: POOL.tensor_copy(out=WO[:], in_=wst2[:]), reads=[b_w2], writes=[b_WO])
                gs_t = [sb(st, f"gs{i}", [128, 2048], BF16) for i in range(2)]
                x_t = [sb(st, f"x4{i}", [128, D], F32) for i in range(2)]
                b_gs, b_x4 = [Buf(), Buf()], [Buf(), Buf()]
                UA = [ps(st, f"UA{i}", [128, 512], F32) for i in range(2)]
                UB = [ps(st, f"UB{i}", [128, 512], F32) for i in range(2)]
                WOp = [ps(st, f"WOp{i}", [128, 512], F32) for i in range(2)]
                TR4a = ps(st, "TR4a", [128, 1024], BF16)
                TR4b = ps(st, "TR4b", [128, 1024], BF16)
                b_UA, b_UB, b_WOp = [Buf(), Buf()], [Buf(), Buf()], [Buf(), Buf()]
                b_TR4a, b_TR4b = Buf(), Buf()
                t1 = [sb(st, f"t1{i}", [128, 512], F32) for i in range(2)]
                t2 = [sb(st, f"t2{i}", [128, 512], F32) for i in range(2)]
                b_t1, b_t2 = [Buf(), Buf()], [Buf(), Buf()]
                mg = [sb(st, f"mg{i}", [128, D], BF16) for i in range(2)]
                mgT = [sb(st, f"mgT{i}", [128, 8, 128], BF16) for i in range(2)]
                b_mg, b_mgT = [Buf(), Buf()], [Buf(), Buf()]
                x2 = [sb(st, f"x2{i}", [128, D], F32) for i in range(2)]
                b_x2 = [Buf(), Buf()]
                h2b = [sb(st, f"h2b{i}", [128, D], BF16) for i in range(2)]
                b_h2b = [Buf(), Buf()]
                ss4 = [ss, sb(st, "ss4", [128, 1], F32)]
                ms4 = [ms, sb(st, "ms4", [128, 1], F32)]
                rs4 = [rstd, sb(st, "rs4", [128, 1], F32)]
                b_ss4, b_ms4, b_rs4 = [b_ss, Buf()], [b_ms, Buf()], [b_rstd, Buf()]

                def stage_4A(m):
                    g_, bg_ = gs_t[m % 2], b_gs[m % 2]
                    x_, bx_ = x_t[m % 2], b_x4[m % 2]
                    mg_, bmg_ = mg[m % 2], b_mg[m % 2]
                    mgT_, bmgT_ = mgT[m % 2], b_mgT[m % 2]
                    dma("sp", g_[:], GS.ap()[m], reads=[b_GS[m]], writes=[bg_])
                    dma("sp", x_[:], xl.ap()[4 * m + 3], writes=[bx_])
                    tk = slice(m * 128, (m + 1) * 128)
                    for half in range(2):
                        cs_ = slice(half * 512, (half + 1) * 512)
                        for kc in range(4):
                            op("pe", lambda kc=kc: PE.matmul(UA[half][:], lhsT=YAT[:, kc, tk], rhs=WUA[:, kc, cs_],
                                                             start=(kc == 0), stop=(kc == 3)),
                               reads=[b_YAT[m], b_WUA], writes=[b_UA[half]], sig=(kc == 3))
                        for kc in range(4):
                            op("pe", lambda kc=kc: PE.matmul(UB[half][:], lhsT=YBT[:, kc, tk], rhs=WUB[:, kc, cs_],
                                                             start=(kc == 0), stop=(kc == 3)),
                               reads=[b_YBT[m], b_WUB], writes=[b_UB[half]], sig=(kc == 3))
                        op("dve", lambda: DVE.tensor_tensor(out=t1[half][:], in0=UA[half][:], in1=g_[:, cs_], op=ALU.mult),
                           reads=[b_UA[half], bg_], writes=[b_t1[half]])
                        op("dve", lambda: DVE.tensor_tensor(out=t2[half][:], in0=UB[half][:],
                                                            in1=g_[:, 1024 + half * 512:1024 + (half + 1) * 512], op=ALU.mult),
                           reads=[b_UB[half], bg_], writes=[b_t2[half]])
                        op("pool", lambda: POOL.tensor_tensor(out=mg_[:, cs_], in0=t1[half][:], in1=t2[half][:], op=ALU.add),
                           reads=[b_t1[half], b_t2[half]], writes=[bmg_])
                    for kc in range(8):
                        op("pe", lambda kc=kc: PE.transpose(out=TR4a[:, kc * 128:(kc + 1) * 128],
                                                            in_=mg_[:, kc * 128:(kc + 1) * 128], identity=identb[:]),
                           reads=[bmg_, b_const], writes=[b_TR4a], sig=(kc == 7))
                    op("act", lambda: ACT.copy(out=mgT_[:].rearrange("p a b -> p (a b)"), in_=TR4a[:]),
                       reads=[b_TR4a], writes=[bmgT_])

                def stage_4B(m):
                    x_, bx_ = x_t[m % 2], b_x4[m % 2]
                    mgT_, bmgT_ = mgT[m % 2], b_mgT[m % 2]
                    x2_, bx2_ = x2[m % 2], b_x2[m % 2]
                    h2_, bh2_ = h2b[m % 2], b_h2b[m % 2]
                    ss_, ms_, rs_ = ss4[m % 2], ms4[m % 2], rs4[m % 2]
                    bss_, bms_, brs_ = b_ss4[m % 2], b_ms4[m % 2], b_rs4[m % 2]
                    tk = slice(m * 128, (m + 1) * 128)
                    for half in range(2):
                        cs_ = slice(half * 512, (half + 1) * 512)
                        for kc in range(8):
                            op("pe", lambda kc=kc: PE.matmul(WOp[half][:], lhsT=mgT_[:, kc, :], rhs=WO[:, kc, cs_],
                                                             start=(kc == 0), stop=(kc == 7)),
                               reads=[bmgT_, b_WO], writes=[b_WOp[half]], sig=(kc == 7))
                        op("dve", lambda: DVE.tensor_tensor(out=x2_[:, cs_], in0=WOp[half][:], in1=x_[:, cs_], op=ALU.add),
                           reads=[b_WOp[half], bx_], writes=[bx2_])
                    dma("pool", X2.ap()[m], x2_[:], reads=[bx2_], writes=[b_X2[m]], fence=("dve",))
                    op("act", lambda: ACT.activation(out=junk[:], in_=x2_[:], func=AF.Square, accum_out=ss_[:]),
                       reads=[bx2_], writes=[b_junk, bss_])
                    op("dve", lambda: DVE.tensor_scalar(out=ms_[:], in0=ss_[:], scalar1=1.0 / D, scalar2=EPS,
                                                        op0=ALU.mult, op1=ALU.add), reads=[bss_], writes=[bms_])
                    op("pool", lambda: POOL.tensor_tensor(out=rs_[:], in0=ms_[:], in1=mhalf[:], op=ALU.pow),
                       reads=[bms_, b_mh], writes=[brs_])
                    op("pool", lambda: POOL.tensor_scalar(out=h2_[:], in0=x2_[:], scalar1=rs_[:, 0:1], scalar2=0.0,
                                                          op0=ALU.mult, op1=ALU.add),
                       reads=[bx2_, brs_], writes=[bh2_])
                    for kc in range(8):
                        op("pe", lambda kc=kc: PE.transpose(out=TR4b[:, kc * 128:(kc + 1) * 128],
                                                            in_=h2_[:, kc * 128:(kc + 1) * 128], identity=identb[:]),
                           reads=[bh2_, b_const], writes=[b_TR4b], sig=(kc == 7))
                    op("act", lambda: ACT.copy(out=H2T[:, :, tk], in_=TR4b[:].rearrange("p (a b) -> p a b", b=128)),
                       reads=[b_TR4b], writes=[b_H2T[m]])

                stage_4A(0)
                for m in range(NOWN):
                    if m + 1 < NOWN:
                        stage_4A(m + 1)
                    stage_4B(m)

            if _DBG == "4":
                dx = dbg_out("X2o", [NOWN, 128, D], F32)
                bo = Buf()
                with ExitStack() as st:
                    tt = sb(st, "dbgt", [128, D], F32)
                    bt = Buf()
                    for m in range(NOWN):
                        dma("sp", tt[:], X2.ap()[m], reads=[b_X2[m]], writes=[bt])
                        dma("sp", dx.ap()[m], tt[:], reads=[bt], writes=[bo])
                    kb.finish([bo])
                return nc, dbg_outs

            kb.barrier()
            with ExitStack() as st:
                gffn = sb(st, "gffn", [128, 8], F32)
                gfin = sb(st, "gfin", [128, D], F32)
                b_g5 = Buf()
                dma("sp", gffn[:], gffn_d.ap(), writes=[b_g5])
                dma("sp", gfin[:], gfin_d.ap(), writes=[b_g5])
                HT = NOWN * 128 // 2
                ACTT = sb(st, "ACTT", [128, NFF, HT], BF16)
                b_ACTT = [Buf(), Buf()]
                WD = sb(st, "WD", [128, NFF, D], BF16)
                b_WD = [Buf() for _ in range(NFF)]
                wdst = [sb(st, "wdst0", [128, D], F32)] * 2
                b_wdst = [Buf()] * 2
                wgst = [sb(st, f"wgst{i}", [128, 8, 128], F32) for i in range(2)]
                wust = [sb(st, f"wust{i}", [128, 8, 128], F32) for i in range(2)]
                wgb = [sb(st, f"wgb{i}", [128, 8, 128], BF16) for i in range(2)]
                wub = [sb(st, f"wub{i}", [128, 8, 128], BF16) for i in range(2)]
                b_wgst, b_wust, b_wgb, b_wub = ([Buf(), Buf()] for _ in range(4))
                PS5 = [ps(st, f"PS5{i}", [128, 512], F32) for i in range(8)]
                b_PS5 = [Buf() for _ in range(8)]
                sgt = [sb(st, f"sgt{i}", [128, 512], F32) for i in range(2)]
                b_sgt = [Buf(), Buf()]
                x2t = [sb(st, f"x2t{i}", [128, D], F32) for i in range(2)]
                x3 = x2t
                ot = [sb(st, "ot0", [128, D], F32)] * 2
                b_x2t = [Buf(), Buf()]
                b_x3 = b_x2t
                b_ot = [Buf()] * 2
                wgv = w_gate.ap().rearrange("(kc p) n -> p kc n", p=128)
                wuv = w_up.ap().rearrange("(kc p) n -> p kc n", p=128)

                def load_wd(ffc):
                    i2 = ffc % 2
                    dma("sp", wdst[i2][:], w_down.ap()[ffc * 128:(ffc + 1) * 128, :], writes=[b_wdst[i2]])
                    op("pool", lambda: POOL.tensor_copy(out=WD[:, ffc, :], in_=wdst[i2][:]),
                       reads=[b_wdst[i2]], writes=[b_WD[ffc]])

                kk = 0
                kw5 = 0
                for hf in range(2):
                    tok0 = hf * HT
                    for ffc in range(NFF):
                        i2 = kw5 % 2
                        kw5 += 1
                        dma("sp", wgst[i2][:], wgv[:, :, ffc * 128:(ffc + 1) * 128], writes=[b_wgst[i2]])
                        dma("sp", wust[i2][:], wuv[:, :, ffc * 128:(ffc + 1) * 128], writes=[b_wust[i2]])
                        op("pool", lambda: POOL.tensor_tensor(out=wgb[i2][:], in0=wgst[i2][:], in1=bc_last(gffn[:, :], 128),
                                                              op=ALU.mult), reads=[b_wgst[i2], b_g5], writes=[b_wgb[i2]])
                        op("pool", lambda: POOL.tensor_tensor(out=wub[i2][:], in0=wust[i2][:], in1=bc_last(gffn[:, :], 128),
                                                              op=ALU.mult), reads=[b_wust[i2], b_g5], writes=[b_wub[i2]])
                        if hf == 0:
                            load_wd(ffc)
                        for tg2 in range(2):
                            ts_ = slice(tok0 + tg2 * 512, tok0 + (tg2 + 1) * 512)
                            la = slice(tg2 * 512, (tg2 + 1) * 512)
                            gi, ui = kk % 2, 2 + kk % 2
                            s_, bs_ = sgt[kk % 2], b_sgt[kk % 2]
                            kk += 1
                            hbufs = b_H2T[(tok0 // 128) + tg2 * 4:(tok0 // 128) + tg2 * 4 + 4]
                            for kc in range(8):
                                op("pe", lambda kc=kc: PE.matmul(PS5[gi][:], lhsT=wgb[i2][:, kc, :], rhs=H2T[:, kc, ts_],
                                                                 start=(kc == 0), stop=(kc == 7)),
                                   reads=[b_wgb[i2]] + hbufs, writes=[b_PS5[gi]], sig=(kc == 7))
                            for kc in range(8):
                                op("pe", lambda kc=kc: PE.matmul(PS5[ui][:], lhsT=wub[i2][:, kc, :], rhs=H2T[:, kc, ts_],
                                                                 start=(kc == 0), stop=(kc == 7)),
                                   reads=[b_wub[i2]] + hbufs, writes=[b_PS5[ui]], sig=(kc == 7))
                            op("act", lambda: ACT.activation(out=s_[:], in_=PS5[gi][:], func=AF.Silu),
                               reads=[b_PS5[gi]], writes=[bs_])
                            op("dve", lambda: DVE.tensor_tensor(out=ACTT[:, ffc, la], in0=PS5[ui][:], in1=s_[:], op=ALU.mult),
                               reads=[b_PS5[ui], bs_], writes=[b_ACTT[tg2]])
                    for tg2 in range(2):
                        for ffc in range(NFF):
                            for tb in range(4):
                                lt = slice((tg2 * 4 + tb) * 128, (tg2 * 4 + tb + 1) * 128)
                                for half in range(2):
                                    op("pe", lambda tb=tb, half=half, lt=lt: PE.matmul(
                                        PS5[tb * 2 + half][:], lhsT=ACTT[:, ffc, lt],
                                        rhs=WD[:, ffc, half * 512:(half + 1) * 512], start=(ffc == 0), stop=(ffc == NFF - 1)),
                                       reads=[b_WD[ffc], b_ACTT[tg2]], writes=[b_PS5[tb * 2 + half]],
                                       sig=(ffc == NFF - 1))
                        for tb in range(4):
                            mm = hf * 8 + tg2 * 4 + tb
                            xx, bxx = x2t[mm % 2], b_x2t[mm % 2]
                            x3_, bx3 = x3[mm % 2], b_x3[mm % 2]
                            o_, bo_ = ot[mm % 2], b_ot[mm % 2]
                            dma("sp", xx[:], X2.ap()[mm], reads=[b_X2[mm]], writes=[bxx])
                            for half in range(2):
                                cs_ = slice(half * 512, (half + 1) * 512)
                                op("dve", lambda: DVE.tensor_tensor(out=x3_[:, cs_], in0=PS5[tb * 2 + half][:], in1=xx[:, cs_],
                                                                    op=ALU.add),
                                   reads=[b_PS5[tb * 2 + half], bxx], writes=[bx3])
                            op("act", lambda: ACT.activation(out=junk[:], in_=x3_[:], func=AF.Square, accum_out=ss[:]),
                               reads=[bx3], writes=[b_junk, b_ss])
                            op("dve", lambda: DVE.tensor_scalar(out=ms[:], in0=ss[:], scalar1=1.0 / D, scalar2=EPS,
                                                                op0=ALU.mult, op1=ALU.add), reads=[b_ss], writes=[b_ms])
                            op("pool", lambda: POOL.tensor_tensor(out=rstd[:], in0=ms[:], in1=mhalf[:], op=ALU.pow),
                               reads=[b_ms, b_mh], writes=[b_rstd])
                            op("dve", lambda: DVE.scalar_tensor_tensor(out=o_[:], in0=x3_[:], scalar=rstd[:, 0:1], in1=gfin[:],
                                                                       op0=ALU.mult, op1=ALU.mult),
                               reads=[bx3, b_rstd, b_g5], writes=[bo_])
                            dma("sp", out_d.ap()[mm], o_[:], reads=[bo_], writes=[b_out[mm]], fence=("dve",))

        kb.finish(b_out)
    return nc, dbg_outs


def host_prep(x, norm_mix, w_in, w_up_a, w_up_b, w_out, norm_ffn, w_gate, w_up, w_down, norm_final):
    B, T, _ = x.shape
    x = np.asarray(x, np.float32)
    half = 32
    inv_freq = (10000.0 ** (-np.arange(half, dtype=np.float32) / half)).astype(np.float32)
    identb = np.eye(128, dtype=np.float32).astype(ml_dtypes.bfloat16)
    s_i = np.arange(128)[:, None]
    t_i = np.arange(128)[None, :]
    mt = np.zeros((128, 17, 128), np.float32)
    for dl in range(17):
        diff = 128 * dl + t_i - s_i
        tot = np.zeros((128, 128), np.float32)
        for (wdw, dil) in ((128, 1), (512, 4), (2048, 16)):
            ok = (diff >= 0) & (diff <= wdw) & (diff % dil == 0)
            tot += ok.astype(np.float32)
        mt[:, dl, :] = tot
    mt = mt.astype(ml_dtypes.bfloat16)
    diagbias = np.zeros((128, 512), np.float32)
    diagbias[:, 384:512] = np.where(np.arange(128)[None, :] > np.arange(128)[:, None], -BIG, 0.0)
    pow2 = np.broadcast_to((2.0 ** (-(np.arange(NIT + 1) + 1.0))).astype(np.float32)[None, :], (128, NIT + 1)).copy()
    gmix = np.ascontiguousarray(np.asarray(norm_mix, np.float32).reshape(8, 128).T)
    gffn = np.ascontiguousarray(np.asarray(norm_ffn, np.float32).reshape(8, 128).T)
    gfin = np.ascontiguousarray(np.broadcast_to(np.asarray(norm_final, np.float32)[None, :], (128, D)))
    common = {
        "identb": identb, "mt": mt, "diagbias": diagbias, "pow2": pow2, "gmix": gmix, "gffn": gffn, "gfin": gfin,
        "w_in": np.ascontiguousarray(np.asarray(w_in, np.float32)[0]),
        "w_up_a": np.ascontiguousarray(np.asarray(w_up_a, np.float32)[0]),
        "w_up_b": np.ascontiguousarray(np.asarray(w_up_b, np.float32)[0]),
        "w_out": np.ascontiguousarray(np.asarray(w_out, np.float32)[0]),
        "w_gate": np.ascontiguousarray(np.asarray(w_gate, np.float32)[0]),
        "w_up": np.ascontiguousarray(np.asarray(w_up, np.float32)[0]),
        "w_down": np.ascontiguousarray(np.asarray(w_down, np.float32)[0]),
    }
    in_maps = []
    for core in range(8):
        b, j = core // 4, core % 4
        xl = np.zeros((NB, 128, D), np.float32)
        pos = np.zeros((NB, 128), np.float32)
        valid = np.zeros((NB,), np.float32)
        for l in range(NB):
            g = l + j - 3
            if g >= 0:
                xl[l] = x[b, g * 128:(g + 1) * 128]
                pos[l] = np.arange(g * 128, (g + 1) * 128, dtype=np.float32)
                valid[l] = 1.0
        ang = pos[:, :, None] * inv_freq[None, None, :]
        cs = np.concatenate([np.cos(ang), np.sin(ang)], axis=-1).astype(np.float32)
        vmask = np.ascontiguousarray(np.broadcast_to(valid[None, :], (128, NB))).astype(np.float32)
        padbias = np.zeros((128, 512), np.float32)
        for l in range(4):
            if valid[l] == 0.0:
                padbias[:, l * 128:(l + 1) * 128] = -BIG
        m = dict(common)
        m.update({"xl": xl, "cs": cs, "vmask": vmask, "padbias": padbias})
        in_maps.append(m)
    return in_maps


def kernel(x, norm_mix, w_in, w_up_a, w_up_b, w_out, norm_ffn, w_gate, w_up, w_down, norm_final):
    in_maps = host_prep(x, norm_mix, w_in, w_up_a, w_up_b, w_out, norm_ffn, w_gate, w_up, w_down, norm_final)
    nc, dbg = build()
    if _DBG:
        res = run_bass_kernel_spmd(nc, in_maps, core_ids=list(range(8)), trace=bool(os.environ.get("KTRACE")))
        print("DBG exec_time_ns", res.exec_time_ns)
        return res
    res = run_bass_kernel_spmd(nc, in_maps, core_ids=list(range(8)))
    B, T, _ = x.shape
    out = np.zeros((B, T, D), np.float32)
    for core in range(8):
        b, j = core // 4, core % 4
        o = res.results[core]["out"]
        for m in range(NOWN):
            g = 4 * m + j
            out[b, g * 128:(g + 1) * 128] = o[m]
    return out
```

```python
import os
from contextlib import ExitStack

import ml_dtypes
import numpy as np

import concourse.bass as bass
import concourse.mybir as mybir
from concourse.bass_types import AP
from concourse.bass_utils import run_bass_kernel_spmd

F32 = mybir.dt.float32
BF16 = mybir.dt.bfloat16
AF = mybir.ActivationFunctionType
ALU = mybir.AluOpType
AX = mybir.AxisListType

NB = 64
NOWN = 16
D = 1024
DFF = 2816
NFF = DFF // 128
DIN = 4808
NIT = 16
BIG = 1.0e30
NEGM = 30000.0
EPS = 1e-6
NDS = 12

_DBG = os.environ.get("KDBG", "")


class Buf:
    __slots__ = ("w", "r")

    def __init__(self):
        self.w = None
        self.r = {}


class KB:
    def __init__(self, nc):
        self.nc = nc
        self.eng = {"pe": nc.tensor, "act": nc.scalar, "dve": nc.vector, "pool": nc.gpsimd, "sp": nc.sync}
        self.sem = {e: nc.alloc_semaphore(name=f"s_{e}") for e in self.eng}
        self.cnt = {e: 0 for e in self.eng}
        self.waited = {e: {} for e in self.eng}
        self.fence_fn = {}
        self.last_fence = {}
        self.dq = {}
        for q in ("sp", "pool", "act"):
            self.dq[q] = {"sems": [nc.alloc_semaphore(name=f"d_{q}{i}") for i in range(NDS)], "k": 0}

    def _wait(self, e, ev):
        sem, val = ev
        if e == "pe" and sem is self.sem["pe"]:
            return
        key = sem.num
        if self.waited[e].get(key, 0) >= val:
            return
        self.eng[e].wait_ge(sem, val)
        self.waited[e][key] = val

    def _deps(self, e, reads, writes):
        for b in reads:
            if b.w is not None:
                self._wait(e, b.w)
        for b in writes:
            if b.w is not None:
                self._wait(e, b.w)
            for ev in b.r.values():
                self._wait(e, ev)

    def _mark(self, ev, reads, writes):
        key = ev[0].num
        for b in reads:
            old = b.r.get(key)
            if old is None or old[1] < ev[1]:
                b.r[key] = ev
        for b in writes:
            b.w = ev
            b.r = {}

    def op(self, e, fn, reads=(), writes=(), sig=True):
        self._deps(e, reads, writes)
        inst = fn()
        if sig:
            self.cnt[e] += 1
            inst.then_inc(self.sem[e], 1)
            ev = (self.sem[e], self.cnt[e])
        else:
            ev = (self.sem[e], self.cnt[e] + 1)
        self._mark(ev, reads, writes)
        return inst

    def dma(self, q, out, in_, reads=(), writes=(), fence=()):
        dq = self.dq[q]
        k = dq["k"]
        P = len(dq["sems"])
        sem = dq["sems"][k % P]
        if k >= P:
            self._wait(q, (sem, 16 * (k // P)))
        self._deps(q, reads, writes)
        for e in fence:
            last = self.last_fence.get(e)
            if last is not None:
                self._wait(e, last)
            self.fence_fn[e]().then_inc(self.sem[e], 1)
            self.cnt[e] += 1
            self.last_fence[e] = (self.sem[e], self.cnt[e])
            self._wait(q, (self.sem[e], self.cnt[e]))
        self.eng[q].dma_start(out=out, in_=in_).then_inc(sem, 16)
        dq["k"] += 1
        ev = (sem, 16 * (k // P + 1))
        self._mark(ev, reads, writes)

    def barrier(self):
        evs = [(self.sem[e], self.cnt[e]) for e in self.eng if self.cnt[e] > 0]
        for q, dq in self.dq.items():
            k = dq["k"]
            P = len(dq["sems"])
            for i in range(min(k, P)):
                kk = k - 1 - i
                evs.append((dq["sems"][kk % P], 16 * (kk // P + 1)))
        for e in self.eng:
            for ev in evs:
                if ev[0] is self.sem[e]:
                    continue
                self._wait(e, ev)

    def finish(self, bufs):
        for b in bufs:
            if b.w is not None:
                self._wait("sp", b.w)
        for q, dq in self.dq.items():
            k = dq["k"]
            P = len(dq["sems"])
            for i in range(min(k, P)):
                kk = k - 1 - i
                self._wait(q, (dq["sems"][kk % P], 16 * (kk // P + 1)))


def bc_mid(ap2d, n):
    a = [list(x) for x in ap2d.ap]
    assert len(a) == 2
    return AP(ap2d.tensor, ap2d.offset, [a[0], [0, n], a[1]])


def bc_last(ap, n):
    a = [list(x) for x in ap.ap]
    return AP(ap.tensor, ap.offset, a + [[0, n]])


def build():
    nc = bass.Bass("TRN2", target_bir_lowering=False)
    kb = KB(nc)
    op = kb.op
    dma = kb.dma
    PE, ACT, DVE, POOL = nc.tensor, nc.scalar, nc.vector, nc.gpsimd

    def din(name, shape, dt=F32):
        return nc.dram_tensor(name, list(shape), dt, kind="ExternalInput")

    def dscr(name, shape, dt):
        return nc.dram_tensor(name, list(shape), dt, kind=("ExternalOutput" if _DBG else "Internal"))

    xl = din("xl", [NB, 128, D])
    cs_t = din("cs", [NB, 128, 64])
    vmask_d = din("vmask", [128, NB])
    padbias_d = din("padbias", [128, 512])
    diagbias_d = din("diagbias", [128, 512])
    mt_d = din("mt", [128, 17, 128], BF16)
    identb_d = din("identb", [128, 128], BF16)
    pow2_d = din("pow2", [128, NIT + 1])
    gmix_d = din("gmix", [128, 8])
    gffn_d = din("gffn", [128, 8])
    gfin_d = din("gfin", [128, D])
    w_in = din("w_in", [D, DIN])
    w_up_a = din("w_up_a", [512, D])
    w_up_b = din("w_up_b", [512, D])
    w_out = din("w_out", [D, D])
    w_gate = din("w_gate", [D, DFF])
    w_up = din("w_up", [D, DFF])
    w_down = din("w_down", [DFF, D])
    out_d = nc.dram_tensor("out", [NOWN, 128, D], F32, kind="ExternalOutput")

    KAT = dscr("KAT", [128, NB, 512], BF16)
    VA = dscr("VA", [128, NB, 520], BF16)
    KBT = dscr("KBT", [64, NB * 128], BF16)
    KIT = dscr("KIT", [64, NB * 128], BF16)
    VB = dscr("VB", [128, NB, 65], BF16)
    QAT = dscr("QAT", [NOWN, 128, 512], BF16)
    QBT = dscr("QBT", [NOWN, 128, 512], BF16)
    QIT = dscr("QIT", [NOWN, 128, 512], BF16)
    WI = dscr("WI", [NOWN, 128, 8], F32)
    GS = dscr("GS", [NOWN, 128, 2048], BF16)
    X2 = dscr("X2", [NOWN, 128, D], F32)
    b_KAT = [Buf() for _ in range(NB)]
    b_VA = [Buf() for _ in range(NB)]
    b_KBT = [Buf() for _ in range(NB)]
    b_KIT = [Buf() for _ in range(NB)]
    b_VB = [Buf() for _ in range(NB)]
    b_QAT = [Buf() for _ in range(NOWN)]
    b_QBT = [Buf() for _ in range(NOWN)]
    b_QIT = [Buf() for _ in range(NOWN)]
    b_WI = [Buf() for _ in range(NOWN)]
    b_GS = [Buf() for _ in range(NOWN)]
    b_X2 = [Buf() for _ in range(NOWN)]
    b_out = [Buf() for _ in range(NOWN)]

    dbg_outs = {}

    def dbg_out(name, shape, dt):
        t = nc.dram_tensor("dbg_" + name, list(shape), dt, kind="ExternalOutput")
        dbg_outs[name] = t
        return t

    with ExitStack() as top:
        def sb(stack, name, shape, dt):
            return stack.enter_context(nc.sbuf_tensor("sb_" + name, list(shape), dt))

        def ps(stack, name, shape, dt):
            return stack.enter_context(nc.psum_tensor("ps_" + name, list(shape), dt))

        identb = sb(top, "identb", [128, 128], BF16)
        b_const = Buf()
        dma("sp", identb[:], identb_d.ap(), writes=[b_const])
        vmask = sb(top, "vmask", [128, NB], F32)
        dma("sp", vmask[:], vmask_d.ap(), writes=[b_const])
        fsc = sb(top, "fsc", [128, 8], F32)
        op("pool", lambda: POOL.memset(fsc[:], 0.0), writes=[Buf()])
        kb.barrier()
        kb.fence_fn["act"] = lambda: ACT.copy(out=fsc[:, 0:1], in_=fsc[:, 1:2])
        kb.fence_fn["dve"] = lambda: DVE.tensor_copy(out=fsc[:, 2:3], in_=fsc[:, 3:4])
        kb.fence_fn["pool"] = lambda: POOL.tensor_copy(out=fsc[:, 4:5], in_=fsc[:, 5:6])
        YAT = sb(top, "YAT", [128, 4, NOWN * 128], BF16)
        YBT = sb(top, "YBT", [128, 4, NOWN * 128], BF16)
        b_YAT = [Buf() for _ in range(NOWN)]
        b_YBT = [Buf() for _ in range(NOWN)]

        def rope(stack_bufs, zview, H, cs_tile, b_cs, b_z, out_view, b_outv):
            tA, tB, tC, tD, bA, bB, bC, bD = stack_bufs
            cosb = bc_mid(cs_tile[:, 0:32], H)
            sinb = bc_mid(cs_tile[:, 32:64], H)
            z1 = zview[:, :, 0:32]
            z2 = zview[:, :, 32:64]
            a = tA[:, 0:H, :]
            b = tB[:, 0:H, :]
            c = tC[:, 0:H, :]
            d = tD[:, 0:H, :]
            op("dve", lambda: DVE.tensor_tensor(out=a, in0=z1, in1=cosb, op=ALU.mult), reads=[b_z, b_cs], writes=[bA])
            op("dve", lambda: DVE.tensor_tensor(out=b, in0=z2, in1=sinb, op=ALU.mult), reads=[b_z, b_cs], writes=[bB])
            op("dve", lambda: DVE.tensor_tensor(out=c, in0=z2, in1=cosb, op=ALU.mult), reads=[b_z, b_cs], writes=[bC])
            op("dve", lambda: DVE.tensor_tensor(out=d, in0=z1, in1=sinb, op=ALU.mult), reads=[b_z, b_cs], writes=[bD])
            op("dve", lambda: DVE.tensor_tensor(out=out_view[:, :, 0:32], in0=a, in1=b, op=ALU.subtract),
               reads=[bA, bB], writes=[b_outv])
            op("dve", lambda: DVE.tensor_tensor(out=out_view[:, :, 32:64], in0=c, in1=d, op=ALU.add),
               reads=[bC, bD], writes=[b_outv])

        with ExitStack() as st:
            WK = sb(st, "WK", [128, 8, 1216], BF16)
            WQ = sb(st, "WQ", [128, 8, 3592], BF16)
            wst = [sb(st, f"wst{i}", [128, 8, 512], F32) for i in range(2)]
            b_wst = [Buf(), Buf()]
            gmix = sb(st, "gmix", [128, 8], F32)
            b_g = Buf()
            dma("sp", gmix[:], gmix_d.ap(), writes=[b_g])
            b_WK = Buf()
            b_WQ = Buf()
            w_in_v = w_in.ap().rearrange("(kc p) n -> p kc n", p=128)
            kparts = [(WK, b_WK, 0, 512, 512), (WK, b_WK, 512, 1024, 512), (WK, b_WK, 1024, 2048, 128),
                      (WK, b_WK, 1152, 2688, 64)]
            qparts = [(WQ, b_WQ, 0, 0, 512), (WQ, b_WQ, 512, 1536, 512), (WQ, b_WQ, 1024, 2176, 512),
                      (WQ, b_WQ, 1536, 2752, 8), (WQ, b_WQ, 1544, 2760, 512), (WQ, b_WQ, 2056, 3272, 512),
                      (WQ, b_WQ, 2568, 3784, 512), (WQ, b_WQ, 3080, 4296, 512)]
            for i, (dst, bdst, dc, sc, n) in enumerate(kparts + qparts):
                s = wst[i % 2]
                bs = b_wst[i % 2]
                dma("sp", s[:, :, 0:n], w_in_v[:, :, sc:sc + n], writes=[bs])
                op("pool", lambda s=s, dst=dst, dc=dc, n=n: POOL.tensor_tensor(
                    out=dst[:, :, dc:dc + n], in0=s[:, :, 0:n], in1=bc_last(gmix[:, :], n), op=ALU.mult),
                   reads=[bs, b_g], writes=[bdst])

            xs = [sb(st, f"xs{i}", [128, D], F32) for i in range(3)]
            b_xs = [Buf() for _ in range(3)]
            cst = [sb(st, f"cst{i}", [128, 64], F32) for i in range(4)]
            b_cst = [Buf() for _ in range(4)]
            junk = sb(st, "junk", [128, D], BF16)
            b_junk = Buf()
            ss = [sb(st, f"ss{i}", [128, 1], F32) for i in range(2)]
            ms = [sb(st, f"ms{i}", [128, 1], F32) for i in range(2)]
            rstd = [sb(st, f"rstd{i}", [128, 1], F32) for i in range(2)]
            mhalf = sb(st, "mhalf", [128, 1], F32)
            b_ss, b_ms, b_rstd = [Buf(), Buf()], [Buf(), Buf()], [Buf(), Buf()]
            b_mh = Buf()
            op("pool", lambda: POOL.memset(mhalf[:], -0.5), writes=[b_mh])
            hb = [sb(st, f"hb{i}", [128, D], BF16) for i in range(2)]
            b_hb = [Buf(), Buf()]
            hT = [sb(st, f"hT{i}", [128, 8, 128], BF16) for i in range(3)]
            b_hT = [Buf() for _ in range(3)]
            TRH = ps(st, "TRH", [128, 1024], BF16)
            b_TRH = Buf()
            NPB = 6
            PB = [ps(st, f"PB{i}", [128, 512], F32) for i in range(NPB)]
            b_PB = [Buf() for _ in range(NPB)]
            TRO = ps(st, "TRO", [128, 1024], BF16)
            b_TRO = Buf()
            rt = [sb(st, f"rt{i}", [128, 8, 32], F32) for i in range(4)]
            rbufs = tuple(rt) + tuple(Buf() for _ in range(4))
            kab = sb(st, "kab", [128, 8, 64], BF16)
            b_kab = Buf()
            kat = [sb(st, f"kat{i}", [128, 512], BF16) for i in range(2)]
            b_kat = [Buf(), Buf()]
            kbi = sb(st, "kbi", [128, 2, 64], BF16)
            b_kbi = Buf()
            kbit = [sb(st, f"kbit{i}", [64, 256], BF16) for i in range(2)]
            b_kbit = [Buf(), Buf()]
            vaa = [sb(st, f"vaa{i}", [128, 8, 65], BF16) for i in range(2)]
            b_vaa = [Buf(), Buf()]
            vba = [sb(st, f"vba{i}", [128, 65], BF16) for i in range(2)]
            b_vba = [Buf(), Buf()]
            qab = sb(st, "qab", [128, 8, 64], BF16)
            b_qab = Buf()
            qat = sb(st, "qat", [128, 512], BF16)
            b_qat = Buf()
            qbt = [sb(st, f"qbt{i}", [128, 512], BF16) for i in range(2)]
            b_qbt = [Buf(), Buf()]
            sgq = sb(st, "sgq", [128, 8], F32)
            absq = sb(st, "absq", [128, 8], F32)
            qis = sb(st, "qis", [128, 8, 64], F32)
            b_sgq, b_absq, b_qis = Buf(), Buf(), Buf()
            wis = sb(st, "wis", [128, 8], F32)
            b_wis = Buf()
            gsb = sb(st, "gsb", [128, 2048], BF16)
            b_gsb = Buf()
            pbc = [0]
            kbanks = {}

            def next_pb():
                i = pbc[0] % NPB
                pbc[0] += 1
                return PB[i], b_PB[i]

            def proj(hT_, bhT_, W, bW, c0, n, bank, bbank, o0=0):
                for kc in range(8):
                    op("pe", lambda kc=kc: PE.matmul(bank[:, o0:o0 + n], lhsT=hT_[:, kc, :], rhs=W[:, kc, c0:c0 + n],
                                                     start=(kc == 0), stop=(kc == 7)),
                       reads=[bhT_, bW], writes=[bbank], sig=(kc == 7))

            def stage_F(l):
                x_, bx = xs[l % 3], b_xs[l % 3]
                c_, bc = cst[l % 4], b_cst[l % 4]
                ss_, ms_, rs_ = ss[l % 2], ms[l % 2], rstd[l % 2]
                hb_, bhb = hb[l % 2], b_hb[l % 2]
                hT_, bhT_ = hT[l % 3], b_hT[l % 3]
                dma("sp", x_[:], xl.ap()[l], writes=[bx])
                dma("sp", c_[:], cs_t.ap()[l], writes=[bc])
                op("act", lambda: ACT.activation(out=junk[:], in_=x_[:], func=AF.Square, accum_out=ss_[:]),
                   reads=[bx], writes=[b_junk, b_ss[l % 2]])
                op("dve", lambda: DVE.tensor_scalar(out=ms_[:], in0=ss_[:], scalar1=1.0 / D, scalar2=EPS,
                                                    op0=ALU.mult, op1=ALU.add), reads=[b_ss[l % 2]], writes=[b_ms[l % 2]])
                op("pool", lambda: POOL.tensor_tensor(out=rs_[:], in0=ms_[:], in1=mhalf[:], op=ALU.pow),
                   reads=[b_ms[l % 2], b_mh], writes=[b_rstd[l % 2]])
                op("pool", lambda: POOL.tensor_scalar(out=hb_[:], in0=x_[:], scalar1=rs_[:, 0:1], scalar2=0.0,
                                                      op0=ALU.mult, op1=ALU.add),
                   reads=[bx, b_rstd[l % 2]], writes=[bhb])
                for kc in range(8):
                    op("pe", lambda kc=kc: PE.transpose(out=TRH[:, kc * 128:(kc + 1) * 128],
                                                        in_=hb_[:, kc * 128:(kc + 1) * 128], identity=identb[:]),
                       reads=[bhb, b_const], writes=[b_TRH], sig=(kc == 7))
                op("act", lambda: ACT.copy(out=hT_[:].rearrange("p a b -> p (a b)"), in_=TRH[:]),
                   reads=[b_TRH], writes=[bhT_])

            def stage_P(l):
                hT_, bhT_ = hT[l % 3], b_hT[l % 3]
                pa, bpa = next_pb()
                proj(hT_, bhT_, WK, b_WK, 0, 512, pa, bpa)
                pv, bpv = next_pb()
                proj(hT_, bhT_, WK, b_WK, 512, 512, pv, bpv)
                pc, bpc = next_pb()
                proj(hT_, bhT_, WK, b_WK, 1024, 128, pc, bpc, 0)
                proj(hT_, bhT_, WK, b_WK, 1152, 64, pc, bpc, 128)
                kbanks[l] = (pa, bpa, pv, bpv, pc, bpc)

            def stage_R(l):
                pa, bpa, pv, bpv, pc, bpc = kbanks.pop(l)
                c_, bc = cst[l % 4], b_cst[l % 4]
                kat_, bkat_ = kat[l % 2], b_kat[l % 2]
                kbit_, bkbit_ = kbit[l % 2], b_kbit[l % 2]
                vaa_, bvaa_ = vaa[l % 2], b_vaa[l % 2]
                vba_, bvba_ = vba[l % 2], b_vba[l % 2]
                op("act", lambda: ACT.activation(out=vaa_[:, :, 0:64], in_=pv[:].rearrange("p (h d) -> p h d", h=8),
                                                 func=AF.Copy, scale=vmask[:, l:l + 1]),
                   reads=[bpv, b_const], writes=[bvaa_])
                op("act", lambda: ACT.copy(out=vaa_[:, :, 64:65], in_=bc_mid(vmask[:, l:l + 1], 8)),
                   reads=[b_const], writes=[bvaa_])
                dma("act", VA.ap()[:, l, :], vaa_[:].rearrange("p h d -> p (h d)"), reads=[bvaa_], writes=[b_VA[l]], fence=("act",))
                op("act", lambda: ACT.activation(out=vba_[:, 0:64], in_=pc[:, 64:128], func=AF.Copy,
                                                 scale=vmask[:, l:l + 1]), reads=[bpc, b_const], writes=[bvba_])
                op("act", lambda: ACT.copy(out=vba_[:, 64:65], in_=vmask[:, l:l + 1]),
                   reads=[b_const], writes=[bvba_])
                dma("act", VB.ap()[:, l, :], vba_[:], reads=[bvba_], writes=[b_VB[l]], fence=("act",))
                rope(rbufs, pa[:].rearrange("p (h d) -> p h d", h=8), 8, c_, bc, bpa, kab[:], b_kab)
                zc = AP(pc, 0, [[512, 128], [128, 2], [1, 64]])
                rope(rbufs, zc, 2, c_, bc, bpc, kbi[:], b_kbi)
                for pr in range(4):
                    op("pe", lambda pr=pr: PE.transpose(out=TRO[:, pr * 128:(pr + 1) * 128],
                                                        in_=kab[:].rearrange("p h d -> p (h d)")[:, pr * 128:(pr + 1) * 128],
                                                        identity=identb[:]),
                       reads=[b_kab, b_const], writes=[b_TRO], sig=False)
                for hh in range(2):
                    op("pe", lambda hh=hh: PE.transpose(out=TRO[0:64, 512 + hh * 128:512 + (hh + 1) * 128],
                                                        in_=kbi[:, hh, :], identity=identb[:]),
                       reads=[b_kbi, b_const], writes=[b_TRO], sig=(hh == 1))
                op("act", lambda: ACT.copy(out=kat_[:], in_=TRO[:, 0:512]), reads=[b_TRO], writes=[bkat_])
                op("act", lambda: ACT.copy(out=kbit_[:], in_=TRO[0:64, 512:768]), reads=[b_TRO], writes=[bkbit_])
                dma("act", KAT.ap()[:, l, :], kat_[:], reads=[bkat_], writes=[b_KAT[l]], fence=("act",))
                dma("act", KBT.ap()[:, l * 128:(l + 1) * 128], kbit_[:, 0:128], reads=[bkbit_], writes=[b_KBT[l]])
                dma("act", KIT.ap()[:, l * 128:(l + 1) * 128], kbit_[:, 128:256], reads=[bkbit_], writes=[b_KIT[l]])

            def stage_Q(l):
                m = l // 4
                hT_, bhT_ = hT[l % 3], b_hT[l % 3]
                c_, bc = cst[l % 4], b_cst[l % 4]
                pq, bpq = next_pb()
                proj(hT_, bhT_, WQ, b_WQ, 0, 512, pq, bpq)
                pq2, bpq2 = next_pb()
                proj(hT_, bhT_, WQ, b_WQ, 512, 512, pq2, bpq2)
                pq3, bpq3 = next_pb()
                proj(hT_, bhT_, WQ, b_WQ, 1024, 512, pq3, bpq3)
                pq4, bpq4 = next_pb()
                proj(hT_, bhT_, WQ, b_WQ, 1536, 8, pq4, bpq4)
                op("dve", lambda: DVE.tensor_copy(out=wis[:], in_=pq4[:, 0:8]), reads=[bpq4], writes=[b_wis])
                dma("pool", WI.ap()[m], wis[:], reads=[b_wis], writes=[b_WI[m]], fence=("dve",))
                rope(rbufs, pq[:].rearrange("p (h d) -> p h d", h=8), 8, c_, bc, bpq, qab[:], b_qab)
                for pr in range(4):
                    op("pe", lambda pr=pr: PE.transpose(out=TRO[:, pr * 128:(pr + 1) * 128],
                                                        in_=qab[:].rearrange("p h d -> p (h d)")[:, pr * 128:(pr + 1) * 128],
                                                        identity=identb[:]),
                       reads=[b_qab, b_const], writes=[b_TRO], sig=(pr == 3))
                op("act", lambda: ACT.copy(out=qat[:], in_=TRO[:, 0:512]), reads=[b_TRO], writes=[b_qat])
                dma("act", QAT.ap()[m], qat[:], reads=[b_qat], writes=[b_QAT[m]], fence=("act",))
                for gq in range(4):
                    pg, bpg = next_pb()
                    proj(hT_, bhT_, WQ, b_WQ, 1544 + gq * 512, 512, pg, bpg)
                    op("act", lambda gq=gq, pg=pg: ACT.activation(out=gsb[:, gq * 512:(gq + 1) * 512], in_=pg[:],
                                                                  func=AF.Sigmoid), reads=[bpg], writes=[b_gsb])
                    if gq == 0:
                        qbt_, bqbt_ = qbt[0], b_qbt[0]
                        rope(rbufs, pq2[:].rearrange("p (h d) -> p h d", h=8), 8, c_, bc, bpq2, qab[:], b_qab)
                        for h8 in range(8):
                            ro = 64 * (h8 // 4)
                            op("pe", lambda h8=h8, ro=ro: PE.transpose(
                                out=TRO[ro:ro + 64, (h8 % 4) * 128:(h8 % 4 + 1) * 128], in_=qab[:, h8, :],
                                identity=identb[:]),
                               reads=[b_qab, b_const], writes=[b_TRO], sig=(h8 == 7))
                        op("act", lambda: ACT.copy(out=qbt_[:], in_=TRO[:, 0:512]), reads=[b_TRO], writes=[bqbt_])
                        dma("act", QBT.ap()[m], qbt_[:], reads=[bqbt_], writes=[b_QBT[m]], fence=("act",))
                    if gq == 1:
                        qbt_, bqbt_ = qbt[1], b_qbt[1]
                        op("dve", lambda: DVE.tensor_scalar(out=sgq[:], in0=wis[:], scalar1=0.0, scalar2=0.5,
                                                            op0=ALU.is_ge, op1=ALU.subtract), reads=[b_wis], writes=[b_sgq])
                        op("dve", lambda: DVE.scalar_tensor_tensor(out=absq[:], in0=sgq[:], scalar=2.0, in1=wis[:],
                                                                   op0=ALU.mult, op1=ALU.mult),
                           reads=[b_wis, b_sgq], writes=[b_absq])
                        op("dve", lambda: DVE.tensor_tensor(out=qis[:], in0=pq3[:].rearrange("p (h d) -> p h d", h=8),
                                                            in1=bc_last(absq[:, :], 64), op=ALU.mult),
                           reads=[bpq3, b_absq], writes=[b_qis])
                        rope(rbufs, qis[:], 8, c_, bc, b_qis, qab[:], b_qab)
                        for pr in range(4):
                            op("pe", lambda pr=pr: PE.transpose(
                                out=TRO[:, pr * 128:(pr + 1) * 128],
                                in_=qab[:].rearrange("p h d -> p (h d)")[:, pr * 128:(pr + 1) * 128], identity=identb[:]),
                               reads=[b_qab, b_const], writes=[b_TRO], sig=(pr == 3))
                        op("act", lambda: ACT.copy(out=qbt_[:], in_=TRO[:, 0:512]), reads=[b_TRO], writes=[bqbt_])
                        dma("act", QIT.ap()[m], qbt_[:], reads=[bqbt_], writes=[b_QIT[m]], fence=("act",))
                dma("act", GS.ap()[m], gsb[:], reads=[b_gsb], writes=[b_GS[m]], fence=("act",))

            stage_F(0)
            stage_F(1)
            stage_P(0)
            for l in range(NB):
                if l + 2 < NB:
                    stage_F(l + 2)
                if l % 4 == 3:
                    stage_R(l)
                    stage_Q(l)
                    if l + 1 < NB:
                        stage_P(l + 1)
                else:
                    if l + 1 < NB:
                        stage_P(l + 1)
                    stage_R(l)

        if _DBG == "1":
            kb.finish(b_KAT + b_VA + b_KBT + b_KIT + b_VB + b_QAT + b_QBT + b_QIT + b_WI + b_GS)
            return nc, dbg_outs

        kb.barrier()
        with ExitStack() as st:
            MT = sb(st, "MT", [128, 17 * 128], BF16)
            b_MT = Buf()
            dma("sp", MT[:], mt_d.ap().rearrange("p a b -> p (a b)"), writes=[b_MT])
            kw = [sb(st, f"kw{i}", [128, 17, 512], BF16) for i in range(2)]
            b_kw = [Buf(), Buf()]
            vw = [sb(st, f"vw{i}", [128, 17, 520], BF16) for i in range(2)]
            b_vw = [Buf(), Buf()]
            qa_t = [sb(st, f"qa{i}", [128, 512], BF16) for i in range(2)]
            b_qa = [Buf(), Buf()]
            STb = [ps(st, f"ST{i}", [128, 512], F32) for i in range(3)]
            b_ST = [Buf() for _ in range(3)]
            OA = [ps(st, f"OA{i}", [128, 512], F32) for i in range(4)]
            b_OA = [Buf() for _ in range(4)]
            TR = ps(st, "TR2", [128, 1024], BF16)
            b_TR = Buf()
            E = [sb(st, f"E{i}", [128, 512], BF16) for i in range(3)]
            b_E = [Buf() for _ in range(3)]
            Pm = [sb(st, f"P{i}", [128, 512], BF16) for i in range(3)]
            b_P = [Buf() for _ in range(3)]
            rc = sb(st, "rc", [128, 8], F32)
            b_rc = Buf()
            ya = sb(st, "ya", [128, 8, 64], BF16)
            b_ya = Buf()
            c2 = {"st": 0, "e": 0}
            pending_fin = []

            def fin2(m):
                oa = OA[2 * (m % 2):2 * (m % 2) + 2]
                boa = b_OA[2 * (m % 2):2 * (m % 2) + 2]
                for bnk in range(2):
                    ov = oa[bnk][:, 0:260].rearrange("p (h d) -> p h d", d=65)
                    op("dve", lambda: DVE.reciprocal(out=rc[:, bnk * 4:(bnk + 1) * 4], in_=ov[:, :, 64]),
                       reads=[boa[bnk]], writes=[b_rc])
                    op("dve", lambda: DVE.tensor_tensor(out=ya[:, bnk * 4:(bnk + 1) * 4, :], in0=ov[:, :, 0:64],
                                                        in1=bc_last(rc[:, bnk * 4:(bnk + 1) * 4], 64), op=ALU.mult),
                       reads=[boa[bnk], b_rc], writes=[b_ya])
                ya2 = ya[:].rearrange("p h d -> p (h d)")
                for kc in range(4):
                    op("pe", lambda kc=kc: PE.transpose(out=TR[:, kc * 128:(kc + 1) * 128],
                                                        in_=ya2[:, kc * 128:(kc + 1) * 128], identity=identb[:]),
                       reads=[b_ya, b_const], writes=[b_TR], sig=(kc == 3))
                op("act", lambda: ACT.copy(out=YAT[:, :, m * 128:(m + 1) * 128],
                                           in_=TR[:, 0:512].rearrange("p (a b) -> p a b", b=128)),
                   reads=[b_TR], writes=[b_YAT[m]])

            for m in range(NOWN):
                lq = 4 * m + 3
                lo = max(0, lq - 16)
                nb = lq - lo + 1
                k_, v_, q_ = kw[m % 2], vw[m % 2], qa_t[m % 2]
                bk, bv, bq = b_kw[m % 2], b_vw[m % 2], b_qa[m % 2]
                oa = OA[2 * (m % 2):2 * (m % 2) + 2]
                boa = b_OA[2 * (m % 2):2 * (m % 2) + 2]
                dma("sp", k_[:, 0:nb, :], KAT.ap()[:, lo:lq + 1, :], reads=b_KAT[lo:lq + 1], writes=[bk])
                dma("sp", v_[:, 0:nb, :], VA.ap()[:, lo:lq + 1, :], reads=b_VA[lo:lq + 1], writes=[bv])
                dma("sp", q_[:], QAT.ap()[m], reads=[b_QAT[m]], writes=[bq])
                first_bank = [True, True]
                groups = [list(range(i, min(i + 4, nb))) for i in range(0, nb, 4)]
                units = [(h, gi) for h in range(8) for gi in range(len(groups))]
                nu = len(units)
                slots = {}

                def emit_qk(u):
                    h, gi = units[u]
                    grp = groups[gi]
                    n = len(grp)
                    base = 64 * (h % 2)
                    pair = h // 2
                    k = c2["st"] % 3
                    c2["st"] += 1
                    slots[u] = k
                    for i, dl in enumerate(grp):
                        slot = nb - 1 - dl
                        op("pe", lambda i=i, slot=slot: PE.matmul(
                            STb[k][:, i * 128:(i + 1) * 128],
                            lhsT=k_[base:base + 64, slot, pair * 128:(pair + 1) * 128],
                            rhs=q_[base:base + 64, pair * 128:(pair + 1) * 128], start=True, stop=True),
                           reads=[bk, bq], writes=[b_ST[k]], sig=(i == n - 1))

                for u in range(min(3, nu)):
                    emit_qk(u)
                for u in range(nu):
                    h, gi = units[u]
                    grp = groups[gi]
                    n = len(grp)
                    k = slots[u]
                    ke = c2["e"] % 3
                    c2["e"] += 1
                    e_, be, p_, bp = E[ke], b_E[ke], Pm[ke], b_P[ke]
                    op("act", lambda: ACT.activation(out=e_[:, 0:n * 128], in_=STb[k][:, 0:n * 128], func=AF.Exp,
                                                     scale=0.125), reads=[b_ST[k]], writes=[be])
                    d0 = grp[0]
                    op("dve", lambda: DVE.tensor_tensor(out=p_[:, 0:n * 128], in0=e_[:, 0:n * 128],
                                                        in1=MT[:, d0 * 128:(d0 + n) * 128], op=ALU.mult),
                       reads=[be, b_MT], writes=[bp])
                    for i, dl in enumerate(grp):
                        slot = nb - 1 - dl
                        is_first = first_bank[h // 4]
                        first_bank[h // 4] = False
                        is_last = (h % 4 == 3 and gi == len(groups) - 1 and i == n - 1)
                        op("pe", lambda i=i, slot=slot, is_first=is_first, is_last=is_last: PE.matmul(
                            oa[h // 4][:, (h % 4) * 65:(h % 4) * 65 + 65], lhsT=p_[:, i * 128:(i + 1) * 128],
                            rhs=v_[:, slot, h * 65:(h + 1) * 65], start=is_first, stop=is_last,
                            skip_group_check=True),
                           reads=[bp, bv], writes=[boa[h // 4]], sig=(i == n - 1))
                    if u + 3 < nu:
                        emit_qk(u + 3)
                    if u == min(3, nu - 1) and pending_fin:
                        fin2(pending_fin.pop(0))
                pending_fin.append(m)
            while pending_fin:
                fin2(pending_fin.pop(0))

        if _DBG == "2":
            dy = dbg_out("YAT", [128, 4, NOWN * 128], BF16)
            bo = Buf()
            dma("sp", dy.ap(), YAT[:], reads=b_YAT, writes=[bo])
            kb.finish([bo])
            return nc, dbg_outs

        kb.barrier()
        with ExitStack() as st:
            KIs = sb(st, "KIs", [128, NB * 128], BF16)
            KBs = sb(st, "KBs", [128, NB * 128], BF16)
            VBs = sb(st, "VBs", [128, NB, 65], BF16)
            b_KIs, b_KBs, b_VBs = Buf(), Buf(), Buf()
            dma("sp", KIs[0:64, :], KIT.ap(), reads=b_KIT, writes=[b_KIs])
            dma("sp", KIs[64:128, :], KIT.ap(), reads=b_KIT, writes=[b_KIs])
            dma("sp", KBs[0:64, :], KBT.ap(), reads=b_KBT, writes=[b_KBs])
            dma("sp", KBs[64:128, :], KBT.ap(), reads=b_KBT, writes=[b_KBs])
            dma("sp", VBs[:], VB.ap(), reads=b_VB, writes=[b_VBs])
            padb = sb(st, "padb", [128, 512], F32)
            diagb = sb(st, "diagb", [128, 512], F32)
            pow2 = sb(st, "pow2", [128, NIT + 1], F32)
            b_c3 = Buf()
            dma("sp", padb[:], padbias_d.ap(), writes=[b_c3])
            dma("sp", diagb[:], diagbias_d.ap(), writes=[b_c3])
            dma("sp", pow2[:], pow2_d.ap(), writes=[b_c3])
            qi_t = [sb(st, f"qi{i}", [128, 512], BF16) for i in range(2)]
            qb_t = [sb(st, f"qb{i}", [128, 512], BF16) for i in range(2)]
            wi_t = [sb(st, f"wi{i}", [128, 8], F32) for i in range(2)]
            b_qi, b_qb, b_wi = [Buf(), Buf()], [Buf(), Buf()], [Buf(), Buf()]
            scores = sb(st, "scores", [128, NB * 128], F32)
            b_sc = [Buf() for _ in range(NOWN)]
            junkc = sb(st, "junkc", [128, NB * 128], BF16)
            b_jc = Buf()
            PXP = [ps(st, f"PXP{i}", [128, 1024], F32) for i in range(2)]
            b_PXP = [Buf(), Buf()]
            SC = ps(st, "SC", [128, 512], F32)
            b_SC = Buf()
            OB = [ps(st, f"OB{i}", [128, 512], F32) for i in range(2)]
            b_OB = [Buf(), Buf()]
            TR3 = ps(st, "TR3", [128, 1024], BF16)
            b_TR3 = Buf()
            R2 = [sb(st, f"R2{i}", [128, 1024], BF16) for i in range(3)]
            b_R2 = [Buf() for _ in range(3)]
            selfull = sb(st, "selfull", [128, NB * 128], BF16)
            b_selfull = Buf()
            absw = sb(st, "absw", [128, 8], F32)
            sgh = sb(st, "sgh", [128, 8], F32)
            Dg = sb(st, "Dg", [128, 8, 128], BF16)
            b_absw, b_sgh, b_Dg = Buf(), Buf(), Buf()
            cmin = sb(st, "cmin", [128, NOWN], F32)
            b_cmin = Buf()
            rmin = sb(st, "rmin", [128, 1], F32)
            rmax = sb(st, "rmax", [128, 1], F32)
            rng = sb(st, "rng", [128, 1], F32)
            tsum = sb(st, "tsum", [128, 1], F32)
            tau = [sb(st, f"tau{i}", [128, 1], F32) for i in range(2)]
            cntt = sb(st, "cntt", [128, 1], F32)
            sg = sb(st, "sg", [128, 1], F32)
            step2 = sb(st, "step2", [128, NIT + 1], F32)
            tsel = sb(st, "tsel", [128, 1], F32)
            b_rmin, b_rmax, b_rng, b_tsum, b_cnt, b_sg, b_step2, b_tsel = (Buf() for _ in range(8))
            b_tau = [Buf(), Buf()]
            sel = [sb(st, f"sel{i}", [128, 512], BF16) for i in range(2)]
            selT = [sb(st, f"selT{i}", [128, 512], BF16) for i in range(3)]
            b_sel, b_selT = [Buf(), Buf()], [Buf(), Buf(), Buf()]
            E2 = [sb(st, f"E2{i}", [128, 1024], BF16) for i in range(2)]
            b_E2 = [Buf(), Buf()]
            P2 = [sb(st, f"P2{i}", [128, 1024], BF16) for i in range(2)]
            b_P2 = [Buf(), Buf()]
            rcb = sb(st, "rcb", [128, 8], F32)
            b_rcb = Buf()
            yb = sb(st, "yb", [128, 8, 64], BF16)
            b_yb = Buf()
            scb = [scores, sb(st, "scores1", [128, NB * 128], F32)]
            b_scb = [b_sc, [Buf() for _ in range(NOWN)]]
            tselb = [tsel, sb(st, "tsel1", [128, 1], F32)]
            b_tselb = [b_tsel, Buf()]
            ctr = {"px": 0, "kr": 0, "ke": 0}

            def stage_S(m):
                nch = m + 1
                qi_, wi_ = qi_t[m % 2], wi_t[m % 2]
                bqi, bwi = b_qi[m % 2], b_wi[m % 2]
                sc_, bsc_ = scb[m % 2], b_scb[m % 2]
                dma("sp", qi_[:], QIT.ap()[m], reads=[b_QIT[m]], writes=[bqi])
                dma("sp", wi_[:], WI.ap()[m], reads=[b_WI[m]], writes=[bwi])
                op("dve", lambda: DVE.tensor_scalar(out=sgh[:], in0=wi_[:], scalar1=0.0, scalar2=0.5,
                                                    op0=ALU.is_ge, op1=ALU.subtract), reads=[bwi], writes=[b_sgh])
                for h in range(8):
                    op("dve", lambda h=h: DVE.tensor_scalar(out=Dg[:, h, :], in0=identb[:], scalar1=sgh[:, h:h + 1],
                                                            scalar2=2.0, op0=ALU.mult, op1=ALU.mult),
                       reads=[b_sgh, b_const], writes=[b_Dg])
                n = 4 * nch
                slots = {}

                def emit_d(j):
                    c, p = divmod(j, 4)
                    k = ctr["px"] % 2
                    ctr["px"] += 1
                    slots[j] = k
                    for half in range(2):
                        rows = slice(64 * half, 64 * half + 64)
                        op("pe", lambda: PE.matmul(PXP[k][:, half * 512:(half + 1) * 512],
                                                   lhsT=qi_[rows, p * 128:(p + 1) * 128],
                                                   rhs=KIs[rows, c * 512:(c + 1) * 512], start=True, stop=True),
                           reads=[bqi, b_KIs], writes=[b_PXP[k]], sig=(half == 1))

                for j in range(min(2, n)):
                    emit_d(j)
                pend_evac = None
                for j in range(n):
                    c, p = divmod(j, 4)
                    k = slots[j]
                    r_, br = R2[ctr["kr"] % 3], b_R2[ctr["kr"] % 3]
                    ctr["kr"] += 1
                    op("act", lambda: ACT.activation(out=r_[:], in_=PXP[k][:], func=AF.Relu),
                       reads=[b_PXP[k]], writes=[br])
                    if pend_evac is not None:
                        cc = pend_evac
                        pend_evac = None
                        op("act", lambda: ACT.copy(out=sc_[:, cc * 512:(cc + 1) * 512], in_=SC[:]),
                           reads=[b_SC], writes=[bsc_[cc]])
                    for half in range(2):
                        h = 2 * p + half
                        op("pe", lambda: PE.matmul(SC[:], lhsT=Dg[:, h, :], rhs=r_[:, half * 512:(half + 1) * 512],
                                                   start=(h == 0), stop=(h == 7)),
                           reads=[br, b_Dg], writes=[b_SC], sig=(half == 1))
                    if j + 2 < n:
                        emit_d(j + 2)
                    if p == 3:
                        pend_evac = c
                if pend_evac is not None:
                    cc = pend_evac
                    op("act", lambda: ACT.copy(out=sc_[:, cc * 512:(cc + 1) * 512], in_=SC[:]),
                       reads=[b_SC], writes=[bsc_[cc]])

            def stage_B(m):
                nch = m + 1
                S = 512 * nch
                sc_, bsc_ = scb[m % 2], b_scb[m % 2]
                bsc = bsc_[0:nch]
                op("dve", lambda: DVE.tensor_reduce(out=rmin[:], in_=sc_[:, 0:S], axis=AX.X, op=ALU.min),
                   reads=bsc, writes=[b_rmin])
                op("dve", lambda: DVE.tensor_tensor(out=sc_[:, 0:512], in0=sc_[:, 0:512], in1=padb[:], op=ALU.add),
                   reads=[b_c3, bsc_[0]], writes=[bsc_[0]])
                op("dve", lambda: DVE.tensor_tensor(out=sc_[:, S - 512:S], in0=sc_[:, S - 512:S], in1=diagb[:], op=ALU.add),
                   reads=[b_c3, bsc_[nch - 1]], writes=[bsc_[nch - 1]])
                op("dve", lambda: DVE.tensor_reduce(out=rmax[:], in_=sc_[:, 0:S], axis=AX.X, op=ALU.max),
                   reads=bsc, writes=[b_rmax])
                op("dve", lambda: DVE.scalar_tensor_tensor(out=rng[:], in0=rmax[:], scalar=2.0, in1=rmin[:],
                                                           op0=ALU.add, op1=ALU.subtract),
                   reads=[b_rmax, b_rmin], writes=[b_rng])
                op("dve", lambda: DVE.tensor_tensor(out=tsum[:], in0=rmax[:], in1=rmin[:], op=ALU.add),
                   reads=[b_rmax, b_rmin], writes=[b_tsum])
                op("dve", lambda: DVE.tensor_scalar(out=tau[0][:], in0=tsum[:], scalar1=0.5, scalar2=None, op0=ALU.mult),
                   reads=[b_tsum], writes=[b_tau[0]])
                op("dve", lambda: DVE.tensor_scalar(out=step2[:], in0=pow2[:], scalar1=rng[:, 0:1], scalar2=None,
                                                    op0=ALU.mult), reads=[b_rng, b_c3], writes=[b_step2])
                for it in range(NIT):
                    tc_, tn_ = tau[it % 2], tau[(it + 1) % 2]
                    btc, btn = b_tau[it % 2], b_tau[(it + 1) % 2]
                    op("dve", lambda: DVE.tensor_scalar(out=junkc[:, 0:S], in0=sc_[:, 0:S], scalar1=tc_[:, 0:1],
                                                        scalar2=None, op0=ALU.is_ge, op1=ALU.add, accum_out=cntt[:]),
                       reads=bsc + [btc], writes=[b_jc, b_cnt])
                    op("dve", lambda: DVE.tensor_scalar(out=sg[:], in0=cntt[:], scalar1=255.5, scalar2=0.5,
                                                        op0=ALU.is_ge, op1=ALU.subtract), reads=[b_cnt], writes=[b_sg])
                    op("dve", lambda: DVE.scalar_tensor_tensor(out=tn_[:], in0=sg[:], scalar=step2[:, it:it + 1],
                                                               in1=tc_[:], op0=ALU.mult, op1=ALU.add),
                       reads=[b_sg, b_step2, btc], writes=[btn])
                tf_, btf = tau[NIT % 2], b_tau[NIT % 2]
                op("dve", lambda: DVE.tensor_tensor(out=tsel[:], in0=tf_[:], in1=step2[:, NIT:NIT + 1], op=ALU.subtract),
                   reads=[btf, b_step2], writes=[b_tsel])
                op("dve", lambda: DVE.tensor_scalar(out=selfull[:, 0:S], in0=sc_[:, 0:S], scalar1=tsel[:, 0:1],
                                                    scalar2=None, op0=ALU.is_ge),
                   reads=bsc + [b_tsel], writes=[b_selfull])

            def stage_A(m):
                nch = m + 1
                qb_, bqb = qb_t[m % 2], b_qb[m % 2]
                dma("sp", qb_[:], QBT.ap()[m], reads=[b_QBT[m]], writes=[bqb])
                first_ob = [True, True]
                n = 4 * nch
                slots = {}

                def prep(c):
                    sT_, bsT_ = selT[c % 3], b_selT[c % 3]
                    for kq in range(4):
                        op("pe", lambda kq=kq: PE.transpose(out=TR3[:, kq * 128:(kq + 1) * 128],
                                                            in_=selfull[:, c * 512 + kq * 128:c * 512 + (kq + 1) * 128],
                                                            identity=identb[:]),
                           reads=[b_selfull, b_const], writes=[b_TR3], sig=(kq == 3))
                    op("act", lambda: ACT.copy(out=sT_[:], in_=TR3[:, 0:512]), reads=[b_TR3], writes=[bsT_])

                def emit_st(u):
                    c, kq = divmod(u, 4)
                    if kq == 0 and c + 1 < nch:
                        prep(c + 1)
                    ls = 4 * c + kq
                    k = ctr["px"] % 2
                    ctr["px"] += 1
                    slots[u] = k
                    for half in range(2):
                        rows = slice(64 * half, 64 * half + 64)
                        op("pe", lambda: PE.matmul(
                            PXP[k][:, half * 512:(half + 1) * 512].rearrange("p (a b) -> p a b", b=128),
                            lhsT=KBs[rows, ls * 128:(ls + 1) * 128],
                            rhs=qb_[rows, :].rearrange("p (a b) -> p a b", b=128), start=True, stop=True),
                           reads=[b_KBs, bqb], writes=[b_PXP[k]], sig=(half == 1))

                prep(0)
                for u in range(min(2, n)):
                    emit_st(u)
                for u in range(n):
                    c, kq = divmod(u, 4)
                    ls = 4 * c + kq
                    k = slots[u]
                    ke = ctr["ke"] % 2
                    ctr["ke"] += 1
                    e_, be, p_, bp = E2[ke], b_E2[ke], P2[ke], b_P2[ke]
                    sT_, bsT_ = selT[c % 3], b_selT[c % 3]
                    op("act", lambda: ACT.activation(out=e_[:], in_=PXP[k][:], func=AF.Exp, scale=0.125),
                       reads=[b_PXP[k]], writes=[be])
                    op("pool", lambda: POOL.tensor_tensor(out=p_[:].rearrange("p (a b) -> p a b", b=128),
                                                          in0=e_[:].rearrange("p (a b) -> p a b", b=128),
                                                          in1=bc_mid(sT_[:, kq * 128:(kq + 1) * 128], 8), op=ALU.mult),
                       reads=[be, bsT_], writes=[bp])
                    for hh in range(8):
                        i = hh // 4
                        is_first = first_ob[i]
                        first_ob[i] = False
                        is_last = (u == n - 1 and hh % 4 == 3)
                        op("pe", lambda hh=hh, i=i, is_first=is_first, is_last=is_last: PE.matmul(
                            OB[i][:, (hh % 4) * 65:(hh % 4) * 65 + 65], lhsT=p_[:, hh * 128:(hh + 1) * 128],
                            rhs=VBs[:, ls, :], start=is_first, stop=is_last, skip_group_check=True),
                           reads=[bp, b_VBs], writes=[b_OB[i]], sig=(hh % 4 == 3))
                    if u + 2 < n:
                        emit_st(u + 2)

            def stage_norm(m):
                for bnk in range(2):
                    ov = OB[bnk][:, 0:260].rearrange("p (h d) -> p h d", d=65)
                    op("dve", lambda: DVE.reciprocal(out=rcb[:, bnk * 4:(bnk + 1) * 4], in_=ov[:, :, 64]),
                       reads=[b_OB[bnk]], writes=[b_rcb])
                    op("dve", lambda: DVE.tensor_tensor(out=yb[:, bnk * 4:(bnk + 1) * 4, :], in0=ov[:, :, 0:64],
                                                        in1=bc_last(rcb[:, bnk * 4:(bnk + 1) * 4], 64), op=ALU.mult),
                       reads=[b_OB[bnk], b_rcb], writes=[b_yb])

            def stage_fin(m):
                yb2 = yb[:].rearrange("p h d -> p (h d)")
                for kc in range(4):
                    op("pe", lambda kc=kc: PE.transpose(out=TR3[:, kc * 128:(kc + 1) * 128],
                                                        in_=yb2[:, kc * 128:(kc + 1) * 128], identity=identb[:]),
                       reads=[b_yb, b_const], writes=[b_TR3], sig=(kc == 3))
                op("act", lambda: ACT.copy(out=YBT[:, :, m * 128:(m + 1) * 128],
                                           in_=TR3[:, 0:512].rearrange("p (a b) -> p a b", b=128)),
                   reads=[b_TR3], writes=[b_YBT[m]])

            stage_S(0)
            for m in range(NOWN):
                if m + 1 < NOWN:
                    stage_S(m + 1)
                stage_B(m)
                if m >= 1:
                    stage_norm(m - 1)
                    stage_fin(m - 1)
                stage_A(m)
            stage_norm(NOWN - 1)
            stage_fin(NOWN - 1)

        if _DBG == "3":
            dy = dbg_out("YBT", [128, 4, NOWN * 128], BF16)
            bo = Buf()
            dma("sp", dy.ap(), YBT[:], reads=b_YBT, writes=[bo])
            kb.finish([bo])
            return nc, dbg_outs

        kb.barrier()
        with ExitStack() as st45:
            H2T = sb(st45, "H2T", [128, 8, NOWN * 128], BF16)
            b_H2T = [Buf() for _ in range(NOWN)]
            ss = sb(st45, "ss2", [128, 1], F32)
            ms = sb(st45, "ms2", [128, 1], F32)
            rstd = sb(st45, "rstd2", [128, 1], F32)
            mhalf = sb(st45, "mhalf2", [128, 1], F32)
            b_ss, b_ms, b_rstd, b_mh = Buf(), Buf(), Buf(), Buf()
            op("pool", lambda: POOL.memset(mhalf[:], -0.5), writes=[b_mh])
            junk = sb(st45, "junk2", [128, D], BF16)
            b_junk = Buf()
            with ExitStack() as st:
                WUA = sb(st, "WUA", [128, 4, D], BF16)
                WUB = sb(st, "WUB", [128, 4, D], BF16)
                WO = sb(st, "WO", [128, 8, D], BF16)
                wst2 = sb(st, "wst2", [128, 8, D], F32)
                b_w2, b_WUA, b_WUB, b_WO = Buf(), Buf(), Buf(), Buf()
                dma("sp", wst2[:, 0:4, :], w_up_a.ap().rearrange("(kc p) n -> p kc n", p=128), writes=[b_w2])
                op("pool", lambda: POOL.tensor_copy(out=WUA[:], in_=wst2[:, 0:4, :]), reads=[b_w2], writes=[b_WUA])
                dma("sp", wst2[:, 0:4, :], w_up_b.ap().rearrange("(kc p) n -> p kc n", p=128), writes=[b_w2])
                op("pool", lambda: POOL.tensor_copy(out=WUB[:], in_=wst2[:, 0:4, :]), reads=[b_w2], writes=[b_WUB])
                dma("sp", wst2[:], w_out.ap().rearrange("(kc p) n -> p kc n", p=128), writes=[b_w2])
                op("pool", lambda: POOL.tensor_copy(out=WO[:], in_=wst2[:]), reads=[b_w2], writes=[b_WO])
                gs_t = [sb(st, f"gs{i}", [128, 2048], BF16) for i in range(2)]
                x_t = [sb(st, f"x4{i}", [128, D], F32) for i in range(2)]
                b_gs, b_x4 = [Buf(), Buf()], [Buf(), Buf()]
                UA = [ps(st, f"UA{i}", [128, 512], F32) for i in range(2)]
                UB = [ps(st, f"UB{i}", [128, 512], F32) for i in range(2)]
                WOp = [ps(st, f"WOp{i}", [128, 512], F32) for i in range(2)]
                TR4a = ps(st, "TR4a", [128, 1024], BF16)
                TR4b = ps(st, "TR4b", [128, 1024], BF16)
                b_UA, b_UB, b_WOp = [Buf(), Buf()], [Buf(), Buf()], [Buf(), Buf()]
                b_TR4a, b_TR4b = Buf(), Buf()
                t1 = [sb(st, f"t1{i}", [128, 512], F32) for i in range(2)]
                t2 = [sb(st, f"t2{i}", [128, 512], F32) for i in range(2)]
                b_t1, b_t2 = [Buf(), Buf()], [Buf(), Buf()]
                mg = [sb(st, f"mg{i}", [128, D], BF16) for i in range(2)]
                mgT = [sb(st, f"mgT{i}", [128, 8, 128], BF16) for i in range(2)]
                b_mg, b_mgT = [Buf(), Buf()], [Buf(), Buf()]
                x2 = [sb(st, f"x2{i}", [128, D], F32) for i in range(2)]
                b_x2 = [Buf(), Buf()]
                h2b = [sb(st, f"h2b{i}", [128, D], BF16) for i in range(2)]
                b_h2b = [Buf(), Buf()]
                ss4 = [ss, sb(st, "ss4", [128, 1], F32)]
                ms4 = [ms, sb(st, "ms4", [128, 1], F32)]
                rs4 = [rstd, sb(st, "rs4", [128, 1], F32)]
                b_ss4, b_ms4, b_rs4 = [b_ss, Buf()], [b_ms, Buf()], [b_rstd, Buf()]

                def stage_4A(m):
                    g_, bg_ = gs_t[m % 2], b_gs[m % 2]
                    x_, bx_ = x_t[m % 2], b_x4[m % 2]
                    mg_, bmg_ = mg[m % 2], b_mg[m % 2]
                    mgT_, bmgT_ = mgT[m % 2], b_mgT[m % 2]
                    dma("sp", g_[:], GS.ap()[m], reads=[b_GS[m]], writes=[bg_])
                    dma("sp", x_[:], xl.ap()[4 * m + 3], writes=[bx_])
                    tk = slice(m * 128, (m + 1) * 128)
                    for half in range(2):
                        cs_ = slice(half * 512, (half + 1) * 512)
                        for kc in range(4):
                            op("pe", lambda kc=kc: PE.matmul(UA[half][:], lhsT=YAT[:, kc, tk], rhs=WUA[:, kc, cs_],
                                                             start=(kc == 0), stop=(kc == 3)),
                               reads=[b_YAT[m], b_WUA], writes=[b_UA[half]], sig=(kc == 3))
                        for kc in range(4):
                            op("pe", lambda kc=kc: PE.matmul(UB[half][:], lhsT=YBT[:, kc, tk], rhs=WUB[:, kc, cs_],
                                                             start=(kc == 0), stop=(kc == 3)),
                               reads=[b_YBT[m], b_WUB], writes=[b_UB[half]], sig=(kc == 3))
                        op("dve", lambda: DVE.tensor_tensor(out=t1[half][:], in0=UA[half][:], in1=g_[:, cs_], op=ALU.mult),
                           reads=[b_UA[half], bg_], writes=[b_t1[half]])
                        op("dve", lambda: DVE.tensor_tensor(out=t2[half][:], in0=UB[half][:],
                                                            in1=g_[:, 1024 + half * 512:1024 + (half + 1) * 512], op=ALU.mult),
                           reads=[b_UB[half], bg_], writes=[b_t2[half]])
                        op("pool", lambda: POOL.tensor_tensor(out=mg_[:, cs_], in0=t1[half][:], in1=t2[half][:], op=ALU.add),
                           reads=[b_t1[half], b_t2[half]], writes=[bmg_])
                    for kc in range(8):
                        op("pe", lambda kc=kc: PE.transpose(out=TR4a[:, kc * 128:(kc + 1) * 128],
                                                            in_=mg_[:, kc * 128:(kc + 1) * 128], identity=identb[:]),
                           reads=[bmg_, b_const], writes=[b_TR4a], sig=(kc == 7))
                    op("act", lambda: ACT.copy(out=mgT_[:].rearrange("p a b -> p (a b)"), in_=TR4a[:]),
                       reads=[b_TR4a], writes=[bmgT_])

                def stage_4B(m):
                    x_, bx_ = x_t[m % 2], b_x4[m % 2]
                    mgT_, bmgT_ = mgT[m % 2], b_mgT[m % 2]
                    x2_, bx2_ = x2[m % 2], b_x2[m % 2]
                    h2_, bh2_ = h2b[m % 2], b_h2b[m % 2]
                    ss_, ms_, rs_ = ss4[m % 2], ms4[m % 2], rs4[m % 2]
                    bss_, bms_, brs_ = b_ss4[m % 2], b_ms4[m % 2], b_rs4[m % 2]
                    tk = slice(m * 128, (m + 1) * 128)
                    for half in range(2):
                        cs_ = slice(half * 512, (half + 1) * 512)
                        for kc in range(8):
                            op("pe", lambda kc=kc: PE.matmul(WOp[half][:], lhsT=mgT_[:, kc, :], rhs=WO[:, kc, cs_],
                                                             start=(kc == 0), stop=(kc == 7)),
                               reads=[bmgT_, b_WO], writes=[b_WOp[half]], sig=(kc == 7))
                        op("dve", lambda: DVE.tensor_tensor(out=x2_[:, cs_], in0=WOp[half][:], in1=x_[:, cs_], op=ALU.add),
                           reads=[b_WOp[half], bx_], writes=[bx2_])
                    dma("pool", X2.ap()[m], x2_[:], reads=[bx2_], writes=[b_X2[m]], fence=("dve",))
                    op("act", lambda: ACT.activation(out=junk[:], in_=x2_[:], func=AF.Square, accum_out=ss_[:]),
                       reads=[bx2_], writes=[b_junk, bss_])
                    op("dve", lambda: DVE.tensor_scalar(out=ms_[:], in0=ss_[:], scalar1=1.0 / D, scalar2=EPS,
                                                        op0=ALU.mult, op1=ALU.add), reads=[bss_], writes=[bms_])
                    op("pool", lambda: POOL.tensor_tensor(out=rs_[:], in0=ms_[:], in1=mhalf[:], op=ALU.pow),
                       reads=[bms_, b_mh], writes=[brs_])
                    op("pool", lambda: POOL.tensor_scalar(out=h2_[:], in0=x2_[:], scalar1=rs_[:, 0:1], scalar2=0.0,
                                                          op0=ALU.mult, op1=ALU.add),
                       reads=[bx2_, brs_], writes=[bh2_])
                    for kc in range(8):
                        op("pe", lambda kc=kc: PE.transpose(out=TR4b[:, kc * 128:(kc + 1) * 128],
                                                            in_=h2_[:, kc * 128:(kc + 1) * 128], identity=identb[:]),
                           reads=[bh2_, b_const], writes=[b_TR4b], sig=(kc == 7))
                    op("act", lambda: ACT.copy(out=H2T[:, :, tk], in_=TR4b[:].rearrange("p (a b) -> p a b", b=128)),
                       reads=[b_TR4b], writes=[b_H2T[m]])

                stage_4A(0)
                for m in range(NOWN):
                    if m + 1 < NOWN:
                        stage_4A(m + 1)
                    stage_4B(m)

            if _DBG == "4":
                dx = dbg_out("X2o", [NOWN, 128, D], F32)
                bo = Buf()
                with ExitStack() as st:
                    tt = sb(st, "dbgt", [128, D], F32)
                    bt = Buf()
                    for m in range(NOWN):
                        dma("sp", tt[:], X2.ap()[m], reads=[b_X2[m]], writes=[bt])
                        dma("sp", dx.ap()[m], tt[:], reads=[bt], writes=[bo])
                    kb.finish([bo])
                return nc, dbg_outs

            kb.barrier()
            with ExitStack() as st:
                gffn = sb(st, "gffn", [128, 8], F32)
                gfin = sb(st, "gfin", [128, D], F32)
                b_g5 = Buf()
                dma("sp", gffn[:], gffn_d.ap(), writes=[b_g5])
                dma("sp", gfin[:], gfin_d.ap(), writes=[b_g5])
                HT = NOWN * 128 // 2
                ACTT = sb(st, "ACTT", [128, NFF, HT], BF16)
                b_ACTT = [Buf(), Buf()]
                WD = sb(st, "WD", [128, NFF, D], BF16)
                b_WD = [Buf() for _ in range(NFF)]
                wdst = [sb(st, "wdst0", [128, D], F32)] * 2
                b_wdst = [Buf()] * 2
                wgst = [sb(st, f"wgst{i}", [128, 8, 128], F32) for i in range(2)]
                wust = [sb(st, f"wust{i}", [128, 8, 128], F32) for i in range(2)]
                wgb = [sb(st, f"wgb{i}", [128, 8, 128], BF16) for i in range(2)]
                wub = [sb(st, f"wub{i}", [128, 8, 128], BF16) for i in range(2)]
                b_wgst, b_wust, b_wgb, b_wub = ([Buf(), Buf()] for _ in range(4))
                PS5 = [ps(st, f"PS5{i}", [128, 512], F32) for i in range(8)]
                b_PS5 = [Buf() for _ in range(8)]
                sgt = [sb(st, f"sgt{i}", [128, 512], F32) for i in range(2)]
                b_sgt = [Buf(), Buf()]
                x2t = [sb(st, f"x2t{i}", [128, D], F32) for i in range(2)]
                x3 = x2t
                ot = [sb(st, "ot0", [128, D], F32)] * 2
                b_x2t = [Buf(), Buf()]
                b_x3 = b_x2t
                b_ot = [Buf()] * 2
                wgv = w_gate.ap().rearrange("(kc p) n -> p kc n", p=128)
                wuv = w_up.ap().rearrange("(kc p) n -> p kc n", p=128)

                def load_wd(ffc):
                    i2 = ffc % 2
                    dma("sp", wdst[i2][:], w_down.ap()[ffc * 128:(ffc + 1) * 128, :], writes=[b_wdst[i2]])
                    op("pool", lambda: POOL.tensor_copy(out=WD[:, ffc, :], in_=wdst[i2][:]),
                       reads=[b_wdst[i2]], writes=[b_WD[ffc]])

                kk = 0
                kw5 = 0
                for hf in range(2):
                    tok0 = hf * HT
                    for ffc in range(NFF):
                        i2 = kw5 % 2
                        kw5 += 1
                        dma("sp", wgst[i2][:], wgv[:, :, ffc * 128:(ffc + 1) * 128], writes=[b_wgst[i2]])
                        dma("sp", wust[i2][:], wuv[:, :, ffc * 128:(ffc + 1) * 128], writes=[b_wust[i2]])
                        op("pool", lambda: POOL.tensor_tensor(out=wgb[i2][:], in0=wgst[i2][:], in1=bc_last(gffn[:, :], 128),
                                                              op=ALU.mult), reads=[b_wgst[i2], b_g5], writes=[b_wgb[i2]])
                        op("pool", lambda: POOL.tensor_tensor(out=wub[i2][:], in0=wust[i2][:], in1=bc_last(gffn[:, :], 128),
                                                              op=ALU.mult), reads=[b_wust[i2], b_g5], writes=[b_wub[i2]])
                        if hf == 0:
                            load_wd(ffc)
                        for tg2 in range(2):
                            ts_ = slice(tok0 + tg2 * 512, tok0 + (tg2 + 1) * 512)
                            la = slice(tg2 * 512, (tg2 + 1) * 512)
                            gi, ui = kk % 2, 2 + kk % 2
                            s_, bs_ = sgt[kk % 2], b_sgt[kk % 2]
                            kk += 1
                            hbufs = b_H2T[(tok0 // 128) + tg2 * 4:(tok0 // 128) + tg2 * 4 + 4]
                            for kc in range(8):
                                op("pe", lambda kc=kc: PE.matmul(PS5[gi][:], lhsT=wgb[i2][:, kc, :], rhs=H2T[:, kc, ts_],
                                                                 start=(kc == 0), stop=(kc == 7)),
                                   reads=[b_wgb[i2]] + hbufs, writes=[b_PS5[gi]], sig=(kc == 7))
                            for kc in range(8):
                                op("pe", lambda kc=kc: PE.matmul(PS5[ui][:], lhsT=wub[i2][:, kc, :], rhs=H2T[:, kc, ts_],
                                                                 start=(kc == 0), stop=(kc == 7)),
                                   reads=[b_wub[i2]] + hbufs, writes=[b_PS5[ui]], sig=(kc == 7))
                            op("act", lambda: ACT.activation(out=s_[:], in_=PS5[gi][:], func=AF.Silu),
                               reads=[b_PS5[gi]], writes=[bs_])
                            op("dve", lambda: DVE.tensor_tensor(out=ACTT[:, ffc, la], in0=PS5[ui][:], in1=s_[:], op=ALU.mult),
                               reads=[b_PS5[ui], bs_], writes=[b_ACTT[tg2]])
                    for tg2 in range(2):
                        for ffc in range(NFF):
                            for tb in range(4):
                                lt = slice((tg2 * 4 + tb) * 128, (tg2 * 4 + tb + 1) * 128)
                                for half in range(2):
                                    op("pe", lambda tb=tb, half=half, lt=lt: PE.matmul(
                                        PS5[tb * 2 + half][:], lhsT=ACTT[:, ffc, lt],
                                        rhs=WD[:, ffc, half * 512:(half + 1) * 512], start=(ffc == 0), stop=(ffc == NFF - 1)),
                                       reads=[b_WD[ffc], b_ACTT[tg2]], writes=[b_PS5[tb * 2 + half]],
                                       sig=(ffc == NFF - 1))
                        for tb in range(4):
                            mm = hf * 8 + tg2 * 4 + tb
                            xx, bxx = x2t[mm % 2], b_x2t[mm % 2]
                            x3_, bx3 = x3[mm % 2], b_x3[mm % 2]
                            o_, bo_ = ot[mm % 2], b_ot[mm % 2]
                            dma("sp", xx[:], X2.ap()[mm], reads=[b_X2[mm]], writes=[bxx])
                            for half in range(2):
                                cs_ = slice(half * 512, (half + 1) * 512)
                                op("dve", lambda: DVE.tensor_tensor(out=x3_[:, cs_], in0=PS5[tb * 2 + half][:], in1=xx[:, cs_],
                                                                    op=ALU.add),
                                   reads=[b_PS5[tb * 2 + half], bxx], writes=[bx3])
                            op("act", lambda: ACT.activation(out=junk[:], in_=x3_[:], func=AF.Square, accum_out=ss[:]),
                               reads=[bx3], writes=[b_junk, b_ss])
                            op("dve", lambda: DVE.tensor_scalar(out=ms[:], in0=ss[:], scalar1=1.0 / D, scalar2=EPS,
                                                                op0=ALU.mult, op1=ALU.add), reads=[b_ss], writes=[b_ms])
                            op("pool", lambda: POOL.tensor_tensor(out=rstd[:], in0=ms[:], in1=mhalf[:], op=ALU.pow),
                               reads=[b_ms, b_mh], writes=[b_rstd])
                            op("dve", lambda: DVE.scalar_tensor_tensor(out=o_[:], in0=x3_[:], scalar=rstd[:, 0:1], in1=gfin[:],
                                                                       op0=ALU.mult, op1=ALU.mult),
                               reads=[bx3, b_rstd, b_g5], writes=[bo_])
                            dma("sp", out_d.ap()[mm], o_[:], reads=[bo_], writes=[b_out[mm]], fence=("dve",))

        kb.finish(b_out)
    return nc, dbg_outs


def host_prep(x, norm_mix, w_in, w_up_a, w_up_b, w_out, norm_ffn, w_gate, w_up, w_down, norm_final):
    B, T, _ = x.shape
    x = np.asarray(x, np.float32)
    half = 32
    inv_freq = (10000.0 ** (-np.arange(half, dtype=np.float32) / half)).astype(np.float32)
    identb = np.eye(128, dtype=np.float32).astype(ml_dtypes.bfloat16)
    s_i = np.arange(128)[:, None]
    t_i = np.arange(128)[None, :]
    mt = np.zeros((128, 17, 128), np.float32)
    for dl in range(17):
        diff = 128 * dl + t_i - s_i
        tot = np.zeros((128, 128), np.float32)
        for (wdw, dil) in ((128, 1), (512, 4), (2048, 16)):
            ok = (diff >= 0) & (diff <= wdw) & (diff % dil == 0)
            tot += ok.astype(np.float32)
        mt[:, dl, :] = tot
    mt = mt.astype(ml_dtypes.bfloat16)
    diagbias = np.zeros((128, 512), np.float32)
    diagbias[:, 384:512] = np.where(np.arange(128)[None, :] > np.arange(128)[:, None], -BIG, 0.0)
    pow2 = np.broadcast_to((2.0 ** (-(np.arange(NIT + 1) + 1.0))).astype(np.float32)[None, :], (128, NIT + 1)).copy()
    gmix = np.ascontiguousarray(np.asarray(norm_mix, np.float32).reshape(8, 128).T)
    gffn = np.ascontiguousarray(np.asarray(norm_ffn, np.float32).reshape(8, 128).T)
    gfin = np.ascontiguousarray(np.broadcast_to(np.asarray(norm_final, np.float32)[None, :], (128, D)))
    common = {
        "identb": identb, "mt": mt, "diagbias": diagbias, "pow2": pow2, "gmix": gmix, "gffn": gffn, "gfin": gfin,
        "w_in": np.ascontiguousarray(np.asarray(w_in, np.float32)[0]),
        "w_up_a": np.ascontiguousarray(np.asarray(w_up_a, np.float32)[0]),
        "w_up_b": np.ascontiguousarray(np.asarray(w_up_b, np.float32)[0]),
        "w_out": np.ascontiguousarray(np.asarray(w_out, np.float32)[0]),
        "w_gate": np.ascontiguousarray(np.asarray(w_gate, np.float32)[0]),
        "w_up": np.ascontiguousarray(np.asarray(w_up, np.float32)[0]),
        "w_down": np.ascontiguousarray(np.asarray(w_down, np.float32)[0]),
    }
    in_maps = []
    for core in range(8):
        b, j = core // 4, core % 4
        xl = np.zeros((NB, 128, D), np.float32)
        pos = np.zeros((NB, 128), np.float32)
        valid = np.zeros((NB,), np.float32)
        for l in range(NB):
            g = l + j - 3
            if g >= 0:
                xl[l] = x[b, g * 128:(g + 1) * 128]
                pos[l] = np.arange(g * 128, (g + 1) * 128, dtype=np.float32)
                valid[l] = 1.0
        ang = pos[:, :, None] * inv_freq[None, None, :]
        cs = np.concatenate([np.cos(ang), np.sin(ang)], axis=-1).astype(np.float32)
        vmask = np.ascontiguousarray(np.broadcast_to(valid[None, :], (128, NB))).astype(np.float32)
        padbias = np.zeros((128, 512), np.float32)
        for l in range(4):
            if valid[l] == 0.0:
                padbias[:, l * 128:(l + 1) * 128] = -BIG
        m = dict(common)
        m.update({"xl": xl, "cs": cs, "vmask": vmask, "padbias": padbias})
        in_maps.append(m)
    return in_maps


def kernel(x, norm_mix, w_in, w_up_a, w_up_b, w_out, norm_ffn, w_gate, w_up, w_down, norm_final):
    in_maps = host_prep(x, norm_mix, w_in, w_up_a, w_up_b, w_out, norm_ffn, w_gate, w_up, w_down, norm_final)
    nc, dbg = build()
    if _DBG:
        res = run_bass_kernel_spmd(nc, in_maps, core_ids=list(range(8)), trace=bool(os.environ.get("KTRACE")))
        print("DBG exec_time_ns", res.exec_time_ns)
        return res
    res = run_bass_kernel_spmd(nc, in_maps, core_ids=list(range(8)))
    B, T, _ = x.shape
    out = np.zeros((B, T, D), np.float32)
    for core in range(8):
        b, j = core // 4, core % 4
        o = res.results[core]["out"]
        for m in range(NOWN):
            g = 4 * m + j
            out[b, g * 128:(g + 1) * 128] = o[m]
    return out
```

```python
import os
from contextlib import ExitStack

import ml_dtypes
import numpy as np

import concourse.bass as bass
import concourse.mybir as mybir
from concourse.bass_types import AP
from concourse.bass_utils import run_bass_kernel_spmd

F32 = mybir.dt.float32
BF16 = mybir.dt.bfloat16
AF = mybir.ActivationFunctionType
ALU = mybir.AluOpType
AX = mybir.AxisListType

NB = 64
NOWN = 16
D = 1024
DFF = 2816
NFF = DFF // 128
DIN = 4808
NIT = 16
BIG = 1.0e30
NEGM = 30000.0
EPS = 1e-6
NDS = 12

_DBG = os.environ.get("KDBG", "")


class Buf:
    __slots__ = ("w", "r")

    def __init__(self):
        self.w = None
        self.r = {}


class KB:
    def __init__(self, nc):
        self.nc = nc
        self.eng = {"pe": nc.tensor, "act": nc.scalar, "dve": nc.vector, "pool": nc.gpsimd, "sp": nc.sync}
        self.sem = {e: nc.alloc_semaphore(name=f"s_{e}") for e in self.eng}
        self.cnt = {e: 0 for e in self.eng}
        self.waited = {e: {} for e in self.eng}
        self.fence_fn = {}
        self.last_fence = {}
        self.dq = {}
        for q in ("sp", "pool"):
            self.dq[q] = {"sems": [nc.alloc_semaphore(name=f"d_{q}{i}") for i in range(NDS)], "k": 0}

    def _wait(self, e, ev):
        sem, val = ev
        if e == "pe" and sem is self.sem["pe"]:
            return
        key = sem.num
        if self.waited[e].get(key, 0) >= val:
            return
        self.eng[e].wait_ge(sem, val)
        self.waited[e][key] = val

    def _deps(self, e, reads, writes):
        for b in reads:
            if b.w is not None:
                self._wait(e, b.w)
        for b in writes:
            if b.w is not None:
                self._wait(e, b.w)
            for ev in b.r.values():
                self._wait(e, ev)

    def _mark(self, ev, reads, writes):
        key = ev[0].num
        for b in reads:
            old = b.r.get(key)
            if old is None or old[1] < ev[1]:
                b.r[key] = ev
        for b in writes:
            b.w = ev
            b.r = {}

    def op(self, e, fn, reads=(), writes=(), sig=True):
        self._deps(e, reads, writes)
        inst = fn()
        if sig:
            self.cnt[e] += 1
            inst.then_inc(self.sem[e], 1)
            ev = (self.sem[e], self.cnt[e])
        else:
            ev = (self.sem[e], self.cnt[e] + 1)
        self._mark(ev, reads, writes)
        return inst

    def dma(self, q, out, in_, reads=(), writes=(), fence=()):
        dq = self.dq[q]
        k = dq["k"]
        P = len(dq["sems"])
        sem = dq["sems"][k % P]
        if k >= P:
            self._wait(q, (sem, 16 * (k // P)))
        self._deps(q, reads, writes)
        for e in fence:
            last = self.last_fence.get(e)
            if last is not None:
                self._wait(e, last)
            self.fence_fn[e]().then_inc(self.sem[e], 1)
            self.cnt[e] += 1
            self.last_fence[e] = (self.sem[e], self.cnt[e])
            self._wait(q, (self.sem[e], self.cnt[e]))
        self.eng[q].dma_start(out=out, in_=in_).then_inc(sem, 16)
        dq["k"] += 1
        ev = (sem, 16 * (k // P + 1))
        self._mark(ev, reads, writes)

    def barrier(self):
        evs = [(self.sem[e], self.cnt[e]) for e in self.eng if self.cnt[e] > 0]
        for q, dq in self.dq.items():
            k = dq["k"]
            P = len(dq["sems"])
            for i in range(min(k, P)):
                kk = k - 1 - i
                evs.append((dq["sems"][kk % P], 16 * (kk // P + 1)))
        for e in self.eng:
            for ev in evs:
                if ev[0] is self.sem[e]:
                    continue
                self._wait(e, ev)

    def finish(self, bufs):
        for b in bufs:
            if b.w is not None:
                self._wait("sp", b.w)
        for q, dq in self.dq.items():
            k = dq["k"]
            P = len(dq["sems"])
            for i in range(min(k, P)):
                kk = k - 1 - i
                self._wait(q, (dq["sems"][kk % P], 16 * (kk // P + 1)))


def bc_mid(ap2d, n):
    a = [list(x) for x in ap2d.ap]
    assert len(a) == 2
    return AP(ap2d.tensor, ap2d.offset, [a[0], [0, n], a[1]])


def bc_last(ap, n):
    a = [list(x) for x in ap.ap]
    return AP(ap.tensor, ap.offset, a + [[0, n]])


def build():
    nc = bass.Bass("TRN2", target_bir_lowering=False)
    kb = KB(nc)
    op = kb.op
    dma = kb.dma
    PE, ACT, DVE, POOL = nc.tensor, nc.scalar, nc.vector, nc.gpsimd

    def din(name, shape, dt=F32):
        return nc.dram_tensor(name, list(shape), dt, kind="ExternalInput")

    def dscr(name, shape, dt):
        return nc.dram_tensor(name, list(shape), dt, kind=("ExternalOutput" if _DBG else "Internal"))

    xl = din("xl", [NB, 128, D])
    cs_t = din("cs", [NB, 128, 64])
    vmask_d = din("vmask", [128, NB])
    padbias_d = din("padbias", [128, 512])
    diagbias_d = din("diagbias", [128, 512])
    mt_d = din("mt", [128, 17, 128], BF16)
    identb_d = din("identb", [128, 128], BF16)
    pow2_d = din("pow2", [128, NIT + 1])
    gmix_d = din("gmix", [128, 8])
    gffn_d = din("gffn", [128, 8])
    gfin_d = din("gfin", [128, D])
    w_in = din("w_in", [D, DIN])
    w_up_a = din("w_up_a", [512, D])
    w_up_b = din("w_up_b", [512, D])
    w_out = din("w_out", [D, D])
    w_gate = din("w_gate", [D, DFF])
    w_up = din("w_up", [D, DFF])
    w_down = din("w_down", [DFF, D])
    out_d = nc.dram_tensor("out", [NOWN, 128, D], F32, kind="ExternalOutput")

    KAT = dscr("KAT", [128, NB, 512], BF16)
    VA = dscr("VA", [128, NB, 520], BF16)
    KBT = dscr("KBT", [64, NB * 128], BF16)
    KIT = dscr("KIT", [64, NB * 128], BF16)
    VB = dscr("VB", [128, NB, 65], BF16)
    QAT = dscr("QAT", [NOWN, 128, 512], BF16)
    QBT = dscr("QBT", [NOWN, 128, 512], BF16)
    QIT = dscr("QIT", [NOWN, 128, 512], BF16)
    WI = dscr("WI", [NOWN, 128, 8], F32)
    GS = dscr("GS", [NOWN, 128, 2048], BF16)
    X2 = dscr("X2", [NOWN, 128, D], F32)
    b_KAT = [Buf() for _ in range(NB)]
    b_VA = [Buf() for _ in range(NB)]
    b_KBT = [Buf() for _ in range(NB)]
    b_KIT = [Buf() for _ in range(NB)]
    b_VB = [Buf() for _ in range(NB)]
    b_QAT = [Buf() for _ in range(NOWN)]
    b_QBT = [Buf() for _ in range(NOWN)]
    b_QIT = [Buf() for _ in range(NOWN)]
    b_WI = [Buf() for _ in range(NOWN)]
    b_GS = [Buf() for _ in range(NOWN)]
    b_X2 = [Buf() for _ in range(NOWN)]
    b_out = [Buf() for _ in range(NOWN)]

    dbg_outs = {}

    def dbg_out(name, shape, dt):
        t = nc.dram_tensor("dbg_" + name, list(shape), dt, kind="ExternalOutput")
        dbg_outs[name] = t
        return t

    with ExitStack() as top:
        def sb(stack, name, shape, dt):
            return stack.enter_context(nc.sbuf_tensor("sb_" + name, list(shape), dt))

        def ps(stack, name, shape, dt):
            return stack.enter_context(nc.psum_tensor("ps_" + name, list(shape), dt))

        identb = sb(top, "identb", [128, 128], BF16)
        b_const = Buf()
        dma("sp", identb[:], identb_d.ap(), writes=[b_const])
        vmask = sb(top, "vmask", [128, NB], F32)
        dma("sp", vmask[:], vmask_d.ap(), writes=[b_const])
        fsc = sb(top, "fsc", [128, 8], F32)
        op("pool", lambda: POOL.memset(fsc[:], 0.0), writes=[Buf()])
        kb.barrier()
        kb.fence_fn["act"] = lambda: ACT.copy(out=fsc[:, 0:1], in_=fsc[:, 1:2])
        kb.fence_fn["dve"] = lambda: DVE.tensor_copy(out=fsc[:, 2:3], in_=fsc[:, 3:4])
        kb.fence_fn["pool"] = lambda: POOL.tensor_copy(out=fsc[:, 4:5], in_=fsc[:, 5:6])
        YAT = sb(top, "YAT", [128, 4, NOWN * 128], BF16)
        YBT = sb(top, "YBT", [128, 4, NOWN * 128], BF16)
        b_YAT = [Buf() for _ in range(NOWN)]
        b_YBT = [Buf() for _ in range(NOWN)]

        def rope(stack_bufs, zview, H, cs_tile, b_cs, b_z, out_view, b_outv):
            tA, tB, tC, tD, bA, bB, bC, bD = stack_bufs
            cosb = bc_mid(cs_tile[:, 0:32], H)
            sinb = bc_mid(cs_tile[:, 32:64], H)
            z1 = zview[:, :, 0:32]
            z2 = zview[:, :, 32:64]
            a = tA[:, 0:H, :]
            b = tB[:, 0:H, :]
            c = tC[:, 0:H, :]
            d = tD[:, 0:H, :]
            op("dve", lambda: DVE.tensor_tensor(out=a, in0=z1, in1=cosb, op=ALU.mult), reads=[b_z, b_cs], writes=[bA])
            op("dve", lambda: DVE.tensor_tensor(out=b, in0=z2, in1=sinb, op=ALU.mult), reads=[b_z, b_cs], writes=[bB])
            op("dve", lambda: DVE.tensor_tensor(out=c, in0=z2, in1=cosb, op=ALU.mult), reads=[b_z, b_cs], writes=[bC])
            op("dve", lambda: DVE.tensor_tensor(out=d, in0=z1, in1=sinb, op=ALU.mult), reads=[b_z, b_cs], writes=[bD])
            op("dve", lambda: DVE.tensor_tensor(out=out_view[:, :, 0:32], in0=a, in1=b, op=ALU.subtract),
               reads=[bA, bB], writes=[b_outv])
            op("dve", lambda: DVE.tensor_tensor(out=out_view[:, :, 32:64], in0=c, in1=d, op=ALU.add),
               reads=[bC, bD], writes=[b_outv])

        with ExitStack() as st:
            WK = sb(st, "WK", [128, 8, 1216], BF16)
            WQ = sb(st, "WQ", [128, 8, 3592], BF16)
            wst = [sb(st, f"wst{i}", [128, 8, 512], F32) for i in range(2)]
            b_wst = [Buf(), Buf()]
            gmix = sb(st, "gmix", [128, 8], F32)
            b_g = Buf()
            dma("sp", gmix[:], gmix_d.ap(), writes=[b_g])
            b_WK = Buf()
            b_WQ = Buf()
            w_in_v = w_in.ap().rearrange("(kc p) n -> p kc n", p=128)
            kparts = [(WK, b_WK, 0, 512, 512), (WK, b_WK, 512, 1024, 512), (WK, b_WK, 1024, 2048, 128),
                      (WK, b_WK, 1152, 2688, 64)]
            qparts = [(WQ, b_WQ, 0, 0, 512), (WQ, b_WQ, 512, 1536, 512), (WQ, b_WQ, 1024, 2176, 512),
                      (WQ, b_WQ, 1536, 2752, 8), (WQ, b_WQ, 1544, 2760, 512), (WQ, b_WQ, 2056, 3272, 512),
                      (WQ, b_WQ, 2568, 3784, 512), (WQ, b_WQ, 3080, 4296, 512)]
            for i, (dst, bdst, dc, sc, n) in enumerate(kparts + qparts):
                s = wst[i % 2]
                bs = b_wst[i % 2]
                dma("sp", s[:, :, 0:n], w_in_v[:, :, sc:sc + n], writes=[bs])
                op("pool", lambda s=s, dst=dst, dc=dc, n=n: POOL.tensor_tensor(
                    out=dst[:, :, dc:dc + n], in0=s[:, :, 0:n], in1=bc_last(gmix[:, :], n), op=ALU.mult),
                   reads=[bs, b_g], writes=[bdst])

            xs = [sb(st, f"xs{i}", [128, D], F32) for i in range(3)]
            b_xs = [Buf() for _ in range(3)]
            cst = [sb(st, f"cst{i}", [128, 64], F32) for i in range(5)]
            b_cst = [Buf() for _ in range(5)]
            junk = sb(st, "junk", [128, D], BF16)
            b_junk = Buf()
            ss = [sb(st, f"ss{i}", [128, 1], F32) for i in range(2)]
            ms = [sb(st, f"ms{i}", [128, 1], F32) for i in range(2)]
            rstd = [sb(st, f"rstd{i}", [128, 1], F32) for i in range(2)]
            mhalf = sb(st, "mhalf", [128, 1], F32)
            b_ss, b_ms, b_rstd = [Buf(), Buf()], [Buf(), Buf()], [Buf(), Buf()]
            b_mh = Buf()
            op("pool", lambda: POOL.memset(mhalf[:], -0.5), writes=[b_mh])
            hb = [sb(st, f"hb{i}", [128, D], BF16) for i in range(2)]
            b_hb = [Buf(), Buf()]
            hT = [sb(st, f"hT{i}", [128, 8, 128], BF16) for i in range(3)]
            b_hT = [Buf() for _ in range(3)]
            TRH = ps(st, "TRH", [128, 1024], BF16)
            b_TRH = Buf()
            NPB = 6
            PB = [ps(st, f"PB{i}", [128, 512], F32) for i in range(NPB)]
            b_PB = [Buf() for _ in range(NPB)]
            TRO = ps(st, "TRO", [128, 1024], BF16)
            b_TRO = Buf()
            rt = [sb(st, f"rt{i}", [128, 8, 32], F32) for i in range(4)]
            rbufs = tuple(rt) + tuple(Buf() for _ in range(4))
            kab = sb(st, "kab", [128, 8, 64], BF16)
            b_kab = Buf()
            kat = [sb(st, f"kat{i}", [128, 512], BF16) for i in range(2)]
            b_kat = [Buf(), Buf()]
            kbi = sb(st, "kbi", [128, 2, 64], BF16)
            b_kbi = Buf()
            kbit = [sb(st, f"kbit{i}", [64, 256], BF16) for i in range(2)]
            b_kbit = [Buf(), Buf()]
            vaa = [sb(st, f"vaa{i}", [128, 8, 65], BF16) for i in range(2)]
            b_vaa = [Buf(), Buf()]
            vba = [sb(st, f"vba{i}", [128, 65], BF16) for i in range(2)]
            b_vba = [Buf(), Buf()]
            qab = sb(st, "qab", [128, 8, 64], BF16)
            b_qab = Buf()
            qat = sb(st, "qat", [128, 512], BF16)
            b_qat = Buf()
            qbt = [sb(st, f"qbt{i}", [128, 512], BF16) for i in range(2)]
            b_qbt = [Buf(), Buf()]
            sgq = sb(st, "sgq", [128, 8], F32)
            absq = sb(st, "absq", [128, 8], F32)
            qis = sb(st, "qis", [128, 8, 64], F32)
            b_sgq, b_absq, b_qis = Buf(), Buf(), Buf()
            wis = sb(st, "wis", [128, 8], F32)
            b_wis = Buf()
            gsb = sb(st, "gsb", [128, 2048], BF16)
            b_gsb = Buf()
            pbc = [0]
            kbanks = {}

            def next_pb():
                i = pbc[0] % NPB
                pbc[0] += 1
                return PB[i], b_PB[i]

            def proj(hT_, bhT_, W, bW, c0, n, bank, bbank, o0=0):
                for kc in range(8):
                    op("pe", lambda kc=kc: PE.matmul(bank[:, o0:o0 + n], lhsT=hT_[:, kc, :], rhs=W[:, kc, c0:c0 + n],
                                                     start=(kc == 0), stop=(kc == 7)),
                       reads=[bhT_, bW], writes=[bbank], sig=(kc == 7))

            def stage_F(l):
                x_, bx = xs[l % 3], b_xs[l % 3]
                c_, bc = cst[l % 5], b_cst[l % 5]
                ss_, ms_, rs_ = ss[l % 2], ms[l % 2], rstd[l % 2]
                hb_, bhb = hb[l % 2], b_hb[l % 2]
                hT_, bhT_ = hT[l % 3], b_hT[l % 3]
                dma("sp", x_[:], xl.ap()[l], writes=[bx])
                dma("sp", c_[:], cs_t.ap()[l], writes=[bc])
                op("act", lambda: ACT.activation(out=junk[:], in_=x_[:], func=AF.Square, accum_out=ss_[:]),
                   reads=[bx], writes=[b_junk, b_ss[l % 2]])
                op("dve", lambda: DVE.tensor_scalar(out=ms_[:], in0=ss_[:], scalar1=1.0 / D, scalar2=EPS,
                                                    op0=ALU.mult, op1=ALU.add), reads=[b_ss[l % 2]], writes=[b_ms[l % 2]])
                op("pool", lambda: POOL.tensor_tensor(out=rs_[:], in0=ms_[:], in1=mhalf[:], op=ALU.pow),
                   reads=[b_ms[l % 2], b_mh], writes=[b_rstd[l % 2]])
                op("pool", lambda: POOL.tensor_scalar(out=hb_[:], in0=x_[:], scalar1=rs_[:, 0:1], scalar2=0.0,
                                                      op0=ALU.mult, op1=ALU.add),
                   reads=[bx, b_rstd[l % 2]], writes=[bhb])

            def stage_F2(l):
                hb_, bhb = hb[l % 2], b_hb[l % 2]
                hT_, bhT_ = hT[l % 3], b_hT[l % 3]
                for kc in range(8):
                    op("pe", lambda kc=kc: PE.transpose(out=TRH[:, kc * 128:(kc + 1) * 128],
                                                        in_=hb_[:, kc * 128:(kc + 1) * 128], identity=identb[:]),
                       reads=[bhb, b_const], writes=[b_TRH], sig=(kc == 7))
                op("act", lambda: ACT.copy(out=hT_[:].rearrange("p a b -> p (a b)"), in_=TRH[:]),
                   reads=[b_TRH], writes=[bhT_])

            def stage_P(l):
                hT_, bhT_ = hT[l % 3], b_hT[l % 3]
                pa, bpa = next_pb()
                proj(hT_, bhT_, WK, b_WK, 0, 512, pa, bpa)
                pv, bpv = next_pb()
                proj(hT_, bhT_, WK, b_WK, 512, 512, pv, bpv)
                pc, bpc = next_pb()
                proj(hT_, bhT_, WK, b_WK, 1024, 128, pc, bpc, 0)
                proj(hT_, bhT_, WK, b_WK, 1152, 64, pc, bpc, 128)
                kbanks[l] = (pa, bpa, pv, bpv, pc, bpc)

            def stage_R(l):
                pa, bpa, pv, bpv, pc, bpc = kbanks.pop(l)
                c_, bc = cst[l % 5], b_cst[l % 5]
                kat_, bkat_ = kat[l % 2], b_kat[l % 2]
                kbit_, bkbit_ = kbit[l % 2], b_kbit[l % 2]
                vaa_, bvaa_ = vaa[l % 2], b_vaa[l % 2]
                vba_, bvba_ = vba[l % 2], b_vba[l % 2]
                op("act", lambda: ACT.activation(out=vaa_[:, :, 0:64], in_=pv[:].rearrange("p (h d) -> p h d", h=8),
                                                 func=AF.Copy, scale=vmask[:, l:l + 1]),
                   reads=[bpv, b_const], writes=[bvaa_])
                op("pool", lambda: POOL.tensor_copy(out=vaa_[:, :, 64:65], in_=bc_mid(vmask[:, l:l + 1], 8)),
                   reads=[b_const], writes=[bvaa_])
                dma("pool", VA.ap()[:, l, :], vaa_[:].rearrange("p h d -> p (h d)"), reads=[bvaa_], writes=[b_VA[l]], fence=("act", "pool"))
                op("act", lambda: ACT.activation(out=vba_[:, 0:64], in_=pc[:, 64:128], func=AF.Copy,
                                                 scale=vmask[:, l:l + 1]), reads=[bpc, b_const], writes=[bvba_])
                op("pool", lambda: POOL.tensor_copy(out=vba_[:, 64:65], in_=vmask[:, l:l + 1]),
                   reads=[b_const], writes=[bvba_])
                dma("pool", VB.ap()[:, l, :], vba_[:], reads=[bvba_], writes=[b_VB[l]], fence=("act", "pool"))
                rope(rbufs, pa[:].rearrange("p (h d) -> p h d", h=8), 8, c_, bc, bpa, kab[:], b_kab)
                zc = AP(pc, 0, [[512, 128], [128, 2], [1, 64]])
                rope(rbufs, zc, 2, c_, bc, bpc, kbi[:], b_kbi)
                for pr in range(4):
                    op("pe", lambda pr=pr: PE.transpose(out=TRO[:, pr * 128:(pr + 1) * 128],
                                                        in_=kab[:].rearrange("p h d -> p (h d)")[:, pr * 128:(pr + 1) * 128],
                                                        identity=identb[:]),
                       reads=[b_kab, b_const], writes=[b_TRO], sig=False)
                for hh in range(2):
                    op("pe", lambda hh=hh: PE.transpose(out=TRO[0:64, 512 + hh * 128:512 + (hh + 1) * 128],
                                                        in_=kbi[:, hh, :], identity=identb[:]),
                       reads=[b_kbi, b_const], writes=[b_TRO], sig=(hh == 1))
                op("act", lambda: ACT.copy(out=kat_[:], in_=TRO[:, 0:512]), reads=[b_TRO], writes=[bkat_])
                op("act", lambda: ACT.copy(out=kbit_[:], in_=TRO[0:64, 512:768]), reads=[b_TRO], writes=[bkbit_])
                dma("pool", KAT.ap()[:, l, :], kat_[:], reads=[bkat_], writes=[b_KAT[l]], fence=("act",))
                dma("pool", KBT.ap()[:, l * 128:(l + 1) * 128], kbit_[:, 0:128], reads=[bkbit_], writes=[b_KBT[l]])
                dma("pool", KIT.ap()[:, l * 128:(l + 1) * 128], kbit_[:, 128:256], reads=[bkbit_], writes=[b_KIT[l]])

            def stage_Q(l):
                m = l // 4
                hT_, bhT_ = hT[l % 3], b_hT[l % 3]
                c_, bc = cst[l % 5], b_cst[l % 5]
                pq, bpq = next_pb()
                proj(hT_, bhT_, WQ, b_WQ, 0, 512, pq, bpq)
                pq2, bpq2 = next_pb()
                proj(hT_, bhT_, WQ, b_WQ, 512, 512, pq2, bpq2)
                pq3, bpq3 = next_pb()
                proj(hT_, bhT_, WQ, b_WQ, 1024, 512, pq3, bpq3)
                pq4, bpq4 = next_pb()
                proj(hT_, bhT_, WQ, b_WQ, 1536, 8, pq4, bpq4)
                op("dve", lambda: DVE.tensor_copy(out=wis[:], in_=pq4[:, 0:8]), reads=[bpq4], writes=[b_wis])
                dma("pool", WI.ap()[m], wis[:], reads=[b_wis], writes=[b_WI[m]], fence=("dve",))
                rope(rbufs, pq[:].rearrange("p (h d) -> p h d", h=8), 8, c_, bc, bpq, qab[:], b_qab)
                for pr in range(4):
                    op("pe", lambda pr=pr: PE.transpose(out=TRO[:, pr * 128:(pr + 1) * 128],
                                                        in_=qab[:].rearrange("p h d -> p (h d)")[:, pr * 128:(pr + 1) * 128],
                                                        identity=identb[:]),
                       reads=[b_qab, b_const], writes=[b_TRO], sig=(pr == 3))
                op("act", lambda: ACT.copy(out=qat[:], in_=TRO[:, 0:512]), reads=[b_TRO], writes=[b_qat])
                dma("pool", QAT.ap()[m], qat[:], reads=[b_qat], writes=[b_QAT[m]], fence=("act",))
                for gq in range(4):
                    pg, bpg = next_pb()
                    proj(hT_, bhT_, WQ, b_WQ, 1544 + gq * 512, 512, pg, bpg)
                    op("act", lambda gq=gq, pg=pg: ACT.activation(out=gsb[:, gq * 512:(gq + 1) * 512], in_=pg[:],
                                                                  func=AF.Sigmoid), reads=[bpg], writes=[b_gsb])
                    if gq == 0:
                        qbt_, bqbt_ = qbt[0], b_qbt[0]
                        rope(rbufs, pq2[:].rearrange("p (h d) -> p h d", h=8), 8, c_, bc, bpq2, qab[:], b_qab)
                        for h8 in range(8):
                            ro = 64 * (h8 // 4)
                            op("pe", lambda h8=h8, ro=ro: PE.transpose(
                                out=TRO[ro:ro + 64, (h8 % 4) * 128:(h8 % 4 + 1) * 128], in_=qab[:, h8, :],
                                identity=identb[:]),
                               reads=[b_qab, b_const], writes=[b_TRO], sig=(h8 == 7))
                        op("act", lambda: ACT.copy(out=qbt_[:], in_=TRO[:, 0:512]), reads=[b_TRO], writes=[bqbt_])
                        dma("pool", QBT.ap()[m], qbt_[:], reads=[bqbt_], writes=[b_QBT[m]], fence=("act",))
                    if gq == 1:
                        qbt_, bqbt_ = qbt[1], b_qbt[1]
                        op("dve", lambda: DVE.tensor_scalar(out=sgq[:], in0=wis[:], scalar1=0.0, scalar2=0.5,
                                                            op0=ALU.is_ge, op1=ALU.subtract), reads=[b_wis], writes=[b_sgq])
                        op("dve", lambda: DVE.scalar_tensor_tensor(out=absq[:], in0=sgq[:], scalar=2.0, in1=wis[:],
                                                                   op0=ALU.mult, op1=ALU.mult),
                           reads=[b_wis, b_sgq], writes=[b_absq])
                        op("dve", lambda: DVE.tensor_tensor(out=qis[:], in0=pq3[:].rearrange("p (h d) -> p h d", h=8),
                                                            in1=bc_last(absq[:, :], 64), op=ALU.mult),
                           reads=[bpq3, b_absq], writes=[b_qis])
                        rope(rbufs, qis[:], 8, c_, bc, b_qis, qab[:], b_qab)
                        for pr in range(4):
                            op("pe", lambda pr=pr: PE.transpose(
                                out=TRO[:, pr * 128:(pr + 1) * 128],
                                in_=qab[:].rearrange("p h d -> p (h d)")[:, pr * 128:(pr + 1) * 128], identity=identb[:]),
                               reads=[b_qab, b_const], writes=[b_TRO], sig=(pr == 3))
                        op("act", lambda: ACT.copy(out=qbt_[:], in_=TRO[:, 0:512]), reads=[b_TRO], writes=[bqbt_])
                        dma("pool", QIT.ap()[m], qbt_[:], reads=[bqbt_], writes=[b_QIT[m]], fence=("act",))
                dma("pool", GS.ap()[m], gsb[:], reads=[b_gsb], writes=[b_GS[m]], fence=("act",))

            stage_F(0)
            stage_F(1)
            stage_F2(0)
            stage_F(2)
            stage_F2(1)
            stage_P(0)
            for l in range(NB):
                if l + 3 < NB:
                    stage_F(l + 3)
                if l + 2 < NB:
                    stage_F2(l + 2)
                if l % 4 == 3:
                    stage_R(l)
                    stage_Q(l)
                    if l + 1 < NB:
                        stage_P(l + 1)
                else:
                    if l + 1 < NB:
                        stage_P(l + 1)
                    stage_R(l)

        if _DBG == "1":
            kb.finish(b_KAT + b_VA + b_KBT + b_KIT + b_VB + b_QAT + b_QBT + b_QIT + b_WI + b_GS)
            return nc, dbg_outs

        kb.barrier()
        with ExitStack() as st:
            MT = sb(st, "MT", [128, 17 * 128], BF16)
            b_MT = Buf()
            dma("sp", MT[:], mt_d.ap().rearrange("p a b -> p (a b)"), writes=[b_MT])
            kw = [sb(st, f"kw{i}", [128, 17, 512], BF16) for i in range(2)]
            b_kw = [Buf(), Buf()]
            vw = [sb(st, f"vw{i}", [128, 17, 520], BF16) for i in range(2)]
            b_vw = [Buf(), Buf()]
            qa_t = [sb(st, f"qa{i}", [128, 512], BF16) for i in range(2)]
            b_qa = [Buf(), Buf()]
            STb = [ps(st, f"ST{i}", [128, 512], F32) for i in range(3)]
            b_ST = [Buf() for _ in range(3)]
            OA = [ps(st, f"OA{i}", [128, 512], F32) for i in range(4)]
            b_OA = [Buf() for _ in range(4)]
            TR = ps(st, "TR2", [128, 1024], BF16)
            b_TR = Buf()
            E = [sb(st, f"E{i}", [128, 512], BF16) for i in range(3)]
            b_E = [Buf() for _ in range(3)]
            Pm = [sb(st, f"P{i}", [128, 512], BF16) for i in range(3)]
            b_P = [Buf() for _ in range(3)]
            rc = sb(st, "rc", [128, 8], F32)
            b_rc = Buf()
            ya = sb(st, "ya", [128, 8, 64], BF16)
            b_ya = Buf()
            c2 = {"st": 0, "e": 0}
            pending_fin = []

            def fin2(m):
                oa = OA[2 * (m % 2):2 * (m % 2) + 2]
                boa = b_OA[2 * (m % 2):2 * (m % 2) + 2]
                for bnk in range(2):
                    ov = oa[bnk][:, 0:260].rearrange("p (h d) -> p h d", d=65)
                    op("dve", lambda: DVE.reciprocal(out=rc[:, bnk * 4:(bnk + 1) * 4], in_=ov[:, :, 64]),
                       reads=[boa[bnk]], writes=[b_rc])
                    op("dve", lambda: DVE.tensor_tensor(out=ya[:, bnk * 4:(bnk + 1) * 4, :], in0=ov[:, :, 0:64],
                                                        in1=bc_last(rc[:, bnk * 4:(bnk + 1) * 4], 64), op=ALU.mult),
                       reads=[boa[bnk], b_rc], writes=[b_ya])
                ya2 = ya[:].rearrange("p h d -> p (h d)")
                for kc in range(4):
                    op("pe", lambda kc=kc: PE.transpose(out=TR[:, kc * 128:(kc + 1) * 128],
                                                        in_=ya2[:, kc * 128:(kc + 1) * 128], identity=identb[:]),
                       reads=[b_ya, b_const], writes=[b_TR], sig=(kc == 3))
                op("act", lambda: ACT.copy(out=YAT[:, :, m * 128:(m + 1) * 128],
                                           in_=TR[:, 0:512].rearrange("p (a b) -> p a b", b=128)),
                   reads=[b_TR], writes=[b_YAT[m]])

            for m in range(NOWN):
                lq = 4 * m + 3
                lo = max(0, lq - 16)
                nb = lq - lo + 1
                k_, v_, q_ = kw[m % 2], vw[m % 2], qa_t[m % 2]
                bk, bv, bq = b_kw[m % 2], b_vw[m % 2], b_qa[m % 2]
                oa = OA[2 * (m % 2):2 * (m % 2) + 2]
                boa = b_OA[2 * (m % 2):2 * (m % 2) + 2]
                dma("sp", k_[:, 0:nb, :], KAT.ap()[:, lo:lq + 1, :], reads=b_KAT[lo:lq + 1], writes=[bk])
                dma("sp", v_[:, 0:nb, :], VA.ap()[:, lo:lq + 1, :], reads=b_VA[lo:lq + 1], writes=[bv])
                dma("sp", q_[:], QAT.ap()[m], reads=[b_QAT[m]], writes=[bq])
                first_bank = [True, True]
                groups = [list(range(i, min(i + 4, nb))) for i in range(0, nb, 4)]
                units = [(h, gi) for h in range(8) for gi in range(len(groups))]
                nu = len(units)
                slots = {}

                def emit_qk(u):
                    h, gi = units[u]
                    grp = groups[gi]
                    n = len(grp)
                    base = 64 * (h % 2)
                    pair = h // 2
                    k = c2["st"] % 3
                    c2["st"] += 1
                    slots[u] = k
                    for i, dl in enumerate(grp):
                        slot = nb - 1 - dl
                        op("pe", lambda i=i, slot=slot: PE.matmul(
                            STb[k][:, i * 128:(i + 1) * 128],
                            lhsT=k_[base:base + 64, slot, pair * 128:(pair + 1) * 128],
                            rhs=q_[base:base + 64, pair * 128:(pair + 1) * 128], start=True, stop=True),
                           reads=[bk, bq], writes=[b_ST[k]], sig=(i == n - 1))

                for u in range(min(3, nu)):
                    emit_qk(u)
                for u in range(nu):
                    h, gi = units[u]
                    grp = groups[gi]
                    n = len(grp)
                    k = slots[u]
                    ke = c2["e"] % 3
                    c2["e"] += 1
                    e_, be, p_, bp = E[ke], b_E[ke], Pm[ke], b_P[ke]
                    op("act", lambda: ACT.activation(out=e_[:, 0:n * 128], in_=STb[k][:, 0:n * 128], func=AF.Exp,
                                                     scale=0.125), reads=[b_ST[k]], writes=[be])
                    d0 = grp[0]
                    op("dve", lambda: DVE.tensor_tensor(out=p_[:, 0:n * 128], in0=e_[:, 0:n * 128],
                                                        in1=MT[:, d0 * 128:(d0 + n) * 128], op=ALU.mult),
                       reads=[be, b_MT], writes=[bp])
                    for i, dl in enumerate(grp):
                        slot = nb - 1 - dl
                        is_first = first_bank[h // 4]
                        first_bank[h // 4] = False
                        is_last = (h % 4 == 3 and gi == len(groups) - 1 and i == n - 1)
                        op("pe", lambda i=i, slot=slot, is_first=is_first, is_last=is_last: PE.matmul(
                            oa[h // 4][:, (h % 4) * 65:(h % 4) * 65 + 65], lhsT=p_[:, i * 128:(i + 1) * 128],
                            rhs=v_[:, slot, h * 65:(h + 1) * 65], start=is_first, stop=is_last,
                            skip_group_check=True),
                           reads=[bp, bv], writes=[boa[h // 4]], sig=(i == n - 1))
                    if u + 3 < nu:
                        emit_qk(u + 3)
                    if u == min(3, nu - 1) and pending_fin:
                        fin2(pending_fin.pop(0))
                pending_fin.append(m)
            while pending_fin:
                fin2(pending_fin.pop(0))

        if _DBG == "2":
            dy = dbg_out("YAT", [128, 4, NOWN * 128], BF16)
            bo = Buf()
            dma("sp", dy.ap(), YAT[:], reads=b_YAT, writes=[bo])
            kb.finish([bo])
            return nc, dbg_outs

        kb.barrier()
        with ExitStack() as st:
            KIs = sb(st, "KIs", [128, NB * 128], BF16)
            KBs = sb(st, "KBs", [128, NB * 128], BF16)
            VBs = sb(st, "VBs", [128, NB, 65], BF16)
            b_KIs, b_KBs, b_VBs = Buf(), Buf(), Buf()
            dma("sp", KIs[0:64, :], KIT.ap(), reads=b_KIT, writes=[b_KIs])
            dma("sp", KIs[64:128, :], KIT.ap(), reads=b_KIT, writes=[b_KIs])
            dma("sp", KBs[0:64, :], KBT.ap(), reads=b_KBT, writes=[b_KBs])
            dma("sp", KBs[64:128, :], KBT.ap(), reads=b_KBT, writes=[b_KBs])
            dma("sp", VBs[:], VB.ap(), reads=b_VB, writes=[b_VBs])
            padb = sb(st, "padb", [128, 512], F32)
            diagb = sb(st, "diagb", [128, 512], F32)
            pow2 = sb(st, "pow2", [128, NIT + 1], F32)
            b_c3 = Buf()
            dma("sp", padb[:], padbias_d.ap(), writes=[b_c3])
            dma("sp", diagb[:], diagbias_d.ap(), writes=[b_c3])
            dma("sp", pow2[:], pow2_d.ap(), writes=[b_c3])
            qi_t = [sb(st, f"qi{i}", [128, 512], BF16) for i in range(2)]
            qb_t = [sb(st, f"qb{i}", [128, 512], BF16) for i in range(2)]
            wi_t = [sb(st, f"wi{i}", [128, 8], F32) for i in range(2)]
            b_qi, b_qb, b_wi = [Buf(), Buf()], [Buf(), Buf()], [Buf(), Buf()]
            scores = sb(st, "scores", [128, NB * 128], F32)
            b_sc = [Buf() for _ in range(NOWN)]
            junkc = sb(st, "junkc", [128, NB * 128], BF16)
            b_jc = Buf()
            PXP = [ps(st, f"PXP{i}", [128, 1024], F32) for i in range(2)]
            b_PXP = [Buf(), Buf()]
            SC = ps(st, "SC", [128, 512], F32)
            b_SC = Buf()
            OB = [ps(st, f"OB{i}", [128, 512], F32) for i in range(2)]
            b_OB = [Buf(), Buf()]
            TR3 = ps(st, "TR3", [128, 1024], BF16)
            b_TR3 = Buf()
            R2 = [sb(st, f"R2{i}", [128, 1024], BF16) for i in range(3)]
            b_R2 = [Buf() for _ in range(3)]
            selfull = sb(st, "selfull", [128, NB * 128], BF16)
            b_selfull = Buf()
            absw = sb(st, "absw", [128, 8], F32)
            sgh = sb(st, "sgh", [128, 8], F32)
            Dg = sb(st, "Dg", [128, 8, 128], BF16)
            b_absw, b_sgh, b_Dg = Buf(), Buf(), Buf()
            cmin = sb(st, "cmin", [128, NOWN], F32)
            b_cmin = Buf()
            rmin = sb(st, "rmin", [128, 1], F32)
            rmax = sb(st, "rmax", [128, 1], F32)
            rng = sb(st, "rng", [128, 1], F32)
            tsum = sb(st, "tsum", [128, 1], F32)
            tau = [sb(st, f"tau{i}", [128, 1], F32) for i in range(2)]
            cntt = sb(st, "cntt", [128, 1], F32)
            sg = sb(st, "sg", [128, 1], F32)
            step2 = sb(st, "step2", [128, NIT + 1], F32)
            tsel = sb(st, "tsel", [128, 1], F32)
            b_rmin, b_rmax, b_rng, b_tsum, b_cnt, b_sg, b_step2, b_tsel = (Buf() for _ in range(8))
            b_tau = [Buf(), Buf()]
            sel = [sb(st, f"sel{i}", [128, 512], BF16) for i in range(2)]
            selT = [sb(st, f"selT{i}", [128, 512], BF16) for i in range(3)]
            b_sel, b_selT = [Buf(), Buf()], [Buf(), Buf(), Buf()]
            E2 = [sb(st, f"E2{i}", [128, 1024], BF16) for i in range(2)]
            b_E2 = [Buf(), Buf()]
            P2 = [sb(st, f"P2{i}", [128, 1024], BF16) for i in range(2)]
            b_P2 = [Buf(), Buf()]
            rcb = sb(st, "rcb", [128, 8], F32)
            b_rcb = Buf()
            yb = sb(st, "yb", [128, 8, 64], BF16)
            b_yb = Buf()
            scb = [scores, sb(st, "scores1", [128, NB * 128], F32)]
            b_scb = [b_sc, [Buf() for _ in range(NOWN)]]
            tselb = [tsel, sb(st, "tsel1", [128, 1], F32)]
            b_tselb = [b_tsel, Buf()]
            ctr = {"px": 0, "kr": 0, "ke": 0}

            def stage_S(m):
                nch = m + 1
                qi_, wi_ = qi_t[m % 2], wi_t[m % 2]
                bqi, bwi = b_qi[m % 2], b_wi[m % 2]
                sc_, bsc_ = scb[m % 2], b_scb[m % 2]
                dma("sp", qi_[:], QIT.ap()[m], reads=[b_QIT[m]], writes=[bqi])
                dma("sp", wi_[:], WI.ap()[m], reads=[b_WI[m]], writes=[bwi])
                op("dve", lambda: DVE.tensor_scalar(out=sgh[:], in0=wi_[:], scalar1=0.0, scalar2=0.5,
                                                    op0=ALU.is_ge, op1=ALU.subtract), reads=[bwi], writes=[b_sgh])
                for h in range(8):
                    op("dve", lambda h=h: DVE.tensor_scalar(out=Dg[:, h, :], in0=identb[:], scalar1=sgh[:, h:h + 1],
                                                            scalar2=2.0, op0=ALU.mult, op1=ALU.mult),
                       reads=[b_sgh, b_const], writes=[b_Dg])
                n = 4 * nch
                slots = {}

                def emit_d(j):
                    c, p = divmod(j, 4)
                    k = ctr["px"] % 2
                    ctr["px"] += 1
                    slots[j] = k
                    for half in range(2):
                        rows = slice(64 * half, 64 * half + 64)
                        op("pe", lambda: PE.matmul(PXP[k][:, half * 512:(half + 1) * 512],
                                                   lhsT=qi_[rows, p * 128:(p + 1) * 128],
                                                   rhs=KIs[rows, c * 512:(c + 1) * 512], start=True, stop=True),
                           reads=[bqi, b_KIs], writes=[b_PXP[k]], sig=(half == 1))

                for j in range(min(2, n)):
                    emit_d(j)
                pend_evac = None
                for j in range(n):
                    c, p = divmod(j, 4)
                    k = slots[j]
                    r_, br = R2[ctr["kr"] % 3], b_R2[ctr["kr"] % 3]
                    ctr["kr"] += 1
                    op("act", lambda: ACT.activation(out=r_[:], in_=PXP[k][:], func=AF.Relu),
                       reads=[b_PXP[k]], writes=[br])
                    if pend_evac is not None:
                        cc = pend_evac
                        pend_evac = None
                        op("act", lambda: ACT.copy(out=sc_[:, cc * 512:(cc + 1) * 512], in_=SC[:]),
                           reads=[b_SC], writes=[bsc_[cc]])
                    for half in range(2):
                        h = 2 * p + half
                        op("pe", lambda: PE.matmul(SC[:], lhsT=Dg[:, h, :], rhs=r_[:, half * 512:(half + 1) * 512],
                                                   start=(h == 0), stop=(h == 7)),
                           reads=[br, b_Dg], writes=[b_SC], sig=(half == 1))
                    if j + 2 < n:
                        emit_d(j + 2)
                    if p == 3:
                        pend_evac = c
                if pend_evac is not None:
                    cc = pend_evac
                    op("act", lambda: ACT.copy(out=sc_[:, cc * 512:(cc + 1) * 512], in_=SC[:]),
                       reads=[b_SC], writes=[bsc_[cc]])

            def stage_B(m):
                nch = m + 1
                S = 512 * nch
                sc_, bsc_ = scb[m % 2], b_scb[m % 2]
                bsc = bsc_[0:nch]
                op("dve", lambda: DVE.tensor_reduce(out=rmin[:], in_=sc_[:, 0:S], axis=AX.X, op=ALU.min),
                   reads=bsc, writes=[b_rmin])
                op("dve", lambda: DVE.tensor_tensor(out=sc_[:, 0:512], in0=sc_[:, 0:512], in1=padb[:], op=ALU.add),
                   reads=[b_c3, bsc_[0]], writes=[bsc_[0]])
                op("dve", lambda: DVE.tensor_tensor(out=sc_[:, S - 512:S], in0=sc_[:, S - 512:S], in1=diagb[:], op=ALU.add),
                   reads=[b_c3, bsc_[nch - 1]], writes=[bsc_[nch - 1]])
                op("dve", lambda: DVE.tensor_reduce(out=rmax[:], in_=sc_[:, 0:S], axis=AX.X, op=ALU.max),
                   reads=bsc, writes=[b_rmax])
                op("dve", lambda: DVE.scalar_tensor_tensor(out=rng[:], in0=rmax[:], scalar=2.0, in1=rmin[:],
                                                           op0=ALU.add, op1=ALU.subtract),
                   reads=[b_rmax, b_rmin], writes=[b_rng])
                op("dve", lambda: DVE.tensor_tensor(out=tsum[:], in0=rmax[:], in1=rmin[:], op=ALU.add),
                   reads=[b_rmax, b_rmin], writes=[b_tsum])
                op("dve", lambda: DVE.tensor_scalar(out=tau[0][:], in0=tsum[:], scalar1=0.5, scalar2=None, op0=ALU.mult),
                   reads=[b_tsum], writes=[b_tau[0]])
                op("dve", lambda: DVE.tensor_scalar(out=step2[:], in0=pow2[:], scalar1=rng[:, 0:1], scalar2=None,
                                                    op0=ALU.mult), reads=[b_rng, b_c3], writes=[b_step2])
                for it in range(NIT):
                    tc_, tn_ = tau[it % 2], tau[(it + 1) % 2]
                    btc, btn = b_tau[it % 2], b_tau[(it + 1) % 2]
                    op("dve", lambda: DVE.tensor_scalar(out=junkc[:, 0:S], in0=sc_[:, 0:S], scalar1=tc_[:, 0:1],
                                                        scalar2=None, op0=ALU.is_ge, op1=ALU.add, accum_out=cntt[:]),
                       reads=bsc + [btc], writes=[b_jc, b_cnt])
                    op("dve", lambda: DVE.tensor_scalar(out=sg[:], in0=cntt[:], scalar1=255.5, scalar2=0.5,
                                                        op0=ALU.is_ge, op1=ALU.subtract), reads=[b_cnt], writes=[b_sg])
                    op("dve", lambda: DVE.scalar_tensor_tensor(out=tn_[:], in0=sg[:], scalar=step2[:, it:it + 1],
                                                               in1=tc_[:], op0=ALU.mult, op1=ALU.add),
                       reads=[b_sg, b_step2, btc], writes=[btn])
                tf_, btf = tau[NIT % 2], b_tau[NIT % 2]
                op("dve", lambda: DVE.tensor_tensor(out=tsel[:], in0=tf_[:], in1=step2[:, NIT:NIT + 1], op=ALU.subtract),
                   reads=[btf, b_step2], writes=[b_tsel])
                op("dve", lambda: DVE.tensor_scalar(out=selfull[:, 0:S], in0=sc_[:, 0:S], scalar1=tsel[:, 0:1],
                                                    scalar2=None, op0=ALU.is_ge),
                   reads=bsc + [b_tsel], writes=[b_selfull])

            def stage_A(m):
                nch = m + 1
                qb_, bqb = qb_t[m % 2], b_qb[m % 2]
                dma("sp", qb_[:], QBT.ap()[m], reads=[b_QBT[m]], writes=[bqb])
                first_ob = [True, True]
                n = 4 * nch
                slots = {}

                def prep(c):
                    sT_, bsT_ = selT[c % 3], b_selT[c % 3]
                    for kq in range(4):
                        op("pe", lambda kq=kq: PE.transpose(out=TR3[:, kq * 128:(kq + 1) * 128],
                                                            in_=selfull[:, c * 512 + kq * 128:c * 512 + (kq + 1) * 128],
                                                            identity=identb[:]),
                           reads=[b_selfull, b_const], writes=[b_TR3], sig=(kq == 3))
                    op("act", lambda: ACT.copy(out=sT_[:], in_=TR3[:, 0:512]), reads=[b_TR3], writes=[bsT_])

                def emit_st(u):
                    c, kq = divmod(u, 4)
                    if kq == 0 and c + 1 < nch:
                        prep(c + 1)
                    ls = 4 * c + kq
                    k = ctr["px"] % 2
                    ctr["px"] += 1
                    slots[u] = k
                    for half in range(2):
                        rows = slice(64 * half, 64 * half + 64)
                        op("pe", lambda: PE.matmul(
                            PXP[k][:, half * 512:(half + 1) * 512].rearrange("p (a b) -> p a b", b=128),
                            lhsT=KBs[rows, ls * 128:(ls + 1) * 128],
                            rhs=qb_[rows, :].rearrange("p (a b) -> p a b", b=128), start=True, stop=True),
                           reads=[b_KBs, bqb], writes=[b_PXP[k]], sig=(half == 1))

                prep(0)
                for u in range(min(2, n)):
                    emit_st(u)
                for u in range(n):
                    c, kq = divmod(u, 4)
                    ls = 4 * c + kq
                    k = slots[u]
                    ke = ctr["ke"] % 2
                    ctr["ke"] += 1
                    e_, be, p_, bp = E2[ke], b_E2[ke], P2[ke], b_P2[ke]
                    sT_, bsT_ = selT[c % 3], b_selT[c % 3]
                    op("act", lambda: ACT.activation(out=e_[:], in_=PXP[k][:], func=AF.Exp, scale=0.125),
                       reads=[b_PXP[k]], writes=[be])
                    op("pool", lambda: POOL.tensor_tensor(out=p_[:].rearrange("p (a b) -> p a b", b=128),
                                                          in0=e_[:].rearrange("p (a b) -> p a b", b=128),
                                                          in1=bc_mid(sT_[:, kq * 128:(kq + 1) * 128], 8), op=ALU.mult),
                       reads=[be, bsT_], writes=[bp])
                    for hh in range(8):
                        i = hh // 4
                        is_first = first_ob[i]
                        first_ob[i] = False
                        is_last = (u == n - 1 and hh % 4 == 3)
                        op("pe", lambda hh=hh, i=i, is_first=is_first, is_last=is_last: PE.matmul(
                            OB[i][:, (hh % 4) * 65:(hh % 4) * 65 + 65], lhsT=p_[:, hh * 128:(hh + 1) * 128],
                            rhs=VBs[:, ls, :], start=is_first, stop=is_last, skip_group_check=True),
                           reads=[bp, b_VBs], writes=[b_OB[i]], sig=(hh % 4 == 3))
                    if u + 2 < n:
                        emit_st(u + 2)

            def stage_norm(m):
                for bnk in range(2):
                    ov = OB[bnk][:, 0:260].rearrange("p (h d) -> p h d", d=65)
                    op("dve", lambda: DVE.reciprocal(out=rcb[:, bnk * 4:(bnk + 1) * 4], in_=ov[:, :, 64]),
                       reads=[b_OB[bnk]], writes=[b_rcb])
                    op("dve", lambda: DVE.tensor_tensor(out=yb[:, bnk * 4:(bnk + 1) * 4, :], in0=ov[:, :, 0:64],
                                                        in1=bc_last(rcb[:, bnk * 4:(bnk + 1) * 4], 64), op=ALU.mult),
                       reads=[b_OB[bnk], b_rcb], writes=[b_yb])

            def stage_fin(m):
                yb2 = yb[:].rearrange("p h d -> p (h d)")
                for kc in range(4):
                    op("pe", lambda kc=kc: PE.transpose(out=TR3[:, kc * 128:(kc + 1) * 128],
                                                        in_=yb2[:, kc * 128:(kc + 1) * 128], identity=identb[:]),
                       reads=[b_yb, b_const], writes=[b_TR3], sig=(kc == 3))
                op("act", lambda: ACT.copy(out=YBT[:, :, m * 128:(m + 1) * 128],
                                           in_=TR3[:, 0:512].rearrange("p (a b) -> p a b", b=128)),
                   reads=[b_TR3], writes=[b_YBT[m]])

            stage_S(0)
            for m in range(NOWN):
                if m + 1 < NOWN:
                    stage_S(m + 1)
                stage_B(m)
                if m >= 1:
                    stage_norm(m - 1)
                    stage_fin(m - 1)
                stage_A(m)
            stage_norm(NOWN - 1)
            stage_fin(NOWN - 1)

        if _DBG == "3":
            dy = dbg_out("YBT", [128, 4, NOWN * 128], BF16)
            bo = Buf()
            dma("sp", dy.ap(), YBT[:], reads=b_YBT, writes=[bo])
            kb.finish([bo])
            return nc, dbg_outs

        kb.barrier()
        with ExitStack() as st45:
            H2T = sb(st45, "H2T", [128, 8, NOWN * 128], BF16)
            b_H2T = [Buf() for _ in range(NOWN)]
            ss = sb(st45, "ss2", [128, 1], F32)
            ms = sb(st45, "ms2", [128, 1], F32)
            rstd = sb(st45, "rstd2", [128, 1], F32)
            mhalf = sb(st45, "mhalf2", [128, 1], F32)
            b_ss, b_ms, b_rstd, b_mh = Buf(), Buf(), Buf(), Buf()
            op("pool", lambda: POOL.memset(mhalf[:], -0.5), writes=[b_mh])
            junk = sb(st45, "junk2", [128, D], BF16)
            b_junk = Buf()
            with ExitStack() as st:
                WUA = sb(st, "WUA", [128, 4, D], BF16)
                WUB = sb(st, "WUB", [128, 4, D], BF16)
                WO = sb(st, "WO", [128, 8, D], BF16)
                wst2 = sb(st, "wst2", [128, 8, D], F32)
                b_w2, b_WUA, b_WUB, b_WO = Buf(), Buf(), Buf(), Buf()
                dma("sp", wst2[:, 0:4, :], w_up_a.ap().rearrange("(kc p) n -> p kc n", p=128), writes=[b_w2])
                op("pool", lambda: POOL.tensor_copy(out=WUA[:], in_=wst2[:, 0:4, :]), reads=[b_w2], writes=[b_WUA])
                dma("sp", wst2[:, 0:4, :], w_up_b.ap().rearrange("(kc p) n -> p kc n", p=128), writes=[b_w2])
                op("pool", lambda: POOL.tensor_copy(out=WUB[:], in_=wst2[:, 0:4, :]), reads=[b_w2], writes=[b_WUB])
                dma("sp", wst2[:], w_out.ap().rearrange("(kc p) n -> p kc n", p=128), writes=[b_w2])
                op("pool", lambda: POOL.tensor_copy(out=WO[:], in_=wst2[:]), reads=[b_w2], writes=[b_WO])
                gs_t = [sb(st, f"gs{i}", [128, 2048], BF16) for i in range(2)]
                x_t = [sb(st, f"x4{i}", [128, D], F32) for i in range(2)]
                b_gs, b_x4 = [Buf(), Buf()], [Buf(), Buf()]
                UA = [ps(st, f"UA{i}", [128, 512], F32) for i in range(2)]
                UB = [ps(st, f"UB{i}", [128, 512], F32) for i in range(2)]
                WOp = [ps(st, f"WOp{i}", [128, 512], F32) for i in range(2)]
                TR4a = ps(st, "TR4a", [128, 1024], BF16)
                TR4b = ps(st, "TR4b", [128, 1024], BF16)
                b_UA, b_UB, b_WOp = [Buf(), Buf()], [Buf(), Buf()], [Buf(), Buf()]
                b_TR4a, b_TR4b = Buf(), Buf()
                t1 = [sb(st, f"t1{i}", [128, 512], F32) for i in range(2)]
                t2 = [sb(st, f"t2{i}", [128, 512], F32) for i in range(2)]
                b_t1, b_t2 = [Buf(), Buf()], [Buf(), Buf()]
                mg = [sb(st, f"mg{i}", [128, D], BF16) for i in range(2)]
                mgT = [sb(st, f"mgT{i}", [128, 8, 128], BF16) for i in range(2)]
                b_mg, b_mgT = [Buf(), Buf()], [Buf(), Buf()]
                x2 = [sb(st, f"x2{i}", [128, D], F32) for i in range(2)]
                b_x2 = [Buf(), Buf()]
                h2b = [sb(st, f"h2b{i}", [128, D], BF16) for i in range(2)]
                b_h2b = [Buf(), Buf()]
                ss4 = [ss, sb(st, "ss4", [128, 1], F32)]
                ms4 = [ms, sb(st, "ms4", [128, 1], F32)]
                rs4 = [rstd, sb(st, "rs4", [128, 1], F32)]
                b_ss4, b_ms4, b_rs4 = [b_ss, Buf()], [b_ms, Buf()], [b_rstd, Buf()]

                def stage_4A(m):
                    g_, bg_ = gs_t[m % 2], b_gs[m % 2]
                    x_, bx_ = x_t[m % 2], b_x4[m % 2]
                    mg_, bmg_ = mg[m % 2], b_mg[m % 2]
                    mgT_, bmgT_ = mgT[m % 2], b_mgT[m % 2]
                    dma("sp", g_[:], GS.ap()[m], reads=[b_GS[m]], writes=[bg_])
                    dma("sp", x_[:], xl.ap()[4 * m + 3], writes=[bx_])
                    tk = slice(m * 128, (m + 1) * 128)
                    for half in range(2):
                        cs_ = slice(half * 512, (half + 1) * 512)
                        for kc in range(4):
                            op("pe", lambda kc=kc: PE.matmul(UA[half][:], lhsT=YAT[:, kc, tk], rhs=WUA[:, kc, cs_],
                                                             start=(kc == 0), stop=(kc == 3)),
                               reads=[b_YAT[m], b_WUA], writes=[b_UA[half]], sig=(kc == 3))
                        for kc in range(4):
                            op("pe", lambda kc=kc: PE.matmul(UB[half][:], lhsT=YBT[:, kc, tk], rhs=WUB[:, kc, cs_],
                                                             start=(kc == 0), stop=(kc == 3)),
                               reads=[b_YBT[m], b_WUB], writes=[b_UB[half]], sig=(kc == 3))
                        op("dve", lambda: DVE.tensor_tensor(out=t1[half][:], in0=UA[half][:], in1=g_[:, cs_], op=ALU.mult),
                           reads=[b_UA[half], bg_], writes=[b_t1[half]])
                        op("dve", lambda: DVE.tensor_tensor(out=t2[half][:], in0=UB[half][:],
                                                            in1=g_[:, 1024 + half * 512:1024 + (half + 1) * 512], op=ALU.mult),
                           reads=[b_UB[half], bg_], writes=[b_t2[half]])
                        op("pool", lambda: POOL.tensor_tensor(out=mg_[:, cs_], in0=t1[half][:], in1=t2[half][:], op=ALU.add),
                           reads=[b_t1[half], b_t2[half]], writes=[bmg_])
                    for kc in range(8):
                        op("pe", lambda kc=kc: PE.transpose(out=TR4a[:, kc * 128:(kc + 1) * 128],
                                                            in_=mg_[:, kc * 128:(kc + 1) * 128], identity=identb[:]),
                           reads=[bmg_, b_const], writes=[b_TR4a], sig=(kc == 7))
                    op("act", lambda: ACT.copy(out=mgT_[:].rearrange("p a b -> p (a b)"), in_=TR4a[:]),
                       reads=[b_TR4a], writes=[bmgT_])

                def stage_4B(m):
                    x_, bx_ = x_t[m % 2], b_x4[m % 2]
                    mgT_, bmgT_ = mgT[m % 2], b_mgT[m % 2]
                    x2_, bx2_ = x2[m % 2], b_x2[m % 2]
                    h2_, bh2_ = h2b[m % 2], b_h2b[m % 2]
                    ss_, ms_, rs_ = ss4[m % 2], ms4[m % 2], rs4[m % 2]
                    bss_, bms_, brs_ = b_ss4[m % 2], b_ms4[m % 2], b_rs4[m % 2]
                    tk = slice(m * 128, (m + 1) * 128)
                    for half in range(2):
                        cs_ = slice(half * 512, (half + 1) * 512)
                        for kc in range(8):
                            op("pe", lambda kc=kc: PE.matmul(WOp[half][:], lhsT=mgT_[:, kc, :], rhs=WO[:, kc, cs_],
                                                             start=(kc == 0), stop=(kc == 7)),
                               reads=[bmgT_, b_WO], writes=[b_WOp[half]], sig=(kc == 7))
                        op("dve", lambda: DVE.tensor_tensor(out=x2_[:, cs_], in0=WOp[half][:], in1=x_[:, cs_], op=ALU.add),
                           reads=[b_WOp[half], bx_], writes=[bx2_])
                    dma("pool", X2.ap()[m], x2_[:], reads=[bx2_], writes=[b_X2[m]], fence=("dve",))
                    op("act", lambda: ACT.activation(out=junk[:], in_=x2_[:], func=AF.Square, accum_out=ss_[:]),
                       reads=[bx2_], writes=[b_junk, bss_])
                    op("dve", lambda: DVE.tensor_scalar(out=ms_[:], in0=ss_[:], scalar1=1.0 / D, scalar2=EPS,
                                                        op0=ALU.mult, op1=ALU.add), reads=[bss_], writes=[bms_])
                    op("pool", lambda: POOL.tensor_tensor(out=rs_[:], in0=ms_[:], in1=mhalf[:], op=ALU.pow),
                       reads=[bms_, b_mh], writes=[brs_])
                    op("pool", lambda: POOL.tensor_scalar(out=h2_[:], in0=x2_[:], scalar1=rs_[:, 0:1], scalar2=0.0,
                                                          op0=ALU.mult, op1=ALU.add),
                       reads=[bx2_, brs_], writes=[bh2_])
                    for kc in range(8):
                        op("pe", lambda kc=kc: PE.transpose(out=TR4b[:, kc * 128:(kc + 1) * 128],
                                                            in_=h2_[:, kc * 128:(kc + 1) * 128], identity=identb[:]),
                           reads=[bh2_, b_const], writes=[b_TR4b], sig=(kc == 7))
                    op("act", lambda: ACT.copy(out=H2T[:, :, tk], in_=TR4b[:].rearrange("p (a b) -> p a b", b=128)),
                       reads=[b_TR4b], writes=[b_H2T[m]])

                stage_4A(0)
                for m in range(NOWN):
                    if m + 1 < NOWN:
                        stage_4A(m + 1)
                    stage_4B(m)

            if _DBG == "4":
                dx = dbg_out("X2o", [NOWN, 128, D], F32)
                bo = Buf()
                with ExitStack() as st:
                    tt = sb(st, "dbgt", [128, D], F32)
                    bt = Buf()
                    for m in range(NOWN):
                        dma("sp", tt[:], X2.ap()[m], reads=[b_X2[m]], writes=[bt])
                        dma("sp", dx.ap()[m], tt[:], reads=[bt], writes=[bo])
                    kb.finish([bo])
                return nc, dbg_outs

            kb.barrier()
            with ExitStack() as st:
                gffn = sb(st, "gffn", [128, 8], F32)
                gfin = sb(st, "gfin", [128, D], F32)
                b_g5 = Buf()
                dma("sp", gffn[:], gffn_d.ap(), writes=[b_g5])
                dma("sp", gfin[:], gfin_d.ap(), writes=[b_g5])
                HT = NOWN * 128 // 2
                ACTT = sb(st, "ACTT", [128, NFF, HT], BF16)
                b_ACTT = [Buf(), Buf()]
                WD = sb(st, "WD", [128, NFF, D], BF16)
                b_WD = [Buf() for _ in range(NFF)]
                wdst = [sb(st, "wdst0", [128, D], F32)] * 2
                b_wdst = [Buf()] * 2
                wgst = [sb(st, f"wgst{i}", [128, 8, 128], F32) for i in range(2)]
                wust = [sb(st, f"wust{i}", [128, 8, 128], F32) for i in range(2)]
                wgb = [sb(st, f"wgb{i}", [128, 8, 128], BF16) for i in range(2)]
                wub = [sb(st, f"wub{i}", [128, 8, 128], BF16) for i in range(2)]
                b_wgst, b_wust, b_wgb, b_wub = ([Buf(), Buf()] for _ in range(4))
                PS5 = [ps(st, f"PS5{i}", [128, 512], F32) for i in range(8)]
                b_PS5 = [Buf() for _ in range(8)]
                sgt = [sb(st, f"sgt{i}", [128, 512], F32) for i in range(2)]
                b_sgt = [Buf(), Buf()]
                x2t = [sb(st, f"x2t{i}", [128, D], F32) for i in range(2)]
                x3 = x2t
                ot = [sb(st, "ot0", [128, D], F32)] * 2
                b_x2t = [Buf(), Buf()]
                b_x3 = b_x2t
                b_ot = [Buf()] * 2
                wgv = w_gate.ap().rearrange("(kc p) n -> p kc n", p=128)
                wuv = w_up.ap().rearrange("(kc p) n -> p kc n", p=128)

                def load_wd(ffc):
                    i2 = ffc % 2
                    dma("sp", wdst[i2][:], w_down.ap()[ffc * 128:(ffc + 1) * 128, :], writes=[b_wdst[i2]])
                    op("pool", lambda: POOL.tensor_copy(out=WD[:, ffc, :], in_=wdst[i2][:]),
                       reads=[b_wdst[i2]], writes=[b_WD[ffc]])

                kk = 0
                kw5 = 0
                for hf in range(2):
                    tok0 = hf * HT
                    for ffc in range(NFF):
                        i2 = kw5 % 2
                        kw5 += 1
                        dma("sp", wgst[i2][:], wgv[:, :, ffc * 128:(ffc + 1) * 128], writes=[b_wgst[i2]])
                        dma("sp", wust[i2][:], wuv[:, :, ffc * 128:(ffc + 1) * 128], writes=[b_wust[i2]])
                        op("pool", lambda: POOL.tensor_tensor(out=wgb[i2][:], in0=wgst[i2][:], in1=bc_last(gffn[:, :], 128),
                                                              op=ALU.mult), reads=[b_wgst[i2], b_g5], writes=[b_wgb[i2]])
                        op("pool", lambda: POOL.tensor_tensor(out=wub[i2][:], in0=wust[i2][:], in1=bc_last(gffn[:, :], 128),
                                                              op=ALU.mult), reads=[b_wust[i2], b_g5], writes=[b_wub[i2]])
                        if hf == 0:
                            load_wd(ffc)
                        for tg2 in range(2):
                            ts_ = slice(tok0 + tg2 * 512, tok0 + (tg2 + 1) * 512)
                            la = slice(tg2 * 512, (tg2 + 1) * 512)
                            gi, ui = kk % 2, 2 + kk % 2
                            s_, bs_ = sgt[kk % 2], b_sgt[kk % 2]
                            kk += 1
                            hbufs = b_H2T[(tok0 // 128) + tg2 * 4:(tok0 // 128) + tg2 * 4 + 4]
                            for kc in range(8):
                                op("pe", lambda kc=kc: PE.matmul(PS5[gi][:], lhsT=wgb[i2][:, kc, :], rhs=H2T[:, kc, ts_],
                                                                 start=(kc == 0), stop=(kc == 7)),
                                   reads=[b_wgb[i2]] + hbufs, writes=[b_PS5[gi]], sig=(kc == 7))
                            for kc in range(8):
                                op("pe", lambda kc=kc: PE.matmul(PS5[ui][:], lhsT=wub[i2][:, kc, :], rhs=H2T[:, kc, ts_],
                                                                 start=(kc == 0), stop=(kc == 7)),
                                   reads=[b_wub[i2]] + hbufs, writes=[b_PS5[ui]], sig=(kc == 7))
                            op("act", lambda: ACT.activation(out=s_[:], in_=PS5[gi][:], func=AF.Silu),
                               reads=[b_PS5[gi]], writes=[bs_])
                            op("dve", lambda: DVE.tensor_tensor(out=ACTT[:, ffc, la], in0=PS5[ui][:], in1=s_[:], op=ALU.mult),
                               reads=[b_PS5[ui], bs_], writes=[b_ACTT[tg2]])
                    for tg2 in range(2):
                        for ffc in range(NFF):
                            for tb in range(4):
                                lt = slice((tg2 * 4 + tb) * 128, (tg2 * 4 + tb + 1) * 128)
                                for half in range(2):
                                    op("pe", lambda tb=tb, half=half, lt=lt: PE.matmul(
                                        PS5[tb * 2 + half][:], lhsT=ACTT[:, ffc, lt],
                                        rhs=WD[:, ffc, half * 512:(half + 1) * 512], start=(ffc == 0), stop=(ffc == NFF - 1)),
                                       reads=[b_WD[ffc], b_ACTT[tg2]], writes=[b_PS5[tb * 2 + half]],
                                       sig=(ffc == NFF - 1))
                        for tb in range(4):
                            mm = hf * 8 + tg2 * 4 + tb
                            xx, bxx = x2t[mm % 2], b_x2t[mm % 2]
                            x3_, bx3 = x3[mm % 2], b_x3[mm % 2]
                            o_, bo_ = ot[mm % 2], b_ot[mm % 2]
                            dma("sp", xx[:], X2.ap()[mm], reads=[b_X2[mm]], writes=[bxx])
                            for half in range(2):
                                cs_ = slice(half * 512, (half + 1) * 512)
                                op("dve", lambda: DVE.tensor_tensor(out=x3_[:, cs_], in0=PS5[tb * 2 + half][:], in1=xx[:, cs_],
                                                                    op=ALU.add),
                                   reads=[b_PS5[tb * 2 + half], bxx], writes=[bx3])
                            op("act", lambda: ACT.activation(out=junk[:], in_=x3_[:], func=AF.Square, accum_out=ss[:]),
                               reads=[bx3], writes=[b_junk, b_ss])
                            op("dve", lambda: DVE.tensor_scalar(out=ms[:], in0=ss[:], scalar1=1.0 / D, scalar2=EPS,
                                                                op0=ALU.mult, op1=ALU.add), reads=[b_ss], writes=[b_ms])
                            op("pool", lambda: POOL.tensor_tensor(out=rstd[:], in0=ms[:], in1=mhalf[:], op=ALU.pow),
                               reads=[b_ms, b_mh], writes=[b_rstd])
                            op("dve", lambda: DVE.scalar_tensor_tensor(out=o_[:], in0=x3_[:], scalar=rstd[:, 0:1], in1=gfin[:],
                                                                       op0=ALU.mult, op1=ALU.mult),
                               reads=[bx3, b_rstd, b_g5], writes=[bo_])
                            dma("sp", out_d.ap()[mm], o_[:], reads=[bo_], writes=[b_out[mm]], fence=("dve",))

        kb.finish(b_out)
    return nc, dbg_outs


def host_prep(x, norm_mix, w_in, w_up_a, w_up_b, w_out, norm_ffn, w_gate, w_up, w_down, norm_final):
    B, T, _ = x.shape
    x = np.asarray(x, np.float32)
    half = 32
    inv_freq = (10000.0 ** (-np.arange(half, dtype=np.float32) / half)).astype(np.float32)
    identb = np.eye(128, dtype=np.float32).astype(ml_dtypes.bfloat16)
    s_i = np.arange(128)[:, None]
    t_i = np.arange(128)[None, :]
    mt = np.zeros((128, 17, 128), np.float32)
    for dl in range(17):
        diff = 128 * dl + t_i - s_i
        tot = np.zeros((128, 128), np.float32)
        for (wdw, dil) in ((128, 1), (512, 4), (2048, 16)):
            ok = (diff >= 0) & (diff <= wdw) & (diff % dil == 0)
            tot += ok.astype(np.float32)
        mt[:, dl, :] = tot
    mt = mt.astype(ml_dtypes.bfloat16)
    diagbias = np.zeros((128, 512), np.float32)
    diagbias[:, 384:512] = np.where(np.arange(128)[None, :] > np.arange(128)[:, None], -BIG, 0.0)
    pow2 = np.broadcast_to((2.0 ** (-(np.arange(NIT + 1) + 1.0))).astype(np.float32)[None, :], (128, NIT + 1)).copy()
    gmix = np.ascontiguousarray(np.asarray(norm_mix, np.float32).reshape(8, 128).T)
    gffn = np.ascontiguousarray(np.asarray(norm_ffn, np.float32).reshape(8, 128).T)
    gfin = np.ascontiguousarray(np.broadcast_to(np.asarray(norm_final, np.float32)[None, :], (128, D)))
    common = {
        "identb": identb, "mt": mt, "diagbias": diagbias, "pow2": pow2, "gmix": gmix, "gffn": gffn, "gfin": gfin,
        "w_in": np.ascontiguousarray(np.asarray(w_in, np.float32)[0]),
        "w_up_a": np.ascontiguousarray(np.asarray(w_up_a, np.float32)[0]),
        "w_up_b": np.ascontiguousarray(np.asarray(w_up_b, np.float32)[0]),
        "w_out": np.ascontiguousarray(np.asarray(w_out, np.float32)[0]),
        "w_gate": np.ascontiguousarray(np.asarray(w_gate, np.float32)[0]),
        "w_up": np.ascontiguousarray(np.asarray(w_up, np.float32)[0]),
        "w_down": np.ascontiguousarray(np.asarray(w_down, np.float32)[0]),
    }
    in_maps = []
    for core in range(8):
        b, j = core // 4, core % 4
        xl = np.zeros((NB, 128, D), np.float32)
        pos = np.zeros((NB, 128), np.float32)
        valid = np.zeros((NB,), np.float32)
        for l in range(NB):
            g = l + j - 3
            if g >= 0:
                xl[l] = x[b, g * 128:(g + 1) * 128]
                pos[l] = np.arange(g * 128, (g + 1) * 128, dtype=np.float32)
                valid[l] = 1.0
        ang = pos[:, :, None] * inv_freq[None, None, :]
        cs = np.concatenate([np.cos(ang), np.sin(ang)], axis=-1).astype(np.float32)
        vmask = np.ascontiguousarray(np.broadcast_to(valid[None, :], (128, NB))).astype(np.float32)
        padbias = np.zeros((128, 512), np.float32)
        for l in range(4):
            if valid[l] == 0.0:
                padbias[:, l * 128:(l + 1) * 128] = -BIG
        m = dict(common)
        m.update({"xl": xl, "cs": cs, "vmask": vmask, "padbias": padbias})
        in_maps.append(m)
    return in_maps


def kernel(x, norm_mix, w_in, w_up_a, w_up_b, w_out, norm_ffn, w_gate, w_up, w_down, norm_final):
    in_maps = host_prep(x, norm_mix, w_in, w_up_a, w_up_b, w_out, norm_ffn, w_gate, w_up, w_down, norm_final)
    nc, dbg = build()
    if _DBG:
        res = run_bass_kernel_spmd(nc, in_maps, core_ids=list(range(8)), trace=bool(os.environ.get("KTRACE")))
        print("DBG exec_time_ns", res.exec_time_ns)
        return res
    res = run_bass_kernel_spmd(nc, in_maps, core_ids=list(range(8)))
    B, T, _ = x.shape
    out = np.zeros((B, T, D), np.float32)
    for core in range(8):
        b, j = core // 4, core % 4
        o = res.results[core]["out"]
        for m in range(NOWN):
            g = 4 * m + j
            out[b, g * 128:(g + 1) * 128] = o[m]
    return out
```
